# Optimizing a Trainium2 kernel written in Bass

```python
import math
import jax, jax.numpy as jnp
from jax import lax
import numpy as np

D_MODEL = 1024
BATCH = 8
SEQ = 4096
DEPTH = 4

N_MIXERS = 2
N_LAYERS_A = (DEPTH + 1) // 2
N_LAYERS_B = DEPTH // 2
D_FF = 2752
FFN_HALF = 0.5
ROPE_THETA = 500000.0
ROT_FRAC = 4
Q_BLOCK = 128
EPS = 1e-6
N_MOD = 9

A_HEADS = 16
A_HEAD_DIM = 64
A_KV_DIM = 64
IDX_HEADS = 8
IDX_DIM = 64
TOPK_MAX = 256
A_Q = A_HEADS * A_HEAD_DIM
A_IN = A_Q + 2 * A_KV_DIM + IDX_HEADS * IDX_DIM + IDX_DIM + IDX_HEADS

B_HEADS = 8
B_HEAD_DIM = 64
B_V_DIM = 2 * B_HEAD_DIM
B_QK = 2 * B_HEADS * B_HEAD_DIM
B_IN = 2 * B_QK + B_HEADS * B_V_DIM

kernel_name = "hybrid_dsa_diffattn_macaron_adaln"


def rmsnorm(x, g):
    xf = x.astype(jnp.float32)
    y = xf * lax.rsqrt(jnp.mean(xf * xf, axis=-1, keepdims=True) + EPS)
    return (y * g.astype(jnp.float32)).astype(x.dtype)


def modulate(h, shift, scale):
    return h * (1.0 + scale[:, None, :]) + shift[:, None, :]


def swiglu(h, w_gate, w_up, w_down):
    return (jax.nn.silu(h @ w_gate) * (h @ w_up)) @ w_down


def rope_tables(positions, head_dim):
    rot = head_dim // ROT_FRAC
    inv = ROPE_THETA ** (-jnp.arange(0, rot, 2, dtype=jnp.float32) / rot)
    ang = positions.astype(jnp.float32)[..., None] * inv
    return jnp.cos(ang), jnp.sin(ang)


def apply_rope(x, cos, sin):
    half = cos.shape[-1]
    rot = 2 * half
    shp = cos.shape[:2] + (1,) * (x.ndim - 3) + (half,)
    cos = cos.reshape(shp)
    sin = sin.reshape(shp)
    x1 = x[..., :half].astype(jnp.float32)
    x2 = x[..., half:rot].astype(jnp.float32)
    xr = jnp.concatenate([x1 * cos - x2 * sin, x2 * cos + x1 * sin], axis=-1)
    return jnp.concatenate([xr.astype(x.dtype), x[..., rot:]], axis=-1)


def dsa_mixer(h, w_in, w_out, cos, sin):
    B, S, _ = h.shape
    top_k = min(TOPK_MAX, S // 4)
    proj = h @ w_in
    o1 = A_Q
    o2 = o1 + A_KV_DIM
    o3 = o2 + A_KV_DIM
    o4 = o3 + IDX_HEADS * IDX_DIM
    o5 = o4 + IDX_DIM
    q = apply_rope(proj[..., :o1].reshape(B, S, A_HEADS, A_HEAD_DIM), cos, sin)
    k = apply_rope(proj[..., o1:o2], cos, sin)
    v = proj[..., o2:o3]
    qi = apply_rope(proj[..., o3:o4].reshape(B, S, IDX_HEADS, IDX_DIM), cos, sin)
    ki = apply_rope(proj[..., o4:o5], cos, sin)
    wi = proj[..., o5:].astype(jnp.float32) * (IDX_HEADS ** -0.5 * IDX_DIM ** -0.5)
    ki32 = ki.astype(jnp.float32)
    key_pos = jnp.arange(S)
    scale = A_HEAD_DIM ** -0.5
    gather = jax.vmap(lambda arr, ids: arr[ids])

    def block(i):
        start = i * Q_BLOCK
        t = start + jnp.arange(Q_BLOCK)
        q_b = lax.dynamic_slice_in_dim(q, start, Q_BLOCK, axis=1)
        qi_b = lax.dynamic_slice_in_dim(qi, start, Q_BLOCK, axis=1)
        wi_b = lax.dynamic_slice_in_dim(wi, start, Q_BLOCK, axis=1)
        logits = jnp.einsum('bthd,bsd->bths', qi_b.astype(jnp.float32), ki32)
        score = jnp.einsum('bth,bths->bts', wi_b, jax.nn.relu(logits))
        causal = key_pos[None, :] <= t[:, None]
        score = jnp.where(causal[None], score, -jnp.inf)
        _, idx = lax.top_k(score, top_k)
        k_sel = gather(k, idx).astype(jnp.float32)
        v_sel = gather(v, idx).astype(jnp.float32)
        s = jnp.einsum('bthd,btkd->bhtk', q_b.astype(jnp.float32), k_sel) * scale
        valid = idx <= t[None, :, None]
        s = jnp.where(valid[:, None], s, -jnp.inf)
        p = jax.nn.softmax(s, axis=-1)
        o = jnp.einsum('bhtk,btkd->bthd', p, v_sel)
        return o.reshape(B, Q_BLOCK, A_HEADS * A_KV_DIM).astype(h.dtype)

    out = lax.map(block, jnp.arange(S // Q_BLOCK))
    out = jnp.transpose(out, (1, 0, 2, 3)).reshape(B, S, A_HEADS * A_KV_DIM)
    return out @ w_out


def diff_mixer(h, w_in, w_out, lam, subln_g, cos, sin, layer_idx):
    B, S, _ = h.shape
    lam_init = 0.8 - 0.6 * math.exp(-0.3 * layer_idx)
    proj = h @ w_in
    q = apply_rope(proj[..., :B_QK].reshape(B, S, 2, B_HEADS, B_HEAD_DIM), cos, sin)
    k = apply_rope(proj[..., B_QK:2 * B_QK].reshape(B, S, 2, B_HEADS, B_HEAD_DIM), cos, sin)
    v = proj[..., 2 * B_QK:].reshape(B, S, B_HEADS, B_V_DIM)
    lam32 = lam.astype(jnp.float32)
    lam_val = (jnp.exp(jnp.sum(lam32[0] * lam32[1])) - jnp.exp(jnp.sum(lam32[2] * lam32[3]))
               + lam_init)
    k32 = k.astype(jnp.float32)
    v32 = v.astype(jnp.float32)
    key_pos = jnp.arange(S)
    scale = B_HEAD_DIM ** -0.5

    def block(i):
        start = i * Q_BLOCK
        t = start + jnp.arange(Q_BLOCK)
        q_b = lax.dynamic_slice_in_dim(q, start, Q_BLOCK, axis=1).astype(jnp.float32)
        s = jnp.einsum('btchd,bschd->bchts', q_b, k32) * scale
        causal = key_pos[None, :] <= t[:, None]
        s = jnp.where(causal, s, -jnp.inf)
        p = jax.nn.softmax(s, axis=-1)
        a = p[:, 0] - lam_val * p[:, 1]
        o = jnp.einsum('bhts,bshe->bthe', a, v32)
        o = rmsnorm(o, subln_g) * (1.0 - lam_init)
        return o.reshape(B, Q_BLOCK, B_HEADS * B_V_DIM).astype(h.dtype)

    out = lax.map(block, jnp.arange(S // Q_BLOCK))
    out = jnp.transpose(out, (1, 0, 2, 3)).reshape(B, S, B_HEADS * B_V_DIM)
    return out @ w_out


def setup_inputs(seed: int = 0) -> dict:
    key = jax.random.key(seed)
    ks = jax.random.split(key, 18)
    f32 = jnp.float32

    def nrm(k, shape, fan_in, scale=1.0):
        return jax.random.normal(k, shape, f32) * (scale * fan_in ** -0.5)

    x = jax.random.normal(ks[0], (BATCH, SEQ, D_MODEL), f32)
    c = jax.random.normal(ks[1], (BATCH, D_MODEL), f32)
    offsets = jax.random.randint(ks[2], (BATCH, 1), 0, 1024, dtype=jnp.int32)
    positions = offsets + jnp.arange(SEQ, dtype=jnp.int32)[None, :]
    return {
        "x": x,
        "c": c,
        "positions": positions,
        "ada_w": nrm(ks[3], (DEPTH, D_MODEL, N_MOD * D_MODEL), D_MODEL, 0.5),
        "ada_b": 0.02 * jax.random.normal(ks[4], (DEPTH, N_MOD * D_MODEL), f32),
        "pre_norm": 1.0 + 0.05 * jax.random.normal(ks[5], (DEPTH, 3, D_MODEL), f32),
        "post_norm": 1.0 + 0.05 * jax.random.normal(ks[6], (DEPTH, 3, D_MODEL), f32),
        "ffn_w_gate": nrm(ks[7], (DEPTH, 2, D_MODEL, D_FF), D_MODEL),
        "ffn_w_up": nrm(ks[8], (DEPTH, 2, D_MODEL, D_FF), D_MODEL),
        "ffn_w_down": nrm(ks[9], (DEPTH, 2, D_FF, D_MODEL), D_FF),
        "dsa_w_in": nrm(ks[10], (N_LAYERS_A, D_MODEL, A_IN), D_MODEL),
        "dsa_w_out": nrm(ks[11], (N_LAYERS_A, A_HEADS * A_KV_DIM, D_MODEL), A_HEADS * A_KV_DIM),
        "diff_w_in": nrm(ks[12], (N_LAYERS_B, D_MODEL, B_IN), D_MODEL),
        "diff_w_out": nrm(ks[13], (N_LAYERS_B, B_HEADS * B_V_DIM, D_MODEL), B_HEADS * B_V_DIM),
        "diff_lambda": 0.1 * jax.random.normal(ks[14], (N_LAYERS_B, 4, B_HEAD_DIM), f32),
        "diff_subln": 1.0 + 0.05 * jax.random.normal(ks[15], (N_LAYERS_B, B_V_DIM), f32),
    }


def reference(x, c, positions, ada_w, ada_b, pre_norm, post_norm, ffn_w_gate, ffn_w_up,
              ffn_w_down, dsa_w_in, dsa_w_out, diff_w_in, diff_w_out, diff_lambda, diff_subln):
    cos, sin = rope_tables(positions, A_HEAD_DIM)
    cond = jax.nn.silu(c)
    for i in range(DEPTH):
        mod = cond @ ada_w[i] + ada_b[i]
        sh0, sc0, g0, sh1, sc1, g1, sh2, sc2, g2 = jnp.split(mod, N_MOD, axis=-1)
        h = modulate(rmsnorm(x, pre_norm[i, 0]), sh0, sc0)
        y = swiglu(h, ffn_w_gate[i, 0], ffn_w_up[i, 0], ffn_w_down[i, 0])
        x = x + FFN_HALF * g0[:, None, :] * rmsnorm(y, post_norm[i, 0])
        h = modulate(rmsnorm(x, pre_norm[i, 1]), sh1, sc1)
        j = i // N_MIXERS
        if i % N_MIXERS == 0:
            y = dsa_mixer(h, dsa_w_in[j], dsa_w_out[j], cos, sin)
        else:
            y = diff_mixer(h, diff_w_in[j], diff_w_out[j], diff_lambda[j], diff_subln[j],
                           cos, sin, i)
        x = x + g1[:, None, :] * rmsnorm(y, post_norm[i, 1])
        h = modulate(rmsnorm(x, pre_norm[i, 2]), sh2, sc2)
        y = swiglu(h, ffn_w_gate[i, 1], ffn_w_up[i, 1], ffn_w_down[i, 1])
        x = x + FFN_HALF * g2[:, None, :] * rmsnorm(y, post_norm[i, 2])
    return x
```

```python
import math
from contextlib import ExitStack

import numpy as np
import concourse.bass as bass
import concourse.mybir as mybir
from concourse.bass_utils import run_bass_kernel_spmd

F32 = mybir.dt.float32
BF16 = mybir.dt.bfloat16
I32 = mybir.dt.int32
AF = mybir.ActivationFunctionType
ALU = mybir.AluOpType
AX = mybir.AxisListType

D = 1024
DFF = 2752
FFP = 2816
NF = 22
DEPTH = 4
EPS = 1e-6
A_IN = 1736
NEG = -30000.0
SEM_LIMIT = 12000
NBIS = 26
import os
STOP = int(os.environ.get('STOP', '99'))

W_GU = 11 * 2 * 8 * 256
W_D = NF * 1024
W_AIN = 8 * A_IN
W_AOUT = 8 * 1024
W_BIN = 8 * 1536
W_BOUT = 4 * 1024


def weight_offsets():
    off = {}
    o = 0
    for i in range(DEPTH):
        for j in range(2):
            off[("gu", i, j)] = o; o += W_GU
            off[("d", i, j)] = o; o += W_D
    for j in range(2):
        off[("ain", j)] = o; o += W_AIN
        off[("aout", j)] = o; o += W_AOUT
    for j in range(2):
        for g in range(2):
            off[("bin", j, g)] = o; o += W_BIN
            off[("bout", j, g)] = o; o += W_BOUT
    return off, o


WOFF, NTOT = weight_offsets()
CASTW = 4096
assert NTOT % CASTW == 0 or True


class Ev:
    __slots__ = ("sem", "val", "eng")

    def __init__(self, eng=None):
        self.sem = None
        self.val = 0
        self.eng = eng


class Buf:
    __slots__ = ("name", "w", "r")

    def __init__(self, name):
        self.name = name
        self.w = None
        self.r = {}


class Eng:
    def __init__(self, K, name, h):
        self.K = K
        self.name = name
        self.h = h
        self.sem = None
        self.cnt = 0
        self.seen = {}
        self.pending = []
        self.last = None
        self.n = 0


class _Slot:
    def __init__(self, K, name):
        self.sem = K.new_sem("d_" + name)
        self.cnt = 0
        self.name = name
        self.last = None
        K.slots.append(self)


def Slot(K, name):
    if K.free_slots:
        s = K.free_slots.pop()
    else:
        s = _Slot(K, name)
        s.name = f"s{len(K.slots)}"
    K.scope_slots.append(s)
    return s


class Kern:
    def __init__(self, nc):
        self.nc = nc
        self.es = ExitStack()
        self.nsem = 0
        self.slots = []
        self.pe = Eng(self, "pe", nc.tensor)
        self.act = Eng(self, "act", nc.scalar)
        self.dve = Eng(self, "dve", nc.vector)
        self.pool = Eng(self, "pool", nc.gpsimd)
        self.sp = Eng(self, "sp", nc.sync)
        self.engs = [self.pe, self.act, self.dve, self.pool, self.sp]
        self.ninstr = 0
        self.free_slots = []
        self.scope_slots = []

    def begin_scope(self):
        self._saved = self.scope_slots
        self.scope_slots = []

    def end_scope(self):
        self.free_slots.extend(self.scope_slots)
        self.scope_slots = self._saved

    def new_sem(self, name):
        self.nsem += 1
        return self.es.enter_context(self.nc.semaphore(f"{name}_{self.nsem}"))

    def _need(self, eng, ev, raw=False):
        if ev is None:
            return
        if ev.eng is eng and (not raw or eng is self.pe):
            return
        assert ev.sem is not None, "dependency on unsignaled instruction"
        k = id(ev.sem)
        if eng.seen.get(k, 0) >= ev.val:
            return
        eng.h.wait_ge(ev.sem, ev.val)
        eng.seen[k] = ev.val

    def _waits(self, eng, reads, writes):
        for b in reads:
            self._need(eng, b.w, raw=True)
        for b in writes:
            self._need(eng, b.w)
            for ev in b.r.values():
                self._need(eng, ev)

    def _record(self, ev, key, reads, writes):
        for b in writes:
            b.w = ev
            b.r = {}
        for b in reads:
            b.r[key] = ev

    def op(self, eng, fn, reads=(), writes=(), sig=True):
        self._waits(eng, reads, writes)
        ins = fn(eng.h)
        ev = Ev(eng)
        eng.n += 1
        self.ninstr += 1
        if sig:
            if eng.sem is None or eng.cnt >= SEM_LIMIT:
                eng.sem = self.new_sem(eng.name)
                eng.cnt = 0
            eng.cnt += 1
            ins.then_inc(eng.sem, 1)
            ev.sem, ev.val = eng.sem, eng.cnt
            for p in eng.pending:
                p.sem, p.val = eng.sem, eng.cnt
            eng.pending = []
            eng.last = ev
        else:
            eng.pending.append(ev)
        self._record(ev, eng.name, reads, writes)
        return ev

    def dma(self, q, slot, out, in_, reads=(), writes=()):
        self._waits(q, reads, writes)
        ins = q.h.dma_start(out=out, in_=in_)
        slot.cnt += 16
        ins.then_inc(slot.sem, 16)
        ev = Ev(None)
        ev.sem, ev.val = slot.sem, slot.cnt
        slot.last = ev
        self.ninstr += 1
        self._record(ev, "dma_" + slot.name, reads, writes)
        return ev

    def barrier(self):
        evs = []
        for e in self.engs:
            assert not e.pending, f"barrier with pending unsignaled instrs on {e.name}"
            if e.last is not None:
                evs.append(e.last)
        for s in self.slots:
            if s.last is not None:
                evs.append(s.last)
        for e in self.engs:
            for ev in evs:
                self._need(e, ev)


class Prog:
    def __init__(self, T, sublayers, do_cast=True):
        self.T = T
        self.NT = T // 128
        self.subl = sublayers
        self.do_cast = do_cast
        nc = bass.Bass("TRN2", target_bir_lowering=False)
        self.nc = nc
        self.K = Kern(nc)
        K = self.K
        NT = self.NT
        dt = nc.dram_tensor
        self.x_in = dt("x", [T, D], F32, kind="ExternalInput").ap()
        self.c_in = dt("c", [128, 8], F32, kind="ExternalInput").ap()
        self.pos_in = dt("pos", [128, NT], I32, kind="ExternalInput").ap()
        self.wall = dt("wall", [128, NTOT], F32, kind="ExternalInput").ap()
        self.ada = dt("ada", [12 * 24, 128, 1024], F32, kind="ExternalInput").ap()
        self.adab = dt("adab", [1, 12 * 3072], F32, kind="ExternalInput").ap()
        self.pre_n = dt("pre_n", [12, D], F32, kind="ExternalInput").ap()
        self.post_n = dt("post_n", [12, D], F32, kind="ExternalInput").ap()
        self.subln = dt("subln", [2, 128], F32, kind="ExternalInput").ap()
        self.lam = dt("lam", [2, 256], F32, kind="ExternalInput").ap()
        self.out = dt("out", [T, D], F32, kind="ExternalOutput").ap()
        self.debug = False
        self.dbg = None
        self.wbf = dt("wbf", [128, NTOT], BF16, kind="Internal").ap()
        self.ypart = dt("ypart", [T, D], F32, kind="Internal").ap()
        self.xbuf = [Buf(f"xd{i}") for i in range(NT)]
        self.ypbuf = [Buf(f"yp{i}") for i in range(NT)]
        self.wbf_buf = Buf("wbf")
        self.gs = ExitStack()
        self._names = 0

    def sb(self, st, name, shape, dtype):
        self._names += 1
        return st.enter_context(self.nc.sbuf_tensor(f"{name}_{self._names}", shape, dtype))

    def ps(self, st, name, shape, dtype):
        self._names += 1
        return st.enter_context(self.nc.psum_tensor(f"{name}_{self._names}", shape, dtype))

    def setup_globals(self):
        K, nc, st, NT = self.K, self.nc, self.gs, self.NT
        sb = lambda n, s, d: self.sb(st, n, s, d)
        self.TA = self.ps(st, "TA", [128, 512], F32)
        self.TAb = self.TA[:].bitcast(BF16)
        self.bTA = Buf("TA")
        self.identf = sb("identf", [128, 128], F32)
        self.ident = sb("ident", [128, 128], BF16)
        self.I4 = sb("I4", [128, 4, 128], BF16)
        self.CN8 = sb("CN8", [128, 8, 128], BF16)
        self.ones_row = sb("ones_row", [1, 128], F32)
        self.neghalf = sb("neghalf", [128, 16], F32)
        self.half = sb("half", [128, 16], F32)
        self.bconst = Buf("consts")
        self.cosT = sb("cosT", [128, NT, 8], F32)
        self.sinT = sb("sinT", [128, NT, 8], F32)
        self.brope = Buf("rope")
        self.condrep = sb("condrep", [128, 8, 128], F32)
        self.bcond = Buf("cond")
        self.vecA = [sb(f"vA{i}", [128, D], F32) for i in range(2)]
        self.vecS = [sb(f"vS{i}", [128, D], F32) for i in range(2)]
        self.vecG = [sb(f"vG{i}", [128, D], F32) for i in range(2)]
        self.bvec = [Buf(f"vec{i}") for i in range(2)]
        self.adaring = [sb(f"adar{i}", [128, 2, 512], F32) for i in range(2)]
        self.badar = [Buf(f"adar{i}") for i in range(2)]
        self.sadar = [Slot(K, f"adar{i}") for i in range(2)]
        self.pgb = [sb(f"pgb{i}", [128, D], F32) for i in range(2)]
        self.bpgb = [Buf(f"pgb{i}") for i in range(2)]
        self.spgb = [Slot(K, f"pgb{i}") for i in range(2)]
        self.brow = sb("brow", [1, 512], F32)
        self.bbrow = Buf("brow")
        self.sbrow = Slot(K, "brow")
        self.neglam = sb("neglam", [128, 2], F32)
        self.sublnb = sb("sublnb", [128, 2, 128], F32)
        self.bdiffc = Buf("diffc")
        self.adacnt = 0

        pool, dve, act = K.pool, K.dve, K.act
        bc = self.bconst
        K.op(pool, lambda e: e.memset(self.identf[:], 0.0), writes=[bc])
        K.op(pool, lambda e: e.affine_select(out=self.identf[:], in_=self.identf[:], pattern=[[-1, 128]],
                                             compare_op=ALU.not_equal, fill=1.0, base=0, channel_multiplier=1),
             writes=[bc])
        K.op(pool, lambda e: e.tensor_copy(out=self.ident[:], in_=self.identf[:]), writes=[bc])
        for r in range(4):
            K.op(pool, lambda e, r=r: e.tensor_copy(out=self.I4[:, r, :], in_=self.identf[:]), writes=[bc])
        K.op(pool, lambda e: e.memset(self.CN8[:], 0.0), writes=[bc])
        K.op(pool, lambda e: e.affine_select(out=self.CN8[:], in_=self.CN8[:], pattern=[[0, 8], [1, 128]],
                                             compare_op=ALU.is_ge, fill=NEG, base=0, channel_multiplier=-1),
             writes=[bc])
        K.op(pool, lambda e: e.memset(self.ones_row[:], 1.0), writes=[bc])
        K.op(pool, lambda e: e.memset(self.neghalf[:], -0.5), writes=[bc])
        K.op(pool, lambda e: e.memset(self.half[:], 0.5), writes=[bc])

        with ExitStack() as ts:
            tsb = lambda n, s, d: self.sb(ts, n, s, d)
            c_sb = tsb("c_sb", [128, 8], F32)
            cond = tsb("cond", [128, 8], F32)
            b_c = Buf("c_sb")
            s_c = Slot(K, "c_sb")
            K.dma(K.sp, s_c, c_sb[:], self.c_in, writes=[b_c])
            K.op(act, lambda e: e.activation(out=cond[:], in_=c_sb[:], func=AF.Silu), reads=[b_c], writes=[self.bcond])
            zer = tsb("zer", [128, 128], F32)
            K.op(dve, lambda e: e.memset(zer[:], 0.0), writes=[b_c])
            for kc in range(8):
                K.op(dve, lambda e, kc=kc: e.tensor_scalar(out=self.condrep[:, kc, :], in0=zer[:],
                                                           scalar1=cond[:, kc:kc + 1], scalar2=None, op0=ALU.add),
                     reads=[b_c, self.bcond], writes=[self.bcond])
            pos_i = tsb("pos_i", [128, NT], I32)
            pos_f = tsb("pos_f", [128, NT], F32)
            invt = tsb("invt", [128, 8], F32)
            ang = tsb("ang", [128, NT, 8], F32)
            ang2 = tsb("ang2", [128, NT, 8], F32)
            b_p = Buf("pos")
            s_p = Slot(K, "pos")
            K.dma(K.sp, s_p, pos_i[:], self.pos_in, writes=[b_p])
            K.op(dve, lambda e: e.tensor_copy(out=pos_f[:], in_=pos_i[:]), reads=[b_p], writes=[b_p])
            for j in range(8):
                inv = float(np.float32(500000.0) ** np.float32(-(2.0 * j) / 16.0))
                K.op(dve, lambda e, j=j, inv=inv: e.memset(invt[:, j:j + 1], inv), writes=[b_p])
            K.op(dve, lambda e: e.tensor_tensor(out=ang[:], in0=pos_f[:].rearrange("p (n o) -> p n o", o=1).to_broadcast([128, NT, 8]),
                                                in1=invt[:].rearrange("p (o j) -> p o j", o=1).to_broadcast([128, NT, 8]),
                                                op=ALU.mult), reads=[b_p], writes=[b_p])
            TWO_PI = 2.0 * math.pi
            C1 = 6.28125
            C2 = TWO_PI - C1
            kf = tsb("kf", [128, NT, 8], F32)
            ki = tsb("ki", [128, NT, 8], I32)
            mm_ = tsb("mm_", [128, NT, 8], F32)

            def sin_of(dst, shift):
                K.op(dve, lambda e: e.tensor_scalar(out=ang2[:], in0=ang[:], scalar1=shift, scalar2=None, op0=ALU.add),
                     reads=[b_p, self.brope], writes=[b_p])
                K.op(dve, lambda e: e.tensor_scalar(out=kf[:], in0=ang2[:], scalar1=1.0 / TWO_PI, scalar2=None, op0=ALU.mult),
                     reads=[b_p], writes=[b_p])
                K.op(dve, lambda e: e.tensor_copy(out=ki[:], in_=kf[:]), reads=[b_p], writes=[b_p])
                K.op(dve, lambda e: e.tensor_copy(out=kf[:], in_=ki[:]), reads=[b_p], writes=[b_p])
                K.op(dve, lambda e: e.scalar_tensor_tensor(out=ang2[:], in0=kf[:], scalar=-C1, in1=ang2[:], op0=ALU.mult, op1=ALU.add),
                     reads=[b_p], writes=[b_p])
                K.op(dve, lambda e: e.scalar_tensor_tensor(out=ang2[:], in0=kf[:], scalar=-C2, in1=ang2[:], op0=ALU.mult, op1=ALU.add),
                     reads=[b_p], writes=[b_p])
                K.op(dve, lambda e: e.tensor_scalar(out=mm_[:], in0=ang2[:], scalar1=math.pi, scalar2=TWO_PI, op0=ALU.is_gt, op1=ALU.mult),
                     reads=[b_p], writes=[b_p])
                K.op(dve, lambda e: e.tensor_tensor(out=ang2[:], in0=ang2[:], in1=mm_[:], op=ALU.subtract), reads=[b_p], writes=[b_p])
                K.op(dve, lambda e: e.tensor_scalar(out=mm_[:], in0=ang2[:], scalar1=-math.pi, scalar2=TWO_PI, op0=ALU.is_lt, op1=ALU.mult),
                     reads=[b_p], writes=[b_p])
                K.op(dve, lambda e: e.tensor_tensor(out=ang2[:], in0=ang2[:], in1=mm_[:], op=ALU.add), reads=[b_p], writes=[b_p])
                K.op(act, lambda e: e.activation(out=dst[:], in_=ang2[:], func=AF.Sin), reads=[b_p], writes=[self.brope])

            sin_of(self.sinT, 0.0)
            sin_of(self.cosT, 0.5 * math.pi)
            lam_sb = tsb("lam_sb", [128, 2, 4, 64], F32)
            sub_sb = tsb("sub_sb", [128, 2, 128], F32)
            lp = tsb("lp", [128, 2, 2, 64], F32)
            ls = tsb("ls", [128, 4], F32)
            b_l = Buf("lam")
            s_l = Slot(K, "lam")
            s_l2 = Slot(K, "lam2")
            K.dma(K.sp, s_l, lam_sb[:].rearrange("p a b c -> p (a b c)"),
                  self.lam.rearrange("a b -> (a b)").partition_broadcast(128), writes=[b_l])
            b_l2 = Buf("sub")
            K.dma(K.sp, s_l2, sub_sb[:].rearrange("p a b -> p (a b)"),
                  self.subln.rearrange("a b -> (a b)").partition_broadcast(128), writes=[b_l2])
            K.op(dve, lambda e: e.tensor_tensor(out=lp[:], in0=lam_sb[:, :, 0:4:2, :], in1=lam_sb[:, :, 1:4:2, :],
                                                op=ALU.mult), reads=[b_l], writes=[b_l])
            K.op(dve, lambda e: e.tensor_reduce(out=ls[:], in_=lp[:].rearrange("p a b c -> p (a b) c"), axis=AX.X,
                                                op=ALU.add), reads=[b_l], writes=[b_l])
            K.op(act, lambda e: e.activation(out=ls[:], in_=ls[:], func=AF.Exp), reads=[b_l], writes=[b_l])
            for j in range(2):
                li = 0.8 - 0.6 * math.exp(-0.3 * (2 * j + 1))
                K.op(dve, lambda e, j=j, li=li: e.scalar_tensor_tensor(out=self.neglam[:, j:j + 1], in0=ls[:, 2 * j + 1:2 * j + 2],
                                                                       scalar=-li, in1=ls[:, 2 * j:2 * j + 1],
                                                                       op0=ALU.add, op1=ALU.subtract),
                     reads=[b_l], writes=[self.bdiffc])
                K.op(dve, lambda e, j=j, li=li: e.tensor_scalar(out=self.sublnb[:, j, :], in0=sub_sb[:, j, :],
                                                                scalar1=1.0 - li, scalar2=None, op0=ALU.mult),
                     reads=[b_l2], writes=[self.bdiffc])
            K.barrier()

    def cast_weights(self, ranges):
        K = self.K
        K.begin_scope()
        with ExitStack() as ts:
            NB = 3
            fin = [self.sb(ts, f"cin{i}", [128, CASTW], F32) for i in range(NB)]
            fout = [self.sb(ts, f"cout{i}", [128, CASTW], BF16) for i in range(NB)]
            bin_ = [Buf(f"cin{i}") for i in range(NB)]
            bout = [Buf(f"cout{i}") for i in range(NB)]
            sin_ = [Slot(K, f"cin{i}") for i in range(NB)]
            sout = [Slot(K, f"cout{i}") for i in range(NB)]
            pieces = []
            for (s0, ln) in ranges:
                o = 0
                while o < ln:
                    w = min(CASTW, ln - o)
                    pieces.append((s0 + o, w))
                    o += w
            engs = [K.dve, K.pool, K.act]
            for n, (c0, w) in enumerate(pieces):
                i = n % NB
                K.dma(K.sp, sin_[i], fin[i][:, 0:w], self.wall[:, c0:c0 + w], writes=[bin_[i]])
                eng = engs[n % 3]
                if eng is K.act:
                    K.op(eng, lambda e, i=i, w=w: e.activation(out=fout[i][:, 0:w], in_=fin[i][:, 0:w], func=AF.Copy),
                         reads=[bin_[i]], writes=[bout[i]])
                else:
                    K.op(eng, lambda e, i=i, w=w: e.tensor_copy(out=fout[i][:, 0:w], in_=fin[i][:, 0:w]),
                         reads=[bin_[i]], writes=[bout[i]])
                K.dma(K.sp, sout[i], self.wbf[:, c0:c0 + w], fout[i][:, 0:w], reads=[bout[i]], writes=[self.wbf_buf])
            K.barrier()
        K.end_scope()

    def ada_steps(self, s_glob, vs, coef):
        K = self.K
        pe, dve, act = K.pe, K.dve, K.act

        def load_pg():
            K.dma(K.sp, self.spgb[0], self.pgb[0][:], self.pre_n[s_glob, :].partition_broadcast(128), writes=[self.bpgb[0]])
            K.dma(K.sp, self.spgb[1], self.pgb[1][:], self.post_n[s_glob, :].partition_broadcast(128), writes=[self.bpgb[1]])

        def step(c):
            if c == 0:
                load_pg()
            K.dma(K.sp, self.sbrow, self.brow[:], self.adab[0:1, s_glob * 3072 + c * 512: s_glob * 3072 + (c + 1) * 512],
                  writes=[self.bbrow])
            for kh in range(4):
                r = self.adacnt % 2
                self.adacnt += 1
                K.dma(K.sp, self.sadar[r], self.adaring[r][:].rearrange("p a b -> p (a b)"),
                      self.ada[s_glob * 24 + c * 4 + kh], writes=[self.badar[r]])
                for kl in range(2):
                    kc = kh * 2 + kl
                    K.op(pe, lambda e, r=r, kl=kl, kc=kc: e.matmul(self.TA[:], lhsT=self.condrep[:, kc, :], rhs=self.adaring[r][:, kl, :],
                                                                    start=(kc == 0), stop=False),
                         reads=[self.bcond, self.badar[r]], writes=[self.bTA], sig=(kl == 1))
            K.op(pe, lambda e: e.matmul(self.TA[:], lhsT=self.ones_row[0:1, :], rhs=self.brow[0:1, :], start=False, stop=True),
                 reads=[self.bconst, self.bbrow], writes=[self.bTA])
            cs = slice((c % 2) * 512, (c % 2) * 512 + 512)
            if c < 2:
                K.op(act, lambda e: e.activation(out=self.vecS[vs][:, cs], in_=self.TA[:], func=AF.Copy),
                     reads=[self.bTA], writes=[self.bvec[vs]])
            elif c < 4:
                K.op(dve, lambda e: e.scalar_tensor_tensor(out=self.vecA[vs][:, cs], in0=self.TA[:], scalar=1.0,
                                                           in1=self.pgb[0][:, cs], op0=ALU.add, op1=ALU.mult),
                     reads=[self.bTA, self.bpgb[0]], writes=[self.bvec[vs]])
            else:
                K.op(dve, lambda e: e.scalar_tensor_tensor(out=self.vecG[vs][:, cs], in0=self.TA[:], scalar=coef,
                                                           in1=self.pgb[1][:, cs], op0=ALU.mult, op1=ALU.mult),
                     reads=[self.bTA, self.bpgb[1]], writes=[self.bvec[vs]])

        return [lambda c=c: step(c) for c in range(6)]

    def norm_mod_T(self, vs, xt, bx, hb, bhb, tmp, btmp, small, bsmall, junk, bjunk, hT_out, bhT):
        K = self.K
        pe, dve, act, pool = K.pe, K.dve, K.act, K.pool
        ss = small[:, 0:1]
        rs = small[:, 1:2]
        K.op(act, lambda e: e.activation(out=junk[:], in_=xt, func=AF.Square, accum_out=ss), reads=[bx], writes=[bjunk, bsmall])
        K.op(pool, lambda e: e.tensor_scalar(out=rs, in0=ss, scalar1=1.0 / D, scalar2=EPS, op0=ALU.mult, op1=ALU.add),
             reads=[bsmall], writes=[bsmall])
        K.op(pool, lambda e: e.tensor_tensor(out=rs, in0=rs, in1=self.neghalf[:, 0:1], op=ALU.pow),
             reads=[bsmall, self.bconst], writes=[bsmall])
        K.op(dve, lambda e: e.scalar_tensor_tensor(out=tmp[:], in0=xt, scalar=rs, in1=self.vecA[vs][:], op0=ALU.mult, op1=ALU.mult),
             reads=[bx, bsmall, self.bvec[vs]], writes=[btmp])
        K.op(dve, lambda e: e.tensor_tensor(out=hb[:], in0=tmp[:], in1=self.vecS[vs][:], op=ALU.add),
             reads=[btmp, self.bvec[vs]], writes=[bhb])
        for kc in range(8):
            K.op(pe, lambda e, kc=kc: e.transpose(out=self.TAb[:, kc * 128:(kc + 1) * 128], in_=hb[:, kc * 128:(kc + 1) * 128],
                                                  identity=self.ident[:]),
                 reads=[bhb, self.bconst], writes=[self.bTA], sig=(kc == 7))
        K.op(act, lambda e: e.activation(out=hT_out, in_=self.TAb[:].rearrange("p (a b) -> p a b", a=8), func=AF.Copy),
             reads=[self.bTA], writes=[bhT])

    def post_resid(self, vs, y_srcs, by, xt, bx, xo, bxo, small, bsmall, junk, bjunk, tmp, btmp):
        K = self.K
        dve, act, pool = K.dve, K.act, K.pool
        n = len(y_srcs)
        for k, (yap, c0, w) in enumerate(y_srcs):
            K.op(act, lambda e, yap=yap, c0=c0, w=w, k=k: e.activation(out=junk[:, c0:c0 + w], in_=yap, func=AF.Square,
                                                                        accum_out=small[:, 4 + k:5 + k]),
                 reads=by, writes=[bjunk, bsmall])
        rs = small[:, 3:4]
        if n == 2:
            K.op(pool, lambda e: e.tensor_tensor(out=rs, in0=small[:, 4:5], in1=small[:, 5:6], op=ALU.add),
                 reads=[bsmall], writes=[bsmall])
            src = rs
        else:
            src = small[:, 4:5]
        K.op(pool, lambda e: e.tensor_scalar(out=rs, in0=src, scalar1=1.0 / D, scalar2=EPS, op0=ALU.mult, op1=ALU.add),
             reads=[bsmall], writes=[bsmall])
        K.op(pool, lambda e: e.tensor_tensor(out=rs, in0=rs, in1=self.neghalf[:, 0:1], op=ALU.pow),
             reads=[bsmall, self.bconst], writes=[bsmall])
        for (yap, c0, w) in y_srcs:
            K.op(dve, lambda e, yap=yap, c0=c0, w=w: e.scalar_tensor_tensor(out=tmp[:, c0:c0 + w], in0=yap, scalar=rs,
                                                                             in1=self.vecG[vs][:, c0:c0 + w],
                                                                             op0=ALU.mult, op1=ALU.mult),
                 reads=list(by) + [bsmall, self.bvec[vs]], writes=[btmp])
        K.op(dve, lambda e: e.tensor_tensor(out=xo[:], in0=tmp[:], in1=xt, op=ALU.add), reads=[btmp, bx], writes=[bxo])

    def ffn(self, x_src, vs, off_gu, off_d, bg):
        K, T, NT = self.K, self.T, self.NT
        pe, dve, act, pool, sp = K.pe, K.dve, K.act, K.pool, K.sp
        NTT = T // 512
        K.begin_scope()
        with ExitStack() as st:
            sb = lambda n, s, d: self.sb(st, n, s, d)
            NG = 3
            wgu = [sb(f"wgu{i}", [128, 2, 8, 256], BF16) for i in range(NG)]
            bwgu = [Buf(f"wgu{i}") for i in range(NG)]
            swgu = [Slot(K, f"wgu{i}") for i in range(NG)]
            wd = sb("wd", [128, NF, 1024], BF16)
            bwd = Buf("wd")
            swd = Slot(K, "wd")
            xa = [sb(f"xa{i}", [128, D], F32) for i in range(2)]
            bxa = [Buf(f"xa{i}") for i in range(2)]
            sxa = [Slot(K, f"xa{i}") for i in range(2)]
            xc = [sb(f"xc{i}", [128, D], F32) for i in range(2)]
            bxc = [Buf(f"xc{i}") for i in range(2)]
            sxc = [Slot(K, f"xc{i}") for i in range(2)]
            xo = [sb(f"xo{i}", [128, D], F32) for i in range(2)]
            bxo = [Buf(f"xo{i}") for i in range(2)]
            sxo = [Slot(K, f"xo{i}") for i in range(2)]
            tmp = sb("tmp", [128, D], F32)
            btmp = Buf("tmp")
            hb = [sb(f"hb{i}", [128, D], BF16) for i in range(2)]
            bhb = [Buf(f"hb{i}") for i in range(2)]
            hT = [sb(f"hT{i}", [128, 8, 512], BF16) for i in range(2)]
            bhT = [Buf(f"hT{i}") for i in range(2)]
            actT = sb("actT", [128, NF, 512], BF16)
            bactT = Buf("actT")
            gt = [sb(f"gt{i}", [128, 512], F32) for i in range(2)]
            bgt = [Buf(f"gt{i}") for i in range(2)]
            junk = sb("junk", [128, D], BF16)
            bjunk = Buf("junk")
            small = [sb(f"small{i}", [128, 8], F32) for i in range(2)]
            bsmall = [Buf(f"small{i}") for i in range(2)]
            GU = [self.ps(st, f"GU{i}", [128, 512], F32) for i in range(3)]
            bGU = [Buf(f"GU{i}") for i in range(3)]
            Y = [self.ps(st, f"Y{i}", [128, 512], F32) for i in range(3)]
            bY = [Buf(f"Y{i}") for i in range(3)]

            wcols = self.wbf
            for q4 in range(2):
                K.dma(sp, swd, wd[:, q4 * 11:(q4 + 1) * 11, :].rearrange("p a b -> p (a b)"),
                      wcols[:, off_d + q4 * 11 * 1024: off_d + (q4 + 1) * 11 * 1024], reads=[self.wbf_buf], writes=[bwd])

            gu_seq = [(n, g) for n in range(NTT) for g in range(11)]
            self._gu_loaded = 0

            def load_gu(upto):
                while self._gu_loaded < min(upto, len(gu_seq)):
                    k = self._gu_loaded
                    n, g = gu_seq[k]
                    i = k % NG
                    K.dma(sp, swgu[i], wgu[i][:].rearrange("p a b c -> p (a b c)"),
                          wcols[:, off_gu + g * 4096: off_gu + (g + 1) * 4096], reads=[self.wbf_buf], writes=[bwgu[i]])
                    self._gu_loaded += 1

            acnt = [0]

            def phaseA(n, j):
                a = acnt[0] % 2
                acnt[0] += 1
                tile = n * 4 + j
                K.dma(pool, sxa[a], xa[a][:], x_src[tile * 128:(tile + 1) * 128, :], reads=[self.xbuf[tile]], writes=[bxa[a]])
                self.norm_mod_T(vs, xa[a][:], bxa[a], hb[a], bhb[a], tmp, btmp, small[0], bsmall[0], junk, bjunk,
                                hT[n % 2][:, :, j * 128:(j + 1) * 128], bhT[n % 2])

            for j in range(4):
                phaseA(0, j)
            load_gu(NG)
            ccnt = [0]
            bgq = list(bg)
            for n in range(NTT):
                cur = n % 2
                for g in range(11):
                    k = n * 11 + g
                    i = k % NG
                    for fl in range(2):
                        f = 2 * g + fl
                        bg_, bu_ = (2 * f) % 3, (2 * f + 1) % 3
                        for kc in range(8):
                            K.op(pe, lambda e, i=i, fl=fl, kc=kc, bg_=bg_: e.matmul(GU[bg_][:], lhsT=wgu[i][:, 0, kc, fl * 128:(fl + 1) * 128],
                                                                                     rhs=hT[cur][:, kc, :], start=(kc == 0), stop=(kc == 7)),
                                 reads=[bwgu[i], bhT[cur]], writes=[bGU[bg_]], sig=(kc == 7))
                        for kc in range(8):
                            K.op(pe, lambda e, i=i, fl=fl, kc=kc, bu_=bu_: e.matmul(GU[bu_][:], lhsT=wgu[i][:, 1, kc, fl * 128:(fl + 1) * 128],
                                                                                     rhs=hT[cur][:, kc, :], start=(kc == 0), stop=(kc == 7)),
                                 reads=[bwgu[i], bhT[cur]], writes=[bGU[bu_]], sig=(kc == 7))
                        K.op(act, lambda e, f=f, bg_=bg_: e.activation(out=gt[f % 2][:], in_=GU[bg_][:], func=AF.Silu),
                             reads=[bGU[bg_]], writes=[bgt[f % 2]])
                        K.op(dve, lambda e, f=f, bu_=bu_: e.tensor_tensor(out=actT[:, f, :], in0=gt[f % 2][:], in1=GU[bu_][:], op=ALU.mult),
                             reads=[bgt[f % 2], bGU[bu_]], writes=[bactT])
                    load_gu(k + 1 + NG)
                    if n + 1 < NTT and g in (1, 3, 5, 7):
                        phaseA(n + 1, (g - 1) // 2)
                    if g == 9 and bgq:
                        bgq.pop(0)()
                for j in range(4):
                    tile = n * 4 + j
                    c = ccnt[0] % 2
                    ccnt[0] += 1
                    K.dma(pool, sxc[c], xc[c][:], x_src[tile * 128:(tile + 1) * 128, :], reads=[self.xbuf[tile]], writes=[bxc[c]])
                    ybs = []
                    for hf in range(2):
                        yb = (2 * j + hf) % 3
                        ybs.append(yb)
                        for f in range(NF):
                            K.op(pe, lambda e, f=f, j=j, hf=hf, yb=yb: e.matmul(Y[yb][:], lhsT=actT[:, f, j * 128:(j + 1) * 128],
                                                                                 rhs=wd[:, f, hf * 512:(hf + 1) * 512],
                                                                                 start=(f == 0), stop=(f == NF - 1)),
                                 reads=[bactT, bwd], writes=[bY[yb]], sig=(f == NF - 1))
                    self.post_resid(vs, [(Y[ybs[0]][:], 0, 512), (Y[ybs[1]][:], 512, 512)], [bY[ybs[0]], bY[ybs[1]]],
                                    xc[c][:], bxc[c], xo[c], bxo[c], small[1], bsmall[1], junk, bjunk, tmp, btmp)
                    K.dma(pool, sxo[c], self.out[tile * 128:(tile + 1) * 128, :], xo[c][:], reads=[bxo[c]], writes=[self.xbuf[tile]])
            while bgq:
                bgq.pop(0)()
            K.barrier()
        K.end_scope()

    def build(self):
        K = self.K
        self.setup_globals()
        if self.do_cast:
            need = []
            for (li, kind) in self.subl:
                if kind in ("f0", "f1"):
                    j = 0 if kind == "f0" else 1
                    need.append((WOFF[("gu", li, j)], W_GU + W_D))
                elif li % 2 == 0:
                    need.append((WOFF[("ain", li // 2)], W_AIN + W_AOUT))
                else:
                    need.append((WOFF[("bin", li // 2, 0)], 2 * (W_BIN + W_BOUT)))
            self.cast_weights(need)
        x_src = self.x_in
        nsub = len(self.subl)

        def sglob(li, kind):
            return li * 3 + {"f0": 0, "mix": 1, "f1": 2}[kind]

        def coef(kind):
            return 1.0 if kind == "mix" else 0.5

        li, kind = self.subl[0]
        for stp in self.ada_steps(sglob(li, kind), 0, coef(kind)):
            stp()
        for si, (li, kind) in enumerate(self.subl):
            vs = si % 2
            bg = []
            if si + 1 < nsub:
                nl, nk = self.subl[si + 1]
                bg = self.ada_steps(sglob(nl, nk), (si + 1) % 2, coef(nk))
            if kind in ("f0", "f1"):
                j = 0 if kind == "f0" else 1
                self.ffn(x_src, vs, WOFF[("gu", li, j)], WOFF[("d", li, j)], bg)
            elif li % 2 == 0:
                self.dsa(x_src, vs, li // 2, bg)
            else:
                self.diff(x_src, vs, li // 2, bg)
            x_src = self.out
        K.barrier()
        return self.nc

    def neg_bound(self, qsq, bq, ksq, bk, run, brun, mneg, bmneg, small, bsmall):
        K = self.K
        pe, dve, act, pool = K.pe, K.dve, K.act, K.pool
        TAr = self.TA[0:1, 0:256]
        K.op(pe, lambda e: e.transpose(out=self.TA[0:1, 0:128], in_=qsq, identity=self.identf[:]),
             reads=[bq, self.bconst], writes=[self.bTA], sig=False)
        K.op(pe, lambda e: e.transpose(out=self.TA[0:1, 128:256], in_=ksq, identity=self.identf[:]),
             reads=[bk, self.bconst], writes=[self.bTA])
        K.op(dve, lambda e: e.tensor_reduce(out=small[0:1, 0:2], in_=TAr.rearrange("p (a b) -> p a b", a=2), axis=AX.X, op=ALU.max),
             reads=[self.bTA], writes=[bsmall])
        K.op(dve, lambda e: e.tensor_tensor(out=run[0:1, 0:1], in0=run[0:1, 0:1], in1=small[0:1, 1:2], op=ALU.max),
             reads=[bsmall], writes=[brun])
        K.op(dve, lambda e: e.tensor_tensor(out=small[0:1, 2:3], in0=small[0:1, 0:1], in1=run[0:1, 0:1], op=ALU.mult),
             reads=[bsmall, brun], writes=[bsmall])
        K.op(pe, lambda e: e.matmul(self.TA[:, 0:1], lhsT=self.ones_row[0:1, :], rhs=small[0:1, 2:3], start=True, stop=True),
             reads=[bsmall, self.bconst], writes=[self.bTA])
        K.op(dve, lambda e: e.tensor_copy(out=mneg[:, 2:3], in_=self.TA[:, 0:1]), reads=[self.bTA], writes=[bmneg])
        K.op(pool, lambda e: e.tensor_tensor(out=mneg[:, 2:3], in0=mneg[:, 2:3], in1=self.half[:, 0:1], op=ALU.pow),
             reads=[bmneg, self.bconst], writes=[bmneg])
        K.op(pool, lambda e: e.tensor_scalar(out=mneg[:, 0:1], in0=mneg[:, 2:3], scalar1=-0.125, scalar2=None, op0=ALU.mult),
             reads=[bmneg], writes=[bmneg])

    def rope(self, pe_t, bpe, h0, nh, ti, rt, brt):
        K = self.K
        dve = K.dve
        v = pe_t[:, h0 * 64:(h0 + nh) * 64].rearrange("p (h d) -> p h d", d=64)
        x1 = v[:, :, 0:8]
        x2 = v[:, :, 8:16]
        cb = self.cosT[:, ti:ti + 1, :].to_broadcast([128, nh, 8])
        sbb = self.sinT[:, ti:ti + 1, :].to_broadcast([128, nh, 8])
        t = [rt[:, k, 0:nh, :] for k in range(4)]
        rd = [bpe, self.brope]
        K.op(dve, lambda e: e.tensor_tensor(out=t[0], in0=x1, in1=cb, op=ALU.mult), reads=rd, writes=[brt])
        K.op(dve, lambda e: e.tensor_tensor(out=t[1], in0=x2, in1=sbb, op=ALU.mult), reads=rd, writes=[brt])
        K.op(dve, lambda e: e.tensor_tensor(out=t[2], in0=x2, in1=cb, op=ALU.mult), reads=rd, writes=[brt])
        K.op(dve, lambda e: e.tensor_tensor(out=t[3], in0=x1, in1=sbb, op=ALU.mult), reads=rd, writes=[brt])
        K.op(dve, lambda e: e.tensor_tensor(out=x1, in0=t[0], in1=t[1], op=ALU.subtract), reads=[brt], writes=[bpe])
        K.op(dve, lambda e: e.tensor_tensor(out=x2, in0=t[2], in1=t[3], op=ALU.add), reads=[brt], writes=[bpe])

    def dsa(self, x_src, vs, jl, bg):
        K, T, NT = self.K, self.T, self.NT
        pe, dve, act, pool, sp = K.pe, K.dve, K.act, K.pool, K.sp
        off_in, off_out = WOFF[("ain", jl)], WOFF[("aout", jl)]
        K.begin_scope()
        with ExitStack() as st:
            sb = lambda n, s, d: self.sb(st, n, s, d)
            w_in = sb("w_in", [128, 8, A_IN], BF16); bw = Buf("w"); sw = Slot(K, "w")
            w_out = sb("w_out", [128, 8, D], BF16)
            kT2 = sb("kT2", [128, T], BF16); bkT = Buf("kT2")
            kiT2 = sb("kiT2", [128, T], BF16); bkiT = Buf("kiT2")
            Vaug = sb("Vaug", [128, NT, 65], BF16); bV = Buf("Vaug")
            score = sb("score", [128, T], F32); bscore = Buf("score")
            NM = sb("NM", [128, T], BF16); bNM = Buf("NM")
            rtmp = [sb(f"rtmp{i}", [128, 512], F32) for i in range(2)]; brtmp = [Buf(f"rtmp{i}") for i in range(2)]
            PT = [sb(f"PT{i}", [128, 16, 128], BF16) for i in range(2)]; bPT = [Buf(f"PT{i}") for i in range(2)]
            pev = sb("pev", [128, A_IN], F32); bpev = Buf("pev")
            rt = sb("rt", [128, 4, 17, 8], F32); brt = Buf("rt")
            qb = sb("qb", [128, 1152], BF16); bqb = Buf("qb")
            qib = sb("qib", [128, 640], BF16); bqib = Buf("qib")
            qT = sb("qT", [128, 8, 128], BF16); bqT = Buf("qT")
            qiT = sb("qiT", [128, 4, 128], BF16); bqiT = Buf("qiT")
            wi = sb("wi", [128, 8], F32); bwi = Buf("wi")
            xa = [sb(f"xa{i}", [128, D], F32) for i in range(2)]; bxa = [Buf(f"xa{i}") for i in range(2)]
            sxa = [Slot(K, f"xa{i}") for i in range(2)]
            sxo = [Slot(K, f"xo{i}") for i in range(2)]
            tmp = sb("tmp", [128, D], F32); btmp = Buf("tmp")
            hb = sb("hb", [128, D], BF16); bhb = Buf("hb")
            hT = sb("hT", [128, 8, 128], BF16); bhT = Buf("hT")
            ob = sb("ob", [128, D], BF16); bob = Buf("ob")
            oT = sb("oT", [128, 8, 128], BF16); boT = Buf("oT")
            junk = sb("junk", [128, D], BF16); bjunk = Buf("junk")
            small = [sb(f"small{i}", [128, 8], F32) for i in range(2)]; bsmall = [Buf(f"small{i}") for i in range(2)]
            sq = sb("sq", [128, 24], F32); bsq = Buf("sq")
            ksqt = sb("ksqt", [128, 64], F32)
            run = sb("run", [128, 2], F32); brun = Buf("run")
            mneg = sb("mneg", [128, 4], F32); bmneg = Buf("mneg")
            bis = sb("bis", [128, 8], F32); bbis = Buf("bis")
            dk = sb("dk", [128, NBIS + 2], F32)
            p2 = sb("p2", [128, NBIS + 2], F32)
            rl = sb("rl", [128, 16], F32); brl = Buf("rl")
            P = [self.ps(st, f"P{i}", [128, 512], F32) for i in range(4)]; bP = [Buf(f"P{i}") for i in range(4)]
            O = self.ps(st, "O", [128, 1536], F32); bO = Buf("O")

            K.dma(sp, sw, w_in[:].rearrange("p a b -> p (a b)"), self.wbf[:, off_in:off_in + W_AIN], reads=[self.wbf_buf], writes=[bw])
            K.dma(sp, sw, w_out[:].rearrange("p a b -> p (a b)"), self.wbf[:, off_out:off_out + W_AOUT], reads=[self.wbf_buf], writes=[bw])
            K.op(pool, lambda e: e.memset(Vaug[:, :, 64:65], 1.0), writes=[bV])
            K.op(pool, lambda e: e.memset(run[:], 0.0), writes=[brun])
            for k in range(NBIS + 2):
                K.op(pool, lambda e, k=k: e.memset(p2[:, k:k + 1], 2.0 ** (-(k + 1))), writes=[bbis])
            bgq = list(bg)
            pcnt = [0]
            if not hasattr(self, "fill_reg"):
                self.fill_reg = self.nc.gpsimd.to_reg(-1e30)

            def pbank():
                b = pcnt[0] % 4
                pcnt[0] += 1
                return b

            for i in range(NT):
                a = i % 2
                n = (i + 1) * 128
                K.dma(pool, sxa[a], xa[a][:], x_src[i * 128:(i + 1) * 128, :], reads=[self.xbuf[i]], writes=[bxa[a]])
                self.norm_mod_T(vs, xa[a][:], bxa[a], hb, bhb, tmp, btmp, small[0], bsmall[0], junk, bjunk, hT[:], bhT)
                for b in range(4):
                    c0, c1 = b * 512, min((b + 1) * 512, A_IN)
                    for kc in range(8):
                        K.op(pe, lambda e, b=b, kc=kc, c0=c0, c1=c1: e.matmul(P[b][:, 0:c1 - c0], lhsT=hT[:, kc, :], rhs=w_in[:, kc, c0:c1],
                                                                             start=(kc == 0), stop=(kc == 7)),
                             reads=[bhT, bw], writes=[bP[b]], sig=(kc == 7))
                    K.op(act, lambda e, b=b, c0=c0, c1=c1: e.activation(out=pev[:, c0:c1], in_=P[b][:, 0:c1 - c0], func=AF.Copy),
                         reads=[bP[b]], writes=[bpev])
                pcnt[0] = 0
                self.rope(pev[:], bpev, 0, 17, i, rt, brt)
                self.rope(pev[:], bpev, 18, 9, i, rt, brt)
                K.op(act, lambda e: e.activation(out=qb[:, 0:1088], in_=pev[:, 0:1088], func=AF.Copy), reads=[bpev], writes=[bqb])
                K.op(act, lambda e: e.activation(out=qb[:, 1088:1152], in_=pev[:, 1024:1088], func=AF.Copy), reads=[bpev], writes=[bqb])
                K.op(act, lambda e, i=i: e.activation(out=Vaug[:, i, 0:64], in_=pev[:, 1088:1152], func=AF.Copy), reads=[bpev], writes=[bV])
                K.op(act, lambda e: e.activation(out=qib[:, 0:576], in_=pev[:, 1152:1728], func=AF.Copy), reads=[bpev], writes=[bqib])
                K.op(act, lambda e: e.activation(out=qib[:, 576:640], in_=pev[:, 1664:1728], func=AF.Copy), reads=[bpev], writes=[bqib])
                K.op(act, lambda e: e.activation(out=wi[:], in_=pev[:, 1728:1736], func=AF.Copy), reads=[bpev], writes=[bwi])
                K.op(act, lambda e: e.activation(out=tmp[:], in_=pev[:, 0:1024], func=AF.Square), reads=[bpev], writes=[btmp])
                K.op(act, lambda e: e.activation(out=ksqt[:], in_=pev[:, 1024:1088], func=AF.Square), reads=[bpev], writes=[btmp])
                K.op(dve, lambda e: e.tensor_reduce(out=sq[:, 0:16], in_=tmp[:].rearrange("p (h d) -> p h d", d=64),
                                                    axis=AX.X, op=ALU.add), reads=[btmp], writes=[bsq])
                K.op(dve, lambda e: e.tensor_reduce(out=sq[:, 16:17], in_=ksqt[:], axis=AX.X, op=ALU.add), reads=[btmp], writes=[bsq])
                K.op(dve, lambda e: e.tensor_reduce(out=sq[:, 20:21], in_=sq[:, 0:16], axis=AX.X, op=ALU.max), reads=[bsq], writes=[bsq])
                for s_ in range(8):
                    K.op(pe, lambda e, s_=s_: e.transpose(out=self.TAb[:, s_ * 128:(s_ + 1) * 128], in_=qb[:, s_ * 128:(s_ + 1) * 128],
                                                           identity=self.ident[:]),
                         reads=[bqb, self.bconst], writes=[self.bTA], sig=(s_ == 7))
                K.op(act, lambda e: e.activation(out=qT[:], in_=self.TAb[:].rearrange("p (a b) -> p a b", a=8), func=AF.Copy),
                     reads=[self.bTA], writes=[bqT])
                K.op(pe, lambda e: e.transpose(out=self.TAb[:, 0:128], in_=qb[:, 1024:1152], identity=self.ident[:]),
                     reads=[bqb, self.bconst], writes=[self.bTA], sig=False)
                for s_ in range(5):
                    K.op(pe, lambda e, s_=s_: e.transpose(out=self.TAb[:, (s_ + 1) * 128:(s_ + 2) * 128], in_=qib[:, s_ * 128:(s_ + 1) * 128],
                                                           identity=self.ident[:]),
                         reads=[bqib, self.bconst], writes=[self.bTA], sig=(s_ == 4))
                K.op(dve, lambda e, i=i: e.tensor_copy(out=kT2[:, i * 128:(i + 1) * 128], in_=self.TAb[:, 0:128]),
                     reads=[self.bTA], writes=[bkT])
                K.op(dve, lambda e: e.tensor_copy(out=qiT[:], in_=self.TAb[:, 128:640].rearrange("p (a b) -> p a b", a=4)),
                     reads=[self.bTA], writes=[bqiT])
                K.op(dve, lambda e, i=i: e.tensor_copy(out=kiT2[:, i * 128:(i + 1) * 128], in_=self.TAb[:, 640:768]),
                     reads=[self.bTA], writes=[bkiT])
                self.neg_bound(sq[:, 20:21], bsq, sq[:, 16:17], bsq, run, brun, mneg, bmneg, small[1], bsmall[1])
                nblk = (n + 511) // 512
                rc = 0
                for sbk in range(nblk):
                    w = min(512, n - sbk * 512)
                    cs = slice(sbk * 512, sbk * 512 + w)
                    for h in range(8):
                        s_, hf = h % 4, h // 4
                        b = pbank()
                        K.op(pe, lambda e, b=b, s_=s_, hf=hf, cs=cs, w=w: e.matmul(P[b][:, 0:w], lhsT=qiT[hf * 64:(hf + 1) * 64, s_, :],
                                                                                    rhs=kiT2[hf * 64:(hf + 1) * 64, cs], start=True, stop=True),
                             reads=[bqiT, bkiT], writes=[bP[b]])
                        r = rc % 2
                        rc += 1
                        K.op(act, lambda e, b=b, r=r, w=w: e.activation(out=rtmp[r][:, 0:w], in_=P[b][:, 0:w], func=AF.Relu),
                             reads=[bP[b]], writes=[brtmp[r]])
                        if h == 0:
                            K.op(dve, lambda e, r=r, w=w, cs=cs, h=h: e.tensor_scalar(out=score[:, cs], in0=rtmp[r][:, 0:w], scalar1=wi[:, h:h + 1],
                                                                                       scalar2=None, op0=ALU.mult),
                                 reads=[brtmp[r], bwi], writes=[bscore])
                        else:
                            K.op(dve, lambda e, r=r, w=w, cs=cs, h=h: e.scalar_tensor_tensor(out=score[:, cs], in0=rtmp[r][:, 0:w], scalar=wi[:, h:h + 1],
                                                                                              in1=score[:, cs], op0=ALU.mult, op1=ALU.add),
                                 reads=[brtmp[r], bwi, bscore], writes=[bscore])
                lo, mid, cnt, u, t2, d0 = (bis[:, k:k + 1] for k in range(6))
                if n > 256:
                    K.op(dve, lambda e: e.tensor_reduce(out=lo, in_=score[:, 0:n], axis=AX.X, op=ALU.min), reads=[bscore], writes=[bbis])
                    K.op(dve, lambda e: e.tensor_reduce(out=d0, in_=score[:, 0:n], axis=AX.X, op=ALU.max), reads=[bscore], writes=[bbis])
                    K.op(dve, lambda e: e.tensor_tensor(out=d0, in0=d0, in1=lo, op=ALU.subtract), reads=[bbis], writes=[bbis])
                    K.op(dve, lambda e: e.tensor_scalar(out=dk[:], in0=p2[:], scalar1=d0, scalar2=None, op0=ALU.mult), reads=[bbis], writes=[bbis])
                    K.op(dve, lambda e: e.tensor_tensor(out=mid, in0=lo, in1=dk[:, 0:1], op=ALU.add), reads=[bbis], writes=[bbis])
                K.op(pool, lambda e, i=i: e.affine_select(out=score[:, i * 128:(i + 1) * 128], in_=score[:, i * 128:(i + 1) * 128],
                                                          pattern=[[-1, 128]], compare_op=ALU.is_ge, fill=self.fill_reg, base=0, channel_multiplier=1),
                     reads=[bscore, bbis], writes=[bscore])
                if n > 256:
                    for k in range(NBIS):
                        K.op(dve, lambda e: e.tensor_scalar(out=NM[:, 0:n], in0=score[:, 0:n], scalar1=mid, scalar2=0.0, op0=ALU.is_ge,
                                                            op1=ALU.add, accum_out=cnt),
                             reads=[bscore, bbis], writes=[bNM, bbis])
                        K.op(dve, lambda e: e.tensor_scalar(out=t2, in0=cnt, scalar1=255.5, scalar2=-1e30, op0=ALU.is_lt, op1=ALU.mult),
                             reads=[bbis], writes=[bbis])
                        K.op(dve, lambda e: e.scalar_tensor_tensor(out=lo, in0=t2, scalar=mid, in1=lo, op0=ALU.add, op1=ALU.max),
                             reads=[bbis], writes=[bbis])
                        K.op(dve, lambda e, k=k: e.tensor_scalar(out=u, in0=cnt, scalar1=255.5, scalar2=dk[:, k:k + 1], op0=ALU.is_ge, op1=ALU.mult),
                             reads=[bbis], writes=[bbis])
                        K.op(dve, lambda e, k=k: e.scalar_tensor_tensor(out=mid, in0=u, scalar=dk[:, k + 1:k + 2], in1=mid, op0=ALU.subtract, op1=ALU.add),
                             reads=[bbis], writes=[bbis])
                    K.op(dve, lambda e: e.tensor_scalar(out=NM[:, 0:n], in0=score[:, 0:n], scalar1=lo, scalar2=NEG, op0=ALU.is_lt, op1=ALU.mult),
                         reads=[bscore, bbis], writes=[bNM])
                else:
                    K.op(dve, lambda e: e.tensor_scalar(out=NM[:, 0:n], in0=score[:, 0:n], scalar1=-1e29, scalar2=NEG, op0=ALU.is_lt, op1=ALU.mult),
                         reads=[bscore], writes=[bNM])
                K.op(dve, lambda e: e.memset(O[:], 0.0), writes=[bO])
                for c in range(i + 1):
                    pt = c % 2
                    for hg in range(4):
                        hf, s0 = hg // 2, (hg % 2) * 4
                        b = pbank()
                        K.op(pe, lambda e, b=b, c=c: e.matmul(P[b][:], lhsT=NM[:, c * 128:(c + 1) * 128], rhs=self.I4[:].rearrange("p a b -> p (a b)"),
                                                              start=True, stop=False),
                             reads=[bNM, self.bconst], writes=[bP[b]], sig=False)
                        K.op(pe, lambda e, b=b, c=c, hf=hf, s0=s0: e.matmul(P[b][:], lhsT=kT2[hf * 64:(hf + 1) * 64, c * 128:(c + 1) * 128],
                                                                             rhs=qT[hf * 64:(hf + 1) * 64, s0:s0 + 4, :], start=False, stop=True),
                             reads=[bkT, bqT], writes=[bP[b]])
                        K.op(act, lambda e, b=b, pt=pt, hg=hg: e.activation(out=PT[pt][:, hg * 4:(hg + 1) * 4, :],
                                                                            in_=P[b][:].rearrange("p (a b) -> p a b", a=4), func=AF.Exp,
                                                                            bias=mneg[:, 0:1], scale=0.125),
                             reads=[bP[b], bmneg], writes=[bPT[pt]])
                    for hd in range(16):
                        col = (hd // 7) * 512 + (hd % 7) * 65
                        K.op(pe, lambda e, pt=pt, hd=hd, col=col, c=c: e.matmul(O[:, col:col + 65], lhsT=PT[pt][:, hd, :], rhs=Vaug[:, c, :],
                                                                                 start=False, stop=False, skip_group_check=True),
                             reads=[bPT[pt], bV, bO], writes=[bO], sig=(hd == 15))
                if self.debug and i == 0:
                    if self.dbg is None:
                        self.dbg = self.nc.dram_tensor("dbg", [128, 4096], F32, kind="ExternalOutput").ap()
                    dsb = sb("dsb", [128, 4096], F32); bd = Buf("dsb"); sd = Slot(K, "dsb")
                    K.op(dve, lambda e: e.memset(dsb[:], 0.0), writes=[bd])
                    K.op(dve, lambda e: e.tensor_copy(out=dsb[:, 0:1536], in_=O[:]), reads=[bO], writes=[bd])
                    K.op(dve, lambda e: e.tensor_copy(out=dsb[:, 1536:1540], in_=mneg[:]), reads=[bmneg], writes=[bd])
                    K.op(dve, lambda e: e.tensor_copy(out=dsb[:, 1600:1600 + 130], in_=Vaug[:, 0:2, :].rearrange("p a b -> p (a b)")), reads=[bV], writes=[bd])
                    K.op(dve, lambda e: e.tensor_copy(out=dsb[:, 2048:4096], in_=PT[0][:].rearrange("p a b -> p (a b)")), reads=[bPT[0]], writes=[bd])
                    K.dma(sp, sd, self.dbg, dsb[:], reads=[bd])
                for bk in range(3):
                    nh = 7 if bk < 2 else 2
                    Ov = O[:, bk * 512: bk * 512 + nh * 65].rearrange("p (h e) -> p h e", e=65)
                    K.op(dve, lambda e, bk=bk, nh=nh, Ov=Ov: e.reciprocal(out=rl[:, bk * 7: bk * 7 + nh], in_=Ov[:, :, 64]),
                         reads=[bO], writes=[brl])
                    K.op(dve, lambda e, bk=bk, nh=nh, Ov=Ov: e.tensor_tensor(
                        out=ob[:, bk * 448: bk * 448 + nh * 64].rearrange("p (h d) -> p h d", d=64), in0=Ov[:, :, 0:64],
                        in1=rl[:, bk * 7: bk * 7 + nh].rearrange("p (h o) -> p h o", o=1).to_broadcast([128, nh, 64]), op=ALU.mult),
                         reads=[bO, brl], writes=[bob])
                for kc in range(8):
                    K.op(pe, lambda e, kc=kc: e.transpose(out=self.TAb[:, kc * 128:(kc + 1) * 128], in_=ob[:, kc * 128:(kc + 1) * 128],
                                                          identity=self.ident[:]),
                         reads=[bob, self.bconst], writes=[self.bTA], sig=(kc == 7))
                K.op(act, lambda e: e.activation(out=oT[:], in_=self.TAb[:].rearrange("p (a b) -> p a b", a=8), func=AF.Copy),
                     reads=[self.bTA], writes=[boT])
                yb = [pbank(), pbank()]
                for hf in range(2):
                    for kc in range(8):
                        K.op(pe, lambda e, hf=hf, kc=kc, yb=yb: e.matmul(P[yb[hf]][:], lhsT=oT[:, kc, :], rhs=w_out[:, kc, hf * 512:(hf + 1) * 512],
                                                                         start=(kc == 0), stop=(kc == 7)),
                             reads=[boT, bw], writes=[bP[yb[hf]]], sig=(kc == 7))
                self.post_resid(vs, [(P[yb[0]][:], 0, 512), (P[yb[1]][:], 512, 512)], [bP[yb[0]], bP[yb[1]]],
                                xa[a][:], bxa[a], xa[a], bxa[a], small[1], bsmall[1], junk, bjunk, tmp, btmp)
                K.dma(pool, sxo[a], self.out[i * 128:(i + 1) * 128, :], xa[a][:], reads=[bxa[a]], writes=[self.xbuf[i]])
                if bgq and i % 4 == 3:
                    bgq.pop(0)()
            while bgq:
                bgq.pop(0)()
            K.barrier()
        K.end_scope()

    def diff(self, x_src, vs, jl, bg):
        K, T, NT = self.K, self.T, self.NT
        pe, dve, act, pool, sp = K.pe, K.dve, K.act, K.pool, K.sp
        bgq = list(bg)
        for g in range(2):
            off_in, off_out = WOFF[("bin", jl, g)], WOFF[("bout", jl, g)]
            K.begin_scope()
            with ExitStack() as st:
                sb = lambda n, s, d: self.sb(st, n, s, d)
                w_in = sb("w_in", [128, 8, 1536], BF16); bw = Buf("w"); sw = Slot(K, "w")
                w_out = sb("w_out", [128, 4, D], BF16)
                kT = sb("kT", [128, 4, T], BF16); bkT = Buf("kT")
                Vaug = sb("Vaug", [128, NT, 4, 129], BF16); bV = Buf("Vaug")
                PT = [sb(f"PT{i}", [128, 8, 128], BF16) for i in range(2)]; bPT = [Buf(f"PT{i}") for i in range(2)]
                pev = sb("pev", [128, 1536], F32); bpev = Buf("pev")
                rt = sb("rt", [128, 4, 16, 8], F32); brt = Buf("rt")
                qb = sb("qb", [128, 1024], BF16); bqb = Buf("qb")
                qT = sb("qT", [128, 4, 128], BF16); bqT = Buf("qT")
                sq = sb("sq", [128, 24], F32); bsq = Buf("sq")
                xa = [sb(f"xa{i}", [128, D], F32) for i in range(2)]; bxa = [Buf(f"xa{i}") for i in range(2)]
                sxa = [Slot(K, f"xa{i}") for i in range(2)]
                sxo = [Slot(K, f"xo{i}") for i in range(2)]
                ysb = [sb(f"ysb{i}", [128, D], F32) for i in range(2)]; bysb = [Buf(f"ysb{i}") for i in range(2)]
                sys_ = [Slot(K, f"ysb{i}") for i in range(2)]
                tmp = sb("tmp", [128, D], F32); btmp = Buf("tmp")
                hb = sb("hb", [128, D], BF16); bhb = Buf("hb")
                hT = sb("hT", [128, 8, 128], BF16); bhT = Buf("hT")
                on = sb("on", [128, 8, 128], F32); bon = Buf("on")
                od = sb("od", [128, 4, 128], F32); bod = Buf("od")
                ob = sb("ob", [128, 512], BF16); bob = Buf("ob")
                oT = sb("oT", [128, 4, 128], BF16); boT = Buf("oT")
                junk = sb("junk", [128, D], BF16); bjunk = Buf("junk")
                small = [sb(f"small{i}", [128, 8], F32) for i in range(2)]; bsmall = [Buf(f"small{i}") for i in range(2)]
                run = sb("run", [128, 2], F32); brun = Buf("run")
                mneg = sb("mneg", [128, 4], F32); bmneg = Buf("mneg")
                rl = sb("rl", [128, 8], F32); brl = Buf("rl")
                ssub = sb("ssub", [128, 8], F32); bssub = Buf("ssub")
                P = [self.ps(st, f"P{i}", [128, 512], F32) for i in range(4)]; bP = [Buf(f"P{i}") for i in range(4)]
                O = self.ps(st, "O", [128, 1536], F32); bO = Buf("O")

                K.dma(sp, sw, w_in[:].rearrange("p a b -> p (a b)"), self.wbf[:, off_in:off_in + W_BIN], reads=[self.wbf_buf], writes=[bw])
                K.dma(sp, sw, w_out[:].rearrange("p a b -> p (a b)"), self.wbf[:, off_out:off_out + W_BOUT], reads=[self.wbf_buf], writes=[bw])
                K.op(pool, lambda e: e.memset(Vaug[:, :, :, 128:129], 1.0), writes=[bV])
                K.op(pool, lambda e: e.memset(run[:], 0.0), writes=[brun])
                pcnt = [0]

                def pbank():
                    b = pcnt[0] % 4
                    pcnt[0] += 1
                    return b

                for i in range(NT):
                    a = i % 2
                    K.dma(pool, sxa[a], xa[a][:], x_src[i * 128:(i + 1) * 128, :], reads=[self.xbuf[i]], writes=[bxa[a]])
                    if g == 1:
                        K.dma(pool, sys_[a], ysb[a][:], self.ypart[i * 128:(i + 1) * 128, :], reads=[self.ypbuf[i]], writes=[bysb[a]])
                    self.norm_mod_T(vs, xa[a][:], bxa[a], hb, bhb, tmp, btmp, small[0], bsmall[0], junk, bjunk, hT[:], bhT)
                    for b in range(3):
                        for kc in range(8):
                            K.op(pe, lambda e, b=b, kc=kc: e.matmul(P[b][:], lhsT=hT[:, kc, :], rhs=w_in[:, kc, b * 512:(b + 1) * 512],
                                                                    start=(kc == 0), stop=(kc == 7)),
                                 reads=[bhT, bw], writes=[bP[b]], sig=(kc == 7))
                        K.op(act, lambda e, b=b: e.activation(out=pev[:, b * 512:(b + 1) * 512], in_=P[b][:], func=AF.Copy),
                             reads=[bP[b]], writes=[bpev])
                    pcnt[0] = 3
                    if STOP <= 1:
                        continue
                    self.rope(pev[:], bpev, 0, 16, i, rt, brt)
                    K.op(act, lambda e: e.activation(out=qb[:], in_=pev[:, 0:1024], func=AF.Copy), reads=[bpev], writes=[bqb])
                    K.op(act, lambda e, i=i: e.activation(out=Vaug[:, i, :, 0:128], in_=pev[:, 1024:1536].rearrange("p (h d) -> p h d", d=128),
                                                          func=AF.Copy), reads=[bpev], writes=[bV])
                    K.op(act, lambda e: e.activation(out=tmp[:], in_=pev[:, 0:1024], func=AF.Square), reads=[bpev], writes=[btmp])
                    K.op(dve, lambda e: e.tensor_reduce(out=sq[:, 0:16], in_=tmp[:].rearrange("p (h d) -> p h d", d=64), axis=AX.X, op=ALU.add),
                         reads=[btmp], writes=[bsq])
                    K.op(dve, lambda e: e.tensor_reduce(out=sq[:, 20:22], in_=sq[:, 0:16].rearrange("p (a b) -> p a b", a=2), axis=AX.X, op=ALU.max),
                         reads=[bsq], writes=[bsq])
                    for s_ in range(8):
                        K.op(pe, lambda e, s_=s_: e.transpose(out=self.TAb[:, s_ * 128:(s_ + 1) * 128], in_=qb[:, s_ * 128:(s_ + 1) * 128],
                                                               identity=self.ident[:]),
                             reads=[bqb, self.bconst], writes=[self.bTA], sig=(s_ == 7))
                    K.op(act, lambda e: e.activation(out=qT[:], in_=self.TAb[:, 0:512].rearrange("p (a b) -> p a b", a=4), func=AF.Copy),
                         reads=[self.bTA], writes=[bqT])
                    K.op(dve, lambda e, i=i: e.tensor_copy(out=kT[:, :, i * 128:(i + 1) * 128],
                                                           in_=self.TAb[:, 512:1024].rearrange("p (a b) -> p a b", a=4)),
                         reads=[self.bTA], writes=[bkT])
                    self.neg_bound(sq[:, 20:21], bsq, sq[:, 21:22], bsq, run, brun, mneg, bmneg, small[1], bsmall[1])
                    if STOP <= 2:
                        continue
                    K.op(dve, lambda e: e.memset(O[:], 0.0), writes=[bO])
                    for c in range(i + 1):
                        pt = c % 2
                        bb = [pbank(), pbank()]
                        if c == i:
                            for bnk in range(2):
                                K.op(pe, lambda e, b=bb[bnk]: e.matmul(P[b][:], lhsT=self.ident[:], rhs=self.CN8[:, 0:4, :].rearrange("p a b -> p (a b)"),
                                                                       start=True, stop=False),
                                     reads=[self.bconst], writes=[bP[bb[bnk]]], sig=False)
                        for ul in range(4):
                            for bnk in range(2):
                                b = bb[bnk]
                                K.op(pe, lambda e, b=b, ul=ul, bnk=bnk, c=c, i=i: e.matmul(
                                    P[b][:, ul * 128:(ul + 1) * 128], lhsT=kT[bnk * 64:(bnk + 1) * 64, ul, c * 128:(c + 1) * 128],
                                    rhs=qT[bnk * 64:(bnk + 1) * 64, ul, :], start=(c != i), stop=(c != i or ul == 3),
                                    skip_group_check=True),
                                     reads=[bkT, bqT], writes=[bP[b]], sig=(ul == 3))
                        for bnk in range(2):
                            b = bb[bnk]
                            K.op(act, lambda e, b=b, pt=pt, bnk=bnk: e.activation(out=PT[pt][:, bnk * 4:(bnk + 1) * 4, :],
                                                                                   in_=P[b][:].rearrange("p (a b) -> p a b", a=4), func=AF.Exp,
                                                                                   bias=mneg[:, 0:1], scale=0.125),
                                 reads=[bP[b], bmneg], writes=[bPT[pt]])
                        for u in range(8):
                            col = (u // 3) * 512 + (u % 3) * 129
                            jx = (u % 2) * 4 + u // 2
                            K.op(pe, lambda e, pt=pt, u=u, jx=jx, col=col, c=c: e.matmul(O[:, col:col + 129], lhsT=PT[pt][:, jx, :], rhs=Vaug[:, c, u % 4, :],
                                                                                          start=False, stop=False, skip_group_check=True),
                                 reads=[bPT[pt], bV, bO], writes=[bO], sig=(u == 7))
                    if STOP <= 3:
                        continue
                    for bk in range(3):
                        nh = 3 if bk < 2 else 2
                        Ov = O[:, bk * 512: bk * 512 + nh * 129].rearrange("p (h e) -> p h e", e=129)
                        K.op(dve, lambda e, bk=bk, nh=nh, Ov=Ov: e.reciprocal(out=rl[:, bk * 3: bk * 3 + nh], in_=Ov[:, :, 128]),
                             reads=[bO], writes=[brl])
                        K.op(dve, lambda e, bk=bk, nh=nh, Ov=Ov: e.tensor_tensor(
                            out=on[:, bk * 3: bk * 3 + nh, :], in0=Ov[:, :, 0:128],
                            in1=rl[:, bk * 3: bk * 3 + nh].rearrange("p (h o) -> p h o", o=1).to_broadcast([128, nh, 128]), op=ALU.mult),
                             reads=[bO, brl], writes=[bon])
                    K.op(dve, lambda e: e.scalar_tensor_tensor(out=od[:].rearrange("p a b -> p (a b)"), in0=on[:, 4:8, :].rearrange("p a b -> p (a b)"),
                                                               scalar=self.neglam[:, jl:jl + 1], in1=on[:, 0:4, :].rearrange("p a b -> p (a b)"),
                                                               op0=ALU.mult, op1=ALU.add),
                         reads=[bon, self.bdiffc], writes=[bod])
                    K.op(act, lambda e: e.activation(out=on[:, 0:4, :], in_=od[:], func=AF.Square), reads=[bod], writes=[bon])
                    K.op(dve, lambda e: e.tensor_reduce(out=ssub[:, 0:4], in_=on[:, 0:4, :], axis=AX.X, op=ALU.add), reads=[bon], writes=[bssub])
                    K.op(pool, lambda e: e.tensor_scalar(out=ssub[:, 4:8], in0=ssub[:, 0:4], scalar1=1.0 / 128.0, scalar2=EPS, op0=ALU.mult, op1=ALU.add),
                         reads=[bssub], writes=[bssub])
                    K.op(pool, lambda e: e.tensor_tensor(out=ssub[:, 4:8], in0=ssub[:, 4:8], in1=self.neghalf[:, 0:4], op=ALU.pow),
                         reads=[bssub, self.bconst], writes=[bssub])
                    K.op(dve, lambda e: e.tensor_tensor(out=od[:], in0=od[:],
                                                        in1=ssub[:, 4:8].rearrange("p (h o) -> p h o", o=1).to_broadcast([128, 4, 128]), op=ALU.mult),
                         reads=[bod, bssub], writes=[bod])
                    K.op(dve, lambda e: e.tensor_tensor(out=ob[:].rearrange("p (h d) -> p h d", d=128), in0=od[:],
                                                        in1=self.sublnb[:, jl:jl + 1, :].to_broadcast([128, 4, 128]), op=ALU.mult),
                         reads=[bod, self.bdiffc], writes=[bob])
                    if STOP <= 4:
                        continue
                    for kc in range(4):
                        K.op(pe, lambda e, kc=kc: e.transpose(out=self.TAb[:, kc * 128:(kc + 1) * 128], in_=ob[:, kc * 128:(kc + 1) * 128],
                                                              identity=self.ident[:]),
                             reads=[bob, self.bconst], writes=[self.bTA], sig=(kc == 3))
                    K.op(act, lambda e: e.activation(out=oT[:], in_=self.TAb[:, 0:512].rearrange("p (a b) -> p a b", a=4), func=AF.Copy),
                         reads=[self.bTA], writes=[boT])
                    yb = [pbank(), pbank()]
                    for hf in range(2):
                        for kc in range(4):
                            K.op(pe, lambda e, hf=hf, kc=kc, yb=yb: e.matmul(P[yb[hf]][:], lhsT=oT[:, kc, :], rhs=w_out[:, kc, hf * 512:(hf + 1) * 512],
                                                                             start=(kc == 0), stop=(kc == 3)),
                                 reads=[boT, bw], writes=[bP[yb[hf]]], sig=(kc == 3))
                    if STOP <= 5:
                        continue
                    if g == 0:
                        for hf in range(2):
                            K.op(act, lambda e, hf=hf, yb=yb, a=a: e.activation(out=ysb[a][:, hf * 512:(hf + 1) * 512], in_=P[yb[hf]][:], func=AF.Copy),
                                 reads=[bP[yb[hf]]], writes=[bysb[a]])
                        K.dma(pool, sys_[a], self.ypart[i * 128:(i + 1) * 128, :], ysb[a][:], reads=[bysb[a]], writes=[self.ypbuf[i]])
                    else:
                        for hf in range(2):
                            K.op(dve, lambda e, hf=hf, yb=yb, a=a: e.tensor_tensor(out=ysb[a][:, hf * 512:(hf + 1) * 512], in0=P[yb[hf]][:],
                                                                                    in1=ysb[a][:, hf * 512:(hf + 1) * 512], op=ALU.add),
                                 reads=[bP[yb[hf]], bysb[a]], writes=[bysb[a]])
                        self.post_resid(vs, [(ysb[a][:], 0, D)], [bysb[a]], xa[a][:], bxa[a], xa[a], bxa[a], small[1], bsmall[1],
                                        junk, bjunk, tmp, btmp)
                        K.dma(pool, sxo[a], self.out[i * 128:(i + 1) * 128, :], xa[a][:], reads=[bxa[a]], writes=[self.xbuf[i]])
                    if bgq and i % 8 == 7:
                        bgq.pop(0)()
                if g == 1:
                    while bgq:
                        bgq.pop(0)()
                K.barrier()
            K.end_scope()


def prep_weights(inp):
    wall = np.zeros((128, NTOT), np.float32)
    for i in range(DEPTH):
        for j in range(2):
            wg = np.zeros((D, FFP), np.float32); wg[:, :DFF] = inp["ffn_w_gate"][i, j]
            wu = np.zeros((D, FFP), np.float32); wu[:, :DFF] = inp["ffn_w_up"][i, j]
            gu = np.stack([wg, wu], 0).reshape(2, 8, 128, 11, 256)
            gu = gu.transpose(2, 3, 0, 1, 4).reshape(128, W_GU)
            o = WOFF[("gu", i, j)]
            wall[:, o:o + W_GU] = gu
            wdn = np.zeros((FFP, D), np.float32); wdn[:DFF] = inp["ffn_w_down"][i, j]
            o = WOFF[("d", i, j)]
            wall[:, o:o + W_D] = wdn.reshape(NF, 128, D).transpose(1, 0, 2).reshape(128, W_D)
    qperm = []
    for s in range(8):
        qperm += list(range(s * 64, s * 64 + 64)) + list(range((s + 8) * 64, (s + 8) * 64 + 64))
    qiperm = []
    for s in range(4):
        qiperm += list(range(1152 + s * 64, 1152 + s * 64 + 64)) + list(range(1152 + (s + 4) * 64, 1152 + (s + 4) * 64 + 64))
    aperm = np.array(qperm + list(range(1024, 1152)) + qiperm + list(range(1664, 1736)))
    for j in range(2):
        w = inp["dsa_w_in"][j][:, aperm]
        o = WOFF[("ain", j)]
        wall[:, o:o + W_AIN] = w.reshape(8, 128, A_IN).transpose(1, 0, 2).reshape(128, W_AIN)
        o = WOFF[("aout", j)]
        wall[:, o:o + W_AOUT] = inp["dsa_w_out"][j].reshape(8, 128, D).transpose(1, 0, 2).reshape(128, W_AOUT)
    for j in range(2):
        for g in range(2):
            cols = []
            for base in (0, 512, 1024, 1536):
                cols += list(range(base + g * 256, base + g * 256 + 256))
            cols += list(range(2048 + g * 512, 2048 + g * 512 + 512))
            w = inp["diff_w_in"][j][:, np.array(cols)]
            o = WOFF[("bin", j, g)]
            wall[:, o:o + W_BIN] = w.reshape(8, 128, 1536).transpose(1, 0, 2).reshape(128, W_BIN)
            wo = inp["diff_w_out"][j][g * 512:(g + 1) * 512]
            o = WOFF[("bout", j, g)]
            wall[:, o:o + W_BOUT] = wo.reshape(4, 128, D).transpose(1, 0, 2).reshape(128, W_BOUT)
    return wall


def prep_ada(inp):
    aw = np.asarray(inp["ada_w"], np.float32)
    a = aw.reshape(4, 4, 2, 128, 3, 6, 512)
    a = a.transpose(0, 4, 5, 1, 3, 2, 6)
    ada = np.ascontiguousarray(a).reshape(12 * 24, 128, 1024)
    adab = np.ascontiguousarray(np.asarray(inp["ada_b"], np.float32).reshape(1, 12 * 3072))
    return ada, adab


def make_in_maps(inp, T, ncores):
    wall = prep_weights(inp)
    ada, adab = prep_ada(inp)
    pre_n = np.ascontiguousarray(np.asarray(inp["pre_norm"], np.float32).reshape(12, D))
    post_n = np.ascontiguousarray(np.asarray(inp["post_norm"], np.float32).reshape(12, D))
    subln = np.ascontiguousarray(np.asarray(inp["diff_subln"], np.float32))
    lam = np.ascontiguousarray(np.asarray(inp["diff_lambda"], np.float32).reshape(2, 256))
    maps = []
    for b in range(ncores):
        maps.append({
            "x": np.ascontiguousarray(np.asarray(inp["x"][b, :T], np.float32)),
            "c": np.ascontiguousarray(np.asarray(inp["c"][b], np.float32).reshape(8, 128).T),
            "pos": np.ascontiguousarray(np.asarray(inp["positions"][b, :T], np.int32).reshape(T // 128, 128).T),
            "wall": wall, "ada": ada, "adab": adab, "pre_n": pre_n, "post_n": post_n, "subln": subln, "lam": lam,
        })
    return maps


ALL_SUBL = [(li, k) for li in range(DEPTH) for k in ("f0", "mix", "f1")]


def kernel(**inputs):
    T = 4096
    prog = Prog(T, ALL_SUBL)
    nc = prog.build()
    maps = make_in_maps(inputs, T, 8)
    res = run_bass_kernel_spmd(nc, maps, core_ids=list(range(8)))
    return np.stack([np.asarray(r["out"], np.float32) for r in res.results], 0)
```

```python
import math
from contextlib import ExitStack

import numpy as np
import concourse.bass as bass
import concourse.mybir as mybir
from concourse.bass_utils import run_bass_kernel_spmd

F32 = mybir.dt.float32
BF16 = mybir.dt.bfloat16
I32 = mybir.dt.int32
AF = mybir.ActivationFunctionType
ALU = mybir.AluOpType
AX = mybir.AxisListType

D = 1024
DFF = 2752
FFP = 2816
NF = 22
DEPTH = 4
EPS = 1e-6
A_IN = 1736
NEG = -30000.0
SEM_LIMIT = 12000
NBIS = 24
import os
STOP = int(os.environ.get('STOP', '99'))

W_GU = 11 * 2 * 8 * 256
W_D = NF * 1024
W_AIN = 8 * A_IN
W_AOUT = 8 * 1024
W_BIN = 8 * 1536
W_BOUT = 4 * 1024


def weight_offsets():
    off = {}
    o = 0
    for i in range(DEPTH):
        for j in range(2):
            off[("gu", i, j)] = o; o += W_GU
            off[("d", i, j)] = o; o += W_D
    for j in range(2):
        off[("ain", j)] = o; o += W_AIN
        off[("aout", j)] = o; o += W_AOUT
    for j in range(2):
        for g in range(2):
            off[("bin", j, g)] = o; o += W_BIN
            off[("bout", j, g)] = o; o += W_BOUT
    return off, o


WOFF, NTOT = weight_offsets()
CASTW = 4096
assert NTOT % CASTW == 0 or True


class Ev:
    __slots__ = ("sem", "val", "eng")

    def __init__(self, eng=None):
        self.sem = None
        self.val = 0
        self.eng = eng


class Buf:
    __slots__ = ("name", "w", "r")

    def __init__(self, name):
        self.name = name
        self.w = None
        self.r = {}


class Eng:
    def __init__(self, K, name, h):
        self.K = K
        self.name = name
        self.h = h
        self.sem = None
        self.cnt = 0
        self.seen = {}
        self.pending = []
        self.last = None
        self.n = 0


class _Slot:
    def __init__(self, K, name):
        self.sem = K.new_sem("d_" + name)
        self.cnt = 0
        self.name = name
        self.last = None
        K.slots.append(self)


def Slot(K, name):
    if K.free_slots:
        s = K.free_slots.pop()
    else:
        s = _Slot(K, name)
        s.name = f"s{len(K.slots)}"
    K.scope_slots.append(s)
    return s


class Kern:
    def __init__(self, nc):
        self.nc = nc
        self.es = ExitStack()
        self.nsem = 0
        self.slots = []
        self.pe = Eng(self, "pe", nc.tensor)
        self.act = Eng(self, "act", nc.scalar)
        self.dve = Eng(self, "dve", nc.vector)
        self.pool = Eng(self, "pool", nc.gpsimd)
        self.sp = Eng(self, "sp", nc.sync)
        self.engs = [self.pe, self.act, self.dve, self.pool, self.sp]
        self.ninstr = 0
        self.free_slots = []
        self.scope_slots = []

    def begin_scope(self):
        self._saved = self.scope_slots
        self.scope_slots = []

    def end_scope(self):
        self.free_slots.extend(self.scope_slots)
        self.scope_slots = self._saved

    def new_sem(self, name):
        self.nsem += 1
        return self.es.enter_context(self.nc.semaphore(f"{name}_{self.nsem}"))

    def _need(self, eng, ev, raw=False):
        if ev is None:
            return
        if ev.eng is eng and (not raw or eng is self.pe):
            return
        assert ev.sem is not None, "dependency on unsignaled instruction"
        k = id(ev.sem)
        if eng.seen.get(k, 0) >= ev.val:
            return
        eng.h.wait_ge(ev.sem, ev.val)
        eng.seen[k] = ev.val

    def _waits(self, eng, reads, writes):
        for b in reads:
            self._need(eng, b.w, raw=True)
        for b in writes:
            self._need(eng, b.w)
            for ev in b.r.values():
                self._need(eng, ev)

    def _record(self, ev, key, reads, writes):
        for b in writes:
            b.w = ev
            b.r = {}
        for b in reads:
            b.r[key] = ev

    def op(self, eng, fn, reads=(), writes=(), sig=True):
        self._waits(eng, reads, writes)
        ins = fn(eng.h)
        ev = Ev(eng)
        eng.n += 1
        self.ninstr += 1
        if sig:
            if eng.sem is None or eng.cnt >= SEM_LIMIT:
                eng.sem = self.new_sem(eng.name)
                eng.cnt = 0
            eng.cnt += 1
            ins.then_inc(eng.sem, 1)
            ev.sem, ev.val = eng.sem, eng.cnt
            for p in eng.pending:
                p.sem, p.val = eng.sem, eng.cnt
            eng.pending = []
            eng.last = ev
        else:
            eng.pending.append(ev)
        self._record(ev, eng.name, reads, writes)
        return ev

    def dma(self, q, slot, out, in_, reads=(), writes=()):
        self._waits(q, reads, writes)
        ins = q.h.dma_start(out=out, in_=in_)
        slot.cnt += 16
        ins.then_inc(slot.sem, 16)
        ev = Ev(None)
        ev.sem, ev.val = slot.sem, slot.cnt
        slot.last = ev
        self.ninstr += 1
        self._record(ev, "dma_" + slot.name, reads, writes)
        return ev

    def barrier(self):
        evs = []
        for e in self.engs:
            assert not e.pending, f"barrier with pending unsignaled instrs on {e.name}"
            if e.last is not None:
                evs.append(e.last)
        for s in self.slots:
            if s.last is not None:
                evs.append(s.last)
        for e in self.engs:
            for ev in evs:
                self._need(e, ev)


class Prog:
    def __init__(self, T, sublayers, do_cast=True):
        self.T = T
        self.NT = T // 128
        self.subl = sublayers
        self.do_cast = do_cast
        nc = bass.Bass("TRN2", target_bir_lowering=False)
        self.nc = nc
        self.K = Kern(nc)
        K = self.K
        NT = self.NT
        dt = nc.dram_tensor
        self.x_in = dt("x", [T, D], F32, kind="ExternalInput").ap()
        self.c_in = dt("c", [128, 8], F32, kind="ExternalInput").ap()
        self.pos_in = dt("pos", [128, NT], I32, kind="ExternalInput").ap()
        self.wall = dt("wall", [128, NTOT], F32, kind="ExternalInput").ap()
        self.ada = dt("ada", [12 * 24, 128, 1024], F32, kind="ExternalInput").ap()
        self.adab = dt("adab", [1, 12 * 3072], F32, kind="ExternalInput").ap()
        self.pre_n = dt("pre_n", [12, D], F32, kind="ExternalInput").ap()
        self.post_n = dt("post_n", [12, D], F32, kind="ExternalInput").ap()
        self.subln = dt("subln", [2, 128], F32, kind="ExternalInput").ap()
        self.lam = dt("lam", [2, 256], F32, kind="ExternalInput").ap()
        self.out = dt("out", [T, D], F32, kind="ExternalOutput").ap()
        self.debug = False
        self.dbg = None
        self.wbf = dt("wbf", [128, NTOT], BF16, kind="Internal").ap()
        self.ypart = dt("ypart", [T, D], F32, kind="Internal").ap()
        self.xbuf = [Buf(f"xd{i}") for i in range(NT)]
        self.ypbuf = [Buf(f"yp{i}") for i in range(NT)]
        self.wbf_buf = Buf("wbf")
        self.cast_pieces = []
        self.cast_slots = None
        self.gs = ExitStack()
        self._names = 0

    def sb(self, st, name, shape, dtype):
        self._names += 1
        return st.enter_context(self.nc.sbuf_tensor(f"{name}_{self._names}", shape, dtype))

    def ps(self, st, name, shape, dtype):
        self._names += 1
        return st.enter_context(self.nc.psum_tensor(f"{name}_{self._names}", shape, dtype))

    def setup_globals(self):
        K, nc, st, NT = self.K, self.nc, self.gs, self.NT
        sb = lambda n, s, d: self.sb(st, n, s, d)
        self.TA = self.ps(st, "TA", [128, 512], F32)
        self.TAb = self.TA[:].bitcast(BF16)
        self.bTA = Buf("TA")
        self.identf = sb("identf", [128, 128], F32)
        self.ident = sb("ident", [128, 128], BF16)
        self.I4 = sb("I4", [128, 4, 128], BF16)
        self.CN8 = sb("CN8", [128, 8, 128], BF16)
        self.ones_row = sb("ones_row", [1, 128], F32)
        self.neghalf = sb("neghalf", [128, 16], F32)
        self.half = sb("half", [128, 16], F32)
        self.bconst = Buf("consts")
        self.cosT = sb("cosT", [128, NT, 8], F32)
        self.sinT = sb("sinT", [128, NT, 8], F32)
        self.brope = Buf("rope")
        self.condrep = sb("condrep", [128, 8, 128], F32)
        self.bcond = Buf("cond")
        self.vecA = [sb(f"vA{i}", [128, D], F32) for i in range(2)]
        self.vecS = [sb(f"vS{i}", [128, D], F32) for i in range(2)]
        self.vecG = [sb(f"vG{i}", [128, D], F32) for i in range(2)]
        self.bvec = [Buf(f"vec{i}") for i in range(2)]
        self.adaring = [sb(f"adar{i}", [128, 2, 512], F32) for i in range(2)]
        self.badar = [Buf(f"adar{i}") for i in range(2)]
        self.sadar = [Slot(K, f"adar{i}") for i in range(2)]
        self.pgb = [sb(f"pgb{i}", [128, D], F32) for i in range(2)]
        self.bpgb = [Buf(f"pgb{i}") for i in range(2)]
        self.spgb = [Slot(K, f"pgb{i}") for i in range(2)]
        self.brow = sb("brow", [1, 512], F32)
        self.bbrow = Buf("brow")
        self.sbrow = Slot(K, "brow")
        self.neglam = sb("neglam", [128, 2], F32)
        self.sublnb = sb("sublnb", [128, 2, 128], F32)
        self.bdiffc = Buf("diffc")
        self.adacnt = 0

        pool, dve, act = K.pool, K.dve, K.act
        bc = self.bconst
        K.op(pool, lambda e: e.memset(self.identf[:], 0.0), writes=[bc])
        K.op(pool, lambda e: e.affine_select(out=self.identf[:], in_=self.identf[:], pattern=[[-1, 128]],
                                             compare_op=ALU.not_equal, fill=1.0, base=0, channel_multiplier=1),
             writes=[bc])
        K.op(pool, lambda e: e.tensor_copy(out=self.ident[:], in_=self.identf[:]), writes=[bc])
        for r in range(4):
            K.op(pool, lambda e, r=r: e.tensor_copy(out=self.I4[:, r, :], in_=self.identf[:]), writes=[bc])
        K.op(pool, lambda e: e.memset(self.CN8[:], 0.0), writes=[bc])
        K.op(pool, lambda e: e.affine_select(out=self.CN8[:], in_=self.CN8[:], pattern=[[0, 8], [1, 128]],
                                             compare_op=ALU.is_ge, fill=NEG, base=0, channel_multiplier=-1),
             writes=[bc])
        K.op(pool, lambda e: e.memset(self.ones_row[:], 1.0), writes=[bc])
        K.op(pool, lambda e: e.memset(self.neghalf[:], -0.5), writes=[bc])
        K.op(pool, lambda e: e.memset(self.half[:], 0.5), writes=[bc])

        with ExitStack() as ts:
            tsb = lambda n, s, d: self.sb(ts, n, s, d)
            c_sb = tsb("c_sb", [128, 8], F32)
            cond = tsb("cond", [128, 8], F32)
            b_c = Buf("c_sb")
            s_c = Slot(K, "c_sb")
            K.dma(K.sp, s_c, c_sb[:], self.c_in, writes=[b_c])
            K.op(act, lambda e: e.activation(out=cond[:], in_=c_sb[:], func=AF.Silu), reads=[b_c], writes=[self.bcond])
            zer = tsb("zer", [128, 128], F32)
            K.op(dve, lambda e: e.memset(zer[:], 0.0), writes=[b_c])
            for kc in range(8):
                K.op(dve, lambda e, kc=kc: e.tensor_scalar(out=self.condrep[:, kc, :], in0=zer[:],
                                                           scalar1=cond[:, kc:kc + 1], scalar2=None, op0=ALU.add),
                     reads=[b_c, self.bcond], writes=[self.bcond])
            pos_i = tsb("pos_i", [128, NT], I32)
            pos_f = tsb("pos_f", [128, NT], F32)
            invt = tsb("invt", [128, 8], F32)
            ang = tsb("ang", [128, NT, 8], F32)
            ang2 = tsb("ang2", [128, NT, 8], F32)
            b_p = Buf("pos")
            s_p = Slot(K, "pos")
            K.dma(K.sp, s_p, pos_i[:], self.pos_in, writes=[b_p])
            K.op(dve, lambda e: e.tensor_copy(out=pos_f[:], in_=pos_i[:]), reads=[b_p], writes=[b_p])
            for j in range(8):
                inv = float(np.float32(500000.0) ** np.float32(-(2.0 * j) / 16.0))
                K.op(dve, lambda e, j=j, inv=inv: e.memset(invt[:, j:j + 1], inv), writes=[b_p])
            K.op(dve, lambda e: e.tensor_tensor(out=ang[:], in0=pos_f[:].rearrange("p (n o) -> p n o", o=1).to_broadcast([128, NT, 8]),
                                                in1=invt[:].rearrange("p (o j) -> p o j", o=1).to_broadcast([128, NT, 8]),
                                                op=ALU.mult), reads=[b_p], writes=[b_p])
            TWO_PI = 2.0 * math.pi
            C1 = 6.28125
            C2 = TWO_PI - C1
            kf = tsb("kf", [128, NT, 8], F32)
            ki = tsb("ki", [128, NT, 8], I32)
            mm_ = tsb("mm_", [128, NT, 8], F32)

            def sin_of(dst, shift):
                K.op(dve, lambda e: e.tensor_scalar(out=ang2[:], in0=ang[:], scalar1=shift, scalar2=None, op0=ALU.add),
                     reads=[b_p, self.brope], writes=[b_p])
                K.op(dve, lambda e: e.tensor_scalar(out=kf[:], in0=ang2[:], scalar1=1.0 / TWO_PI, scalar2=None, op0=ALU.mult),
                     reads=[b_p], writes=[b_p])
                K.op(dve, lambda e: e.tensor_copy(out=ki[:], in_=kf[:]), reads=[b_p], writes=[b_p])
                K.op(dve, lambda e: e.tensor_copy(out=kf[:], in_=ki[:]), reads=[b_p], writes=[b_p])
                K.op(dve, lambda e: e.scalar_tensor_tensor(out=ang2[:], in0=kf[:], scalar=-C1, in1=ang2[:], op0=ALU.mult, op1=ALU.add),
                     reads=[b_p], writes=[b_p])
                K.op(dve, lambda e: e.scalar_tensor_tensor(out=ang2[:], in0=kf[:], scalar=-C2, in1=ang2[:], op0=ALU.mult, op1=ALU.add),
                     reads=[b_p], writes=[b_p])
                K.op(dve, lambda e: e.tensor_scalar(out=mm_[:], in0=ang2[:], scalar1=math.pi, scalar2=TWO_PI, op0=ALU.is_gt, op1=ALU.mult),
                     reads=[b_p], writes=[b_p])
                K.op(dve, lambda e: e.tensor_tensor(out=ang2[:], in0=ang2[:], in1=mm_[:], op=ALU.subtract), reads=[b_p], writes=[b_p])
                K.op(dve, lambda e: e.tensor_scalar(out=mm_[:], in0=ang2[:], scalar1=-math.pi, scalar2=TWO_PI, op0=ALU.is_lt, op1=ALU.mult),
                     reads=[b_p], writes=[b_p])
                K.op(dve, lambda e: e.tensor_tensor(out=ang2[:], in0=ang2[:], in1=mm_[:], op=ALU.add), reads=[b_p], writes=[b_p])
                K.op(act, lambda e: e.activation(out=dst[:], in_=ang2[:], func=AF.Sin), reads=[b_p], writes=[self.brope])

            sin_of(self.sinT, 0.0)
            sin_of(self.cosT, 0.5 * math.pi)
            lam_sb = tsb("lam_sb", [128, 2, 4, 64], F32)
            sub_sb = tsb("sub_sb", [128, 2, 128], F32)
            lp = tsb("lp", [128, 2, 2, 64], F32)
            ls = tsb("ls", [128, 4], F32)
            b_l = Buf("lam")
            s_l = Slot(K, "lam")
            s_l2 = Slot(K, "lam2")
            K.dma(K.sp, s_l, lam_sb[:].rearrange("p a b c -> p (a b c)"),
                  self.lam.rearrange("a b -> (a b)").partition_broadcast(128), writes=[b_l])
            b_l2 = Buf("sub")
            K.dma(K.sp, s_l2, sub_sb[:].rearrange("p a b -> p (a b)"),
                  self.subln.rearrange("a b -> (a b)").partition_broadcast(128), writes=[b_l2])
            K.op(dve, lambda e: e.tensor_tensor(out=lp[:], in0=lam_sb[:, :, 0:4:2, :], in1=lam_sb[:, :, 1:4:2, :],
                                                op=ALU.mult), reads=[b_l], writes=[b_l])
            K.op(dve, lambda e: e.tensor_reduce(out=ls[:], in_=lp[:].rearrange("p a b c -> p (a b) c"), axis=AX.X,
                                                op=ALU.add), reads=[b_l], writes=[b_l])
            K.op(act, lambda e: e.activation(out=ls[:], in_=ls[:], func=AF.Exp), reads=[b_l], writes=[b_l])
            for j in range(2):
                li = 0.8 - 0.6 * math.exp(-0.3 * (2 * j + 1))
                K.op(dve, lambda e, j=j, li=li: e.scalar_tensor_tensor(out=self.neglam[:, j:j + 1], in0=ls[:, 2 * j + 1:2 * j + 2],
                                                                       scalar=-li, in1=ls[:, 2 * j:2 * j + 1],
                                                                       op0=ALU.add, op1=ALU.subtract),
                     reads=[b_l], writes=[self.bdiffc])
                K.op(dve, lambda e, j=j, li=li: e.tensor_scalar(out=self.sublnb[:, j, :], in0=sub_sb[:, j, :],
                                                                scalar1=1.0 - li, scalar2=None, op0=ALU.mult),
                     reads=[b_l2], writes=[self.bdiffc])
            K.barrier()

    def cast_plan(self, ranges):
        CW = 8192
        self.wgrp = [[Buf(f"wgrp{g}_{r}") for r in range(3)] for g in range(len(ranges))]
        self.cast_ring = [Buf(f"castring{r}") for r in range(3)]
        self.cast_n = 0
        for g, (s0, ln) in enumerate(ranges):
            o = 0
            while o < ln:
                w = min(CW, ln - o)
                self.cast_pieces.append((s0 + o, w, g))
                o += w
        self.cast_slots = [Slot(self.K, f"cast{i}") for i in range(3)]

    def cast_bg(self, k=1, upto_group=None):
        K = self.K
        while self.cast_pieces and (k > 0 or (upto_group is not None and self.cast_pieces[0][2] <= upto_group)):
            c0, w, g = self.cast_pieces.pop(0)
            r = self.cast_n % 3
            self.cast_n += 1
            K.dma(K.pool, self.cast_slots[r], self.wbf[:, c0:c0 + w], self.wall[:, c0:c0 + w],
                  writes=[self.wgrp[g][r], self.cast_ring[r]])
            k -= 1

    def cast_weights(self, ranges):
        K = self.K
        K.begin_scope()
        with ExitStack() as ts:
            NB = 3
            fin = [self.sb(ts, f"cin{i}", [128, CASTW], F32) for i in range(NB)]
            fout = [self.sb(ts, f"cout{i}", [128, CASTW], BF16) for i in range(NB)]
            bin_ = [Buf(f"cin{i}") for i in range(NB)]
            bout = [Buf(f"cout{i}") for i in range(NB)]
            sin_ = [Slot(K, f"cin{i}") for i in range(NB)]
            sout = [Slot(K, f"cout{i}") for i in range(NB)]
            pieces = []
            for (s0, ln) in ranges:
                o = 0
                while o < ln:
                    w = min(CASTW, ln - o)
                    pieces.append((s0 + o, w))
                    o += w
            engs = [K.dve, K.pool, K.act]
            for n, (c0, w) in enumerate(pieces):
                i = n % NB
                K.dma(K.sp, sin_[i], fin[i][:, 0:w], self.wall[:, c0:c0 + w], writes=[bin_[i]])
                eng = engs[n % 3]
                if eng is K.act:
                    K.op(eng, lambda e, i=i, w=w: e.activation(out=fout[i][:, 0:w], in_=fin[i][:, 0:w], func=AF.Copy),
                         reads=[bin_[i]], writes=[bout[i]])
                else:
                    K.op(eng, lambda e, i=i, w=w: e.tensor_copy(out=fout[i][:, 0:w], in_=fin[i][:, 0:w]),
                         reads=[bin_[i]], writes=[bout[i]])
                K.dma(K.sp, sout[i], self.wbf[:, c0:c0 + w], fout[i][:, 0:w], reads=[bout[i]], writes=[self.wbf_buf])
            K.barrier()
        K.end_scope()

    def ada_steps(self, s_glob, vs, coef):
        K = self.K
        pe, dve, act = K.pe, K.dve, K.act

        def load_pg():
            K.dma(K.sp, self.spgb[0], self.pgb[0][:], self.pre_n[s_glob, :].partition_broadcast(128), writes=[self.bpgb[0]])
            K.dma(K.sp, self.spgb[1], self.pgb[1][:], self.post_n[s_glob, :].partition_broadcast(128), writes=[self.bpgb[1]])

        def step(c):
            if c == 0:
                load_pg()
            K.dma(K.sp, self.sbrow, self.brow[:], self.adab[0:1, s_glob * 3072 + c * 512: s_glob * 3072 + (c + 1) * 512],
                  writes=[self.bbrow])
            for kh in range(4):
                r = self.adacnt % 2
                self.adacnt += 1
                K.dma(K.sp, self.sadar[r], self.adaring[r][:].rearrange("p a b -> p (a b)"),
                      self.ada[s_glob * 24 + c * 4 + kh], writes=[self.badar[r]])
                for kl in range(2):
                    kc = kh * 2 + kl
                    K.op(pe, lambda e, r=r, kl=kl, kc=kc: e.matmul(self.TA[:], lhsT=self.condrep[:, kc, :], rhs=self.adaring[r][:, kl, :],
                                                                    start=(kc == 0), stop=False),
                         reads=[self.bcond, self.badar[r]], writes=[self.bTA], sig=(kl == 1))
            K.op(pe, lambda e: e.matmul(self.TA[:], lhsT=self.ones_row[0:1, :], rhs=self.brow[0:1, :], start=False, stop=True),
                 reads=[self.bconst, self.bbrow], writes=[self.bTA])
            cs = slice((c % 2) * 512, (c % 2) * 512 + 512)
            if c < 2:
                K.op(act, lambda e: e.activation(out=self.vecS[vs][:, cs], in_=self.TA[:], func=AF.Copy),
                     reads=[self.bTA], writes=[self.bvec[vs]])
            elif c < 4:
                K.op(dve, lambda e: e.scalar_tensor_tensor(out=self.vecA[vs][:, cs], in0=self.TA[:], scalar=1.0,
                                                           in1=self.pgb[0][:, cs], op0=ALU.add, op1=ALU.mult),
                     reads=[self.bTA, self.bpgb[0]], writes=[self.bvec[vs]])
            else:
                K.op(dve, lambda e: e.scalar_tensor_tensor(out=self.vecG[vs][:, cs], in0=self.TA[:], scalar=coef,
                                                           in1=self.pgb[1][:, cs], op0=ALU.mult, op1=ALU.mult),
                     reads=[self.bTA, self.bpgb[1]], writes=[self.bvec[vs]])

        return [lambda c=c: step(c) for c in range(6)]

    def norm_mod_T(self, vs, xt, bx, hb, bhb, tmp, btmp, small, bsmall, junk, bjunk, hT_out, bhT):
        K = self.K
        pe, dve, act, pool = K.pe, K.dve, K.act, K.pool
        ss = small[:, 0:1]
        rs = small[:, 1:2]
        K.op(act, lambda e: e.activation(out=junk[:], in_=xt, func=AF.Square, accum_out=ss), reads=[bx], writes=[bjunk, bsmall])
        K.op(pool, lambda e: e.tensor_scalar(out=rs, in0=ss, scalar1=1.0 / D, scalar2=EPS, op0=ALU.mult, op1=ALU.add),
             reads=[bsmall], writes=[bsmall])
        K.op(pool, lambda e: e.tensor_tensor(out=rs, in0=rs, in1=self.neghalf[:, 0:1], op=ALU.pow),
             reads=[bsmall, self.bconst], writes=[bsmall])
        K.op(dve, lambda e: e.scalar_tensor_tensor(out=tmp[:], in0=xt, scalar=rs, in1=self.vecA[vs][:], op0=ALU.mult, op1=ALU.mult),
             reads=[bx, bsmall, self.bvec[vs]], writes=[btmp])
        K.op(dve, lambda e: e.tensor_tensor(out=hb[:], in0=tmp[:], in1=self.vecS[vs][:], op=ALU.add),
             reads=[btmp, self.bvec[vs]], writes=[bhb])
        for kc in range(8):
            K.op(pe, lambda e, kc=kc: e.transpose(out=self.TAb[:, kc * 128:(kc + 1) * 128], in_=hb[:, kc * 128:(kc + 1) * 128],
                                                  identity=self.ident[:]),
                 reads=[bhb, self.bconst], writes=[self.bTA], sig=(kc == 7))
        K.op(act, lambda e: e.activation(out=hT_out, in_=self.TAb[:].rearrange("p (a b) -> p a b", a=8), func=AF.Copy),
             reads=[self.bTA], writes=[bhT])

    def post_resid(self, vs, y_srcs, by, xt, bx, xo, bxo, small, bsmall, junk, bjunk, tmp, btmp):
        K = self.K
        dve, act, pool = K.dve, K.act, K.pool
        n = len(y_srcs)
        for k, (yap, c0, w) in enumerate(y_srcs):
            K.op(act, lambda e, yap=yap, c0=c0, w=w, k=k: e.activation(out=junk[:, c0:c0 + w], in_=yap, func=AF.Square,
                                                                        accum_out=small[:, 4 + k:5 + k]),
                 reads=by, writes=[bjunk, bsmall])
        rs = small[:, 3:4]
        if n == 2:
            K.op(pool, lambda e: e.tensor_tensor(out=rs, in0=small[:, 4:5], in1=small[:, 5:6], op=ALU.add),
                 reads=[bsmall], writes=[bsmall])
            src = rs
        else:
            src = small[:, 4:5]
        K.op(pool, lambda e: e.tensor_scalar(out=rs, in0=src, scalar1=1.0 / D, scalar2=EPS, op0=ALU.mult, op1=ALU.add),
             reads=[bsmall], writes=[bsmall])
        K.op(pool, lambda e: e.tensor_tensor(out=rs, in0=rs, in1=self.neghalf[:, 0:1], op=ALU.pow),
             reads=[bsmall, self.bconst], writes=[bsmall])
        for (yap, c0, w) in y_srcs:
            K.op(dve, lambda e, yap=yap, c0=c0, w=w: e.scalar_tensor_tensor(out=tmp[:, c0:c0 + w], in0=yap, scalar=rs,
                                                                             in1=self.vecG[vs][:, c0:c0 + w],
                                                                             op0=ALU.mult, op1=ALU.mult),
                 reads=list(by) + [bsmall, self.bvec[vs]], writes=[btmp])
        K.op(dve, lambda e: e.tensor_tensor(out=xo[:], in0=tmp[:], in1=xt, op=ALU.add), reads=[btmp, bx], writes=[bxo])

    def ffn(self, x_src, vs, off_gu, off_d, bg):
        K, T, NT = self.K, self.T, self.NT
        pe, dve, act, pool, sp = K.pe, K.dve, K.act, K.pool, K.sp
        NTT = T // 512
        K.begin_scope()
        with ExitStack() as st:
            sb = lambda n, s, d: self.sb(st, n, s, d)
            NG = 3
            wgu = [sb(f"wgu{i}", [128, 2, 8, 256], BF16) for i in range(NG)]
            bwgu = [Buf(f"wgu{i}") for i in range(NG)]
            swgu = [Slot(K, f"wgu{i}") for i in range(NG)]
            wd = sb("wd", [128, NF, 1024], BF16)
            bwd = Buf("wd")
            swd = Slot(K, "wd")
            xa = [sb(f"xa{i}", [128, D], F32) for i in range(2)]
            bxa = [Buf(f"xa{i}") for i in range(2)]
            sxa = [Slot(K, f"xa{i}") for i in range(2)]
            xc = [sb(f"xc{i}", [128, D], F32) for i in range(2)]
            bxc = [Buf(f"xc{i}") for i in range(2)]
            sxc = [Slot(K, f"xc{i}") for i in range(2)]
            xo = [sb(f"xo{i}", [128, D], F32) for i in range(2)]
            bxo = [Buf(f"xo{i}") for i in range(2)]
            sxo = [Slot(K, f"xo{i}") for i in range(2)]
            tmp = sb("tmp", [128, D], F32)
            btmp = Buf("tmp")
            hb = [sb(f"hb{i}", [128, D], BF16) for i in range(2)]
            bhb = [Buf(f"hb{i}") for i in range(2)]
            hT = [sb(f"hT{i}", [128, 8, 512], BF16) for i in range(2)]
            bhT = [Buf(f"hT{i}") for i in range(2)]
            actT = sb("actT", [128, NF, 512], BF16)
            bactT = Buf("actT")
            gt = [sb(f"gt{i}", [128, 512], F32) for i in range(2)]
            bgt = [Buf(f"gt{i}") for i in range(2)]
            junk = sb("junk", [128, D], BF16)
            bjunk = Buf("junk")
            small = [sb(f"small{i}", [128, 8], F32) for i in range(2)]
            bsmall = [Buf(f"small{i}") for i in range(2)]
            GU = [self.ps(st, f"GU{i}", [128, 512], F32) for i in range(3)]
            bGU = [Buf(f"GU{i}") for i in range(3)]
            Y = [self.ps(st, f"Y{i}", [128, 512], F32) for i in range(3)]
            bY = [Buf(f"Y{i}") for i in range(3)]

            wcols = self.wbf
            for q4 in range(2):
                K.dma(sp, swd, wd[:, q4 * 11:(q4 + 1) * 11, :].rearrange("p a b -> p (a b)"),
                      wcols[:, off_d + q4 * 11 * 1024: off_d + (q4 + 1) * 11 * 1024], reads=self.wbf_grp, writes=[bwd])

            gu_seq = [(n, g) for n in range(NTT) for g in range(11)]
            self._gu_loaded = 0

            def load_gu(upto):
                while self._gu_loaded < min(upto, len(gu_seq)):
                    k = self._gu_loaded
                    n, g = gu_seq[k]
                    i = k % NG
                    K.dma(sp, swgu[i], wgu[i][:].rearrange("p a b c -> p (a b c)"),
                          wcols[:, off_gu + g * 4096: off_gu + (g + 1) * 4096], reads=self.wbf_grp, writes=[bwgu[i]])
                    self._gu_loaded += 1

            acnt = [0]

            def phaseA(n, j):
                a = acnt[0] % 2
                acnt[0] += 1
                tile = n * 4 + j
                K.dma(pool, sxa[a], xa[a][:], x_src[tile * 128:(tile + 1) * 128, :], reads=[self.xbuf[tile]], writes=[bxa[a]])
                self.norm_mod_T(vs, xa[a][:], bxa[a], hb[a], bhb[a], tmp, btmp, small[0], bsmall[0], junk, bjunk,
                                hT[n % 2][:, :, j * 128:(j + 1) * 128], bhT[n % 2])

            for j in range(4):
                phaseA(0, j)
            load_gu(NG)
            ccnt = [0]
            bgq = list(bg)
            for n in range(NTT):
                cur = n % 2
                for g in range(11):
                    k = n * 11 + g
                    i = k % NG
                    for fl in range(2):
                        f = 2 * g + fl
                        bg_, bu_ = (2 * f) % 3, (2 * f + 1) % 3
                        for kc in range(8):
                            K.op(pe, lambda e, i=i, fl=fl, kc=kc, bg_=bg_: e.matmul(GU[bg_][:], lhsT=wgu[i][:, 0, kc, fl * 128:(fl + 1) * 128],
                                                                                     rhs=hT[cur][:, kc, :], start=(kc == 0), stop=(kc == 7)),
                                 reads=[bwgu[i], bhT[cur]], writes=[bGU[bg_]], sig=(kc == 7))
                        for kc in range(8):
                            K.op(pe, lambda e, i=i, fl=fl, kc=kc, bu_=bu_: e.matmul(GU[bu_][:], lhsT=wgu[i][:, 1, kc, fl * 128:(fl + 1) * 128],
                                                                                     rhs=hT[cur][:, kc, :], start=(kc == 0), stop=(kc == 7)),
                                 reads=[bwgu[i], bhT[cur]], writes=[bGU[bu_]], sig=(kc == 7))
                        K.op(act, lambda e, f=f, bg_=bg_: e.activation(out=gt[f % 2][:], in_=GU[bg_][:], func=AF.Silu),
                             reads=[bGU[bg_]], writes=[bgt[f % 2]])
                        K.op(dve, lambda e, f=f, bu_=bu_: e.tensor_tensor(out=actT[:, f, :], in0=gt[f % 2][:], in1=GU[bu_][:], op=ALU.mult),
                             reads=[bgt[f % 2], bGU[bu_]], writes=[bactT])
                    load_gu(k + 1 + NG)
                    if n + 1 < NTT and g in (1, 3, 5, 7):
                        phaseA(n + 1, (g - 1) // 2)
                    if g == 9 and bgq:
                        bgq.pop(0)()
                for j in range(4):
                    tile = n * 4 + j
                    c = ccnt[0] % 2
                    ccnt[0] += 1
                    K.dma(pool, sxc[c], xc[c][:], x_src[tile * 128:(tile + 1) * 128, :], reads=[self.xbuf[tile]], writes=[bxc[c]])
                    ybs = []
                    for hf in range(2):
                        yb = (2 * j + hf) % 3
                        ybs.append(yb)
                        for f in range(NF):
                            K.op(pe, lambda e, f=f, j=j, hf=hf, yb=yb: e.matmul(Y[yb][:], lhsT=actT[:, f, j * 128:(j + 1) * 128],
                                                                                 rhs=wd[:, f, hf * 512:(hf + 1) * 512],
                                                                                 start=(f == 0), stop=(f == NF - 1)),
                                 reads=[bactT, bwd], writes=[bY[yb]], sig=(f == NF - 1))
                    self.post_resid(vs, [(Y[ybs[0]][:], 0, 512), (Y[ybs[1]][:], 512, 512)], [bY[ybs[0]], bY[ybs[1]]],
                                    xc[c][:], bxc[c], xo[c], bxo[c], small[1], bsmall[1], junk, bjunk, tmp, btmp)
                    K.dma(pool, sxo[c], self.out[tile * 128:(tile + 1) * 128, :], xo[c][:], reads=[bxo[c]], writes=[self.xbuf[tile]])
            while bgq:
                bgq.pop(0)()
            K.barrier()
        K.end_scope()

    def build(self):
        K = self.K
        self.setup_globals()
        if self.do_cast:
            need = []
            for (li, kind) in self.subl:
                if kind in ("f0", "f1"):
                    j = 0 if kind == "f0" else 1
                    need.append((WOFF[("gu", li, j)], W_GU + W_D))
                elif li % 2 == 0:
                    need.append((WOFF[("ain", li // 2)], W_AIN + W_AOUT))
                else:
                    need.append((WOFF[("bin", li // 2, 0)], 2 * (W_BIN + W_BOUT)))
            self.cast_plan(need)
            self.cast_bg(0, upto_group=0)
        x_src = self.x_in
        nsub = len(self.subl)

        def sglob(li, kind):
            return li * 3 + {"f0": 0, "mix": 1, "f1": 2}[kind]

        def coef(kind):
            return 1.0 if kind == "mix" else 0.5

        li, kind = self.subl[0]
        for stp in self.ada_steps(sglob(li, kind), 0, coef(kind)):
            stp()
        for si, (li, kind) in enumerate(self.subl):
            vs = si % 2
            bg = []
            self.wbf_grp = self.wgrp[si]
            if si + 1 < nsub:
                nl, nk = self.subl[si + 1]
                steps = self.ada_steps(sglob(nl, nk), (si + 1) % 2, coef(nk))
                bg = [(lambda s_=s_, si=si: (self.cast_bg(2), s_())) for s_ in steps]
                bg.append(lambda si=si: self.cast_bg(0, upto_group=si + 1))
            if kind in ("f0", "f1"):
                j = 0 if kind == "f0" else 1
                self.ffn(x_src, vs, WOFF[("gu", li, j)], WOFF[("d", li, j)], bg)
            elif li % 2 == 0:
                self.dsa(x_src, vs, li // 2, bg)
            else:
                self.diff(x_src, vs, li // 2, bg)
            x_src = self.out
        K.barrier()
        return self.nc

    def neg_bound(self, qsq, bq, ksq, bk, run, brun, mneg, bmneg, small, bsmall):
        K = self.K
        pe, dve, act, pool = K.pe, K.dve, K.act, K.pool
        TAr = self.TA[0:1, 0:256]
        K.op(pe, lambda e: e.transpose(out=self.TA[0:1, 0:128], in_=qsq, identity=self.identf[:]),
             reads=[bq, self.bconst], writes=[self.bTA], sig=False)
        K.op(pe, lambda e: e.transpose(out=self.TA[0:1, 128:256], in_=ksq, identity=self.identf[:]),
             reads=[bk, self.bconst], writes=[self.bTA])
        K.op(dve, lambda e: e.tensor_reduce(out=small[0:1, 0:2], in_=TAr.rearrange("p (a b) -> p a b", a=2), axis=AX.X, op=ALU.max),
             reads=[self.bTA], writes=[bsmall])
        K.op(dve, lambda e: e.tensor_tensor(out=run[0:1, 0:1], in0=run[0:1, 0:1], in1=small[0:1, 1:2], op=ALU.max),
             reads=[bsmall], writes=[brun])
        K.op(dve, lambda e: e.tensor_tensor(out=small[0:1, 2:3], in0=small[0:1, 0:1], in1=run[0:1, 0:1], op=ALU.mult),
             reads=[bsmall, brun], writes=[bsmall])
        K.op(pe, lambda e: e.matmul(self.TA[:, 0:1], lhsT=self.ones_row[0:1, :], rhs=small[0:1, 2:3], start=True, stop=True),
             reads=[bsmall, self.bconst], writes=[self.bTA])
        K.op(dve, lambda e: e.tensor_copy(out=mneg[:, 2:3], in_=self.TA[:, 0:1]), reads=[self.bTA], writes=[bmneg])
        K.op(pool, lambda e: e.tensor_tensor(out=mneg[:, 2:3], in0=mneg[:, 2:3], in1=self.half[:, 0:1], op=ALU.pow),
             reads=[bmneg, self.bconst], writes=[bmneg])
        K.op(pool, lambda e: e.tensor_scalar(out=mneg[:, 0:1], in0=mneg[:, 2:3], scalar1=-0.125, scalar2=None, op0=ALU.mult),
             reads=[bmneg], writes=[bmneg])

    def rope(self, pe_t, bpe, h0, nh, ti, rt, brt):
        K = self.K
        dve = K.dve
        v = pe_t[:, h0 * 64:(h0 + nh) * 64].rearrange("p (h d) -> p h d", d=64)
        x1 = v[:, :, 0:8]
        x2 = v[:, :, 8:16]
        cb = self.cosT[:, ti:ti + 1, :].to_broadcast([128, nh, 8])
        sbb = self.sinT[:, ti:ti + 1, :].to_broadcast([128, nh, 8])
        t = [rt[:, k, 0:nh, :] for k in range(4)]
        rd = [bpe, self.brope]
        K.op(dve, lambda e: e.tensor_tensor(out=t[0], in0=x1, in1=cb, op=ALU.mult), reads=rd, writes=[brt])
        K.op(dve, lambda e: e.tensor_tensor(out=t[1], in0=x2, in1=sbb, op=ALU.mult), reads=rd, writes=[brt])
        K.op(dve, lambda e: e.tensor_tensor(out=t[2], in0=x2, in1=cb, op=ALU.mult), reads=rd, writes=[brt])
        K.op(dve, lambda e: e.tensor_tensor(out=t[3], in0=x1, in1=sbb, op=ALU.mult), reads=rd, writes=[brt])
        K.op(dve, lambda e: e.tensor_tensor(out=x1, in0=t[0], in1=t[1], op=ALU.subtract), reads=[brt], writes=[bpe])
        K.op(dve, lambda e: e.tensor_tensor(out=x2, in0=t[2], in1=t[3], op=ALU.add), reads=[brt], writes=[bpe])

    def dsa(self, x_src, vs, jl, bg):
        K, T, NT = self.K, self.T, self.NT
        pe, dve, act, pool, sp = K.pe, K.dve, K.act, K.pool, K.sp
        off_in, off_out = WOFF[("ain", jl)], WOFF[("aout", jl)]
        K.begin_scope()
        with ExitStack() as st:
            sb = lambda n, s, d: self.sb(st, n, s, d)
            w_in = sb("w_in", [128, 8, A_IN], BF16); bw = Buf("w"); sw = Slot(K, "w")
            w_out = sb("w_out", [128, 8, D], BF16)
            kT2 = sb("kT2", [128, T], BF16); bkT = Buf("kT2")
            kiT2 = sb("kiT2", [128, T], BF16); bkiT = Buf("kiT2")
            Vaug = sb("Vaug", [128, NT, 65], BF16); bV = Buf("Vaug")
            score = sb("score", [128, T], F32); bscore = Buf("score")
            NM = sb("NM", [128, T], BF16); bNM = Buf("NM")
            rtmp = [sb(f"rtmp{i}", [128, 512], F32) for i in range(2)]; brtmp = [Buf(f"rtmp{i}") for i in range(2)]
            PT = [sb(f"PT{i}", [128, 16, 128], BF16) for i in range(2)]; bPT = [Buf(f"PT{i}") for i in range(2)]
            pev = sb("pev", [128, A_IN], F32); bpev = Buf("pev")
            rt = sb("rt", [128, 4, 17, 8], F32); brt = Buf("rt")
            qb = sb("qb", [128, 1152], BF16); bqb = Buf("qb")
            qib = sb("qib", [128, 640], BF16); bqib = Buf("qib")
            qT = sb("qT", [128, 8, 128], BF16); bqT = Buf("qT")
            qiT = sb("qiT", [128, 4, 128], BF16); bqiT = Buf("qiT")
            wi = sb("wi", [128, 8], F32); bwi = Buf("wi")
            xa = [sb(f"xa{i}", [128, D], F32) for i in range(2)]; bxa = [Buf(f"xa{i}") for i in range(2)]
            sxa = [Slot(K, f"xa{i}") for i in range(2)]
            sxo = [Slot(K, f"xo{i}") for i in range(2)]
            tmp = sb("tmp", [128, D], F32); btmp = Buf("tmp")
            hb = sb("hb", [128, D], BF16); bhb = Buf("hb")
            hT = sb("hT", [128, 8, 128], BF16); bhT = Buf("hT")
            ob = sb("ob", [128, D], BF16); bob = Buf("ob")
            oT = sb("oT", [128, 8, 128], BF16); boT = Buf("oT")
            junk = sb("junk", [128, D], BF16); bjunk = Buf("junk")
            small = [sb(f"small{i}", [128, 8], F32) for i in range(2)]; bsmall = [Buf(f"small{i}") for i in range(2)]
            sq = sb("sq", [128, 24], F32); bsq = Buf("sq")
            ksqt = sb("ksqt", [128, 64], F32)
            run = sb("run", [128, 2], F32); brun = Buf("run")
            mneg = sb("mneg", [128, 4], F32); bmneg = Buf("mneg")
            bis = sb("bis", [128, 8], F32); bbis = Buf("bis")
            dk = sb("dk", [128, NBIS + 2], F32)
            p2 = sb("p2", [128, NBIS + 2], F32)
            rl = sb("rl", [128, 16], F32); brl = Buf("rl")
            P = [self.ps(st, f"P{i}", [128, 512], F32) for i in range(4)]; bP = [Buf(f"P{i}") for i in range(4)]
            O = self.ps(st, "O", [128, 1536], F32); bO = Buf("O")

            K.dma(sp, sw, w_in[:].rearrange("p a b -> p (a b)"), self.wbf[:, off_in:off_in + W_AIN], reads=self.wbf_grp, writes=[bw])
            K.dma(sp, sw, w_out[:].rearrange("p a b -> p (a b)"), self.wbf[:, off_out:off_out + W_AOUT], reads=self.wbf_grp, writes=[bw])
            K.op(pool, lambda e: e.memset(Vaug[:, :, 64:65], 1.0), writes=[bV])
            K.op(pool, lambda e: e.memset(run[:], 0.0), writes=[brun])
            for k in range(NBIS + 2):
                K.op(pool, lambda e, k=k: e.memset(p2[:, k:k + 1], 2.0 ** (-(k + 1))), writes=[bbis])
            bgq = list(bg)
            pcnt = [0]
            if not hasattr(self, "fill_reg"):
                self.fill_reg = self.nc.gpsimd.to_reg(-1e30)

            def pbank():
                b = pcnt[0] % 4
                pcnt[0] += 1
                return b

            for i in range(NT):
                a = i % 2
                n = (i + 1) * 128
                K.dma(pool, sxa[a], xa[a][:], x_src[i * 128:(i + 1) * 128, :], reads=[self.xbuf[i]], writes=[bxa[a]])
                self.norm_mod_T(vs, xa[a][:], bxa[a], hb, bhb, tmp, btmp, small[0], bsmall[0], junk, bjunk, hT[:], bhT)
                for b in range(4):
                    c0, c1 = b * 512, min((b + 1) * 512, A_IN)
                    for kc in range(8):
                        K.op(pe, lambda e, b=b, kc=kc, c0=c0, c1=c1: e.matmul(P[b][:, 0:c1 - c0], lhsT=hT[:, kc, :], rhs=w_in[:, kc, c0:c1],
                                                                             start=(kc == 0), stop=(kc == 7)),
                             reads=[bhT, bw], writes=[bP[b]], sig=(kc == 7))
                    K.op(act, lambda e, b=b, c0=c0, c1=c1: e.activation(out=pev[:, c0:c1], in_=P[b][:, 0:c1 - c0], func=AF.Copy),
                         reads=[bP[b]], writes=[bpev])
                pcnt[0] = 0
                self.rope(pev[:], bpev, 0, 17, i, rt, brt)
                self.rope(pev[:], bpev, 18, 9, i, rt, brt)
                K.op(act, lambda e: e.activation(out=qb[:, 0:1088], in_=pev[:, 0:1088], func=AF.Copy), reads=[bpev], writes=[bqb])
                K.op(act, lambda e: e.activation(out=qb[:, 1088:1152], in_=pev[:, 1024:1088], func=AF.Copy), reads=[bpev], writes=[bqb])
                K.op(act, lambda e, i=i: e.activation(out=Vaug[:, i, 0:64], in_=pev[:, 1088:1152], func=AF.Copy), reads=[bpev], writes=[bV])
                K.op(act, lambda e: e.activation(out=qib[:, 0:576], in_=pev[:, 1152:1728], func=AF.Copy), reads=[bpev], writes=[bqib])
                K.op(act, lambda e: e.activation(out=qib[:, 576:640], in_=pev[:, 1664:1728], func=AF.Copy), reads=[bpev], writes=[bqib])
                K.op(act, lambda e: e.activation(out=wi[:], in_=pev[:, 1728:1736], func=AF.Copy), reads=[bpev], writes=[bwi])
                K.op(act, lambda e: e.activation(out=tmp[:], in_=pev[:, 0:1024], func=AF.Square), reads=[bpev], writes=[btmp])
                K.op(act, lambda e: e.activation(out=ksqt[:], in_=pev[:, 1024:1088], func=AF.Square), reads=[bpev], writes=[btmp])
                K.op(dve, lambda e: e.tensor_reduce(out=sq[:, 0:16], in_=tmp[:].rearrange("p (h d) -> p h d", d=64),
                                                    axis=AX.X, op=ALU.add), reads=[btmp], writes=[bsq])
                K.op(dve, lambda e: e.tensor_reduce(out=sq[:, 16:17], in_=ksqt[:], axis=AX.X, op=ALU.add), reads=[btmp], writes=[bsq])
                K.op(dve, lambda e: e.tensor_reduce(out=sq[:, 20:21], in_=sq[:, 0:16], axis=AX.X, op=ALU.max), reads=[bsq], writes=[bsq])
                for s_ in range(8):
                    K.op(pe, lambda e, s_=s_: e.transpose(out=self.TAb[:, s_ * 128:(s_ + 1) * 128], in_=qb[:, s_ * 128:(s_ + 1) * 128],
                                                           identity=self.ident[:]),
                         reads=[bqb, self.bconst], writes=[self.bTA], sig=(s_ == 7))
                K.op(act, lambda e: e.activation(out=qT[:], in_=self.TAb[:].rearrange("p (a b) -> p a b", a=8), func=AF.Copy),
                     reads=[self.bTA], writes=[bqT])
                K.op(pe, lambda e: e.transpose(out=self.TAb[:, 0:128], in_=qb[:, 1024:1152], identity=self.ident[:]),
                     reads=[bqb, self.bconst], writes=[self.bTA], sig=False)
                for s_ in range(5):
                    K.op(pe, lambda e, s_=s_: e.transpose(out=self.TAb[:, (s_ + 1) * 128:(s_ + 2) * 128], in_=qib[:, s_ * 128:(s_ + 1) * 128],
                                                           identity=self.ident[:]),
                         reads=[bqib, self.bconst], writes=[self.bTA], sig=(s_ == 4))
                K.op(dve, lambda e, i=i: e.tensor_copy(out=kT2[:, i * 128:(i + 1) * 128], in_=self.TAb[:, 0:128]),
                     reads=[self.bTA], writes=[bkT])
                K.op(dve, lambda e: e.tensor_copy(out=qiT[:], in_=self.TAb[:, 128:640].rearrange("p (a b) -> p a b", a=4)),
                     reads=[self.bTA], writes=[bqiT])
                K.op(dve, lambda e, i=i: e.tensor_copy(out=kiT2[:, i * 128:(i + 1) * 128], in_=self.TAb[:, 640:768]),
                     reads=[self.bTA], writes=[bkiT])
                self.neg_bound(sq[:, 20:21], bsq, sq[:, 16:17], bsq, run, brun, mneg, bmneg, small[1], bsmall[1])
                nblk = (n + 511) // 512
                rc = 0
                for sbk in range(nblk):
                    w = min(512, n - sbk * 512)
                    cs = slice(sbk * 512, sbk * 512 + w)
                    for h in range(8):
                        s_, hf = h % 4, h // 4
                        b = pbank()
                        K.op(pe, lambda e, b=b, s_=s_, hf=hf, cs=cs, w=w: e.matmul(P[b][:, 0:w], lhsT=qiT[hf * 64:(hf + 1) * 64, s_, :],
                                                                                    rhs=kiT2[hf * 64:(hf + 1) * 64, cs], start=True, stop=True),
                             reads=[bqiT, bkiT], writes=[bP[b]])
                        r = rc % 2
                        rc += 1
                        K.op(act, lambda e, b=b, r=r, w=w: e.activation(out=rtmp[r][:, 0:w], in_=P[b][:, 0:w], func=AF.Relu),
                             reads=[bP[b]], writes=[brtmp[r]])
                        if h == 0:
                            K.op(dve, lambda e, r=r, w=w, cs=cs, h=h: e.tensor_scalar(out=score[:, cs], in0=rtmp[r][:, 0:w], scalar1=wi[:, h:h + 1],
                                                                                       scalar2=None, op0=ALU.mult),
                                 reads=[brtmp[r], bwi], writes=[bscore])
                        else:
                            K.op(dve, lambda e, r=r, w=w, cs=cs, h=h: e.scalar_tensor_tensor(out=score[:, cs], in0=rtmp[r][:, 0:w], scalar=wi[:, h:h + 1],
                                                                                              in1=score[:, cs], op0=ALU.mult, op1=ALU.add),
                                 reads=[brtmp[r], bwi, bscore], writes=[bscore])
                lo, mid, cnt, u, t2, d0 = (bis[:, k:k + 1] for k in range(6))
                if n > 256:
                    K.op(dve, lambda e: e.tensor_reduce(out=lo, in_=score[:, 0:n], axis=AX.X, op=ALU.min), reads=[bscore], writes=[bbis])
                    K.op(dve, lambda e: e.tensor_reduce(out=d0, in_=score[:, 0:n], axis=AX.X, op=ALU.max), reads=[bscore], writes=[bbis])
                    K.op(dve, lambda e: e.tensor_tensor(out=d0, in0=d0, in1=lo, op=ALU.subtract), reads=[bbis], writes=[bbis])
                    K.op(dve, lambda e: e.tensor_scalar(out=dk[:], in0=p2[:], scalar1=d0, scalar2=None, op0=ALU.mult), reads=[bbis], writes=[bbis])
                    K.op(dve, lambda e: e.tensor_tensor(out=mid, in0=lo, in1=dk[:, 0:1], op=ALU.add), reads=[bbis], writes=[bbis])
                K.op(pool, lambda e, i=i: e.affine_select(out=score[:, i * 128:(i + 1) * 128], in_=score[:, i * 128:(i + 1) * 128],
                                                          pattern=[[-1, 128]], compare_op=ALU.is_ge, fill=self.fill_reg, base=0, channel_multiplier=1),
                     reads=[bscore, bbis], writes=[bscore])
                if n > 256:
                    for k in range(NBIS):
                        K.op(dve, lambda e: e.tensor_scalar(out=NM[:, 0:n], in0=score[:, 0:n], scalar1=mid, scalar2=0.0, op0=ALU.is_ge,
                                                            op1=ALU.add, accum_out=cnt),
                             reads=[bscore, bbis], writes=[bNM, bbis])
                        K.op(dve, lambda e, k=k: e.tensor_scalar(out=u, in0=cnt, scalar1=255.5, scalar2=dk[:, k:k + 1], op0=ALU.is_ge, op1=ALU.mult),
                             reads=[bbis], writes=[bbis])
                        K.op(dve, lambda e, k=k: e.scalar_tensor_tensor(out=mid, in0=u, scalar=dk[:, k + 1:k + 2], in1=mid, op0=ALU.subtract, op1=ALU.add),
                             reads=[bbis], writes=[bbis])
                    K.op(dve, lambda e: e.tensor_tensor(out=lo, in0=mid, in1=dk[:, NBIS:NBIS + 1], op=ALU.subtract), reads=[bbis], writes=[bbis])
                    K.op(dve, lambda e: e.tensor_scalar(out=NM[:, 0:n], in0=score[:, 0:n], scalar1=lo, scalar2=NEG, op0=ALU.is_lt, op1=ALU.mult),
                         reads=[bscore, bbis], writes=[bNM])
                else:
                    K.op(dve, lambda e: e.tensor_scalar(out=NM[:, 0:n], in0=score[:, 0:n], scalar1=-1e29, scalar2=NEG, op0=ALU.is_lt, op1=ALU.mult),
                         reads=[bscore], writes=[bNM])
                K.op(dve, lambda e: e.memset(O[:], 0.0), writes=[bO])
                for c in range(i + 1):
                    pt = c % 2
                    for hg in range(4):
                        hf, s0 = hg // 2, (hg % 2) * 4
                        b = pbank()
                        K.op(pe, lambda e, b=b, c=c: e.matmul(P[b][:], lhsT=NM[:, c * 128:(c + 1) * 128], rhs=self.I4[:].rearrange("p a b -> p (a b)"),
                                                              start=True, stop=False),
                             reads=[bNM, self.bconst], writes=[bP[b]], sig=False)
                        K.op(pe, lambda e, b=b, c=c, hf=hf, s0=s0: e.matmul(P[b][:], lhsT=kT2[hf * 64:(hf + 1) * 64, c * 128:(c + 1) * 128],
                                                                             rhs=qT[hf * 64:(hf + 1) * 64, s0:s0 + 4, :], start=False, stop=True),
                             reads=[bkT, bqT], writes=[bP[b]])
                        K.op(act, lambda e, b=b, pt=pt, hg=hg: e.activation(out=PT[pt][:, hg * 4:(hg + 1) * 4, :],
                                                                            in_=P[b][:].rearrange("p (a b) -> p a b", a=4), func=AF.Exp,
                                                                            bias=mneg[:, 0:1], scale=0.125),
                             reads=[bP[b], bmneg], writes=[bPT[pt]])
                    for hd in range(16):
                        col = (hd // 7) * 512 + (hd % 7) * 65
                        K.op(pe, lambda e, pt=pt, hd=hd, col=col, c=c: e.matmul(O[:, col:col + 65], lhsT=PT[pt][:, hd, :], rhs=Vaug[:, c, :],
                                                                                 start=False, stop=False, skip_group_check=True),
                             reads=[bPT[pt], bV, bO], writes=[bO], sig=(hd == 15))
                if self.debug and i == 0:
                    if self.dbg is None:
                        self.dbg = self.nc.dram_tensor("dbg", [128, 4096], F32, kind="ExternalOutput").ap()
                    dsb = sb("dsb", [128, 4096], F32); bd = Buf("dsb"); sd = Slot(K, "dsb")
                    K.op(dve, lambda e: e.memset(dsb[:], 0.0), writes=[bd])
                    K.op(dve, lambda e: e.tensor_copy(out=dsb[:, 0:1536], in_=O[:]), reads=[bO], writes=[bd])
                    K.op(dve, lambda e: e.tensor_copy(out=dsb[:, 1536:1540], in_=mneg[:]), reads=[bmneg], writes=[bd])
                    K.op(dve, lambda e: e.tensor_copy(out=dsb[:, 1600:1600 + 130], in_=Vaug[:, 0:2, :].rearrange("p a b -> p (a b)")), reads=[bV], writes=[bd])
                    K.op(dve, lambda e: e.tensor_copy(out=dsb[:, 2048:4096], in_=PT[0][:].rearrange("p a b -> p (a b)")), reads=[bPT[0]], writes=[bd])
                    K.dma(sp, sd, self.dbg, dsb[:], reads=[bd])
                for bk in range(3):
                    nh = 7 if bk < 2 else 2
                    Ov = O[:, bk * 512: bk * 512 + nh * 65].rearrange("p (h e) -> p h e", e=65)
                    K.op(dve, lambda e, bk=bk, nh=nh, Ov=Ov: e.reciprocal(out=rl[:, bk * 7: bk * 7 + nh], in_=Ov[:, :, 64]),
                         reads=[bO], writes=[brl])
                    K.op(dve, lambda e, bk=bk, nh=nh, Ov=Ov: e.tensor_tensor(
                        out=ob[:, bk * 448: bk * 448 + nh * 64].rearrange("p (h d) -> p h d", d=64), in0=Ov[:, :, 0:64],
                        in1=rl[:, bk * 7: bk * 7 + nh].rearrange("p (h o) -> p h o", o=1).to_broadcast([128, nh, 64]), op=ALU.mult),
                         reads=[bO, brl], writes=[bob])
                for kc in range(8):
                    K.op(pe, lambda e, kc=kc: e.transpose(out=self.TAb[:, kc * 128:(kc + 1) * 128], in_=ob[:, kc * 128:(kc + 1) * 128],
                                                          identity=self.ident[:]),
                         reads=[bob, self.bconst], writes=[self.bTA], sig=(kc == 7))
                K.op(act, lambda e: e.activation(out=oT[:], in_=self.TAb[:].rearrange("p (a b) -> p a b", a=8), func=AF.Copy),
                     reads=[self.bTA], writes=[boT])
                yb = [pbank(), pbank()]
                for hf in range(2):
                    for kc in range(8):
                        K.op(pe, lambda e, hf=hf, kc=kc, yb=yb: e.matmul(P[yb[hf]][:], lhsT=oT[:, kc, :], rhs=w_out[:, kc, hf * 512:(hf + 1) * 512],
                                                                         start=(kc == 0), stop=(kc == 7)),
                             reads=[boT, bw], writes=[bP[yb[hf]]], sig=(kc == 7))
                self.post_resid(vs, [(P[yb[0]][:], 0, 512), (P[yb[1]][:], 512, 512)], [bP[yb[0]], bP[yb[1]]],
                                xa[a][:], bxa[a], xa[a], bxa[a], small[1], bsmall[1], junk, bjunk, tmp, btmp)
                K.dma(pool, sxo[a], self.out[i * 128:(i + 1) * 128, :], xa[a][:], reads=[bxa[a]], writes=[self.xbuf[i]])
                if bgq and i % 4 == 3:
                    bgq.pop(0)()
            while bgq:
                bgq.pop(0)()
            K.barrier()
        K.end_scope()

    def diff(self, x_src, vs, jl, bg):
        K, T, NT = self.K, self.T, self.NT
        pe, dve, act, pool, sp = K.pe, K.dve, K.act, K.pool, K.sp
        bgq = list(bg)
        for g in range(2):
            off_in, off_out = WOFF[("bin", jl, g)], WOFF[("bout", jl, g)]
            K.begin_scope()
            with ExitStack() as st:
                sb = lambda n, s, d: self.sb(st, n, s, d)
                w_in = sb("w_in", [128, 8, 1536], BF16); bw = Buf("w"); sw = Slot(K, "w")
                w_out = sb("w_out", [128, 4, D], BF16)
                kT = sb("kT", [128, 4, T], BF16); bkT = Buf("kT")
                Vaug = sb("Vaug", [128, NT, 4, 129], BF16); bV = Buf("Vaug")
                PT = [sb(f"PT{i}", [128, 8, 128], BF16) for i in range(2)]; bPT = [Buf(f"PT{i}") for i in range(2)]
                pev = sb("pev", [128, 1536], F32); bpev = Buf("pev")
                rt = sb("rt", [128, 4, 16, 8], F32); brt = Buf("rt")
                qb = sb("qb", [128, 1024], BF16); bqb = Buf("qb")
                qT = sb("qT", [128, 4, 128], BF16); bqT = Buf("qT")
                sq = sb("sq", [128, 24], F32); bsq = Buf("sq")
                xa = [sb(f"xa{i}", [128, D], F32) for i in range(2)]; bxa = [Buf(f"xa{i}") for i in range(2)]
                sxa = [Slot(K, f"xa{i}") for i in range(2)]
                sxo = [Slot(K, f"xo{i}") for i in range(2)]
                ysb = [sb(f"ysb{i}", [128, D], F32) for i in range(2)]; bysb = [Buf(f"ysb{i}") for i in range(2)]
                sys_ = [Slot(K, f"ysb{i}") for i in range(2)]
                tmp = sb("tmp", [128, D], F32); btmp = Buf("tmp")
                hb = sb("hb", [128, D], BF16); bhb = Buf("hb")
                hT = sb("hT", [128, 8, 128], BF16); bhT = Buf("hT")
                on = sb("on", [128, 8, 128], F32); bon = Buf("on")
                od = sb("od", [128, 4, 128], F32); bod = Buf("od")
                ob = sb("ob", [128, 512], BF16); bob = Buf("ob")
                oT = sb("oT", [128, 4, 128], BF16); boT = Buf("oT")
                junk = sb("junk", [128, D], BF16); bjunk = Buf("junk")
                small = [sb(f"small{i}", [128, 8], F32) for i in range(2)]; bsmall = [Buf(f"small{i}") for i in range(2)]
                run = sb("run", [128, 2], F32); brun = Buf("run")
                mneg = sb("mneg", [128, 4], F32); bmneg = Buf("mneg")
                rl = sb("rl", [128, 8], F32); brl = Buf("rl")
                ssub = sb("ssub", [128, 8], F32); bssub = Buf("ssub")
                P = [self.ps(st, f"P{i}", [128, 512], F32) for i in range(4)]; bP = [Buf(f"P{i}") for i in range(4)]
                O = self.ps(st, "O", [128, 1536], F32); bO = Buf("O")

                K.dma(sp, sw, w_in[:].rearrange("p a b -> p (a b)"), self.wbf[:, off_in:off_in + W_BIN], reads=self.wbf_grp, writes=[bw])
                K.dma(sp, sw, w_out[:].rearrange("p a b -> p (a b)"), self.wbf[:, off_out:off_out + W_BOUT], reads=self.wbf_grp, writes=[bw])
                K.op(pool, lambda e: e.memset(Vaug[:, :, :, 128:129], 1.0), writes=[bV])
                K.op(pool, lambda e: e.memset(run[:], 0.0), writes=[brun])
                pcnt = [0]

                def pbank():
                    b = pcnt[0] % 4
                    pcnt[0] += 1
                    return b

                for i in range(NT):
                    a = i % 2
                    K.dma(pool, sxa[a], xa[a][:], x_src[i * 128:(i + 1) * 128, :], reads=[self.xbuf[i]], writes=[bxa[a]])
                    if g == 1:
                        K.dma(pool, sys_[a], ysb[a][:], self.ypart[i * 128:(i + 1) * 128, :], reads=[self.ypbuf[i]], writes=[bysb[a]])
                    self.norm_mod_T(vs, xa[a][:], bxa[a], hb, bhb, tmp, btmp, small[0], bsmall[0], junk, bjunk, hT[:], bhT)
                    for b in range(3):
                        for kc in range(8):
                            K.op(pe, lambda e, b=b, kc=kc: e.matmul(P[b][:], lhsT=hT[:, kc, :], rhs=w_in[:, kc, b * 512:(b + 1) * 512],
                                                                    start=(kc == 0), stop=(kc == 7)),
                                 reads=[bhT, bw], writes=[bP[b]], sig=(kc == 7))
                        K.op(act, lambda e, b=b: e.activation(out=pev[:, b * 512:(b + 1) * 512], in_=P[b][:], func=AF.Copy),
                             reads=[bP[b]], writes=[bpev])
                    pcnt[0] = 3
                    if STOP <= 1:
                        continue
                    self.rope(pev[:], bpev, 0, 16, i, rt, brt)
                    K.op(act, lambda e: e.activation(out=qb[:], in_=pev[:, 0:1024], func=AF.Copy), reads=[bpev], writes=[bqb])
                    K.op(act, lambda e, i=i: e.activation(out=Vaug[:, i, :, 0:128], in_=pev[:, 1024:1536].rearrange("p (h d) -> p h d", d=128),
                                                          func=AF.Copy), reads=[bpev], writes=[bV])
                    K.op(act, lambda e: e.activation(out=tmp[:], in_=pev[:, 0:1024], func=AF.Square), reads=[bpev], writes=[btmp])
                    K.op(dve, lambda e: e.tensor_reduce(out=sq[:, 0:16], in_=tmp[:].rearrange("p (h d) -> p h d", d=64), axis=AX.X, op=ALU.add),
                         reads=[btmp], writes=[bsq])
                    K.op(dve, lambda e: e.tensor_reduce(out=sq[:, 20:22], in_=sq[:, 0:16].rearrange("p (a b) -> p a b", a=2), axis=AX.X, op=ALU.max),
                         reads=[bsq], writes=[bsq])
                    for s_ in range(8):
                        K.op(pe, lambda e, s_=s_: e.transpose(out=self.TAb[:, s_ * 128:(s_ + 1) * 128], in_=qb[:, s_ * 128:(s_ + 1) * 128],
                                                               identity=self.ident[:]),
                             reads=[bqb, self.bconst], writes=[self.bTA], sig=(s_ == 7))
                    K.op(act, lambda e: e.activation(out=qT[:], in_=self.TAb[:, 0:512].rearrange("p (a b) -> p a b", a=4), func=AF.Copy),
                         reads=[self.bTA], writes=[bqT])
                    K.op(dve, lambda e, i=i: e.tensor_copy(out=kT[:, :, i * 128:(i + 1) * 128],
                                                           in_=self.TAb[:, 512:1024].rearrange("p (a b) -> p a b", a=4)),
                         reads=[self.bTA], writes=[bkT])
                    self.neg_bound(sq[:, 20:21], bsq, sq[:, 21:22], bsq, run, brun, mneg, bmneg, small[1], bsmall[1])
                    if STOP <= 2:
                        continue
                    K.op(dve, lambda e: e.memset(O[:], 0.0), writes=[bO])
                    for c in range(i + 1):
                        pt = c % 2
                        bb = [pbank(), pbank()]
                        if c == i:
                            for bnk in range(2):
                                K.op(pe, lambda e, b=bb[bnk]: e.matmul(P[b][:], lhsT=self.ident[:], rhs=self.CN8[:, 0:4, :].rearrange("p a b -> p (a b)"),
                                                                       start=True, stop=False),
                                     reads=[self.bconst], writes=[bP[bb[bnk]]], sig=False)
                        for ul in range(4):
                            for bnk in range(2):
                                b = bb[bnk]
                                K.op(pe, lambda e, b=b, ul=ul, bnk=bnk, c=c, i=i: e.matmul(
                                    P[b][:, ul * 128:(ul + 1) * 128], lhsT=kT[bnk * 64:(bnk + 1) * 64, ul, c * 128:(c + 1) * 128],
                                    rhs=qT[bnk * 64:(bnk + 1) * 64, ul, :], start=(c != i), stop=(c != i or ul == 3),
                                    skip_group_check=True),
                                     reads=[bkT, bqT], writes=[bP[b]], sig=(ul == 3))
                        for bnk in range(2):
                            b = bb[bnk]
                            K.op(act, lambda e, b=b, pt=pt, bnk=bnk: e.activation(out=PT[pt][:, bnk * 4:(bnk + 1) * 4, :],
                                                                                   in_=P[b][:].rearrange("p (a b) -> p a b", a=4), func=AF.Exp,
                                                                                   bias=mneg[:, 0:1], scale=0.125),
                                 reads=[bP[b], bmneg], writes=[bPT[pt]])
                        for u in range(8):
                            col = (u // 3) * 512 + (u % 3) * 129
                            jx = (u % 2) * 4 + u // 2
                            K.op(pe, lambda e, pt=pt, u=u, jx=jx, col=col, c=c: e.matmul(O[:, col:col + 129], lhsT=PT[pt][:, jx, :], rhs=Vaug[:, c, u % 4, :],
                                                                                          start=False, stop=False, skip_group_check=True),
                                 reads=[bPT[pt], bV, bO], writes=[bO], sig=(u == 7))
                    if STOP <= 3:
                        continue
                    for bk in range(3):
                        nh = 3 if bk < 2 else 2
                        Ov = O[:, bk * 512: bk * 512 + nh * 129].rearrange("p (h e) -> p h e", e=129)
                        K.op(dve, lambda e, bk=bk, nh=nh, Ov=Ov: e.reciprocal(out=rl[:, bk * 3: bk * 3 + nh], in_=Ov[:, :, 128]),
                             reads=[bO], writes=[brl])
                        K.op(dve, lambda e, bk=bk, nh=nh, Ov=Ov: e.tensor_tensor(
                            out=on[:, bk * 3: bk * 3 + nh, :], in0=Ov[:, :, 0:128],
                            in1=rl[:, bk * 3: bk * 3 + nh].rearrange("p (h o) -> p h o", o=1).to_broadcast([128, nh, 128]), op=ALU.mult),
                             reads=[bO, brl], writes=[bon])
                    K.op(dve, lambda e: e.scalar_tensor_tensor(out=od[:].rearrange("p a b -> p (a b)"), in0=on[:, 4:8, :].rearrange("p a b -> p (a b)"),
                                                               scalar=self.neglam[:, jl:jl + 1], in1=on[:, 0:4, :].rearrange("p a b -> p (a b)"),
                                                               op0=ALU.mult, op1=ALU.add),
                         reads=[bon, self.bdiffc], writes=[bod])
                    K.op(act, lambda e: e.activation(out=on[:, 0:4, :], in_=od[:], func=AF.Square), reads=[bod], writes=[bon])
                    K.op(dve, lambda e: e.tensor_reduce(out=ssub[:, 0:4], in_=on[:, 0:4, :], axis=AX.X, op=ALU.add), reads=[bon], writes=[bssub])
                    K.op(pool, lambda e: e.tensor_scalar(out=ssub[:, 4:8], in0=ssub[:, 0:4], scalar1=1.0 / 128.0, scalar2=EPS, op0=ALU.mult, op1=ALU.add),
                         reads=[bssub], writes=[bssub])
                    K.op(pool, lambda e: e.tensor_tensor(out=ssub[:, 4:8], in0=ssub[:, 4:8], in1=self.neghalf[:, 0:4], op=ALU.pow),
                         reads=[bssub, self.bconst], writes=[bssub])
                    K.op(dve, lambda e: e.tensor_tensor(out=od[:], in0=od[:],
                                                        in1=ssub[:, 4:8].rearrange("p (h o) -> p h o", o=1).to_broadcast([128, 4, 128]), op=ALU.mult),
                         reads=[bod, bssub], writes=[bod])
                    K.op(dve, lambda e: e.tensor_tensor(out=ob[:].rearrange("p (h d) -> p h d", d=128), in0=od[:],
                                                        in1=self.sublnb[:, jl:jl + 1, :].to_broadcast([128, 4, 128]), op=ALU.mult),
                         reads=[bod, self.bdiffc], writes=[bob])
                    if STOP <= 4:
                        continue
                    for kc in range(4):
                        K.op(pe, lambda e, kc=kc: e.transpose(out=self.TAb[:, kc * 128:(kc + 1) * 128], in_=ob[:, kc * 128:(kc + 1) * 128],
                                                              identity=self.ident[:]),
                             reads=[bob, self.bconst], writes=[self.bTA], sig=(kc == 3))
                    K.op(act, lambda e: e.activation(out=oT[:], in_=self.TAb[:, 0:512].rearrange("p (a b) -> p a b", a=4), func=AF.Copy),
                         reads=[self.bTA], writes=[boT])
                    yb = [pbank(), pbank()]
                    for hf in range(2):
                        for kc in range(4):
                            K.op(pe, lambda e, hf=hf, kc=kc, yb=yb: e.matmul(P[yb[hf]][:], lhsT=oT[:, kc, :], rhs=w_out[:, kc, hf * 512:(hf + 1) * 512],
                                                                             start=(kc == 0), stop=(kc == 3)),
                                 reads=[boT, bw], writes=[bP[yb[hf]]], sig=(kc == 3))
                    if STOP <= 5:
                        continue
                    if g == 0:
                        for hf in range(2):
                            K.op(act, lambda e, hf=hf, yb=yb, a=a: e.activation(out=ysb[a][:, hf * 512:(hf + 1) * 512], in_=P[yb[hf]][:], func=AF.Copy),
                                 reads=[bP[yb[hf]]], writes=[bysb[a]])
                        K.dma(pool, sys_[a], self.ypart[i * 128:(i + 1) * 128, :], ysb[a][:], reads=[bysb[a]], writes=[self.ypbuf[i]])
                    else:
                        for hf in range(2):
                            K.op(dve, lambda e, hf=hf, yb=yb, a=a: e.tensor_tensor(out=ysb[a][:, hf * 512:(hf + 1) * 512], in0=P[yb[hf]][:],
                                                                                    in1=ysb[a][:, hf * 512:(hf + 1) * 512], op=ALU.add),
                                 reads=[bP[yb[hf]], bysb[a]], writes=[bysb[a]])
                        self.post_resid(vs, [(ysb[a][:], 0, D)], [bysb[a]], xa[a][:], bxa[a], xa[a], bxa[a], small[1], bsmall[1],
                                        junk, bjunk, tmp, btmp)
                        K.dma(pool, sxo[a], self.out[i * 128:(i + 1) * 128, :], xa[a][:], reads=[bxa[a]], writes=[self.xbuf[i]])
                    if bgq and i % 8 == 7:
                        bgq.pop(0)()
                if g == 1:
                    while bgq:
                        bgq.pop(0)()
                K.barrier()
            K.end_scope()


def prep_weights(inp):
    wall = np.zeros((128, NTOT), np.float32)
    for i in range(DEPTH):
        for j in range(2):
            wg = np.zeros((D, FFP), np.float32); wg[:, :DFF] = inp["ffn_w_gate"][i, j]
            wu = np.zeros((D, FFP), np.float32); wu[:, :DFF] = inp["ffn_w_up"][i, j]
            gu = np.stack([wg, wu], 0).reshape(2, 8, 128, 11, 256)
            gu = gu.transpose(2, 3, 0, 1, 4).reshape(128, W_GU)
            o = WOFF[("gu", i, j)]
            wall[:, o:o + W_GU] = gu
            wdn = np.zeros((FFP, D), np.float32); wdn[:DFF] = inp["ffn_w_down"][i, j]
            o = WOFF[("d", i, j)]
            wall[:, o:o + W_D] = wdn.reshape(NF, 128, D).transpose(1, 0, 2).reshape(128, W_D)
    qperm = []
    for s in range(8):
        qperm += list(range(s * 64, s * 64 + 64)) + list(range((s + 8) * 64, (s + 8) * 64 + 64))
    qiperm = []
    for s in range(4):
        qiperm += list(range(1152 + s * 64, 1152 + s * 64 + 64)) + list(range(1152 + (s + 4) * 64, 1152 + (s + 4) * 64 + 64))
    aperm = np.array(qperm + list(range(1024, 1152)) + qiperm + list(range(1664, 1736)))
    for j in range(2):
        w = inp["dsa_w_in"][j][:, aperm]
        o = WOFF[("ain", j)]
        wall[:, o:o + W_AIN] = w.reshape(8, 128, A_IN).transpose(1, 0, 2).reshape(128, W_AIN)
        o = WOFF[("aout", j)]
        wall[:, o:o + W_AOUT] = inp["dsa_w_out"][j].reshape(8, 128, D).transpose(1, 0, 2).reshape(128, W_AOUT)
    for j in range(2):
        for g in range(2):
            cols = []
            for base in (0, 512, 1024, 1536):
                cols += list(range(base + g * 256, base + g * 256 + 256))
            cols += list(range(2048 + g * 512, 2048 + g * 512 + 512))
            w = inp["diff_w_in"][j][:, np.array(cols)]
            o = WOFF[("bin", j, g)]
            wall[:, o:o + W_BIN] = w.reshape(8, 128, 1536).transpose(1, 0, 2).reshape(128, W_BIN)
            wo = inp["diff_w_out"][j][g * 512:(g + 1) * 512]
            o = WOFF[("bout", j, g)]
            wall[:, o:o + W_BOUT] = wo.reshape(4, 128, D).transpose(1, 0, 2).reshape(128, W_BOUT)
    return wall


def prep_ada(inp):
    aw = np.asarray(inp["ada_w"], np.float32)
    a = aw.reshape(4, 4, 2, 128, 3, 6, 512)
    a = a.transpose(0, 4, 5, 1, 3, 2, 6)
    ada = np.ascontiguousarray(a).reshape(12 * 24, 128, 1024)
    adab = np.ascontiguousarray(np.asarray(inp["ada_b"], np.float32).reshape(1, 12 * 3072))
    return ada, adab


def make_in_maps(inp, T, ncores):
    wall = prep_weights(inp)
    ada, adab = prep_ada(inp)
    pre_n = np.ascontiguousarray(np.asarray(inp["pre_norm"], np.float32).reshape(12, D))
    post_n = np.ascontiguousarray(np.asarray(inp["post_norm"], np.float32).reshape(12, D))
    subln = np.ascontiguousarray(np.asarray(inp["diff_subln"], np.float32))
    lam = np.ascontiguousarray(np.asarray(inp["diff_lambda"], np.float32).reshape(2, 256))
    maps = []
    for b in range(ncores):
        maps.append({
            "x": np.ascontiguousarray(np.asarray(inp["x"][b, :T], np.float32)),
            "c": np.ascontiguousarray(np.asarray(inp["c"][b], np.float32).reshape(8, 128).T),
            "pos": np.ascontiguousarray(np.asarray(inp["positions"][b, :T], np.int32).reshape(T // 128, 128).T),
            "wall": wall, "ada": ada, "adab": adab, "pre_n": pre_n, "post_n": post_n, "subln": subln, "lam": lam,
        })
    return maps


ALL_SUBL = [(li, k) for li in range(DEPTH) for k in ("f0", "mix", "f1")]


def kernel(**inputs):
    T = 4096
    prog = Prog(T, ALL_SUBL)
    nc = prog.build()
    maps = make_in_maps(inputs, T, 8)
    res = run_bass_kernel_spmd(nc, maps, core_ids=list(range(8)))
    return np.stack([np.asarray(r["out"], np.float32) for r in res.results], 0)
```

```python
import math
from contextlib import ExitStack

import numpy as np
import concourse.bass as bass
import concourse.mybir as mybir
from concourse.bass_utils import run_bass_kernel_spmd

F32 = mybir.dt.float32
BF16 = mybir.dt.bfloat16
I32 = mybir.dt.int32
AF = mybir.ActivationFunctionType
ALU = mybir.AluOpType
AX = mybir.AxisListType

D = 1024
DFF = 2752
FFP = 2816
NF = 22
DEPTH = 4
EPS = 1e-6
A_IN = 1736
NEG = -30000.0
SEM_LIMIT = 12000
NBIS = 24
STOP = 99
PIPE = 2
YMASK = 0

W_GU = 11 * 2 * 8 * 256
W_D = NF * 1024
W_AIN = 8 * A_IN
W_AOUT = 8 * 1024
W_BIN = 8 * 1536
W_BOUT = 4 * 1024


def weight_offsets():
    off = {}
    o = 0
    for i in range(DEPTH):
        for j in range(2):
            off[("gu", i, j)] = o; o += W_GU
            off[("d", i, j)] = o; o += W_D
    for j in range(2):
        off[("ain", j)] = o; o += W_AIN
        off[("aout", j)] = o; o += W_AOUT
    for j in range(2):
        for g in range(2):
            off[("bin", j, g)] = o; o += W_BIN
            off[("bout", j, g)] = o; o += W_BOUT
    return off, o


WOFF, NTOT = weight_offsets()
CASTW = 4096
assert NTOT % CASTW == 0 or True


class Ev:
    __slots__ = ("sem", "val", "eng")

    def __init__(self, eng=None):
        self.sem = None
        self.val = 0
        self.eng = eng


class Buf:
    __slots__ = ("name", "w", "r")

    def __init__(self, name):
        self.name = name
        self.w = None
        self.r = {}


class Eng:
    def __init__(self, K, name, h):
        self.K = K
        self.name = name
        self.h = h
        self.sem = None
        self.cnt = 0
        self.seen = {}
        self.pending = []
        self.last = None
        self.n = 0


class _Slot:
    def __init__(self, K, name):
        self.sem = K.new_sem("d_" + name)
        self.cnt = 0
        self.name = name
        self.last = None
        K.slots.append(self)


def Slot(K, name):
    if K.free_slots:
        s = K.free_slots.pop()
    else:
        s = _Slot(K, name)
        s.name = f"s{len(K.slots)}"
    K.scope_slots.append(s)
    return s


class Kern:
    def __init__(self, nc):
        self.nc = nc
        self.es = ExitStack()
        self.nsem = 0
        self.slots = []
        self.pe = Eng(self, "pe", nc.tensor)
        self.act = Eng(self, "act", nc.scalar)
        self.dve = Eng(self, "dve", nc.vector)
        self.pool = Eng(self, "pool", nc.gpsimd)
        self.sp = Eng(self, "sp", nc.sync)
        self.engs = [self.pe, self.act, self.dve, self.pool, self.sp]
        self.ninstr = 0
        self.free_slots = []
        self.scope_slots = []

    def begin_scope(self):
        self._saved = self.scope_slots
        self.scope_slots = []

    def end_scope(self):
        self.free_slots.extend(self.scope_slots)
        self.scope_slots = self._saved

    def new_sem(self, name):
        self.nsem += 1
        return self.es.enter_context(self.nc.semaphore(f"{name}_{self.nsem}"))

    def _need(self, eng, ev, raw=False):
        if ev is None:
            return
        if ev.eng is eng and (not raw or eng is self.pe):
            return
        assert ev.sem is not None, "dependency on unsignaled instruction"
        k = id(ev.sem)
        if eng.seen.get(k, 0) >= ev.val:
            return
        eng.h.wait_ge(ev.sem, ev.val)
        eng.seen[k] = ev.val

    def _waits(self, eng, reads, writes):
        for b in reads:
            self._need(eng, b.w, raw=True)
        for b in writes:
            self._need(eng, b.w)
            for ev in b.r.values():
                self._need(eng, ev)

    def _record(self, ev, key, reads, writes):
        for b in writes:
            b.w = ev
            b.r = {}
        for b in reads:
            b.r[key] = ev

    def op(self, eng, fn, reads=(), writes=(), sig=True):
        self._waits(eng, reads, writes)
        ins = fn(eng.h)
        ev = Ev(eng)
        eng.n += 1
        self.ninstr += 1
        if sig:
            if eng.sem is None or eng.cnt >= SEM_LIMIT:
                eng.sem = self.new_sem(eng.name)
                eng.cnt = 0
            eng.cnt += 1
            ins.then_inc(eng.sem, 1)
            ev.sem, ev.val = eng.sem, eng.cnt
            for p in eng.pending:
                p.sem, p.val = eng.sem, eng.cnt
            eng.pending = []
            eng.last = ev
        else:
            eng.pending.append(ev)
        self._record(ev, eng.name, reads, writes)
        return ev

    def dma(self, q, slot, out, in_, reads=(), writes=()):
        self._waits(q, reads, writes)
        ins = q.h.dma_start(out=out, in_=in_)
        slot.cnt += 16
        ins.then_inc(slot.sem, 16)
        ev = Ev(None)
        ev.sem, ev.val = slot.sem, slot.cnt
        slot.last = ev
        self.ninstr += 1
        self._record(ev, "dma_" + slot.name, reads, writes)
        return ev

    def barrier(self):
        evs = []
        for e in self.engs:
            assert not e.pending, f"barrier with pending unsignaled instrs on {e.name}"
            if e.last is not None:
                evs.append(e.last)
        for s in self.slots:
            if s.last is not None:
                evs.append(s.last)
        for e in self.engs:
            for ev in evs:
                self._need(e, ev)


class Prog:
    def __init__(self, T, sublayers, do_cast=True):
        self.T = T
        self.NT = T // 128
        self.subl = sublayers
        self.do_cast = do_cast
        nc = bass.Bass("TRN2", target_bir_lowering=False)
        self.nc = nc
        self.K = Kern(nc)
        K = self.K
        NT = self.NT
        dt = nc.dram_tensor
        self.x_in = dt("x", [T, D], F32, kind="ExternalInput").ap()
        self.c_in = dt("c", [128, 8], F32, kind="ExternalInput").ap()
        self.pos_in = dt("pos", [128, NT], I32, kind="ExternalInput").ap()
        self.wall = dt("wall", [128, NTOT], F32, kind="ExternalInput").ap()
        self.ada = dt("ada", [12 * 24, 128, 1024], F32, kind="ExternalInput").ap()
        self.adab = dt("adab", [1, 12 * 3072], F32, kind="ExternalInput").ap()
        self.pre_n = dt("pre_n", [12, D], F32, kind="ExternalInput").ap()
        self.post_n = dt("post_n", [12, D], F32, kind="ExternalInput").ap()
        self.subln = dt("subln", [2, 128], F32, kind="ExternalInput").ap()
        self.lam = dt("lam", [2, 256], F32, kind="ExternalInput").ap()
        self.out = dt("out", [T, D], F32, kind="ExternalOutput").ap()
        self.debug = False
        self.dbg = None
        self.wbf = dt("wbf", [128, NTOT], BF16, kind="Internal").ap()
        self.ypart = dt("ypart", [T, D], F32, kind="Internal").ap()
        self.xbuf = [Buf(f"xd{i}") for i in range(NT)]
        self.ypbuf = [Buf(f"yp{i}") for i in range(NT)]
        self.wbf_buf = Buf("wbf")
        self.cast_pieces = []
        self.cast_slots = None
        self.gs = ExitStack()
        self._names = 0

    def sb(self, st, name, shape, dtype):
        self._names += 1
        return st.enter_context(self.nc.sbuf_tensor(f"{name}_{self._names}", shape, dtype))

    def ps(self, st, name, shape, dtype):
        self._names += 1
        return st.enter_context(self.nc.psum_tensor(f"{name}_{self._names}", shape, dtype))

    def setup_globals(self):
        K, nc, st, NT = self.K, self.nc, self.gs, self.NT
        sb = lambda n, s, d: self.sb(st, n, s, d)
        self.TA = self.ps(st, "TA", [128, 512], F32)
        self.TAb = self.TA[:].bitcast(BF16)
        self.bTA = Buf("TA")
        self.identf = sb("identf", [128, 128], F32)
        self.ident = sb("ident", [128, 128], BF16)
        self.I4 = sb("I4", [128, 4, 128], BF16)
        self.CN8 = sb("CN8", [128, 8, 128], BF16)
        self.ones_row = sb("ones_row", [1, 128], F32)
        self.neghalf = sb("neghalf", [128, 16], F32)
        self.half = sb("half", [128, 16], F32)
        self.bconst = Buf("consts")
        self.cosT = sb("cosT", [128, NT, 8], F32)
        self.sinT = sb("sinT", [128, NT, 8], F32)
        self.brope = Buf("rope")
        self.condrep = sb("condrep", [128, 8, 128], F32)
        self.bcond = Buf("cond")
        self.vecA = [sb(f"vA{i}", [128, D], F32) for i in range(2)]
        self.vecS = [sb(f"vS{i}", [128, D], F32) for i in range(2)]
        self.vecG = [sb(f"vG{i}", [128, D], F32) for i in range(2)]
        self.bvec = [Buf(f"vec{i}") for i in range(2)]
        self.adaring = [sb(f"adar{i}", [128, 2, 512], F32) for i in range(2)]
        self.badar = [Buf(f"adar{i}") for i in range(2)]
        self.sadar = [Slot(K, f"adar{i}") for i in range(2)]
        self.pgb = [sb(f"pgb{i}", [128, D], F32) for i in range(2)]
        self.bpgb = [Buf(f"pgb{i}") for i in range(2)]
        self.spgb = [Slot(K, f"pgb{i}") for i in range(2)]
        self.brow = sb("brow", [1, 512], F32)
        self.bbrow = Buf("brow")
        self.sbrow = Slot(K, "brow")
        self.neglam = sb("neglam", [128, 2], F32)
        self.sublnb = sb("sublnb", [128, 2, 128], F32)
        self.bdiffc = Buf("diffc")
        self.adacnt = 0

        pool, dve, act = K.pool, K.dve, K.act
        bc = self.bconst
        K.op(pool, lambda e: e.memset(self.identf[:], 0.0), writes=[bc])
        K.op(pool, lambda e: e.affine_select(out=self.identf[:], in_=self.identf[:], pattern=[[-1, 128]],
                                             compare_op=ALU.not_equal, fill=1.0, base=0, channel_multiplier=1),
             writes=[bc])
        K.op(pool, lambda e: e.tensor_copy(out=self.ident[:], in_=self.identf[:]), writes=[bc])
        for r in range(4):
            K.op(pool, lambda e, r=r: e.tensor_copy(out=self.I4[:, r, :], in_=self.identf[:]), writes=[bc])
        K.op(pool, lambda e: e.memset(self.CN8[:], 0.0), writes=[bc])
        K.op(pool, lambda e: e.affine_select(out=self.CN8[:], in_=self.CN8[:], pattern=[[0, 8], [1, 128]],
                                             compare_op=ALU.is_ge, fill=NEG, base=0, channel_multiplier=-1),
             writes=[bc])
        K.op(pool, lambda e: e.memset(self.ones_row[:], 1.0), writes=[bc])
        K.op(pool, lambda e: e.memset(self.neghalf[:], -0.5), writes=[bc])
        K.op(pool, lambda e: e.memset(self.half[:], 0.5), writes=[bc])

        with ExitStack() as ts:
            tsb = lambda n, s, d: self.sb(ts, n, s, d)
            c_sb = tsb("c_sb", [128, 8], F32)
            cond = tsb("cond", [128, 8], F32)
            b_c = Buf("c_sb")
            s_c = Slot(K, "c_sb")
            K.dma(K.sp, s_c, c_sb[:], self.c_in, writes=[b_c])
            K.op(act, lambda e: e.activation(out=cond[:], in_=c_sb[:], func=AF.Silu), reads=[b_c], writes=[self.bcond])
            zer = tsb("zer", [128, 128], F32)
            K.op(dve, lambda e: e.memset(zer[:], 0.0), writes=[b_c])
            for kc in range(8):
                K.op(dve, lambda e, kc=kc: e.tensor_scalar(out=self.condrep[:, kc, :], in0=zer[:],
                                                           scalar1=cond[:, kc:kc + 1], scalar2=None, op0=ALU.add),
                     reads=[b_c, self.bcond], writes=[self.bcond])
            pos_i = tsb("pos_i", [128, NT], I32)
            pos_f = tsb("pos_f", [128, NT], F32)
            invt = tsb("invt", [128, 8], F32)
            ang = tsb("ang", [128, NT, 8], F32)
            ang2 = tsb("ang2", [128, NT, 8], F32)
            b_p = Buf("pos")
            s_p = Slot(K, "pos")
            K.dma(K.sp, s_p, pos_i[:], self.pos_in, writes=[b_p])
            K.op(dve, lambda e: e.tensor_copy(out=pos_f[:], in_=pos_i[:]), reads=[b_p], writes=[b_p])
            for j in range(8):
                inv = float(np.float32(500000.0) ** np.float32(-(2.0 * j) / 16.0))
                K.op(dve, lambda e, j=j, inv=inv: e.memset(invt[:, j:j + 1], inv), writes=[b_p])
            K.op(dve, lambda e: e.tensor_tensor(out=ang[:], in0=pos_f[:].rearrange("p (n o) -> p n o", o=1).to_broadcast([128, NT, 8]),
                                                in1=invt[:].rearrange("p (o j) -> p o j", o=1).to_broadcast([128, NT, 8]),
                                                op=ALU.mult), reads=[b_p], writes=[b_p])
            TWO_PI = 2.0 * math.pi
            C1 = 6.28125
            C2 = TWO_PI - C1
            kf = tsb("kf", [128, NT, 8], F32)
            ki = tsb("ki", [128, NT, 8], I32)
            mm_ = tsb("mm_", [128, NT, 8], F32)

            def sin_of(dst, shift):
                K.op(dve, lambda e: e.tensor_scalar(out=ang2[:], in0=ang[:], scalar1=shift, scalar2=None, op0=ALU.add),
                     reads=[b_p, self.brope], writes=[b_p])
                K.op(dve, lambda e: e.tensor_scalar(out=kf[:], in0=ang2[:], scalar1=1.0 / TWO_PI, scalar2=None, op0=ALU.mult),
                     reads=[b_p], writes=[b_p])
                K.op(dve, lambda e: e.tensor_copy(out=ki[:], in_=kf[:]), reads=[b_p], writes=[b_p])
                K.op(dve, lambda e: e.tensor_copy(out=kf[:], in_=ki[:]), reads=[b_p], writes=[b_p])
                K.op(dve, lambda e: e.scalar_tensor_tensor(out=ang2[:], in0=kf[:], scalar=-C1, in1=ang2[:], op0=ALU.mult, op1=ALU.add),
                     reads=[b_p], writes=[b_p])
                K.op(dve, lambda e: e.scalar_tensor_tensor(out=ang2[:], in0=kf[:], scalar=-C2, in1=ang2[:], op0=ALU.mult, op1=ALU.add),
                     reads=[b_p], writes=[b_p])
                K.op(dve, lambda e: e.tensor_scalar(out=mm_[:], in0=ang2[:], scalar1=math.pi, scalar2=TWO_PI, op0=ALU.is_gt, op1=ALU.mult),
                     reads=[b_p], writes=[b_p])
                K.op(dve, lambda e: e.tensor_tensor(out=ang2[:], in0=ang2[:], in1=mm_[:], op=ALU.subtract), reads=[b_p], writes=[b_p])
                K.op(dve, lambda e: e.tensor_scalar(out=mm_[:], in0=ang2[:], scalar1=-math.pi, scalar2=TWO_PI, op0=ALU.is_lt, op1=ALU.mult),
                     reads=[b_p], writes=[b_p])
                K.op(dve, lambda e: e.tensor_tensor(out=ang2[:], in0=ang2[:], in1=mm_[:], op=ALU.add), reads=[b_p], writes=[b_p])
                K.op(act, lambda e: e.activation(out=dst[:], in_=ang2[:], func=AF.Sin), reads=[b_p], writes=[self.brope])

            sin_of(self.sinT, 0.0)
            sin_of(self.cosT, 0.5 * math.pi)
            lam_sb = tsb("lam_sb", [128, 2, 4, 64], F32)
            sub_sb = tsb("sub_sb", [128, 2, 128], F32)
            lp = tsb("lp", [128, 2, 2, 64], F32)
            ls = tsb("ls", [128, 4], F32)
            b_l = Buf("lam")
            s_l = Slot(K, "lam")
            s_l2 = Slot(K, "lam2")
            K.dma(K.sp, s_l, lam_sb[:].rearrange("p a b c -> p (a b c)"),
                  self.lam.rearrange("a b -> (a b)").partition_broadcast(128), writes=[b_l])
            b_l2 = Buf("sub")
            K.dma(K.sp, s_l2, sub_sb[:].rearrange("p a b -> p (a b)"),
                  self.subln.rearrange("a b -> (a b)").partition_broadcast(128), writes=[b_l2])
            K.op(dve, lambda e: e.tensor_tensor(out=lp[:], in0=lam_sb[:, :, 0:4:2, :], in1=lam_sb[:, :, 1:4:2, :],
                                                op=ALU.mult), reads=[b_l], writes=[b_l])
            K.op(dve, lambda e: e.tensor_reduce(out=ls[:], in_=lp[:].rearrange("p a b c -> p (a b) c"), axis=AX.X,
                                                op=ALU.add), reads=[b_l], writes=[b_l])
            K.op(act, lambda e: e.activation(out=ls[:], in_=ls[:], func=AF.Exp), reads=[b_l], writes=[b_l])
            for j in range(2):
                li = 0.8 - 0.6 * math.exp(-0.3 * (2 * j + 1))
                K.op(dve, lambda e, j=j, li=li: e.scalar_tensor_tensor(out=self.neglam[:, j:j + 1], in0=ls[:, 2 * j + 1:2 * j + 2],
                                                                       scalar=-li, in1=ls[:, 2 * j:2 * j + 1],
                                                                       op0=ALU.add, op1=ALU.subtract),
                     reads=[b_l], writes=[self.bdiffc])
                K.op(dve, lambda e, j=j, li=li: e.tensor_scalar(out=self.sublnb[:, j, :], in0=sub_sb[:, j, :],
                                                                scalar1=1.0 - li, scalar2=None, op0=ALU.mult),
                     reads=[b_l2], writes=[self.bdiffc])
            K.barrier()

    def cast_plan(self, ranges):
        CW = 8192
        self.wgrp = [[Buf(f"wgrp{g}_{r}") for r in range(3)] for g in range(len(ranges))]
        self.cast_ring = [Buf(f"castring{r}") for r in range(3)]
        self.cast_n = 0
        for g, (s0, ln) in enumerate(ranges):
            o = 0
            while o < ln:
                w = min(CW, ln - o)
                self.cast_pieces.append((s0 + o, w, g))
                o += w
        self.cast_slots = [Slot(self.K, f"cast{i}") for i in range(3)]

    def cast_bg(self, k=1, upto_group=None):
        K = self.K
        while self.cast_pieces and (k > 0 or (upto_group is not None and self.cast_pieces[0][2] <= upto_group)):
            c0, w, g = self.cast_pieces.pop(0)
            r = self.cast_n % 3
            self.cast_n += 1
            K.dma(K.pool, self.cast_slots[r], self.wbf[:, c0:c0 + w], self.wall[:, c0:c0 + w],
                  writes=[self.wgrp[g][r], self.cast_ring[r]])
            k -= 1

    def cast_weights(self, ranges):
        K = self.K
        K.begin_scope()
        with ExitStack() as ts:
            NB = 3
            fin = [self.sb(ts, f"cin{i}", [128, CASTW], F32) for i in range(NB)]
            fout = [self.sb(ts, f"cout{i}", [128, CASTW], BF16) for i in range(NB)]
            bin_ = [Buf(f"cin{i}") for i in range(NB)]
            bout = [Buf(f"cout{i}") for i in range(NB)]
            sin_ = [Slot(K, f"cin{i}") for i in range(NB)]
            sout = [Slot(K, f"cout{i}") for i in range(NB)]
            pieces = []
            for (s0, ln) in ranges:
                o = 0
                while o < ln:
                    w = min(CASTW, ln - o)
                    pieces.append((s0 + o, w))
                    o += w
            engs = [K.dve, K.pool, K.act]
            for n, (c0, w) in enumerate(pieces):
                i = n % NB
                K.dma(K.sp, sin_[i], fin[i][:, 0:w], self.wall[:, c0:c0 + w], writes=[bin_[i]])
                eng = engs[n % 3]
                if eng is K.act:
                    K.op(eng, lambda e, i=i, w=w: e.activation(out=fout[i][:, 0:w], in_=fin[i][:, 0:w], func=AF.Copy),
                         reads=[bin_[i]], writes=[bout[i]])
                else:
                    K.op(eng, lambda e, i=i, w=w: e.tensor_copy(out=fout[i][:, 0:w], in_=fin[i][:, 0:w]),
                         reads=[bin_[i]], writes=[bout[i]])
                K.dma(K.sp, sout[i], self.wbf[:, c0:c0 + w], fout[i][:, 0:w], reads=[bout[i]], writes=[self.wbf_buf])
            K.barrier()
        K.end_scope()

    def ada_steps(self, s_glob, vs, coef):
        K = self.K
        pe, dve, act = K.pe, K.dve, K.act

        def load_pg():
            K.dma(K.sp, self.spgb[0], self.pgb[0][:], self.pre_n[s_glob, :].partition_broadcast(128), writes=[self.bpgb[0]])
            K.dma(K.sp, self.spgb[1], self.pgb[1][:], self.post_n[s_glob, :].partition_broadcast(128), writes=[self.bpgb[1]])

        def step(c):
            if c == 0:
                load_pg()
            K.dma(K.sp, self.sbrow, self.brow[:], self.adab[0:1, s_glob * 3072 + c * 512: s_glob * 3072 + (c + 1) * 512],
                  writes=[self.bbrow])
            for kh in range(4):
                r = self.adacnt % 2
                self.adacnt += 1
                K.dma(K.sp, self.sadar[r], self.adaring[r][:].rearrange("p a b -> p (a b)"),
                      self.ada[s_glob * 24 + c * 4 + kh], writes=[self.badar[r]])
                for kl in range(2):
                    kc = kh * 2 + kl
                    K.op(pe, lambda e, r=r, kl=kl, kc=kc: e.matmul(self.TA[:], lhsT=self.condrep[:, kc, :], rhs=self.adaring[r][:, kl, :],
                                                                    start=(kc == 0), stop=False),
                         reads=[self.bcond, self.badar[r]], writes=[self.bTA], sig=(kl == 1))
            K.op(pe, lambda e: e.matmul(self.TA[:], lhsT=self.ones_row[0:1, :], rhs=self.brow[0:1, :], start=False, stop=True),
                 reads=[self.bconst, self.bbrow], writes=[self.bTA])
            cs = slice((c % 2) * 512, (c % 2) * 512 + 512)
            if c < 2:
                K.op(act, lambda e: e.activation(out=self.vecS[vs][:, cs], in_=self.TA[:], func=AF.Copy),
                     reads=[self.bTA], writes=[self.bvec[vs]])
            elif c < 4:
                K.op(dve, lambda e: e.scalar_tensor_tensor(out=self.vecA[vs][:, cs], in0=self.TA[:], scalar=1.0,
                                                           in1=self.pgb[0][:, cs], op0=ALU.add, op1=ALU.mult),
                     reads=[self.bTA, self.bpgb[0]], writes=[self.bvec[vs]])
            else:
                K.op(dve, lambda e: e.scalar_tensor_tensor(out=self.vecG[vs][:, cs], in0=self.TA[:], scalar=coef,
                                                           in1=self.pgb[1][:, cs], op0=ALU.mult, op1=ALU.mult),
                     reads=[self.bTA, self.bpgb[1]], writes=[self.bvec[vs]])

        return [lambda c=c: step(c) for c in range(6)]

    def norm_mod_T(self, vs, xt, bx, hb, bhb, tmp, btmp, small, bsmall, junk, bjunk, hT_out, bhT):
        K = self.K
        pe, dve, act, pool = K.pe, K.dve, K.act, K.pool
        ss = small[:, 0:1]
        rs = small[:, 1:2]
        K.op(act, lambda e: e.activation(out=junk[:], in_=xt, func=AF.Square, accum_out=ss), reads=[bx], writes=[bjunk, bsmall])
        K.op(pool, lambda e: e.tensor_scalar(out=rs, in0=ss, scalar1=1.0 / D, scalar2=EPS, op0=ALU.mult, op1=ALU.add),
             reads=[bsmall], writes=[bsmall])
        K.op(pool, lambda e: e.tensor_tensor(out=rs, in0=rs, in1=self.neghalf[:, 0:1], op=ALU.pow),
             reads=[bsmall, self.bconst], writes=[bsmall])
        K.op(dve, lambda e: e.scalar_tensor_tensor(out=tmp[:], in0=xt, scalar=rs, in1=self.vecA[vs][:], op0=ALU.mult, op1=ALU.mult),
             reads=[bx, bsmall, self.bvec[vs]], writes=[btmp])
        K.op(dve, lambda e: e.tensor_tensor(out=hb[:], in0=tmp[:], in1=self.vecS[vs][:], op=ALU.add),
             reads=[btmp, self.bvec[vs]], writes=[bhb])
        for kc in range(8):
            K.op(pe, lambda e, kc=kc: e.transpose(out=self.TAb[:, kc * 128:(kc + 1) * 128], in_=hb[:, kc * 128:(kc + 1) * 128],
                                                  identity=self.ident[:]),
                 reads=[bhb, self.bconst], writes=[self.bTA], sig=(kc == 7))
        K.op(act, lambda e: e.activation(out=hT_out, in_=self.TAb[:].rearrange("p (a b) -> p a b", a=8), func=AF.Copy),
             reads=[self.bTA], writes=[bhT])

    def post_resid(self, vs, y_srcs, by, xt, bx, xo, bxo, small, bsmall, junk, bjunk, tmp, btmp):
        K = self.K
        dve, act, pool = K.dve, K.act, K.pool
        n = len(y_srcs)
        for k, (yap, c0, w) in enumerate(y_srcs):
            K.op(act, lambda e, yap=yap, c0=c0, w=w, k=k: e.activation(out=junk[:, c0:c0 + w], in_=yap, func=AF.Square,
                                                                        accum_out=small[:, 4 + k:5 + k]),
                 reads=by, writes=[bjunk, bsmall])
        rs = small[:, 3:4]
        if n == 2:
            K.op(pool, lambda e: e.tensor_tensor(out=rs, in0=small[:, 4:5], in1=small[:, 5:6], op=ALU.add),
                 reads=[bsmall], writes=[bsmall])
            src = rs
        else:
            src = small[:, 4:5]
        K.op(pool, lambda e: e.tensor_scalar(out=rs, in0=src, scalar1=1.0 / D, scalar2=EPS, op0=ALU.mult, op1=ALU.add),
             reads=[bsmall], writes=[bsmall])
        K.op(pool, lambda e: e.tensor_tensor(out=rs, in0=rs, in1=self.neghalf[:, 0:1], op=ALU.pow),
             reads=[bsmall, self.bconst], writes=[bsmall])
        for (yap, c0, w) in y_srcs:
            K.op(dve, lambda e, yap=yap, c0=c0, w=w: e.scalar_tensor_tensor(out=tmp[:, c0:c0 + w], in0=yap, scalar=rs,
                                                                             in1=self.vecG[vs][:, c0:c0 + w],
                                                                             op0=ALU.mult, op1=ALU.mult),
                 reads=list(by) + [bsmall, self.bvec[vs]], writes=[btmp])
        K.op(dve, lambda e: e.tensor_tensor(out=xo[:], in0=tmp[:], in1=xt, op=ALU.add), reads=[btmp, bx], writes=[bxo])

    def ffn(self, x_src, vs, off_gu, off_d, bg):
        K, T, NT = self.K, self.T, self.NT
        pe, dve, act, pool, sp = K.pe, K.dve, K.act, K.pool, K.sp
        NTT = T // 512
        K.begin_scope()
        with ExitStack() as st:
            sb = lambda n, s, d: self.sb(st, n, s, d)
            NG = 3
            wgu = [sb(f"wgu{i}", [128, 2, 8, 256], BF16) for i in range(NG)]
            bwgu = [Buf(f"wgu{i}") for i in range(NG)]
            swgu = [Slot(K, f"wgu{i}") for i in range(NG)]
            wd = sb("wd", [128, NF, 1024], BF16)
            bwd = Buf("wd")
            swd = Slot(K, "wd")
            xa = [sb(f"xa{i}", [128, D], F32) for i in range(2)]
            bxa = [Buf(f"xa{i}") for i in range(2)]
            sxa = [Slot(K, f"xa{i}") for i in range(2)]
            xc = [sb(f"xc{i}", [128, D], F32) for i in range(2)]
            bxc = [Buf(f"xc{i}") for i in range(2)]
            sxc = [Slot(K, f"xc{i}") for i in range(2)]
            xo = [sb(f"xo{i}", [128, D], F32) for i in range(2)]
            bxo = [Buf(f"xo{i}") for i in range(2)]
            sxo = [Slot(K, f"xo{i}") for i in range(2)]
            tmp = sb("tmp", [128, D], F32)
            btmp = Buf("tmp")
            hb = [sb(f"hb{i}", [128, D], BF16) for i in range(2)]
            bhb = [Buf(f"hb{i}") for i in range(2)]
            hT = [sb(f"hT{i}", [128, 8, 512], BF16) for i in range(2)]
            bhT = [Buf(f"hT{i}") for i in range(2)]
            actT = sb("actT", [128, NF, 512], BF16)
            bactT = Buf("actT")
            gt = [sb(f"gt{i}", [128, 512], F32) for i in range(2)]
            bgt = [Buf(f"gt{i}") for i in range(2)]
            junk = sb("junk", [128, D], BF16)
            bjunk = Buf("junk")
            small = [sb(f"small{i}", [128, 8], F32) for i in range(2)]
            bsmall = [Buf(f"small{i}") for i in range(2)]
            GU = [self.ps(st, f"GU{i}", [128, 512], F32) for i in range(3)]
            bGU = [Buf(f"GU{i}") for i in range(3)]
            Y = [self.ps(st, f"Y{i}", [128, 512], F32) for i in range(3)]
            bY = [Buf(f"Y{i}") for i in range(3)]

            wcols = self.wbf
            for q4 in range(2):
                K.dma(sp, swd, wd[:, q4 * 11:(q4 + 1) * 11, :].rearrange("p a b -> p (a b)"),
                      wcols[:, off_d + q4 * 11 * 1024: off_d + (q4 + 1) * 11 * 1024], reads=self.wbf_grp, writes=[bwd])

            gu_seq = [(n, g) for n in range(NTT) for g in range(11)]
            self._gu_loaded = 0

            def load_gu(upto):
                while self._gu_loaded < min(upto, len(gu_seq)):
                    k = self._gu_loaded
                    n, g = gu_seq[k]
                    i = k % NG
                    K.dma(sp, swgu[i], wgu[i][:].rearrange("p a b c -> p (a b c)"),
                          wcols[:, off_gu + g * 4096: off_gu + (g + 1) * 4096], reads=self.wbf_grp, writes=[bwgu[i]])
                    self._gu_loaded += 1

            acnt = [0]

            def phaseA(n, j):
                a = acnt[0] % 2
                acnt[0] += 1
                tile = n * 4 + j
                K.dma(pool, sxa[a], xa[a][:], x_src[tile * 128:(tile + 1) * 128, :], reads=[self.xbuf[tile]], writes=[bxa[a]])
                self.norm_mod_T(vs, xa[a][:], bxa[a], hb[a], bhb[a], tmp, btmp, small[0], bsmall[0], junk, bjunk,
                                hT[n % 2][:, :, j * 128:(j + 1) * 128], bhT[n % 2])

            for j in range(4):
                phaseA(0, j)
            load_gu(NG)
            ccnt = [0]
            bgq = list(bg)
            for n in range(NTT):
                cur = n % 2
                for g in range(11):
                    k = n * 11 + g
                    i = k % NG
                    for fl in range(2):
                        f = 2 * g + fl
                        bg_, bu_ = (2 * f) % 3, (2 * f + 1) % 3
                        for kc in range(8):
                            K.op(pe, lambda e, i=i, fl=fl, kc=kc, bg_=bg_: e.matmul(GU[bg_][:], lhsT=wgu[i][:, 0, kc, fl * 128:(fl + 1) * 128],
                                                                                     rhs=hT[cur][:, kc, :], start=(kc == 0), stop=(kc == 7)),
                                 reads=[bwgu[i], bhT[cur]], writes=[bGU[bg_]], sig=(kc == 7))
                        for kc in range(8):
                            K.op(pe, lambda e, i=i, fl=fl, kc=kc, bu_=bu_: e.matmul(GU[bu_][:], lhsT=wgu[i][:, 1, kc, fl * 128:(fl + 1) * 128],
                                                                                     rhs=hT[cur][:, kc, :], start=(kc == 0), stop=(kc == 7)),
                                 reads=[bwgu[i], bhT[cur]], writes=[bGU[bu_]], sig=(kc == 7))
                        K.op(act, lambda e, f=f, bg_=bg_: e.activation(out=gt[f % 2][:], in_=GU[bg_][:], func=AF.Silu),
                             reads=[bGU[bg_]], writes=[bgt[f % 2]])
                        K.op(dve, lambda e, f=f, bu_=bu_: e.tensor_tensor(out=actT[:, f, :], in0=gt[f % 2][:], in1=GU[bu_][:], op=ALU.mult),
                             reads=[bgt[f % 2], bGU[bu_]], writes=[bactT])
                    load_gu(k + 1 + NG)
                    if n + 1 < NTT and g in (1, 3, 5, 7):
                        phaseA(n + 1, (g - 1) // 2)
                    if g == 9 and bgq:
                        bgq.pop(0)()
                for j in range(4):
                    tile = n * 4 + j
                    c = ccnt[0] % 2
                    ccnt[0] += 1
                    K.dma(pool, sxc[c], xc[c][:], x_src[tile * 128:(tile + 1) * 128, :], reads=[self.xbuf[tile]], writes=[bxc[c]])
                    ybs = []
                    for hf in range(2):
                        yb = (2 * j + hf) % 3
                        ybs.append(yb)
                        for f in range(NF):
                            K.op(pe, lambda e, f=f, j=j, hf=hf, yb=yb: e.matmul(Y[yb][:], lhsT=actT[:, f, j * 128:(j + 1) * 128],
                                                                                 rhs=wd[:, f, hf * 512:(hf + 1) * 512],
                                                                                 start=(f == 0), stop=(f == NF - 1)),
                                 reads=[bactT, bwd], writes=[bY[yb]], sig=(f == NF - 1))
                    self.post_resid(vs, [(Y[ybs[0]][:], 0, 512), (Y[ybs[1]][:], 512, 512)], [bY[ybs[0]], bY[ybs[1]]],
                                    xc[c][:], bxc[c], xo[c], bxo[c], small[1], bsmall[1], junk, bjunk, tmp, btmp)
                    K.dma(pool, sxo[c], self.out[tile * 128:(tile + 1) * 128, :], xo[c][:], reads=[bxo[c]], writes=[self.xbuf[tile]])
            while bgq:
                bgq.pop(0)()
            K.barrier()
        K.end_scope()

    def build(self):
        K = self.K
        self.setup_globals()
        if self.do_cast:
            need = []
            for (li, kind) in self.subl:
                if kind in ("f0", "f1"):
                    j = 0 if kind == "f0" else 1
                    need.append((WOFF[("gu", li, j)], W_GU + W_D))
                elif li % 2 == 0:
                    need.append((WOFF[("ain", li // 2)], W_AIN + W_AOUT))
                else:
                    need.append((WOFF[("bin", li // 2, 0)], 2 * (W_BIN + W_BOUT)))
            self.cast_plan(need)
            self.cast_bg(0, upto_group=0)
        x_src = self.x_in
        nsub = len(self.subl)

        def sglob(li, kind):
            return li * 3 + {"f0": 0, "mix": 1, "f1": 2}[kind]

        def coef(kind):
            return 1.0 if kind == "mix" else 0.5

        li, kind = self.subl[0]
        for stp in self.ada_steps(sglob(li, kind), 0, coef(kind)):
            stp()
        for si, (li, kind) in enumerate(self.subl):
            vs = si % 2
            bg = []
            self.wbf_grp = self.wgrp[si]
            if si + 1 < nsub:
                nl, nk = self.subl[si + 1]
                steps = self.ada_steps(sglob(nl, nk), (si + 1) % 2, coef(nk))
                bg = [(lambda s_=s_, si=si: (self.cast_bg(2), s_())) for s_ in steps]
                bg.append(lambda si=si: self.cast_bg(0, upto_group=si + 1))
            if kind in ("f0", "f1"):
                j = 0 if kind == "f0" else 1
                self.ffn(x_src, vs, WOFF[("gu", li, j)], WOFF[("d", li, j)], bg)
            elif li % 2 == 0:
                self.dsa(x_src, vs, li // 2, bg)
            else:
                self.diff(x_src, vs, li // 2, bg)
            x_src = self.out
        K.barrier()
        return self.nc

    @staticmethod
    def interleave(*gens):
        gens = [g for g in gens if g is not None]
        while gens:
            for g in list(gens):
                try:
                    next(g)
                except StopIteration:
                    gens.remove(g)

    @staticmethod
    def chain(*gens):
        for g in gens:
            if g is not None:
                yield from g

    def neg_bound(self, qsq, bq, ksq, bk, run, brun, mneg, bmneg, small, bsmall):
        K = self.K
        pe, dve, act, pool = K.pe, K.dve, K.act, K.pool
        TAr = self.TA[0:1, 0:256]
        K.op(pe, lambda e: e.transpose(out=self.TA[0:1, 0:128], in_=qsq, identity=self.identf[:]),
             reads=[bq, self.bconst], writes=[self.bTA], sig=False)
        K.op(pe, lambda e: e.transpose(out=self.TA[0:1, 128:256], in_=ksq, identity=self.identf[:]),
             reads=[bk, self.bconst], writes=[self.bTA])
        K.op(dve, lambda e: e.tensor_reduce(out=small[0:1, 0:2], in_=TAr.rearrange("p (a b) -> p a b", a=2), axis=AX.X, op=ALU.max),
             reads=[self.bTA], writes=[bsmall])
        K.op(dve, lambda e: e.tensor_tensor(out=run[0:1, 0:1], in0=run[0:1, 0:1], in1=small[0:1, 1:2], op=ALU.max),
             reads=[bsmall], writes=[brun])
        K.op(dve, lambda e: e.tensor_tensor(out=small[0:1, 2:3], in0=small[0:1, 0:1], in1=run[0:1, 0:1], op=ALU.mult),
             reads=[bsmall, brun], writes=[bsmall])
        K.op(pe, lambda e: e.matmul(self.TA[:, 0:1], lhsT=self.ones_row[0:1, :], rhs=small[0:1, 2:3], start=True, stop=True),
             reads=[bsmall, self.bconst], writes=[self.bTA])
        K.op(dve, lambda e: e.tensor_copy(out=mneg[:, 2:3], in_=self.TA[:, 0:1]), reads=[self.bTA], writes=[bmneg])
        K.op(pool, lambda e: e.tensor_tensor(out=mneg[:, 2:3], in0=mneg[:, 2:3], in1=self.half[:, 0:1], op=ALU.pow),
             reads=[bmneg, self.bconst], writes=[bmneg])
        K.op(pool, lambda e: e.tensor_scalar(out=mneg[:, 0:1], in0=mneg[:, 2:3], scalar1=-0.125, scalar2=None, op0=ALU.mult),
             reads=[bmneg], writes=[bmneg])

    def rope(self, pe_t, bpe, h0, nh, ti, rt, brt):
        K = self.K
        dve = K.dve
        v = pe_t[:, h0 * 64:(h0 + nh) * 64].rearrange("p (h d) -> p h d", d=64)
        x1 = v[:, :, 0:8]
        x2 = v[:, :, 8:16]
        cb = self.cosT[:, ti:ti + 1, :].to_broadcast([128, nh, 8])
        sbb = self.sinT[:, ti:ti + 1, :].to_broadcast([128, nh, 8])
        t = [rt[:, k, 0:nh, :] for k in range(4)]
        rd = [bpe, self.brope]
        K.op(dve, lambda e: e.tensor_tensor(out=t[0], in0=x1, in1=cb, op=ALU.mult), reads=rd, writes=[brt])
        K.op(dve, lambda e: e.tensor_tensor(out=t[1], in0=x2, in1=sbb, op=ALU.mult), reads=rd, writes=[brt])
        K.op(dve, lambda e: e.tensor_tensor(out=t[2], in0=x2, in1=cb, op=ALU.mult), reads=rd, writes=[brt])
        K.op(dve, lambda e: e.tensor_tensor(out=t[3], in0=x1, in1=sbb, op=ALU.mult), reads=rd, writes=[brt])
        K.op(dve, lambda e: e.tensor_tensor(out=x1, in0=t[0], in1=t[1], op=ALU.subtract), reads=[brt], writes=[bpe])
        K.op(dve, lambda e: e.tensor_tensor(out=x2, in0=t[2], in1=t[3], op=ALU.add), reads=[brt], writes=[bpe])

    def dsa(self, x_src, vs, jl, bg):
        K, T, NT = self.K, self.T, self.NT
        pe, dve, act, pool, sp = K.pe, K.dve, K.act, K.pool, K.sp
        off_in, off_out = WOFF[("ain", jl)], WOFF[("aout", jl)]
        K.begin_scope()
        with ExitStack() as st:
            sb = lambda n, s, d: self.sb(st, n, s, d)
            w_in = sb("w_in", [128, 8, A_IN], BF16); bw = Buf("w"); sw = Slot(K, "w")
            w_out = sb("w_out", [128, 8, D], BF16)
            kT2 = sb("kT2", [128, T], BF16); bkT = Buf("kT2")
            kiT2 = sb("kiT2", [128, T], BF16); bkiT = Buf("kiT2")
            Vaug = sb("Vaug", [128, NT, 65], BF16); bV = Buf("Vaug")
            score = sb("score", [128, T], F32); bscore = Buf("score")
            NM2 = [sb(f"NM{i}", [128, T], BF16) for i in range(2)]; bNM2 = [Buf(f"NM{i}") for i in range(2)]
            rtmp = [sb(f"rtmp{i}", [128, 512], F32) for i in range(2)]; brtmp = [Buf(f"rtmp{i}") for i in range(2)]
            PT = [sb(f"PT{i}", [128, 16, 128], BF16) for i in range(2)]; bPT = [Buf(f"PT{i}") for i in range(2)]
            pev = sb("pev", [128, A_IN], F32); bpev = Buf("pev")
            rt = sb("rt", [128, 4, 17, 8], F32); brt = Buf("rt")
            qb = sb("qb", [128, 1152], BF16); bqb = Buf("qb")
            qib = sb("qib", [128, 640], BF16); bqib = Buf("qib")
            qT2 = [sb(f"qT{i}", [128, 8, 128], BF16) for i in range(2)]; bqT2 = [Buf(f"qT{i}") for i in range(2)]
            qiT = sb("qiT", [128, 4, 128], BF16); bqiT = Buf("qiT")
            wi = sb("wi", [128, 8], F32); bwi = Buf("wi")
            xa = [sb(f"xa{i}", [128, D], F32) for i in range(2)]; bxa = [Buf(f"xa{i}") for i in range(2)]
            sxa = [Slot(K, f"xa{i}") for i in range(2)]
            sxo = [Slot(K, f"xo{i}") for i in range(2)]
            tmp = sb("tmp", [128, D], F32); btmp = Buf("tmp")
            hb = sb("hb", [128, D], BF16); bhb = Buf("hb")
            hT = sb("hT", [128, 8, 128], BF16); bhT = Buf("hT")
            ob = sb("ob", [128, D], BF16); bob = Buf("ob")
            oT = sb("oT", [128, 8, 128], BF16); boT = Buf("oT")
            junk = sb("junk", [128, D], BF16); bjunk = Buf("junk")
            small = [sb(f"small{i}", [128, 8], F32) for i in range(2)]; bsmall = [Buf(f"small{i}") for i in range(2)]
            sq = sb("sq", [128, 24], F32); bsq = Buf("sq")
            ksqt = sb("ksqt", [128, 64], F32)
            run = sb("run", [128, 2], F32); brun = Buf("run")
            mneg2 = [sb(f"mneg{i}", [128, 4], F32) for i in range(2)]; bmneg2 = [Buf(f"mneg{i}") for i in range(2)]
            smallb = sb("smallb", [128, 8], F32); bsmallb = Buf("smallb")
            bis = sb("bis", [128, 8], F32); bbis = Buf("bis")
            dk = sb("dk", [128, NBIS + 2], F32)
            p2 = sb("p2", [128, NBIS + 2], F32)
            rl = sb("rl", [128, 16], F32); brl = Buf("rl")
            P = [self.ps(st, f"P{i}", [128, 512], F32) for i in range(4)]; bP = [Buf(f"P{i}") for i in range(4)]
            O = self.ps(st, "O", [128, 1536], F32); bO = Buf("O")

            K.dma(sp, sw, w_in[:].rearrange("p a b -> p (a b)"), self.wbf[:, off_in:off_in + W_AIN], reads=self.wbf_grp, writes=[bw])
            K.dma(sp, sw, w_out[:].rearrange("p a b -> p (a b)"), self.wbf[:, off_out:off_out + W_AOUT], reads=self.wbf_grp, writes=[bw])
            K.op(pool, lambda e: e.memset(Vaug[:, :, 64:65], 1.0), writes=[bV])
            K.op(pool, lambda e: e.memset(run[:], 0.0), writes=[brun])
            for k in range(NBIS + 2):
                K.op(pool, lambda e, k=k: e.memset(p2[:, k:k + 1], 2.0 ** (-(k + 1))), writes=[bbis])
            bgq = list(bg)
            pcnt = [0]
            if not hasattr(self, "fill_reg"):
                self.fill_reg = self.nc.gpsimd.to_reg(-1e30)

            def pbank():
                b = pcnt[0] % 4
                pcnt[0] += 1
                return b

            def stageA(i):
                a = i % 2
                n = (i + 1) * 128
                qT, bqT, mneg, bmneg = qT2[a], bqT2[a], mneg2[a], bmneg2[a]
                K.dma(pool, sxa[a], xa[a][:], x_src[i * 128:(i + 1) * 128, :], reads=[self.xbuf[i]], writes=[bxa[a]])
                self.norm_mod_T(vs, xa[a][:], bxa[a], hb, bhb, tmp, btmp, small[0], bsmall[0], junk, bjunk, hT[:], bhT)
                yield
                for b in range(4):
                    c0, c1 = b * 512, min((b + 1) * 512, A_IN)
                    for kc in range(8):
                        K.op(pe, lambda e, b=b, kc=kc, c0=c0, c1=c1: e.matmul(P[b][:, 0:c1 - c0], lhsT=hT[:, kc, :], rhs=w_in[:, kc, c0:c1],
                                                                             start=(kc == 0), stop=(kc == 7)),
                             reads=[bhT, bw], writes=[bP[b]], sig=(kc == 7))
                    K.op(act, lambda e, b=b, c0=c0, c1=c1: e.activation(out=pev[:, c0:c1], in_=P[b][:, 0:c1 - c0], func=AF.Copy),
                         reads=[bP[b]], writes=[bpev])
                yield
                self.rope(pev[:], bpev, 0, 17, i, rt, brt)
                self.rope(pev[:], bpev, 18, 9, i, rt, brt)
                yield
                K.op(act, lambda e: e.activation(out=qb[:, 0:1088], in_=pev[:, 0:1088], func=AF.Copy), reads=[bpev], writes=[bqb])
                K.op(act, lambda e: e.activation(out=qb[:, 1088:1152], in_=pev[:, 1024:1088], func=AF.Copy), reads=[bpev], writes=[bqb])
                K.op(act, lambda e, i=i: e.activation(out=Vaug[:, i, 0:64], in_=pev[:, 1088:1152], func=AF.Copy), reads=[bpev], writes=[bV])
                K.op(act, lambda e: e.activation(out=qib[:, 0:576], in_=pev[:, 1152:1728], func=AF.Copy), reads=[bpev], writes=[bqib])
                K.op(act, lambda e: e.activation(out=qib[:, 576:640], in_=pev[:, 1664:1728], func=AF.Copy), reads=[bpev], writes=[bqib])
                K.op(act, lambda e: e.activation(out=wi[:], in_=pev[:, 1728:1736], func=AF.Copy), reads=[bpev], writes=[bwi])
                K.op(act, lambda e: e.activation(out=tmp[:], in_=pev[:, 0:1024], func=AF.Square), reads=[bpev], writes=[btmp])
                K.op(act, lambda e: e.activation(out=ksqt[:], in_=pev[:, 1024:1088], func=AF.Square), reads=[bpev], writes=[btmp])
                K.op(dve, lambda e: e.tensor_reduce(out=sq[:, 0:16], in_=tmp[:].rearrange("p (h d) -> p h d", d=64),
                                                    axis=AX.X, op=ALU.add), reads=[btmp], writes=[bsq])
                K.op(dve, lambda e: e.tensor_reduce(out=sq[:, 16:17], in_=ksqt[:], axis=AX.X, op=ALU.add), reads=[btmp], writes=[bsq])
                K.op(dve, lambda e: e.tensor_reduce(out=sq[:, 20:21], in_=sq[:, 0:16], axis=AX.X, op=ALU.max), reads=[bsq], writes=[bsq])
                yield
                for s_ in range(8):
                    K.op(pe, lambda e, s_=s_: e.transpose(out=self.TAb[:, s_ * 128:(s_ + 1) * 128], in_=qb[:, s_ * 128:(s_ + 1) * 128],
                                                           identity=self.ident[:]),
                         reads=[bqb, self.bconst], writes=[self.bTA], sig=(s_ == 7))
                K.op(act, lambda e: e.activation(out=qT[:], in_=self.TAb[:].rearrange("p (a b) -> p a b", a=8), func=AF.Copy),
                     reads=[self.bTA], writes=[bqT])
                K.op(pe, lambda e: e.transpose(out=self.TAb[:, 0:128], in_=qb[:, 1024:1152], identity=self.ident[:]),
                     reads=[bqb, self.bconst], writes=[self.bTA], sig=False)
                for s_ in range(5):
                    K.op(pe, lambda e, s_=s_: e.transpose(out=self.TAb[:, (s_ + 1) * 128:(s_ + 2) * 128], in_=qib[:, s_ * 128:(s_ + 1) * 128],
                                                           identity=self.ident[:]),
                         reads=[bqib, self.bconst], writes=[self.bTA], sig=(s_ == 4))
                K.op(dve, lambda e, i=i: e.tensor_copy(out=kT2[:, i * 128:(i + 1) * 128], in_=self.TAb[:, 0:128]),
                     reads=[self.bTA], writes=[bkT])
                K.op(dve, lambda e: e.tensor_copy(out=qiT[:], in_=self.TAb[:, 128:640].rearrange("p (a b) -> p a b", a=4)),
                     reads=[self.bTA], writes=[bqiT])
                K.op(dve, lambda e, i=i: e.tensor_copy(out=kiT2[:, i * 128:(i + 1) * 128], in_=self.TAb[:, 640:768]),
                     reads=[self.bTA], writes=[bkiT])
                yield
                self.neg_bound(sq[:, 20:21], bsq, sq[:, 16:17], bsq, run, brun, mneg, bmneg, smallb, bsmallb)
                yield
                nblk = (n + 511) // 512
                rc = 0
                for sbk in range(nblk):
                    w = min(512, n - sbk * 512)
                    cs = slice(sbk * 512, sbk * 512 + w)
                    for h in range(8):
                        s_, hf = h % 4, h // 4
                        b = pbank()
                        K.op(pe, lambda e, b=b, s_=s_, hf=hf, cs=cs, w=w: e.matmul(P[b][:, 0:w], lhsT=qiT[hf * 64:(hf + 1) * 64, s_, :],
                                                                                    rhs=kiT2[hf * 64:(hf + 1) * 64, cs], start=True, stop=True),
                             reads=[bqiT, bkiT], writes=[bP[b]])
                        r = rc % 2
                        rc += 1
                        K.op(act, lambda e, b=b, r=r, w=w: e.activation(out=rtmp[r][:, 0:w], in_=P[b][:, 0:w], func=AF.Relu),
                             reads=[bP[b]], writes=[brtmp[r]])
                        if h == 0:
                            K.op(dve, lambda e, r=r, w=w, cs=cs, h=h: e.tensor_scalar(out=score[:, cs], in0=rtmp[r][:, 0:w], scalar1=wi[:, h:h + 1],
                                                                                       scalar2=None, op0=ALU.mult),
                                 reads=[brtmp[r], bwi], writes=[bscore])
                        else:
                            K.op(dve, lambda e, r=r, w=w, cs=cs, h=h: e.scalar_tensor_tensor(out=score[:, cs], in0=rtmp[r][:, 0:w], scalar=wi[:, h:h + 1],
                                                                                              in1=score[:, cs], op0=ALU.mult, op1=ALU.add),
                                 reads=[brtmp[r], bwi, bscore], writes=[bscore])
                    yield

            def stageB(i):
                a = i % 2
                n = (i + 1) * 128
                NM, bNM = NM2[a], bNM2[a]
                lo, mid, cnt, u, t2, d0 = (bis[:, k:k + 1] for k in range(6))
                if n > 256:
                    K.op(dve, lambda e: e.tensor_reduce(out=lo, in_=score[:, 0:n], axis=AX.X, op=ALU.min), reads=[bscore], writes=[bbis])
                    K.op(dve, lambda e: e.tensor_reduce(out=d0, in_=score[:, 0:n], axis=AX.X, op=ALU.max), reads=[bscore], writes=[bbis])
                    K.op(dve, lambda e: e.tensor_tensor(out=d0, in0=d0, in1=lo, op=ALU.subtract), reads=[bbis], writes=[bbis])
                    K.op(dve, lambda e: e.tensor_scalar(out=dk[:], in0=p2[:], scalar1=d0, scalar2=None, op0=ALU.mult), reads=[bbis], writes=[bbis])
                    K.op(dve, lambda e: e.tensor_tensor(out=mid, in0=lo, in1=dk[:, 0:1], op=ALU.add), reads=[bbis], writes=[bbis])
                K.op(pool, lambda e, i=i: e.affine_select(out=score[:, i * 128:(i + 1) * 128], in_=score[:, i * 128:(i + 1) * 128],
                                                          pattern=[[-1, 128]], compare_op=ALU.is_ge, fill=self.fill_reg, base=0, channel_multiplier=1),
                     reads=[bscore, bbis], writes=[bscore])
                if n > 256:
                    for k in range(NBIS):
                        K.op(dve, lambda e: e.tensor_scalar(out=NM[:, 0:n], in0=score[:, 0:n], scalar1=mid, scalar2=0.0, op0=ALU.is_ge,
                                                            op1=ALU.add, accum_out=cnt),
                             reads=[bscore, bbis], writes=[bNM, bbis])
                        K.op(dve, lambda e, k=k: e.tensor_scalar(out=u, in0=cnt, scalar1=255.5, scalar2=dk[:, k:k + 1], op0=ALU.is_ge, op1=ALU.mult),
                             reads=[bbis], writes=[bbis])
                        K.op(dve, lambda e, k=k: e.scalar_tensor_tensor(out=mid, in0=u, scalar=dk[:, k + 1:k + 2], in1=mid, op0=ALU.subtract, op1=ALU.add),
                             reads=[bbis], writes=[bbis])
                        yield
                    K.op(dve, lambda e: e.tensor_tensor(out=lo, in0=mid, in1=dk[:, NBIS:NBIS + 1], op=ALU.subtract), reads=[bbis], writes=[bbis])
                    K.op(dve, lambda e: e.tensor_scalar(out=NM[:, 0:n], in0=score[:, 0:n], scalar1=lo, scalar2=NEG, op0=ALU.is_lt, op1=ALU.mult),
                         reads=[bscore, bbis], writes=[bNM])
                else:
                    K.op(dve, lambda e: e.tensor_scalar(out=NM[:, 0:n], in0=score[:, 0:n], scalar1=-1e29, scalar2=NEG, op0=ALU.is_lt, op1=ALU.mult),
                         reads=[bscore], writes=[bNM])
                yield

            def stageC(i):
                a = i % 2
                n = (i + 1) * 128
                NM, bNM = NM2[a], bNM2[a]
                qT, bqT, mneg, bmneg = qT2[a], bqT2[a], mneg2[a], bmneg2[a]
                K.op(dve, lambda e: e.memset(O[:], 0.0), writes=[bO])
                for c in range(i + 1):
                    pt = c % 2
                    for hg in range(4):
                        hf, s0 = hg // 2, (hg % 2) * 4
                        b = pbank()
                        K.op(pe, lambda e, b=b, c=c: e.matmul(P[b][:], lhsT=NM[:, c * 128:(c + 1) * 128], rhs=self.I4[:].rearrange("p a b -> p (a b)"),
                                                              start=True, stop=False),
                             reads=[bNM, self.bconst], writes=[bP[b]], sig=False)
                        K.op(pe, lambda e, b=b, c=c, hf=hf, s0=s0: e.matmul(P[b][:], lhsT=kT2[hf * 64:(hf + 1) * 64, c * 128:(c + 1) * 128],
                                                                             rhs=qT[hf * 64:(hf + 1) * 64, s0:s0 + 4, :], start=False, stop=True),
                             reads=[bkT, bqT], writes=[bP[b]])
                        K.op(act, lambda e, b=b, pt=pt, hg=hg: e.activation(out=PT[pt][:, hg * 4:(hg + 1) * 4, :],
                                                                            in_=P[b][:].rearrange("p (a b) -> p a b", a=4), func=AF.Exp,
                                                                            bias=mneg[:, 0:1], scale=0.125),
                             reads=[bP[b], bmneg], writes=[bPT[pt]])
                    for hd in range(16):
                        col = (hd // 7) * 512 + (hd % 7) * 65
                        K.op(pe, lambda e, pt=pt, hd=hd, col=col, c=c: e.matmul(O[:, col:col + 65], lhsT=PT[pt][:, hd, :], rhs=Vaug[:, c, :],
                                                                                 start=False, stop=False, skip_group_check=True),
                             reads=[bPT[pt], bV, bO], writes=[bO], sig=(hd == 15))
                    yield
                if self.debug and i == 0:
                    if self.dbg is None:
                        self.dbg = self.nc.dram_tensor("dbg", [128, 4096], F32, kind="ExternalOutput").ap()
                    dsb = sb("dsb", [128, 4096], F32); bd = Buf("dsb"); sd = Slot(K, "dsb")
                    K.op(dve, lambda e: e.memset(dsb[:], 0.0), writes=[bd])
                    K.op(dve, lambda e: e.tensor_copy(out=dsb[:, 0:1536], in_=O[:]), reads=[bO], writes=[bd])
                    K.op(dve, lambda e: e.tensor_copy(out=dsb[:, 1536:1540], in_=mneg[:]), reads=[bmneg], writes=[bd])
                    K.op(dve, lambda e: e.tensor_copy(out=dsb[:, 1600:1600 + 130], in_=Vaug[:, 0:2, :].rearrange("p a b -> p (a b)")), reads=[bV], writes=[bd])
                    K.op(dve, lambda e: e.tensor_copy(out=dsb[:, 2048:4096], in_=PT[0][:].rearrange("p a b -> p (a b)")), reads=[bPT[0]], writes=[bd])
                    K.dma(sp, sd, self.dbg, dsb[:], reads=[bd])
                for bk in range(3):
                    nh = 7 if bk < 2 else 2
                    Ov = O[:, bk * 512: bk * 512 + nh * 65].rearrange("p (h e) -> p h e", e=65)
                    K.op(dve, lambda e, bk=bk, nh=nh, Ov=Ov: e.reciprocal(out=rl[:, bk * 7: bk * 7 + nh], in_=Ov[:, :, 64]),
                         reads=[bO], writes=[brl])
                    K.op(dve, lambda e, bk=bk, nh=nh, Ov=Ov: e.tensor_tensor(
                        out=ob[:, bk * 448: bk * 448 + nh * 64].rearrange("p (h d) -> p h d", d=64), in0=Ov[:, :, 0:64],
                        in1=rl[:, bk * 7: bk * 7 + nh].rearrange("p (h o) -> p h o", o=1).to_broadcast([128, nh, 64]), op=ALU.mult),
                         reads=[bO, brl], writes=[bob])
                yield
                for kc in range(8):
                    K.op(pe, lambda e, kc=kc: e.transpose(out=self.TAb[:, kc * 128:(kc + 1) * 128], in_=ob[:, kc * 128:(kc + 1) * 128],
                                                          identity=self.ident[:]),
                         reads=[bob, self.bconst], writes=[self.bTA], sig=(kc == 7))
                K.op(act, lambda e: e.activation(out=oT[:], in_=self.TAb[:].rearrange("p (a b) -> p a b", a=8), func=AF.Copy),
                     reads=[self.bTA], writes=[boT])
                yb = [pbank(), pbank()]
                for hf in range(2):
                    for kc in range(8):
                        K.op(pe, lambda e, hf=hf, kc=kc, yb=yb: e.matmul(P[yb[hf]][:], lhsT=oT[:, kc, :], rhs=w_out[:, kc, hf * 512:(hf + 1) * 512],
                                                                         start=(kc == 0), stop=(kc == 7)),
                             reads=[boT, bw], writes=[bP[yb[hf]]], sig=(kc == 7))
                self.post_resid(vs, [(P[yb[0]][:], 0, 512), (P[yb[1]][:], 512, 512)], [bP[yb[0]], bP[yb[1]]],
                                xa[a][:], bxa[a], xa[a], bxa[a], small[1], bsmall[1], junk, bjunk, tmp, btmp)
                K.dma(pool, sxo[a], self.out[i * 128:(i + 1) * 128, :], xa[a][:], reads=[bxa[a]], writes=[self.xbuf[i]])
                yield

            self.interleave(self.chain(stageA(0), stageB(0)))
            for i in range(NT):
                nxt = self.chain(stageA(i + 1), stageB(i + 1)) if i + 1 < NT else None
                if PIPE == 0:
                    self.interleave(stageC(i)); self.interleave(nxt)
                elif PIPE == 1:
                    self.interleave(nxt); self.interleave(stageC(i))
                else:
                    self.interleave(stageC(i), nxt)
                if bgq and i % 4 == 3:
                    bgq.pop(0)()
            while bgq:
                bgq.pop(0)()
            K.barrier()
        K.end_scope()

    def diff(self, x_src, vs, jl, bg):
        K, T, NT = self.K, self.T, self.NT
        pe, dve, act, pool, sp = K.pe, K.dve, K.act, K.pool, K.sp
        bgq = list(bg)
        for g in range(2):
            off_in, off_out = WOFF[("bin", jl, g)], WOFF[("bout", jl, g)]
            K.begin_scope()
            with ExitStack() as st:
                sb = lambda n, s, d: self.sb(st, n, s, d)
                w_in = sb("w_in", [128, 8, 1536], BF16); bw = Buf("w"); sw = Slot(K, "w")
                w_out = sb("w_out", [128, 4, D], BF16)
                kT = sb("kT", [128, 4, T], BF16); bkT = Buf("kT")
                Vaug = sb("Vaug", [128, NT, 4, 129], BF16); bV = Buf("Vaug")
                PT = [sb(f"PT{i}", [128, 8, 128], BF16) for i in range(2)]; bPT = [Buf(f"PT{i}") for i in range(2)]
                pev = sb("pev", [128, 1536], F32); bpev = Buf("pev")
                rt = sb("rt", [128, 4, 16, 8], F32); brt = Buf("rt")
                qb = sb("qb", [128, 1024], BF16); bqb = Buf("qb")
                qT2 = [sb(f"qT{i}", [128, 4, 128], BF16) for i in range(2)]; bqT2 = [Buf(f"qT{i}") for i in range(2)]
                smallb = sb("smallb", [128, 8], F32); bsmallb = Buf("smallb")
                sq = sb("sq", [128, 24], F32); bsq = Buf("sq")
                xa = [sb(f"xa{i}", [128, D], F32) for i in range(2)]; bxa = [Buf(f"xa{i}") for i in range(2)]
                sxa = [Slot(K, f"xa{i}") for i in range(2)]
                sxo = [Slot(K, f"xo{i}") for i in range(2)]
                ysb = [sb(f"ysb{i}", [128, D], F32) for i in range(2)]; bysb = [Buf(f"ysb{i}") for i in range(2)]
                sys_ = [Slot(K, f"ysb{i}") for i in range(2)]
                tmp = sb("tmp", [128, D], F32); btmp = Buf("tmp")
                hb = sb("hb", [128, D], BF16); bhb = Buf("hb")
                hT = sb("hT", [128, 8, 128], BF16); bhT = Buf("hT")
                on = sb("on", [128, 8, 128], F32); bon = Buf("on")
                od = sb("od", [128, 4, 128], F32); bod = Buf("od")
                ob = sb("ob", [128, 512], BF16); bob = Buf("ob")
                oT = sb("oT", [128, 4, 128], BF16); boT = Buf("oT")
                junk = sb("junk", [128, D], BF16); bjunk = Buf("junk")
                small = [sb(f"small{i}", [128, 8], F32) for i in range(2)]; bsmall = [Buf(f"small{i}") for i in range(2)]
                run = sb("run", [128, 2], F32); brun = Buf("run")
                mneg2 = [sb(f"mneg{i}", [128, 4], F32) for i in range(2)]; bmneg2 = [Buf(f"mneg{i}") for i in range(2)]
                rl = sb("rl", [128, 8], F32); brl = Buf("rl")
                ssub = sb("ssub", [128, 8], F32); bssub = Buf("ssub")
                P = [self.ps(st, f"P{i}", [128, 512], F32) for i in range(4)]; bP = [Buf(f"P{i}") for i in range(4)]
                O = self.ps(st, "O", [128, 1536], F32); bO = Buf("O")

                K.dma(sp, sw, w_in[:].rearrange("p a b -> p (a b)"), self.wbf[:, off_in:off_in + W_BIN], reads=self.wbf_grp, writes=[bw])
                K.dma(sp, sw, w_out[:].rearrange("p a b -> p (a b)"), self.wbf[:, off_out:off_out + W_BOUT], reads=self.wbf_grp, writes=[bw])
                K.op(pool, lambda e: e.memset(Vaug[:, :, :, 128:129], 1.0), writes=[bV])
                K.op(pool, lambda e: e.memset(run[:], 0.0), writes=[brun])
                pcnt = [0]

                def pbank():
                    b = pcnt[0] % 4
                    pcnt[0] += 1
                    return b

                def stageA(i):
                    a = i % 2
                    qT, bqT, mneg, bmneg = qT2[a], bqT2[a], mneg2[a], bmneg2[a]
                    K.dma(pool, sxa[a], xa[a][:], x_src[i * 128:(i + 1) * 128, :], reads=[self.xbuf[i]], writes=[bxa[a]])
                    if g == 1:
                        K.dma(pool, sys_[a], ysb[a][:], self.ypart[i * 128:(i + 1) * 128, :], reads=[self.ypbuf[i]], writes=[bysb[a]])
                    self.norm_mod_T(vs, xa[a][:], bxa[a], hb, bhb, tmp, btmp, small[0], bsmall[0], junk, bjunk, hT[:], bhT)
                    if YMASK & 1:
                        yield
                    for b in range(3):
                        for kc in range(8):
                            K.op(pe, lambda e, b=b, kc=kc: e.matmul(P[b][:], lhsT=hT[:, kc, :], rhs=w_in[:, kc, b * 512:(b + 1) * 512],
                                                                    start=(kc == 0), stop=(kc == 7)),
                                 reads=[bhT, bw], writes=[bP[b]], sig=(kc == 7))
                        K.op(act, lambda e, b=b: e.activation(out=pev[:, b * 512:(b + 1) * 512], in_=P[b][:], func=AF.Copy),
                             reads=[bP[b]], writes=[bpev])
                    if YMASK & 2:
                        yield
                    self.rope(pev[:], bpev, 0, 16, i, rt, brt)
                    if YMASK & 4:
                        yield
                    K.op(act, lambda e: e.activation(out=qb[:], in_=pev[:, 0:1024], func=AF.Copy), reads=[bpev], writes=[bqb])
                    K.op(act, lambda e, i=i: e.activation(out=Vaug[:, i, :, 0:128], in_=pev[:, 1024:1536].rearrange("p (h d) -> p h d", d=128),
                                                          func=AF.Copy), reads=[bpev], writes=[bV])
                    K.op(act, lambda e: e.activation(out=tmp[:], in_=pev[:, 0:1024], func=AF.Square), reads=[bpev], writes=[btmp])
                    K.op(dve, lambda e: e.tensor_reduce(out=sq[:, 0:16], in_=tmp[:].rearrange("p (h d) -> p h d", d=64), axis=AX.X, op=ALU.add),
                         reads=[btmp], writes=[bsq])
                    K.op(dve, lambda e: e.tensor_reduce(out=sq[:, 20:22], in_=sq[:, 0:16].rearrange("p (a b) -> p a b", a=2), axis=AX.X, op=ALU.max),
                         reads=[bsq], writes=[bsq])
                    if YMASK & 8:
                        yield
                    for s_ in range(8):
                        K.op(pe, lambda e, s_=s_: e.transpose(out=self.TAb[:, s_ * 128:(s_ + 1) * 128], in_=qb[:, s_ * 128:(s_ + 1) * 128],
                                                               identity=self.ident[:]),
                             reads=[bqb, self.bconst], writes=[self.bTA], sig=(s_ == 7))
                    K.op(act, lambda e: e.activation(out=qT[:], in_=self.TAb[:, 0:512].rearrange("p (a b) -> p a b", a=4), func=AF.Copy),
                         reads=[self.bTA], writes=[bqT])
                    K.op(dve, lambda e, i=i: e.tensor_copy(out=kT[:, :, i * 128:(i + 1) * 128],
                                                           in_=self.TAb[:, 512:1024].rearrange("p (a b) -> p a b", a=4)),
                         reads=[self.bTA], writes=[bkT])
                    if YMASK & 16:
                        yield
                    self.neg_bound(sq[:, 20:21], bsq, sq[:, 21:22], bsq, run, brun, mneg, bmneg, smallb, bsmallb)
                    if YMASK & 32:
                        yield

                def stageC(i):
                    a = i % 2
                    qT, bqT, mneg, bmneg = qT2[a], bqT2[a], mneg2[a], bmneg2[a]
                    K.op(dve, lambda e: e.memset(O[:], 0.0), writes=[bO])
                    for c in range(i + 1):
                        pt = c % 2
                        bb = [pbank(), pbank()]
                        if c == i:
                            for bnk in range(2):
                                K.op(pe, lambda e, b=bb[bnk]: e.matmul(P[b][:], lhsT=self.ident[:], rhs=self.CN8[:, 0:4, :].rearrange("p a b -> p (a b)"),
                                                                       start=True, stop=False),
                                     reads=[self.bconst], writes=[bP[bb[bnk]]], sig=False)
                        for ul in range(4):
                            for bnk in range(2):
                                b = bb[bnk]
                                K.op(pe, lambda e, b=b, ul=ul, bnk=bnk, c=c, i=i: e.matmul(
                                    P[b][:, ul * 128:(ul + 1) * 128], lhsT=kT[bnk * 64:(bnk + 1) * 64, ul, c * 128:(c + 1) * 128],
                                    rhs=qT[bnk * 64:(bnk + 1) * 64, ul, :], start=(c != i), stop=(c != i or ul == 3),
                                    skip_group_check=True),
                                     reads=[bkT, bqT], writes=[bP[b]], sig=(ul == 3))
                        for bnk in range(2):
                            b = bb[bnk]
                            K.op(act, lambda e, b=b, pt=pt, bnk=bnk: e.activation(out=PT[pt][:, bnk * 4:(bnk + 1) * 4, :],
                                                                                   in_=P[b][:].rearrange("p (a b) -> p a b", a=4), func=AF.Exp,
                                                                                   bias=mneg[:, 0:1], scale=0.125),
                                 reads=[bP[b], bmneg], writes=[bPT[pt]])
                        for u in range(8):
                            col = (u // 3) * 512 + (u % 3) * 129
                            jx = (u % 2) * 4 + u // 2
                            K.op(pe, lambda e, pt=pt, u=u, jx=jx, col=col, c=c: e.matmul(O[:, col:col + 129], lhsT=PT[pt][:, jx, :], rhs=Vaug[:, c, u % 4, :],
                                                                                          start=False, stop=False, skip_group_check=True),
                                 reads=[bPT[pt], bV, bO], writes=[bO], sig=(u == 7))
                        yield
                    for bk in range(3):
                        nh = 3 if bk < 2 else 2
                        Ov = O[:, bk * 512: bk * 512 + nh * 129].rearrange("p (h e) -> p h e", e=129)
                        K.op(dve, lambda e, bk=bk, nh=nh, Ov=Ov: e.reciprocal(out=rl[:, bk * 3: bk * 3 + nh], in_=Ov[:, :, 128]),
                             reads=[bO], writes=[brl])
                        K.op(dve, lambda e, bk=bk, nh=nh, Ov=Ov: e.tensor_tensor(
                            out=on[:, bk * 3: bk * 3 + nh, :], in0=Ov[:, :, 0:128],
                            in1=rl[:, bk * 3: bk * 3 + nh].rearrange("p (h o) -> p h o", o=1).to_broadcast([128, nh, 128]), op=ALU.mult),
                             reads=[bO, brl], writes=[bon])
                    K.op(dve, lambda e: e.scalar_tensor_tensor(out=od[:].rearrange("p a b -> p (a b)"), in0=on[:, 4:8, :].rearrange("p a b -> p (a b)"),
                                                               scalar=self.neglam[:, jl:jl + 1], in1=on[:, 0:4, :].rearrange("p a b -> p (a b)"),
                                                               op0=ALU.mult, op1=ALU.add),
                         reads=[bon, self.bdiffc], writes=[bod])
                    K.op(act, lambda e: e.activation(out=on[:, 0:4, :], in_=od[:], func=AF.Square), reads=[bod], writes=[bon])
                    K.op(dve, lambda e: e.tensor_reduce(out=ssub[:, 0:4], in_=on[:, 0:4, :], axis=AX.X, op=ALU.add), reads=[bon], writes=[bssub])
                    K.op(pool, lambda e: e.tensor_scalar(out=ssub[:, 4:8], in0=ssub[:, 0:4], scalar1=1.0 / 128.0, scalar2=EPS, op0=ALU.mult, op1=ALU.add),
                         reads=[bssub], writes=[bssub])
                    K.op(pool, lambda e: e.tensor_tensor(out=ssub[:, 4:8], in0=ssub[:, 4:8], in1=self.neghalf[:, 0:4], op=ALU.pow),
                         reads=[bssub, self.bconst], writes=[bssub])
                    K.op(dve, lambda e: e.tensor_tensor(out=od[:], in0=od[:],
                                                        in1=ssub[:, 4:8].rearrange("p (h o) -> p h o", o=1).to_broadcast([128, 4, 128]), op=ALU.mult),
                         reads=[bod, bssub], writes=[bod])
                    K.op(dve, lambda e: e.tensor_tensor(out=ob[:].rearrange("p (h d) -> p h d", d=128), in0=od[:],
                                                        in1=self.sublnb[:, jl:jl + 1, :].to_broadcast([128, 4, 128]), op=ALU.mult),
                         reads=[bod, self.bdiffc], writes=[bob])
                    yield
                    for kc in range(4):
                        K.op(pe, lambda e, kc=kc: e.transpose(out=self.TAb[:, kc * 128:(kc + 1) * 128], in_=ob[:, kc * 128:(kc + 1) * 128],
                                                              identity=self.ident[:]),
                             reads=[bob, self.bconst], writes=[self.bTA], sig=(kc == 3))
                    K.op(act, lambda e: e.activation(out=oT[:], in_=self.TAb[:, 0:512].rearrange("p (a b) -> p a b", a=4), func=AF.Copy),
                         reads=[self.bTA], writes=[boT])
                    yb = [pbank(), pbank()]
                    for hf in range(2):
                        for kc in range(4):
                            K.op(pe, lambda e, hf=hf, kc=kc, yb=yb: e.matmul(P[yb[hf]][:], lhsT=oT[:, kc, :], rhs=w_out[:, kc, hf * 512:(hf + 1) * 512],
                                                                             start=(kc == 0), stop=(kc == 3)),
                                 reads=[boT, bw], writes=[bP[yb[hf]]], sig=(kc == 3))
                    yield
                    if g == 0:
                        for hf in range(2):
                            K.op(act, lambda e, hf=hf, yb=yb, a=a: e.activation(out=ysb[a][:, hf * 512:(hf + 1) * 512], in_=P[yb[hf]][:], func=AF.Copy),
                                 reads=[bP[yb[hf]]], writes=[bysb[a]])
                        K.dma(pool, sys_[a], self.ypart[i * 128:(i + 1) * 128, :], ysb[a][:], reads=[bysb[a]], writes=[self.ypbuf[i]])
                    else:
                        for hf in range(2):
                            K.op(dve, lambda e, hf=hf, yb=yb, a=a: e.tensor_tensor(out=ysb[a][:, hf * 512:(hf + 1) * 512], in0=P[yb[hf]][:],
                                                                                    in1=ysb[a][:, hf * 512:(hf + 1) * 512], op=ALU.add),
                                 reads=[bP[yb[hf]], bysb[a]], writes=[bysb[a]])
                        self.post_resid(vs, [(ysb[a][:], 0, D)], [bysb[a]], xa[a][:], bxa[a], xa[a], bxa[a], small[1], bsmall[1],
                                        junk, bjunk, tmp, btmp)
                        K.dma(pool, sxo[a], self.out[i * 128:(i + 1) * 128, :], xa[a][:], reads=[bxa[a]], writes=[self.xbuf[i]])
                    yield

                self.interleave(stageA(0))
                for i in range(NT):
                    nxt = stageA(i + 1) if i + 1 < NT else None
                    self.interleave(stageC(i)); self.interleave(nxt)
                    if bgq and i % 8 == 7:
                        bgq.pop(0)()
                if g == 1:
                    while bgq:
                        bgq.pop(0)()
                K.barrier()
            K.end_scope()


def prep_weights(inp):
    wall = np.zeros((128, NTOT), np.float32)
    for i in range(DEPTH):
        for j in range(2):
            wg = np.zeros((D, FFP), np.float32); wg[:, :DFF] = inp["ffn_w_gate"][i, j]
            wu = np.zeros((D, FFP), np.float32); wu[:, :DFF] = inp["ffn_w_up"][i, j]
            gu = np.stack([wg, wu], 0).reshape(2, 8, 128, 11, 256)
            gu = gu.transpose(2, 3, 0, 1, 4).reshape(128, W_GU)
            o = WOFF[("gu", i, j)]
            wall[:, o:o + W_GU] = gu
            wdn = np.zeros((FFP, D), np.float32); wdn[:DFF] = inp["ffn_w_down"][i, j]
            o = WOFF[("d", i, j)]
            wall[:, o:o + W_D] = wdn.reshape(NF, 128, D).transpose(1, 0, 2).reshape(128, W_D)
    qperm = []
    for s in range(8):
        qperm += list(range(s * 64, s * 64 + 64)) + list(range((s + 8) * 64, (s + 8) * 64 + 64))
    qiperm = []
    for s in range(4):
        qiperm += list(range(1152 + s * 64, 1152 + s * 64 + 64)) + list(range(1152 + (s + 4) * 64, 1152 + (s + 4) * 64 + 64))
    aperm = np.array(qperm + list(range(1024, 1152)) + qiperm + list(range(1664, 1736)))
    for j in range(2):
        w = inp["dsa_w_in"][j][:, aperm]
        o = WOFF[("ain", j)]
        wall[:, o:o + W_AIN] = w.reshape(8, 128, A_IN).transpose(1, 0, 2).reshape(128, W_AIN)
        o = WOFF[("aout", j)]
        wall[:, o:o + W_AOUT] = inp["dsa_w_out"][j].reshape(8, 128, D).transpose(1, 0, 2).reshape(128, W_AOUT)
    for j in range(2):
        for g in range(2):
            cols = []
            for base in (0, 512, 1024, 1536):
                cols += list(range(base + g * 256, base + g * 256 + 256))
            cols += list(range(2048 + g * 512, 2048 + g * 512 + 512))
            w = inp["diff_w_in"][j][:, np.array(cols)]
            o = WOFF[("bin", j, g)]
            wall[:, o:o + W_BIN] = w.reshape(8, 128, 1536).transpose(1, 0, 2).reshape(128, W_BIN)
            wo = inp["diff_w_out"][j][g * 512:(g + 1) * 512]
            o = WOFF[("bout", j, g)]
            wall[:, o:o + W_BOUT] = wo.reshape(4, 128, D).transpose(1, 0, 2).reshape(128, W_BOUT)
    return wall


def prep_ada(inp):
    aw = np.asarray(inp["ada_w"], np.float32)
    a = aw.reshape(4, 4, 2, 128, 3, 6, 512)
    a = a.transpose(0, 4, 5, 1, 3, 2, 6)
    ada = np.ascontiguousarray(a).reshape(12 * 24, 128, 1024)
    adab = np.ascontiguousarray(np.asarray(inp["ada_b"], np.float32).reshape(1, 12 * 3072))
    return ada, adab


def make_in_maps(inp, T, ncores):
    wall = prep_weights(inp)
    ada, adab = prep_ada(inp)
    pre_n = np.ascontiguousarray(np.asarray(inp["pre_norm"], np.float32).reshape(12, D))
    post_n = np.ascontiguousarray(np.asarray(inp["post_norm"], np.float32).reshape(12, D))
    subln = np.ascontiguousarray(np.asarray(inp["diff_subln"], np.float32))
    lam = np.ascontiguousarray(np.asarray(inp["diff_lambda"], np.float32).reshape(2, 256))
    maps = []
    for b in range(ncores):
        maps.append({
            "x": np.ascontiguousarray(np.asarray(inp["x"][b, :T], np.float32)),
            "c": np.ascontiguousarray(np.asarray(inp["c"][b], np.float32).reshape(8, 128).T),
            "pos": np.ascontiguousarray(np.asarray(inp["positions"][b, :T], np.int32).reshape(T // 128, 128).T),
            "wall": wall, "ada": ada, "adab": adab, "pre_n": pre_n, "post_n": post_n, "subln": subln, "lam": lam,
        })
    return maps


ALL_SUBL = [(li, k) for li in range(DEPTH) for k in ("f0", "mix", "f1")]


def kernel(**inputs):
    T = 4096
    prog = Prog(T, ALL_SUBL)
    nc = prog.build()
    maps = make_in_maps(inputs, T, 8)
    res = run_bass_kernel_spmd(nc, maps, core_ids=list(range(8)))
    return np.stack([np.asarray(r["out"], np.float32) for r in res.results], 0)
```

```python
import math
from contextlib import ExitStack

import numpy as np
import concourse.bass as bass
import concourse.mybir as mybir
from concourse.bass_utils import run_bass_kernel_spmd

F32 = mybir.dt.float32
BF16 = mybir.dt.bfloat16
I32 = mybir.dt.int32
AF = mybir.ActivationFunctionType
ALU = mybir.AluOpType
AX = mybir.AxisListType

D = 1024
DFF = 2752
FFP = 2816
NF = 22
DEPTH = 4
EPS = 1e-6
A_IN = 1736
NEG = -30000.0
SEM_LIMIT = 12000
NBIS = 24
STOP = 99
PIPE = 2
YMASK = 0

W_GU = 11 * 2 * 8 * 256
W_D = NF * 1024
W_AIN = 8 * A_IN
W_AOUT = 8 * 1024
W_BIN = 8 * 1536
W_BOUT = 4 * 1024


def weight_offsets():
    off = {}
    o = 0
    for i in range(DEPTH):
        for j in range(2):
            off[("gu", i, j)] = o; o += W_GU
            off[("d", i, j)] = o; o += W_D
    for j in range(2):
        off[("ain", j)] = o; o += W_AIN
        off[("aout", j)] = o; o += W_AOUT
    for j in range(2):
        for g in range(2):
            off[("bin", j, g)] = o; o += W_BIN
            off[("bout", j, g)] = o; o += W_BOUT
    return off, o


WOFF, NTOT = weight_offsets()
CASTW = 4096
assert NTOT % CASTW == 0 or True


class Ev:
    __slots__ = ("sem", "val", "eng")

    def __init__(self, eng=None):
        self.sem = None
        self.val = 0
        self.eng = eng


class Buf:
    __slots__ = ("name", "w", "r")

    def __init__(self, name):
        self.name = name
        self.w = None
        self.r = {}


class Eng:
    def __init__(self, K, name, h):
        self.K = K
        self.name = name
        self.h = h
        self.sem = None
        self.cnt = 0
        self.seen = {}
        self.pending = []
        self.last = None
        self.n = 0


class _Slot:
    def __init__(self, K, name):
        self.sem = K.new_sem("d_" + name)
        self.cnt = 0
        self.name = name
        self.last = None
        K.slots.append(self)


def Slot(K, name):
    if K.free_slots:
        s = K.free_slots.pop()
    else:
        s = _Slot(K, name)
        s.name = f"s{len(K.slots)}"
    K.scope_slots.append(s)
    return s


class Kern:
    def __init__(self, nc):
        self.nc = nc
        self.es = ExitStack()
        self.nsem = 0
        self.slots = []
        self.pe = Eng(self, "pe", nc.tensor)
        self.act = Eng(self, "act", nc.scalar)
        self.dve = Eng(self, "dve", nc.vector)
        self.pool = Eng(self, "pool", nc.gpsimd)
        self.sp = Eng(self, "sp", nc.sync)
        self.engs = [self.pe, self.act, self.dve, self.pool, self.sp]
        self.ninstr = 0
        self.free_slots = []
        self.scope_slots = []

    def begin_scope(self):
        self._saved = self.scope_slots
        self.scope_slots = []

    def end_scope(self):
        self.free_slots.extend(self.scope_slots)
        self.scope_slots = self._saved

    def new_sem(self, name):
        self.nsem += 1
        return self.es.enter_context(self.nc.semaphore(f"{name}_{self.nsem}"))

    def _need(self, eng, ev, raw=False):
        if ev is None:
            return
        if ev.eng is eng and (not raw or eng is self.pe):
            return
        assert ev.sem is not None, "dependency on unsignaled instruction"
        k = id(ev.sem)
        if eng.seen.get(k, 0) >= ev.val:
            return
        eng.h.wait_ge(ev.sem, ev.val)
        eng.seen[k] = ev.val

    def _waits(self, eng, reads, writes):
        for b in reads:
            self._need(eng, b.w, raw=True)
        for b in writes:
            self._need(eng, b.w)
            for ev in b.r.values():
                self._need(eng, ev)

    def _record(self, ev, key, reads, writes):
        for b in writes:
            b.w = ev
            b.r = {}
        for b in reads:
            b.r[key] = ev

    def op(self, eng, fn, reads=(), writes=(), sig=True):
        self._waits(eng, reads, writes)
        ins = fn(eng.h)
        ev = Ev(eng)
        eng.n += 1
        self.ninstr += 1
        if sig:
            if eng.sem is None or eng.cnt >= SEM_LIMIT:
                eng.sem = self.new_sem(eng.name)
                eng.cnt = 0
            eng.cnt += 1
            ins.then_inc(eng.sem, 1)
            ev.sem, ev.val = eng.sem, eng.cnt
            for p in eng.pending:
                p.sem, p.val = eng.sem, eng.cnt
            eng.pending = []
            eng.last = ev
        else:
            eng.pending.append(ev)
        self._record(ev, eng.name, reads, writes)
        return ev

    def dma(self, q, slot, out, in_, reads=(), writes=()):
        self._waits(q, reads, writes)
        ins = q.h.dma_start(out=out, in_=in_)
        slot.cnt += 16
        ins.then_inc(slot.sem, 16)
        ev = Ev(None)
        ev.sem, ev.val = slot.sem, slot.cnt
        slot.last = ev
        self.ninstr += 1
        self._record(ev, "dma_" + slot.name, reads, writes)
        return ev

    def barrier(self):
        evs = []
        for e in self.engs:
            assert not e.pending, f"barrier with pending unsignaled instrs on {e.name}"
            if e.last is not None:
                evs.append(e.last)
        for s in self.slots:
            if s.last is not None:
                evs.append(s.last)
        for e in self.engs:
            for ev in evs:
                self._need(e, ev)


class Prog:
    def __init__(self, T, sublayers, do_cast=True):
        self.T = T
        self.NT = T // 128
        self.subl = sublayers
        self.do_cast = do_cast
        nc = bass.Bass("TRN2", target_bir_lowering=False)
        self.nc = nc
        self.K = Kern(nc)
        K = self.K
        NT = self.NT
        dt = nc.dram_tensor
        self.x_in = dt("x", [T, D], F32, kind="ExternalInput").ap()
        self.c_in = dt("c", [128, 8], F32, kind="ExternalInput").ap()
        self.pos_in = dt("pos", [128, NT], I32, kind="ExternalInput").ap()
        self.wall = dt("wall", [128, NTOT], F32, kind="ExternalInput").ap()
        self.ada = dt("ada", [12 * 24, 128, 1024], F32, kind="ExternalInput").ap()
        self.adab = dt("adab", [1, 12 * 3072], F32, kind="ExternalInput").ap()
        self.pre_n = dt("pre_n", [12, D], F32, kind="ExternalInput").ap()
        self.post_n = dt("post_n", [12, D], F32, kind="ExternalInput").ap()
        self.subln = dt("subln", [2, 128], F32, kind="ExternalInput").ap()
        self.lam = dt("lam", [2, 256], F32, kind="ExternalInput").ap()
        self.out = dt("out", [T, D], F32, kind="ExternalOutput").ap()
        self.debug = False
        self.dbg = None
        self.wbf = dt("wbf", [128, NTOT], BF16, kind="Internal").ap()
        self.ypart = dt("ypart", [T, D], F32, kind="Internal").ap()
        self.xbuf = [Buf(f"xd{i}") for i in range(NT)]
        self.ypbuf = [Buf(f"yp{i}") for i in range(NT)]
        self.wbf_buf = Buf("wbf")
        self.cast_pieces = []
        self.cast_slots = None
        self.gs = ExitStack()
        self._names = 0

    def sb(self, st, name, shape, dtype):
        self._names += 1
        return st.enter_context(self.nc.sbuf_tensor(f"{name}_{self._names}", shape, dtype))

    def ps(self, st, name, shape, dtype):
        self._names += 1
        return st.enter_context(self.nc.psum_tensor(f"{name}_{self._names}", shape, dtype))

    def setup_globals(self):
        K, nc, st, NT = self.K, self.nc, self.gs, self.NT
        sb = lambda n, s, d: self.sb(st, n, s, d)
        self.TA = self.ps(st, "TA", [128, 512], F32)
        self.TAb = self.TA[:].bitcast(BF16)
        self.bTA = Buf("TA")
        self.identf = sb("identf", [128, 128], F32)
        self.ident = sb("ident", [128, 128], BF16)
        self.I4 = sb("I4", [128, 4, 128], BF16)
        self.CN8 = sb("CN8", [128, 8, 128], BF16)
        self.ones_row = sb("ones_row", [1, 128], F32)
        self.neghalf = sb("neghalf", [128, 16], F32)
        self.half = sb("half", [128, 16], F32)
        self.bconst = Buf("consts")
        self.cosT = sb("cosT", [128, NT, 8], F32)
        self.sinT = sb("sinT", [128, NT, 8], F32)
        self.brope = Buf("rope")
        self.condrep = sb("condrep", [128, 8, 128], F32)
        self.bcond = Buf("cond")
        self.vecA = [sb(f"vA{i}", [128, D], F32) for i in range(2)]
        self.vecS = [sb(f"vS{i}", [128, D], F32) for i in range(2)]
        self.vecG = [sb(f"vG{i}", [128, D], F32) for i in range(2)]
        self.bvec = [Buf(f"vec{i}") for i in range(2)]
        self.adaring = [sb(f"adar{i}", [128, 2, 512], F32) for i in range(2)]
        self.badar = [Buf(f"adar{i}") for i in range(2)]
        self.sadar = [Slot(K, f"adar{i}") for i in range(2)]
        self.pgb = [sb(f"pgb{i}", [128, D], F32) for i in range(2)]
        self.bpgb = [Buf(f"pgb{i}") for i in range(2)]
        self.spgb = [Slot(K, f"pgb{i}") for i in range(2)]
        self.brow = sb("brow", [1, 512], F32)
        self.bbrow = Buf("brow")
        self.sbrow = Slot(K, "brow")
        self.neglam = sb("neglam", [128, 2], F32)
        self.sublnb = sb("sublnb", [128, 2, 128], F32)
        self.bdiffc = Buf("diffc")
        self.adacnt = 0

        pool, dve, act = K.pool, K.dve, K.act
        bc = self.bconst
        K.op(pool, lambda e: e.memset(self.identf[:], 0.0), writes=[bc])
        K.op(pool, lambda e: e.affine_select(out=self.identf[:], in_=self.identf[:], pattern=[[-1, 128]],
                                             compare_op=ALU.not_equal, fill=1.0, base=0, channel_multiplier=1),
             writes=[bc])
        K.op(pool, lambda e: e.tensor_copy(out=self.ident[:], in_=self.identf[:]), writes=[bc])
        for r in range(4):
            K.op(pool, lambda e, r=r: e.tensor_copy(out=self.I4[:, r, :], in_=self.identf[:]), writes=[bc])
        K.op(pool, lambda e: e.memset(self.CN8[:], 0.0), writes=[bc])
        K.op(pool, lambda e: e.affine_select(out=self.CN8[:], in_=self.CN8[:], pattern=[[0, 8], [1, 128]],
                                             compare_op=ALU.is_ge, fill=NEG, base=0, channel_multiplier=-1),
             writes=[bc])
        K.op(pool, lambda e: e.memset(self.ones_row[:], 1.0), writes=[bc])
        K.op(pool, lambda e: e.memset(self.neghalf[:], -0.5), writes=[bc])
        K.op(pool, lambda e: e.memset(self.half[:], 0.5), writes=[bc])

        with ExitStack() as ts:
            tsb = lambda n, s, d: self.sb(ts, n, s, d)
            c_sb = tsb("c_sb", [128, 8], F32)
            cond = tsb("cond", [128, 8], F32)
            b_c = Buf("c_sb")
            s_c = Slot(K, "c_sb")
            K.dma(K.sp, s_c, c_sb[:], self.c_in, writes=[b_c])
            K.op(act, lambda e: e.activation(out=cond[:], in_=c_sb[:], func=AF.Silu), reads=[b_c], writes=[self.bcond])
            zer = tsb("zer", [128, 128], F32)
            K.op(dve, lambda e: e.memset(zer[:], 0.0), writes=[b_c])
            for kc in range(8):
                K.op(dve, lambda e, kc=kc: e.tensor_scalar(out=self.condrep[:, kc, :], in0=zer[:],
                                                           scalar1=cond[:, kc:kc + 1], scalar2=None, op0=ALU.add),
                     reads=[b_c, self.bcond], writes=[self.bcond])
            pos_i = tsb("pos_i", [128, NT], I32)
            pos_f = tsb("pos_f", [128, NT], F32)
            invt = tsb("invt", [128, 8], F32)
            ang = tsb("ang", [128, NT, 8], F32)
            ang2 = tsb("ang2", [128, NT, 8], F32)
            b_p = Buf("pos")
            s_p = Slot(K, "pos")
            K.dma(K.sp, s_p, pos_i[:], self.pos_in, writes=[b_p])
            K.op(dve, lambda e: e.tensor_copy(out=pos_f[:], in_=pos_i[:]), reads=[b_p], writes=[b_p])
            for j in range(8):
                inv = float(np.float32(500000.0) ** np.float32(-(2.0 * j) / 16.0))
                K.op(dve, lambda e, j=j, inv=inv: e.memset(invt[:, j:j + 1], inv), writes=[b_p])
            K.op(dve, lambda e: e.tensor_tensor(out=ang[:], in0=pos_f[:].rearrange("p (n o) -> p n o", o=1).to_broadcast([128, NT, 8]),
                                                in1=invt[:].rearrange("p (o j) -> p o j", o=1).to_broadcast([128, NT, 8]),
                                                op=ALU.mult), reads=[b_p], writes=[b_p])
            TWO_PI = 2.0 * math.pi
            C1 = 6.28125
            C2 = TWO_PI - C1
            kf = tsb("kf", [128, NT, 8], F32)
            ki = tsb("ki", [128, NT, 8], I32)
            mm_ = tsb("mm_", [128, NT, 8], F32)

            def sin_of(dst, shift):
                K.op(dve, lambda e: e.tensor_scalar(out=ang2[:], in0=ang[:], scalar1=shift, scalar2=None, op0=ALU.add),
                     reads=[b_p, self.brope], writes=[b_p])
                K.op(dve, lambda e: e.tensor_scalar(out=kf[:], in0=ang2[:], scalar1=1.0 / TWO_PI, scalar2=None, op0=ALU.mult),
                     reads=[b_p], writes=[b_p])
                K.op(dve, lambda e: e.tensor_copy(out=ki[:], in_=kf[:]), reads=[b_p], writes=[b_p])
                K.op(dve, lambda e: e.tensor_copy(out=kf[:], in_=ki[:]), reads=[b_p], writes=[b_p])
                K.op(dve, lambda e: e.scalar_tensor_tensor(out=ang2[:], in0=kf[:], scalar=-C1, in1=ang2[:], op0=ALU.mult, op1=ALU.add),
                     reads=[b_p], writes=[b_p])
                K.op(dve, lambda e: e.scalar_tensor_tensor(out=ang2[:], in0=kf[:], scalar=-C2, in1=ang2[:], op0=ALU.mult, op1=ALU.add),
                     reads=[b_p], writes=[b_p])
                K.op(dve, lambda e: e.tensor_scalar(out=mm_[:], in0=ang2[:], scalar1=math.pi, scalar2=TWO_PI, op0=ALU.is_gt, op1=ALU.mult),
                     reads=[b_p], writes=[b_p])
                K.op(dve, lambda e: e.tensor_tensor(out=ang2[:], in0=ang2[:], in1=mm_[:], op=ALU.subtract), reads=[b_p], writes=[b_p])
                K.op(dve, lambda e: e.tensor_scalar(out=mm_[:], in0=ang2[:], scalar1=-math.pi, scalar2=TWO_PI, op0=ALU.is_lt, op1=ALU.mult),
                     reads=[b_p], writes=[b_p])
                K.op(dve, lambda e: e.tensor_tensor(out=ang2[:], in0=ang2[:], in1=mm_[:], op=ALU.add), reads=[b_p], writes=[b_p])
                K.op(act, lambda e: e.activation(out=dst[:], in_=ang2[:], func=AF.Sin), reads=[b_p], writes=[self.brope])

            sin_of(self.sinT, 0.0)
            sin_of(self.cosT, 0.5 * math.pi)
            lam_sb = tsb("lam_sb", [128, 2, 4, 64], F32)
            sub_sb = tsb("sub_sb", [128, 2, 128], F32)
            lp = tsb("lp", [128, 2, 2, 64], F32)
            ls = tsb("ls", [128, 4], F32)
            b_l = Buf("lam")
            s_l = Slot(K, "lam")
            s_l2 = Slot(K, "lam2")
            K.dma(K.sp, s_l, lam_sb[:].rearrange("p a b c -> p (a b c)"),
                  self.lam.rearrange("a b -> (a b)").partition_broadcast(128), writes=[b_l])
            b_l2 = Buf("sub")
            K.dma(K.sp, s_l2, sub_sb[:].rearrange("p a b -> p (a b)"),
                  self.subln.rearrange("a b -> (a b)").partition_broadcast(128), writes=[b_l2])
            K.op(dve, lambda e: e.tensor_tensor(out=lp[:], in0=lam_sb[:, :, 0:4:2, :], in1=lam_sb[:, :, 1:4:2, :],
                                                op=ALU.mult), reads=[b_l], writes=[b_l])
            K.op(dve, lambda e: e.tensor_reduce(out=ls[:], in_=lp[:].rearrange("p a b c -> p (a b) c"), axis=AX.X,
                                                op=ALU.add), reads=[b_l], writes=[b_l])
            K.op(act, lambda e: e.activation(out=ls[:], in_=ls[:], func=AF.Exp), reads=[b_l], writes=[b_l])
            for j in range(2):
                li = 0.8 - 0.6 * math.exp(-0.3 * (2 * j + 1))
                K.op(dve, lambda e, j=j, li=li: e.scalar_tensor_tensor(out=self.neglam[:, j:j + 1], in0=ls[:, 2 * j + 1:2 * j + 2],
                                                                       scalar=-li, in1=ls[:, 2 * j:2 * j + 1],
                                                                       op0=ALU.add, op1=ALU.subtract),
                     reads=[b_l], writes=[self.bdiffc])
                K.op(dve, lambda e, j=j, li=li: e.tensor_scalar(out=self.sublnb[:, j, :], in0=sub_sb[:, j, :],
                                                                scalar1=1.0 - li, scalar2=None, op0=ALU.mult),
                     reads=[b_l2], writes=[self.bdiffc])
            K.barrier()

    def cast_plan(self, ranges):
        CW = 8192
        self.wgrp = [[Buf(f"wgrp{g}_{r}") for r in range(3)] for g in range(len(ranges))]
        self.cast_ring = [Buf(f"castring{r}") for r in range(3)]
        self.cast_n = 0
        for g, (s0, ln) in enumerate(ranges):
            o = 0
            while o < ln:
                w = min(CW, ln - o)
                self.cast_pieces.append((s0 + o, w, g))
                o += w
        self.cast_slots = [Slot(self.K, f"cast{i}") for i in range(3)]

    def cast_bg(self, k=1, upto_group=None):
        K = self.K
        while self.cast_pieces and (k > 0 or (upto_group is not None and self.cast_pieces[0][2] <= upto_group)):
            c0, w, g = self.cast_pieces.pop(0)
            r = self.cast_n % 3
            self.cast_n += 1
            K.dma(K.pool, self.cast_slots[r], self.wbf[:, c0:c0 + w], self.wall[:, c0:c0 + w],
                  writes=[self.wgrp[g][r], self.cast_ring[r]])
            k -= 1

    def cast_weights(self, ranges):
        K = self.K
        K.begin_scope()
        with ExitStack() as ts:
            NB = 3
            fin = [self.sb(ts, f"cin{i}", [128, CASTW], F32) for i in range(NB)]
            fout = [self.sb(ts, f"cout{i}", [128, CASTW], BF16) for i in range(NB)]
            bin_ = [Buf(f"cin{i}") for i in range(NB)]
            bout = [Buf(f"cout{i}") for i in range(NB)]
            sin_ = [Slot(K, f"cin{i}") for i in range(NB)]
            sout = [Slot(K, f"cout{i}") for i in range(NB)]
            pieces = []
            for (s0, ln) in ranges:
                o = 0
                while o < ln:
                    w = min(CASTW, ln - o)
                    pieces.append((s0 + o, w))
                    o += w
            engs = [K.dve, K.pool, K.act]
            for n, (c0, w) in enumerate(pieces):
                i = n % NB
                K.dma(K.sp, sin_[i], fin[i][:, 0:w], self.wall[:, c0:c0 + w], writes=[bin_[i]])
                eng = engs[n % 3]
                if eng is K.act:
                    K.op(eng, lambda e, i=i, w=w: e.activation(out=fout[i][:, 0:w], in_=fin[i][:, 0:w], func=AF.Copy),
                         reads=[bin_[i]], writes=[bout[i]])
                else:
                    K.op(eng, lambda e, i=i, w=w: e.tensor_copy(out=fout[i][:, 0:w], in_=fin[i][:, 0:w]),
                         reads=[bin_[i]], writes=[bout[i]])
                K.dma(K.sp, sout[i], self.wbf[:, c0:c0 + w], fout[i][:, 0:w], reads=[bout[i]], writes=[self.wbf_buf])
            K.barrier()
        K.end_scope()

    def ada_steps(self, s_glob, vs, coef):
        K = self.K
        pe, dve, act = K.pe, K.dve, K.act

        def load_pg():
            K.dma(K.sp, self.spgb[0], self.pgb[0][:], self.pre_n[s_glob, :].partition_broadcast(128), writes=[self.bpgb[0]])
            K.dma(K.sp, self.spgb[1], self.pgb[1][:], self.post_n[s_glob, :].partition_broadcast(128), writes=[self.bpgb[1]])

        def step(c):
            if c == 0:
                load_pg()
            K.dma(K.sp, self.sbrow, self.brow[:], self.adab[0:1, s_glob * 3072 + c * 512: s_glob * 3072 + (c + 1) * 512],
                  writes=[self.bbrow])
            for kh in range(4):
                r = self.adacnt % 2
                self.adacnt += 1
                K.dma(K.sp, self.sadar[r], self.adaring[r][:].rearrange("p a b -> p (a b)"),
                      self.ada[s_glob * 24 + c * 4 + kh], writes=[self.badar[r]])
                for kl in range(2):
                    kc = kh * 2 + kl
                    K.op(pe, lambda e, r=r, kl=kl, kc=kc: e.matmul(self.TA[:], lhsT=self.condrep[:, kc, :], rhs=self.adaring[r][:, kl, :],
                                                                    start=(kc == 0), stop=False),
                         reads=[self.bcond, self.badar[r]], writes=[self.bTA], sig=(kl == 1))
            K.op(pe, lambda e: e.matmul(self.TA[:], lhsT=self.ones_row[0:1, :], rhs=self.brow[0:1, :], start=False, stop=True),
                 reads=[self.bconst, self.bbrow], writes=[self.bTA])
            cs = slice((c % 2) * 512, (c % 2) * 512 + 512)
            if c < 2:
                K.op(act, lambda e: e.activation(out=self.vecS[vs][:, cs], in_=self.TA[:], func=AF.Copy),
                     reads=[self.bTA], writes=[self.bvec[vs]])
            elif c < 4:
                K.op(dve, lambda e: e.scalar_tensor_tensor(out=self.vecA[vs][:, cs], in0=self.TA[:], scalar=1.0,
                                                           in1=self.pgb[0][:, cs], op0=ALU.add, op1=ALU.mult),
                     reads=[self.bTA, self.bpgb[0]], writes=[self.bvec[vs]])
            else:
                K.op(dve, lambda e: e.scalar_tensor_tensor(out=self.vecG[vs][:, cs], in0=self.TA[:], scalar=coef,
                                                           in1=self.pgb[1][:, cs], op0=ALU.mult, op1=ALU.mult),
                     reads=[self.bTA, self.bpgb[1]], writes=[self.bvec[vs]])

        return [lambda c=c: step(c) for c in range(6)]

    def norm_mod_T(self, vs, xt, bx, hb, bhb, tmp, btmp, small, bsmall, junk, bjunk, hT_out, bhT):
        K = self.K
        pe, dve, act, pool = K.pe, K.dve, K.act, K.pool
        ss = small[:, 0:1]
        rs = small[:, 1:2]
        K.op(act, lambda e: e.activation(out=junk[:], in_=xt, func=AF.Square, accum_out=ss), reads=[bx], writes=[bjunk, bsmall])
        K.op(pool, lambda e: e.tensor_scalar(out=rs, in0=ss, scalar1=1.0 / D, scalar2=EPS, op0=ALU.mult, op1=ALU.add),
             reads=[bsmall], writes=[bsmall])
        K.op(pool, lambda e: e.tensor_tensor(out=rs, in0=rs, in1=self.neghalf[:, 0:1], op=ALU.pow),
             reads=[bsmall, self.bconst], writes=[bsmall])
        K.op(dve, lambda e: e.scalar_tensor_tensor(out=tmp[:], in0=xt, scalar=rs, in1=self.vecA[vs][:], op0=ALU.mult, op1=ALU.mult),
             reads=[bx, bsmall, self.bvec[vs]], writes=[btmp])
        K.op(dve, lambda e: e.tensor_tensor(out=hb[:], in0=tmp[:], in1=self.vecS[vs][:], op=ALU.add),
             reads=[btmp, self.bvec[vs]], writes=[bhb])
        for kc in range(8):
            K.op(pe, lambda e, kc=kc: e.transpose(out=self.TAb[:, kc * 128:(kc + 1) * 128], in_=hb[:, kc * 128:(kc + 1) * 128],
                                                  identity=self.ident[:]),
                 reads=[bhb, self.bconst], writes=[self.bTA], sig=(kc == 7))
        K.op(act, lambda e: e.activation(out=hT_out, in_=self.TAb[:].rearrange("p (a b) -> p a b", a=8), func=AF.Copy),
             reads=[self.bTA], writes=[bhT])

    def post_resid(self, vs, y_srcs, by, xt, bx, xo, bxo, small, bsmall, junk, bjunk, tmp, btmp):
        K = self.K
        dve, act, pool = K.dve, K.act, K.pool
        n = len(y_srcs)
        for k, (yap, c0, w) in enumerate(y_srcs):
            K.op(act, lambda e, yap=yap, c0=c0, w=w, k=k: e.activation(out=junk[:, c0:c0 + w], in_=yap, func=AF.Square,
                                                                        accum_out=small[:, 4 + k:5 + k]),
                 reads=by, writes=[bjunk, bsmall])
        rs = small[:, 3:4]
        if n == 2:
            K.op(pool, lambda e: e.tensor_tensor(out=rs, in0=small[:, 4:5], in1=small[:, 5:6], op=ALU.add),
                 reads=[bsmall], writes=[bsmall])
            src = rs
        else:
            src = small[:, 4:5]
        K.op(pool, lambda e: e.tensor_scalar(out=rs, in0=src, scalar1=1.0 / D, scalar2=EPS, op0=ALU.mult, op1=ALU.add),
             reads=[bsmall], writes=[bsmall])
        K.op(pool, lambda e: e.tensor_tensor(out=rs, in0=rs, in1=self.neghalf[:, 0:1], op=ALU.pow),
             reads=[bsmall, self.bconst], writes=[bsmall])
        for (yap, c0, w) in y_srcs:
            K.op(dve, lambda e, yap=yap, c0=c0, w=w: e.scalar_tensor_tensor(out=tmp[:, c0:c0 + w], in0=yap, scalar=rs,
                                                                             in1=self.vecG[vs][:, c0:c0 + w],
                                                                             op0=ALU.mult, op1=ALU.mult),
                 reads=list(by) + [bsmall, self.bvec[vs]], writes=[btmp])
        K.op(dve, lambda e: e.tensor_tensor(out=xo[:], in0=tmp[:], in1=xt, op=ALU.add), reads=[btmp, bx], writes=[bxo])

    def ffn(self, x_src, vs, off_gu, off_d, bg):
        K, T, NT = self.K, self.T, self.NT
        pe, dve, act, pool, sp = K.pe, K.dve, K.act, K.pool, K.sp
        NTT = T // 512
        K.begin_scope()
        with ExitStack() as st:
            sb = lambda n, s, d: self.sb(st, n, s, d)
            NG = 4
            wgu = [sb(f"wgu{i}", [128, 2, 8, 256], BF16) for i in range(NG)]
            bwgu = [Buf(f"wgu{i}") for i in range(NG)]
            swgu = [Slot(K, f"wgu{i}") for i in range(NG)]
            wd = sb("wd", [128, NF, 1024], BF16)
            bwd = Buf("wd")
            swd = Slot(K, "wd")
            xa = [sb(f"xa{i}", [128, D], F32) for i in range(2)]
            bxa = [Buf(f"xa{i}") for i in range(2)]
            sxa = [Slot(K, f"xa{i}") for i in range(2)]
            xc = [sb(f"xc{i}", [128, D], F32) for i in range(2)]
            bxc = [Buf(f"xc{i}") for i in range(2)]
            sxc = [Slot(K, f"xc{i}") for i in range(2)]
            xo = [sb(f"xo{i}", [128, D], F32) for i in range(2)]
            bxo = [Buf(f"xo{i}") for i in range(2)]
            sxo = [Slot(K, f"xo{i}") for i in range(2)]
            tmp = sb("tmp", [128, D], F32)
            btmp = Buf("tmp")
            hb = [sb(f"hb{i}", [128, D], BF16) for i in range(2)]
            bhb = [Buf(f"hb{i}") for i in range(2)]
            hT = [sb(f"hT{i}", [128, 8, 512], BF16) for i in range(2)]
            bhT = [Buf(f"hT{i}") for i in range(2)]
            actT = sb("actT", [128, NF, 512], BF16)
            bactT = Buf("actT")
            gt = [sb(f"gt{i}", [128, 512], F32) for i in range(2)]
            bgt = [Buf(f"gt{i}") for i in range(2)]
            junk = sb("junk", [128, D], BF16)
            bjunk = Buf("junk")
            small = [sb(f"small{i}", [128, 8], F32) for i in range(2)]
            bsmall = [Buf(f"small{i}") for i in range(2)]
            GU = [self.ps(st, f"GU{i}", [128, 512], F32) for i in range(3)]
            bGU = [Buf(f"GU{i}") for i in range(3)]
            Y = [self.ps(st, f"Y{i}", [128, 512], F32) for i in range(3)]
            bY = [Buf(f"Y{i}") for i in range(3)]

            wcols = self.wbf
            for q4 in range(2):
                K.dma(sp, swd, wd[:, q4 * 11:(q4 + 1) * 11, :].rearrange("p a b -> p (a b)"),
                      wcols[:, off_d + q4 * 11 * 1024: off_d + (q4 + 1) * 11 * 1024], reads=self.wbf_grp, writes=[bwd])

            gu_seq = [(n, g) for n in range(NTT) for g in range(11)]
            self._gu_loaded = 0

            def load_gu(upto):
                while self._gu_loaded < min(upto, len(gu_seq)):
                    k = self._gu_loaded
                    n, g = gu_seq[k]
                    i = k % NG
                    K.dma(sp, swgu[i], wgu[i][:].rearrange("p a b c -> p (a b c)"),
                          wcols[:, off_gu + g * 4096: off_gu + (g + 1) * 4096], reads=self.wbf_grp, writes=[bwgu[i]])
                    self._gu_loaded += 1

            acnt = [0]

            def phaseA(n, j):
                a = acnt[0] % 2
                acnt[0] += 1
                tile = n * 4 + j
                K.dma(pool, sxa[a], xa[a][:], x_src[tile * 128:(tile + 1) * 128, :], reads=[self.xbuf[tile]], writes=[bxa[a]])
                self.norm_mod_T(vs, xa[a][:], bxa[a], hb[a], bhb[a], tmp, btmp, small[0], bsmall[0], junk, bjunk,
                                hT[n % 2][:, :, j * 128:(j + 1) * 128], bhT[n % 2])

            for j in range(4):
                phaseA(0, j)
            load_gu(NG)
            ccnt = [0]
            bgq = list(bg)
            for n in range(NTT):
                cur = n % 2
                for g in range(11):
                    k = n * 11 + g
                    i = k % NG
                    for fl in range(2):
                        f = 2 * g + fl
                        bg_, bu_ = (2 * f) % 3, (2 * f + 1) % 3
                        for kc in range(8):
                            K.op(pe, lambda e, i=i, fl=fl, kc=kc, bg_=bg_: e.matmul(GU[bg_][:], lhsT=wgu[i][:, 0, kc, fl * 128:(fl + 1) * 128],
                                                                                     rhs=hT[cur][:, kc, :], start=(kc == 0), stop=(kc == 7)),
                                 reads=[bwgu[i], bhT[cur]], writes=[bGU[bg_]], sig=(kc == 7))
                        for kc in range(8):
                            K.op(pe, lambda e, i=i, fl=fl, kc=kc, bu_=bu_: e.matmul(GU[bu_][:], lhsT=wgu[i][:, 1, kc, fl * 128:(fl + 1) * 128],
                                                                                     rhs=hT[cur][:, kc, :], start=(kc == 0), stop=(kc == 7)),
                                 reads=[bwgu[i], bhT[cur]], writes=[bGU[bu_]], sig=(kc == 7))
                        K.op(act, lambda e, f=f, bg_=bg_: e.activation(out=gt[f % 2][:], in_=GU[bg_][:], func=AF.Silu),
                             reads=[bGU[bg_]], writes=[bgt[f % 2]])
                        K.op(dve, lambda e, f=f, bu_=bu_: e.tensor_tensor(out=actT[:, f, :], in0=gt[f % 2][:], in1=GU[bu_][:], op=ALU.mult),
                             reads=[bgt[f % 2], bGU[bu_]], writes=[bactT])
                    load_gu(k + 1 + NG)
                    if n + 1 < NTT and g in (1, 3, 5, 7):
                        phaseA(n + 1, (g - 1) // 2)
                    if g == 9 and bgq:
                        bgq.pop(0)()
                for j in range(4):
                    tile = n * 4 + j
                    c = ccnt[0] % 2
                    ccnt[0] += 1
                    K.dma(pool, sxc[c], xc[c][:], x_src[tile * 128:(tile + 1) * 128, :], reads=[self.xbuf[tile]], writes=[bxc[c]])
                    ybs = []
                    for hf in range(2):
                        yb = (2 * j + hf) % 3
                        ybs.append(yb)
                        for f in range(NF):
                            K.op(pe, lambda e, f=f, j=j, hf=hf, yb=yb: e.matmul(Y[yb][:], lhsT=actT[:, f, j * 128:(j + 1) * 128],
                                                                                 rhs=wd[:, f, hf * 512:(hf + 1) * 512],
                                                                                 start=(f == 0), stop=(f == NF - 1)),
                                 reads=[bactT, bwd], writes=[bY[yb]], sig=(f == NF - 1))
                    self.post_resid(vs, [(Y[ybs[0]][:], 0, 512), (Y[ybs[1]][:], 512, 512)], [bY[ybs[0]], bY[ybs[1]]],
                                    xc[c][:], bxc[c], xo[c], bxo[c], small[1], bsmall[1], junk, bjunk, tmp, btmp)
                    K.dma(pool, sxo[c], self.out[tile * 128:(tile + 1) * 128, :], xo[c][:], reads=[bxo[c]], writes=[self.xbuf[tile]])
            while bgq:
                bgq.pop(0)()
            K.barrier()
        K.end_scope()

    def build(self):
        K = self.K
        self.setup_globals()
        if self.do_cast:
            need = []
            for (li, kind) in self.subl:
                if kind in ("f0", "f1"):
                    j = 0 if kind == "f0" else 1
                    need.append((WOFF[("gu", li, j)], W_GU + W_D))
                elif li % 2 == 0:
                    need.append((WOFF[("ain", li // 2)], W_AIN + W_AOUT))
                else:
                    need.append((WOFF[("bin", li // 2, 0)], 2 * (W_BIN + W_BOUT)))
            self.cast_plan(need)
            self.cast_bg(0, upto_group=0)
        x_src = self.x_in
        nsub = len(self.subl)

        def sglob(li, kind):
            return li * 3 + {"f0": 0, "mix": 1, "f1": 2}[kind]

        def coef(kind):
            return 1.0 if kind == "mix" else 0.5

        li, kind = self.subl[0]
        for stp in self.ada_steps(sglob(li, kind), 0, coef(kind)):
            stp()
        for si, (li, kind) in enumerate(self.subl):
            vs = si % 2
            bg = []
            self.wbf_grp = self.wgrp[si]
            if si + 1 < nsub:
                nl, nk = self.subl[si + 1]
                steps = self.ada_steps(sglob(nl, nk), (si + 1) % 2, coef(nk))
                bg = [(lambda s_=s_, si=si: (self.cast_bg(2), s_())) for s_ in steps]
                bg.append(lambda si=si: self.cast_bg(0, upto_group=si + 1))
            if kind in ("f0", "f1"):
                j = 0 if kind == "f0" else 1
                self.ffn(x_src, vs, WOFF[("gu", li, j)], WOFF[("d", li, j)], bg)
            elif li % 2 == 0:
                self.dsa(x_src, vs, li // 2, bg)
            else:
                self.diff(x_src, vs, li // 2, bg)
            x_src = self.out
        K.barrier()
        return self.nc

    @staticmethod
    def interleave(*gens):
        gens = [g for g in gens if g is not None]
        while gens:
            for g in list(gens):
                try:
                    next(g)
                except StopIteration:
                    gens.remove(g)

    @staticmethod
    def chain(*gens):
        for g in gens:
            if g is not None:
                yield from g

    def neg_bound(self, qsq, bq, ksq, bk, run, brun, mneg, bmneg, small, bsmall):
        K = self.K
        pe, dve, act, pool = K.pe, K.dve, K.act, K.pool
        TAr = self.TA[0:1, 0:256]
        K.op(pe, lambda e: e.transpose(out=self.TA[0:1, 0:128], in_=qsq, identity=self.identf[:]),
             reads=[bq, self.bconst], writes=[self.bTA], sig=False)
        K.op(pe, lambda e: e.transpose(out=self.TA[0:1, 128:256], in_=ksq, identity=self.identf[:]),
             reads=[bk, self.bconst], writes=[self.bTA])
        K.op(dve, lambda e: e.tensor_reduce(out=small[0:1, 0:2], in_=TAr.rearrange("p (a b) -> p a b", a=2), axis=AX.X, op=ALU.max),
             reads=[self.bTA], writes=[bsmall])
        K.op(dve, lambda e: e.tensor_tensor(out=run[0:1, 0:1], in0=run[0:1, 0:1], in1=small[0:1, 1:2], op=ALU.max),
             reads=[bsmall], writes=[brun])
        K.op(dve, lambda e: e.tensor_tensor(out=small[0:1, 2:3], in0=small[0:1, 0:1], in1=run[0:1, 0:1], op=ALU.mult),
             reads=[bsmall, brun], writes=[bsmall])
        K.op(pe, lambda e: e.matmul(self.TA[:, 0:1], lhsT=self.ones_row[0:1, :], rhs=small[0:1, 2:3], start=True, stop=True),
             reads=[bsmall, self.bconst], writes=[self.bTA])
        K.op(dve, lambda e: e.tensor_copy(out=mneg[:, 2:3], in_=self.TA[:, 0:1]), reads=[self.bTA], writes=[bmneg])
        K.op(pool, lambda e: e.tensor_tensor(out=mneg[:, 2:3], in0=mneg[:, 2:3], in1=self.half[:, 0:1], op=ALU.pow),
             reads=[bmneg, self.bconst], writes=[bmneg])
        K.op(pool, lambda e: e.tensor_scalar(out=mneg[:, 0:1], in0=mneg[:, 2:3], scalar1=-0.125, scalar2=None, op0=ALU.mult),
             reads=[bmneg], writes=[bmneg])

    def rope(self, pe_t, bpe, h0, nh, ti, rt, brt):
        K = self.K
        dve = K.dve
        v = pe_t[:, h0 * 64:(h0 + nh) * 64].rearrange("p (h d) -> p h d", d=64)
        x1 = v[:, :, 0:8]
        x2 = v[:, :, 8:16]
        cb = self.cosT[:, ti:ti + 1, :].to_broadcast([128, nh, 8])
        sbb = self.sinT[:, ti:ti + 1, :].to_broadcast([128, nh, 8])
        t = [rt[:, k, 0:nh, :] for k in range(4)]
        rd = [bpe, self.brope]
        K.op(dve, lambda e: e.tensor_tensor(out=t[0], in0=x1, in1=cb, op=ALU.mult), reads=rd, writes=[brt])
        K.op(dve, lambda e: e.tensor_tensor(out=t[1], in0=x2, in1=sbb, op=ALU.mult), reads=rd, writes=[brt])
        K.op(dve, lambda e: e.tensor_tensor(out=t[2], in0=x2, in1=cb, op=ALU.mult), reads=rd, writes=[brt])
        K.op(dve, lambda e: e.tensor_tensor(out=t[3], in0=x1, in1=sbb, op=ALU.mult), reads=rd, writes=[brt])
        K.op(dve, lambda e: e.tensor_tensor(out=x1, in0=t[0], in1=t[1], op=ALU.subtract), reads=[brt], writes=[bpe])
        K.op(dve, lambda e: e.tensor_tensor(out=x2, in0=t[2], in1=t[3], op=ALU.add), reads=[brt], writes=[bpe])

    def dsa(self, x_src, vs, jl, bg):
        K, T, NT = self.K, self.T, self.NT
        pe, dve, act, pool, sp = K.pe, K.dve, K.act, K.pool, K.sp
        off_in, off_out = WOFF[("ain", jl)], WOFF[("aout", jl)]
        K.begin_scope()
        with ExitStack() as st:
            sb = lambda n, s, d: self.sb(st, n, s, d)
            w_in = sb("w_in", [128, 8, A_IN], BF16); bw = Buf("w"); sw = Slot(K, "w")
            w_out = sb("w_out", [128, 8, D], BF16)
            kT2 = sb("kT2", [128, T], BF16); bkT = Buf("kT2")
            kiT2 = sb("kiT2", [128, T], BF16); bkiT = Buf("kiT2")
            Vaug = sb("Vaug", [128, NT, 65], BF16); bV = Buf("Vaug")
            score = sb("score", [128, T], F32); bscore = Buf("score")
            NM2 = [sb(f"NM{i}", [128, T], BF16) for i in range(2)]; bNM2 = [Buf(f"NM{i}") for i in range(2)]
            rtmp = [sb(f"rtmp{i}", [128, 512], F32) for i in range(2)]; brtmp = [Buf(f"rtmp{i}") for i in range(2)]
            PT = [sb(f"PT{i}", [128, 16, 128], BF16) for i in range(2)]; bPT = [Buf(f"PT{i}") for i in range(2)]
            pev = sb("pev", [128, A_IN], F32); bpev = Buf("pev")
            rt = sb("rt", [128, 4, 17, 8], F32); brt = Buf("rt")
            qb = sb("qb", [128, 1152], BF16); bqb = Buf("qb")
            qib = sb("qib", [128, 640], BF16); bqib = Buf("qib")
            qT2 = [sb(f"qT{i}", [128, 8, 128], BF16) for i in range(2)]; bqT2 = [Buf(f"qT{i}") for i in range(2)]
            qiT = sb("qiT", [128, 4, 128], BF16); bqiT = Buf("qiT")
            wi = sb("wi", [128, 8], F32); bwi = Buf("wi")
            xa = [sb(f"xa{i}", [128, D], F32) for i in range(2)]; bxa = [Buf(f"xa{i}") for i in range(2)]
            sxa = [Slot(K, f"xa{i}") for i in range(2)]
            sxo = [Slot(K, f"xo{i}") for i in range(2)]
            tmp = sb("tmp", [128, D], F32); btmp = Buf("tmp")
            hb = sb("hb", [128, D], BF16); bhb = Buf("hb")
            hT = sb("hT", [128, 8, 128], BF16); bhT = Buf("hT")
            ob = sb("ob", [128, D], BF16); bob = Buf("ob")
            oT = sb("oT", [128, 8, 128], BF16); boT = Buf("oT")
            junk = sb("junk", [128, D], BF16); bjunk = Buf("junk")
            small = [sb(f"small{i}", [128, 8], F32) for i in range(2)]; bsmall = [Buf(f"small{i}") for i in range(2)]
            sq = sb("sq", [128, 24], F32); bsq = Buf("sq")
            ksqt = sb("ksqt", [128, 64], F32)
            run = sb("run", [128, 2], F32); brun = Buf("run")
            mneg2 = [sb(f"mneg{i}", [128, 4], F32) for i in range(2)]; bmneg2 = [Buf(f"mneg{i}") for i in range(2)]
            smallb = sb("smallb", [128, 8], F32); bsmallb = Buf("smallb")
            bis = sb("bis", [128, 8], F32); bbis = Buf("bis")
            dk = sb("dk", [128, NBIS + 2], F32)
            p2 = sb("p2", [128, NBIS + 2], F32)
            rl = sb("rl", [128, 16], F32); brl = Buf("rl")
            P = [self.ps(st, f"P{i}", [128, 512], F32) for i in range(4)]; bP = [Buf(f"P{i}") for i in range(4)]
            O = self.ps(st, "O", [128, 1536], F32); bO = Buf("O")

            K.dma(sp, sw, w_in[:].rearrange("p a b -> p (a b)"), self.wbf[:, off_in:off_in + W_AIN], reads=self.wbf_grp, writes=[bw])
            K.dma(sp, sw, w_out[:].rearrange("p a b -> p (a b)"), self.wbf[:, off_out:off_out + W_AOUT], reads=self.wbf_grp, writes=[bw])
            K.op(pool, lambda e: e.memset(Vaug[:, :, 64:65], 1.0), writes=[bV])
            K.op(pool, lambda e: e.memset(run[:], 0.0), writes=[brun])
            for k in range(NBIS + 2):
                K.op(pool, lambda e, k=k: e.memset(p2[:, k:k + 1], 2.0 ** (-(k + 1))), writes=[bbis])
            bgq = list(bg)
            pcnt = [0]
            if not hasattr(self, "fill_reg"):
                self.fill_reg = self.nc.gpsimd.to_reg(-1e30)

            def pbank():
                b = pcnt[0] % 4
                pcnt[0] += 1
                return b

            def stageA(i):
                a = i % 2
                n = (i + 1) * 128
                qT, bqT, mneg, bmneg = qT2[a], bqT2[a], mneg2[a], bmneg2[a]
                K.dma(pool, sxa[a], xa[a][:], x_src[i * 128:(i + 1) * 128, :], reads=[self.xbuf[i]], writes=[bxa[a]])
                self.norm_mod_T(vs, xa[a][:], bxa[a], hb, bhb, tmp, btmp, small[0], bsmall[0], junk, bjunk, hT[:], bhT)
                yield
                for b in range(4):
                    c0, c1 = b * 512, min((b + 1) * 512, A_IN)
                    for kc in range(8):
                        K.op(pe, lambda e, b=b, kc=kc, c0=c0, c1=c1: e.matmul(P[b][:, 0:c1 - c0], lhsT=hT[:, kc, :], rhs=w_in[:, kc, c0:c1],
                                                                             start=(kc == 0), stop=(kc == 7)),
                             reads=[bhT, bw], writes=[bP[b]], sig=(kc == 7))
                    K.op(act, lambda e, b=b, c0=c0, c1=c1: e.activation(out=pev[:, c0:c1], in_=P[b][:, 0:c1 - c0], func=AF.Copy),
                         reads=[bP[b]], writes=[bpev])
                yield
                self.rope(pev[:], bpev, 0, 17, i, rt, brt)
                self.rope(pev[:], bpev, 18, 9, i, rt, brt)
                yield
                K.op(act, lambda e: e.activation(out=qb[:, 0:1088], in_=pev[:, 0:1088], func=AF.Copy), reads=[bpev], writes=[bqb])
                K.op(act, lambda e: e.activation(out=qb[:, 1088:1152], in_=pev[:, 1024:1088], func=AF.Copy), reads=[bpev], writes=[bqb])
                K.op(act, lambda e, i=i: e.activation(out=Vaug[:, i, 0:64], in_=pev[:, 1088:1152], func=AF.Copy), reads=[bpev], writes=[bV])
                K.op(act, lambda e: e.activation(out=qib[:, 0:576], in_=pev[:, 1152:1728], func=AF.Copy), reads=[bpev], writes=[bqib])
                K.op(act, lambda e: e.activation(out=qib[:, 576:640], in_=pev[:, 1664:1728], func=AF.Copy), reads=[bpev], writes=[bqib])
                K.op(act, lambda e: e.activation(out=wi[:], in_=pev[:, 1728:1736], func=AF.Copy), reads=[bpev], writes=[bwi])
                K.op(act, lambda e: e.activation(out=tmp[:], in_=pev[:, 0:1024], func=AF.Square), reads=[bpev], writes=[btmp])
                K.op(act, lambda e: e.activation(out=ksqt[:], in_=pev[:, 1024:1088], func=AF.Square), reads=[bpev], writes=[btmp])
                K.op(dve, lambda e: e.tensor_reduce(out=sq[:, 0:16], in_=tmp[:].rearrange("p (h d) -> p h d", d=64),
                                                    axis=AX.X, op=ALU.add), reads=[btmp], writes=[bsq])
                K.op(dve, lambda e: e.tensor_reduce(out=sq[:, 16:17], in_=ksqt[:], axis=AX.X, op=ALU.add), reads=[btmp], writes=[bsq])
                K.op(dve, lambda e: e.tensor_reduce(out=sq[:, 20:21], in_=sq[:, 0:16], axis=AX.X, op=ALU.max), reads=[bsq], writes=[bsq])
                yield
                for s_ in range(8):
                    K.op(pe, lambda e, s_=s_: e.transpose(out=self.TAb[:, s_ * 128:(s_ + 1) * 128], in_=qb[:, s_ * 128:(s_ + 1) * 128],
                                                           identity=self.ident[:]),
                         reads=[bqb, self.bconst], writes=[self.bTA], sig=(s_ == 7))
                K.op(act, lambda e: e.activation(out=qT[:], in_=self.TAb[:].rearrange("p (a b) -> p a b", a=8), func=AF.Copy),
                     reads=[self.bTA], writes=[bqT])
                K.op(pe, lambda e: e.transpose(out=self.TAb[:, 0:128], in_=qb[:, 1024:1152], identity=self.ident[:]),
                     reads=[bqb, self.bconst], writes=[self.bTA], sig=False)
                for s_ in range(5):
                    K.op(pe, lambda e, s_=s_: e.transpose(out=self.TAb[:, (s_ + 1) * 128:(s_ + 2) * 128], in_=qib[:, s_ * 128:(s_ + 1) * 128],
                                                           identity=self.ident[:]),
                         reads=[bqib, self.bconst], writes=[self.bTA], sig=(s_ == 4))
                K.op(dve, lambda e, i=i: e.tensor_copy(out=kT2[:, i * 128:(i + 1) * 128], in_=self.TAb[:, 0:128]),
                     reads=[self.bTA], writes=[bkT])
                K.op(dve, lambda e: e.tensor_copy(out=qiT[:], in_=self.TAb[:, 128:640].rearrange("p (a b) -> p a b", a=4)),
                     reads=[self.bTA], writes=[bqiT])
                K.op(dve, lambda e, i=i: e.tensor_copy(out=kiT2[:, i * 128:(i + 1) * 128], in_=self.TAb[:, 640:768]),
                     reads=[self.bTA], writes=[bkiT])
                yield
                self.neg_bound(sq[:, 20:21], bsq, sq[:, 16:17], bsq, run, brun, mneg, bmneg, smallb, bsmallb)
                yield
                nblk = (n + 511) // 512
                rc = 0
                for sbk in range(nblk):
                    w = min(512, n - sbk * 512)
                    cs = slice(sbk * 512, sbk * 512 + w)
                    for h in range(8):
                        s_, hf = h % 4, h // 4
                        b = pbank()
                        K.op(pe, lambda e, b=b, s_=s_, hf=hf, cs=cs, w=w: e.matmul(P[b][:, 0:w], lhsT=qiT[hf * 64:(hf + 1) * 64, s_, :],
                                                                                    rhs=kiT2[hf * 64:(hf + 1) * 64, cs], start=True, stop=True),
                             reads=[bqiT, bkiT], writes=[bP[b]])
                        r = rc % 2
                        rc += 1
                        K.op(act, lambda e, b=b, r=r, w=w: e.activation(out=rtmp[r][:, 0:w], in_=P[b][:, 0:w], func=AF.Relu),
                             reads=[bP[b]], writes=[brtmp[r]])
                        if h == 0:
                            K.op(dve, lambda e, r=r, w=w, cs=cs, h=h: e.tensor_scalar(out=score[:, cs], in0=rtmp[r][:, 0:w], scalar1=wi[:, h:h + 1],
                                                                                       scalar2=None, op0=ALU.mult),
                                 reads=[brtmp[r], bwi], writes=[bscore])
                        else:
                            K.op(dve, lambda e, r=r, w=w, cs=cs, h=h: e.scalar_tensor_tensor(out=score[:, cs], in0=rtmp[r][:, 0:w], scalar=wi[:, h:h + 1],
                                                                                              in1=score[:, cs], op0=ALU.mult, op1=ALU.add),
                                 reads=[brtmp[r], bwi, bscore], writes=[bscore])
                    yield

            def stageB(i):
                a = i % 2
                n = (i + 1) * 128
                NM, bNM = NM2[a], bNM2[a]
                lo, mid, cnt, u, t2, d0 = (bis[:, k:k + 1] for k in range(6))
                if n > 256:
                    K.op(dve, lambda e: e.tensor_reduce(out=lo, in_=score[:, 0:n], axis=AX.X, op=ALU.min), reads=[bscore], writes=[bbis])
                    K.op(dve, lambda e: e.tensor_reduce(out=d0, in_=score[:, 0:n], axis=AX.X, op=ALU.max), reads=[bscore], writes=[bbis])
                    K.op(dve, lambda e: e.tensor_tensor(out=d0, in0=d0, in1=lo, op=ALU.subtract), reads=[bbis], writes=[bbis])
                    K.op(dve, lambda e: e.tensor_scalar(out=dk[:], in0=p2[:], scalar1=d0, scalar2=None, op0=ALU.mult), reads=[bbis], writes=[bbis])
                    K.op(dve, lambda e: e.tensor_tensor(out=mid, in0=lo, in1=dk[:, 0:1], op=ALU.add), reads=[bbis], writes=[bbis])
                K.op(pool, lambda e, i=i: e.affine_select(out=score[:, i * 128:(i + 1) * 128], in_=score[:, i * 128:(i + 1) * 128],
                                                          pattern=[[-1, 128]], compare_op=ALU.is_ge, fill=self.fill_reg, base=0, channel_multiplier=1),
                     reads=[bscore, bbis], writes=[bscore])
                if n > 256:
                    for k in range(NBIS):
                        K.op(dve, lambda e: e.tensor_scalar(out=NM[:, 0:n], in0=score[:, 0:n], scalar1=mid, scalar2=0.0, op0=ALU.is_ge,
                                                            op1=ALU.add, accum_out=cnt),
                             reads=[bscore, bbis], writes=[bNM, bbis])
                        K.op(dve, lambda e, k=k: e.tensor_scalar(out=u, in0=cnt, scalar1=255.5, scalar2=dk[:, k:k + 1], op0=ALU.is_ge, op1=ALU.mult),
                             reads=[bbis], writes=[bbis])
                        K.op(dve, lambda e, k=k: e.scalar_tensor_tensor(out=mid, in0=u, scalar=dk[:, k + 1:k + 2], in1=mid, op0=ALU.subtract, op1=ALU.add),
                             reads=[bbis], writes=[bbis])
                        yield
                    K.op(dve, lambda e: e.tensor_tensor(out=lo, in0=mid, in1=dk[:, NBIS:NBIS + 1], op=ALU.subtract), reads=[bbis], writes=[bbis])
                    K.op(dve, lambda e: e.tensor_scalar(out=NM[:, 0:n], in0=score[:, 0:n], scalar1=lo, scalar2=NEG, op0=ALU.is_lt, op1=ALU.mult),
                         reads=[bscore, bbis], writes=[bNM])
                else:
                    K.op(dve, lambda e: e.tensor_scalar(out=NM[:, 0:n], in0=score[:, 0:n], scalar1=-1e29, scalar2=NEG, op0=ALU.is_lt, op1=ALU.mult),
                         reads=[bscore], writes=[bNM])
                yield

            def stageC(i):
                a = i % 2
                n = (i + 1) * 128
                NM, bNM = NM2[a], bNM2[a]
                qT, bqT, mneg, bmneg = qT2[a], bqT2[a], mneg2[a], bmneg2[a]
                K.op(dve, lambda e: e.memset(O[:], 0.0), writes=[bO])
                for c in range(i + 1):
                    pt = c % 2
                    for hg in range(4):
                        hf, s0 = hg // 2, (hg % 2) * 4
                        b = pbank()
                        K.op(pe, lambda e, b=b, c=c: e.matmul(P[b][:], lhsT=NM[:, c * 128:(c + 1) * 128], rhs=self.I4[:].rearrange("p a b -> p (a b)"),
                                                              start=True, stop=False),
                             reads=[bNM, self.bconst], writes=[bP[b]], sig=False)
                        K.op(pe, lambda e, b=b, c=c, hf=hf, s0=s0: e.matmul(P[b][:], lhsT=kT2[hf * 64:(hf + 1) * 64, c * 128:(c + 1) * 128],
                                                                             rhs=qT[hf * 64:(hf + 1) * 64, s0:s0 + 4, :], start=False, stop=True),
                             reads=[bkT, bqT], writes=[bP[b]])
                        K.op(act, lambda e, b=b, pt=pt, hg=hg: e.activation(out=PT[pt][:, hg * 4:(hg + 1) * 4, :],
                                                                            in_=P[b][:].rearrange("p (a b) -> p a b", a=4), func=AF.Exp,
                                                                            bias=mneg[:, 0:1], scale=0.125),
                             reads=[bP[b], bmneg], writes=[bPT[pt]])
                    for hd in range(16):
                        col = (hd // 7) * 512 + (hd % 7) * 65
                        K.op(pe, lambda e, pt=pt, hd=hd, col=col, c=c: e.matmul(O[:, col:col + 65], lhsT=PT[pt][:, hd, :], rhs=Vaug[:, c, :],
                                                                                 start=False, stop=False, skip_group_check=True),
                             reads=[bPT[pt], bV, bO], writes=[bO], sig=(hd == 15))
                    yield
                if self.debug and i == 0:
                    if self.dbg is None:
                        self.dbg = self.nc.dram_tensor("dbg", [128, 4096], F32, kind="ExternalOutput").ap()
                    dsb = sb("dsb", [128, 4096], F32); bd = Buf("dsb"); sd = Slot(K, "dsb")
                    K.op(dve, lambda e: e.memset(dsb[:], 0.0), writes=[bd])
                    K.op(dve, lambda e: e.tensor_copy(out=dsb[:, 0:1536], in_=O[:]), reads=[bO], writes=[bd])
                    K.op(dve, lambda e: e.tensor_copy(out=dsb[:, 1536:1540], in_=mneg[:]), reads=[bmneg], writes=[bd])
                    K.op(dve, lambda e: e.tensor_copy(out=dsb[:, 1600:1600 + 130], in_=Vaug[:, 0:2, :].rearrange("p a b -> p (a b)")), reads=[bV], writes=[bd])
                    K.op(dve, lambda e: e.tensor_copy(out=dsb[:, 2048:4096], in_=PT[0][:].rearrange("p a b -> p (a b)")), reads=[bPT[0]], writes=[bd])
                    K.dma(sp, sd, self.dbg, dsb[:], reads=[bd])
                for bk in range(3):
                    nh = 7 if bk < 2 else 2
                    Ov = O[:, bk * 512: bk * 512 + nh * 65].rearrange("p (h e) -> p h e", e=65)
                    K.op(dve, lambda e, bk=bk, nh=nh, Ov=Ov: e.reciprocal(out=rl[:, bk * 7: bk * 7 + nh], in_=Ov[:, :, 64]),
                         reads=[bO], writes=[brl])
                    K.op(dve, lambda e, bk=bk, nh=nh, Ov=Ov: e.tensor_tensor(
                        out=ob[:, bk * 448: bk * 448 + nh * 64].rearrange("p (h d) -> p h d", d=64), in0=Ov[:, :, 0:64],
                        in1=rl[:, bk * 7: bk * 7 + nh].rearrange("p (h o) -> p h o", o=1).to_broadcast([128, nh, 64]), op=ALU.mult),
                         reads=[bO, brl], writes=[bob])
                yield
                for kc in range(8):
                    K.op(pe, lambda e, kc=kc: e.transpose(out=self.TAb[:, kc * 128:(kc + 1) * 128], in_=ob[:, kc * 128:(kc + 1) * 128],
                                                          identity=self.ident[:]),
                         reads=[bob, self.bconst], writes=[self.bTA], sig=(kc == 7))
                K.op(act, lambda e: e.activation(out=oT[:], in_=self.TAb[:].rearrange("p (a b) -> p a b", a=8), func=AF.Copy),
                     reads=[self.bTA], writes=[boT])
                yb = [pbank(), pbank()]
                for hf in range(2):
                    for kc in range(8):
                        K.op(pe, lambda e, hf=hf, kc=kc, yb=yb: e.matmul(P[yb[hf]][:], lhsT=oT[:, kc, :], rhs=w_out[:, kc, hf * 512:(hf + 1) * 512],
                                                                         start=(kc == 0), stop=(kc == 7)),
                             reads=[boT, bw], writes=[bP[yb[hf]]], sig=(kc == 7))
                self.post_resid(vs, [(P[yb[0]][:], 0, 512), (P[yb[1]][:], 512, 512)], [bP[yb[0]], bP[yb[1]]],
                                xa[a][:], bxa[a], xa[a], bxa[a], small[1], bsmall[1], junk, bjunk, tmp, btmp)
                K.dma(pool, sxo[a], self.out[i * 128:(i + 1) * 128, :], xa[a][:], reads=[bxa[a]], writes=[self.xbuf[i]])
                yield

            self.interleave(self.chain(stageA(0), stageB(0)))
            for i in range(NT):
                nxt = self.chain(stageA(i + 1), stageB(i + 1)) if i + 1 < NT else None
                if PIPE == 0:
                    self.interleave(stageC(i)); self.interleave(nxt)
                elif PIPE == 1:
                    self.interleave(nxt); self.interleave(stageC(i))
                else:
                    self.interleave(stageC(i), nxt)
                if bgq and i % 4 == 3:
                    bgq.pop(0)()
            while bgq:
                bgq.pop(0)()
            K.barrier()
        K.end_scope()

    def diff(self, x_src, vs, jl, bg):
        K, T, NT = self.K, self.T, self.NT
        pe, dve, act, pool, sp = K.pe, K.dve, K.act, K.pool, K.sp
        bgq = list(bg)
        for g in range(2):
            off_in, off_out = WOFF[("bin", jl, g)], WOFF[("bout", jl, g)]
            K.begin_scope()
            with ExitStack() as st:
                sb = lambda n, s, d: self.sb(st, n, s, d)
                w_in = sb("w_in", [128, 8, 1536], BF16); bw = Buf("w"); sw = Slot(K, "w")
                w_out = sb("w_out", [128, 4, D], BF16)
                kT = sb("kT", [128, 4, T], BF16); bkT = Buf("kT")
                Vaug = sb("Vaug", [128, NT, 4, 129], BF16); bV = Buf("Vaug")
                PT = [sb(f"PT{i}", [128, 8, 128], BF16) for i in range(2)]; bPT = [Buf(f"PT{i}") for i in range(2)]
                pev = sb("pev", [128, 1536], F32); bpev = Buf("pev")
                rt = sb("rt", [128, 4, 16, 8], F32); brt = Buf("rt")
                qb = sb("qb", [128, 1024], BF16); bqb = Buf("qb")
                qT2 = [sb(f"qT{i}", [128, 4, 128], BF16) for i in range(2)]; bqT2 = [Buf(f"qT{i}") for i in range(2)]
                smallb = sb("smallb", [128, 8], F32); bsmallb = Buf("smallb")
                sq = sb("sq", [128, 24], F32); bsq = Buf("sq")
                xa = [sb(f"xa{i}", [128, D], F32) for i in range(2)]; bxa = [Buf(f"xa{i}") for i in range(2)]
                sxa = [Slot(K, f"xa{i}") for i in range(2)]
                sxo = [Slot(K, f"xo{i}") for i in range(2)]
                ysb = [sb(f"ysb{i}", [128, D], F32) for i in range(2)]; bysb = [Buf(f"ysb{i}") for i in range(2)]
                sys_ = [Slot(K, f"ysb{i}") for i in range(2)]
                tmp = sb("tmp", [128, D], F32); btmp = Buf("tmp")
                hb = sb("hb", [128, D], BF16); bhb = Buf("hb")
                hT = sb("hT", [128, 8, 128], BF16); bhT = Buf("hT")
                on = sb("on", [128, 8, 128], F32); bon = Buf("on")
                od = sb("od", [128, 4, 128], F32); bod = Buf("od")
                ob = sb("ob", [128, 512], BF16); bob = Buf("ob")
                oT = sb("oT", [128, 4, 128], BF16); boT = Buf("oT")
                junk = sb("junk", [128, D], BF16); bjunk = Buf("junk")
                small = [sb(f"small{i}", [128, 8], F32) for i in range(2)]; bsmall = [Buf(f"small{i}") for i in range(2)]
                run = sb("run", [128, 2], F32); brun = Buf("run")
                mneg2 = [sb(f"mneg{i}", [128, 4], F32) for i in range(2)]; bmneg2 = [Buf(f"mneg{i}") for i in range(2)]
                rl = sb("rl", [128, 8], F32); brl = Buf("rl")
                ssub = sb("ssub", [128, 8], F32); bssub = Buf("ssub")
                P = [self.ps(st, f"P{i}", [128, 512], F32) for i in range(4)]; bP = [Buf(f"P{i}") for i in range(4)]
                O = self.ps(st, "O", [128, 1536], F32); bO = Buf("O")

                K.dma(sp, sw, w_in[:].rearrange("p a b -> p (a b)"), self.wbf[:, off_in:off_in + W_BIN], reads=self.wbf_grp, writes=[bw])
                K.dma(sp, sw, w_out[:].rearrange("p a b -> p (a b)"), self.wbf[:, off_out:off_out + W_BOUT], reads=self.wbf_grp, writes=[bw])
                K.op(pool, lambda e: e.memset(Vaug[:, :, :, 128:129], 1.0), writes=[bV])
                K.op(pool, lambda e: e.memset(run[:], 0.0), writes=[brun])
                pcnt = [0]

                def pbank():
                    b = pcnt[0] % 4
                    pcnt[0] += 1
                    return b

                def stageA(i):
                    a = i % 2
                    qT, bqT, mneg, bmneg = qT2[a], bqT2[a], mneg2[a], bmneg2[a]
                    K.dma(pool, sxa[a], xa[a][:], x_src[i * 128:(i + 1) * 128, :], reads=[self.xbuf[i]], writes=[bxa[a]])
                    if g == 1:
                        K.dma(pool, sys_[a], ysb[a][:], self.ypart[i * 128:(i + 1) * 128, :], reads=[self.ypbuf[i]], writes=[bysb[a]])
                    self.norm_mod_T(vs, xa[a][:], bxa[a], hb, bhb, tmp, btmp, small[0], bsmall[0], junk, bjunk, hT[:], bhT)
                    if YMASK & 1:
                        yield
                    for b in range(3):
                        for kc in range(8):
                            K.op(pe, lambda e, b=b, kc=kc: e.matmul(P[b][:], lhsT=hT[:, kc, :], rhs=w_in[:, kc, b * 512:(b + 1) * 512],
                                                                    start=(kc == 0), stop=(kc == 7)),
                                 reads=[bhT, bw], writes=[bP[b]], sig=(kc == 7))
                        K.op(act, lambda e, b=b: e.activation(out=pev[:, b * 512:(b + 1) * 512], in_=P[b][:], func=AF.Copy),
                             reads=[bP[b]], writes=[bpev])
                    if YMASK & 2:
                        yield
                    self.rope(pev[:], bpev, 0, 16, i, rt, brt)
                    if YMASK & 4:
                        yield
                    K.op(act, lambda e: e.activation(out=qb[:], in_=pev[:, 0:1024], func=AF.Copy), reads=[bpev], writes=[bqb])
                    K.op(act, lambda e, i=i: e.activation(out=Vaug[:, i, :, 0:128], in_=pev[:, 1024:1536].rearrange("p (h d) -> p h d", d=128),
                                                          func=AF.Copy), reads=[bpev], writes=[bV])
                    K.op(act, lambda e: e.activation(out=tmp[:], in_=pev[:, 0:1024], func=AF.Square), reads=[bpev], writes=[btmp])
                    K.op(dve, lambda e: e.tensor_reduce(out=sq[:, 0:16], in_=tmp[:].rearrange("p (h d) -> p h d", d=64), axis=AX.X, op=ALU.add),
                         reads=[btmp], writes=[bsq])
                    K.op(dve, lambda e: e.tensor_reduce(out=sq[:, 20:22], in_=sq[:, 0:16].rearrange("p (a b) -> p a b", a=2), axis=AX.X, op=ALU.max),
                         reads=[bsq], writes=[bsq])
                    if YMASK & 8:
                        yield
                    for s_ in range(8):
                        K.op(pe, lambda e, s_=s_: e.transpose(out=self.TAb[:, s_ * 128:(s_ + 1) * 128], in_=qb[:, s_ * 128:(s_ + 1) * 128],
                                                               identity=self.ident[:]),
                             reads=[bqb, self.bconst], writes=[self.bTA], sig=(s_ == 7))
                    K.op(act, lambda e: e.activation(out=qT[:], in_=self.TAb[:, 0:512].rearrange("p (a b) -> p a b", a=4), func=AF.Copy),
                         reads=[self.bTA], writes=[bqT])
                    K.op(dve, lambda e, i=i: e.tensor_copy(out=kT[:, :, i * 128:(i + 1) * 128],
                                                           in_=self.TAb[:, 512:1024].rearrange("p (a b) -> p a b", a=4)),
                         reads=[self.bTA], writes=[bkT])
                    if YMASK & 16:
                        yield
                    self.neg_bound(sq[:, 20:21], bsq, sq[:, 21:22], bsq, run, brun, mneg, bmneg, smallb, bsmallb)
                    if YMASK & 32:
                        yield

                def stageC(i):
                    a = i % 2
                    qT, bqT, mneg, bmneg = qT2[a], bqT2[a], mneg2[a], bmneg2[a]
                    K.op(dve, lambda e: e.memset(O[:], 0.0), writes=[bO])
                    for c in range(i + 1):
                        pt = c % 2
                        bb = [pbank(), pbank()]
                        if c == i:
                            for bnk in range(2):
                                K.op(pe, lambda e, b=bb[bnk]: e.matmul(P[b][:], lhsT=self.ident[:], rhs=self.CN8[:, 0:4, :].rearrange("p a b -> p (a b)"),
                                                                       start=True, stop=False),
                                     reads=[self.bconst], writes=[bP[bb[bnk]]], sig=False)
                        for ul in range(4):
                            for bnk in range(2):
                                b = bb[bnk]
                                K.op(pe, lambda e, b=b, ul=ul, bnk=bnk, c=c, i=i: e.matmul(
                                    P[b][:, ul * 128:(ul + 1) * 128], lhsT=kT[bnk * 64:(bnk + 1) * 64, ul, c * 128:(c + 1) * 128],
                                    rhs=qT[bnk * 64:(bnk + 1) * 64, ul, :], start=(c != i), stop=(c != i or ul == 3),
                                    skip_group_check=True),
                                     reads=[bkT, bqT], writes=[bP[b]], sig=(ul == 3))
                        for bnk in range(2):
                            b = bb[bnk]
                            K.op(act, lambda e, b=b, pt=pt, bnk=bnk: e.activation(out=PT[pt][:, bnk * 4:(bnk + 1) * 4, :],
                                                                                   in_=P[b][:].rearrange("p (a b) -> p a b", a=4), func=AF.Exp,
                                                                                   bias=mneg[:, 0:1], scale=0.125),
                                 reads=[bP[b], bmneg], writes=[bPT[pt]])
                        for u in range(8):
                            col = (u // 3) * 512 + (u % 3) * 129
                            jx = (u % 2) * 4 + u // 2
                            K.op(pe, lambda e, pt=pt, u=u, jx=jx, col=col, c=c: e.matmul(O[:, col:col + 129], lhsT=PT[pt][:, jx, :], rhs=Vaug[:, c, u % 4, :],
                                                                                          start=False, stop=False, skip_group_check=True),
                                 reads=[bPT[pt], bV, bO], writes=[bO], sig=(u == 7))
                        yield
                    for bk in range(3):
                        nh = 3 if bk < 2 else 2
                        Ov = O[:, bk * 512: bk * 512 + nh * 129].rearrange("p (h e) -> p h e", e=129)
                        K.op(dve, lambda e, bk=bk, nh=nh, Ov=Ov: e.reciprocal(out=rl[:, bk * 3: bk * 3 + nh], in_=Ov[:, :, 128]),
                             reads=[bO], writes=[brl])
                        K.op(dve, lambda e, bk=bk, nh=nh, Ov=Ov: e.tensor_tensor(
                            out=on[:, bk * 3: bk * 3 + nh, :], in0=Ov[:, :, 0:128],
                            in1=rl[:, bk * 3: bk * 3 + nh].rearrange("p (h o) -> p h o", o=1).to_broadcast([128, nh, 128]), op=ALU.mult),
                             reads=[bO, brl], writes=[bon])
                    K.op(dve, lambda e: e.scalar_tensor_tensor(out=od[:].rearrange("p a b -> p (a b)"), in0=on[:, 4:8, :].rearrange("p a b -> p (a b)"),
                                                               scalar=self.neglam[:, jl:jl + 1], in1=on[:, 0:4, :].rearrange("p a b -> p (a b)"),
                                                               op0=ALU.mult, op1=ALU.add),
                         reads=[bon, self.bdiffc], writes=[bod])
                    K.op(act, lambda e: e.activation(out=on[:, 0:4, :], in_=od[:], func=AF.Square), reads=[bod], writes=[bon])
                    K.op(dve, lambda e: e.tensor_reduce(out=ssub[:, 0:4], in_=on[:, 0:4, :], axis=AX.X, op=ALU.add), reads=[bon], writes=[bssub])
                    K.op(pool, lambda e: e.tensor_scalar(out=ssub[:, 4:8], in0=ssub[:, 0:4], scalar1=1.0 / 128.0, scalar2=EPS, op0=ALU.mult, op1=ALU.add),
                         reads=[bssub], writes=[bssub])
                    K.op(pool, lambda e: e.tensor_tensor(out=ssub[:, 4:8], in0=ssub[:, 4:8], in1=self.neghalf[:, 0:4], op=ALU.pow),
                         reads=[bssub, self.bconst], writes=[bssub])
                    K.op(dve, lambda e: e.tensor_tensor(out=od[:], in0=od[:],
                                                        in1=ssub[:, 4:8].rearrange("p (h o) -> p h o", o=1).to_broadcast([128, 4, 128]), op=ALU.mult),
                         reads=[bod, bssub], writes=[bod])
                    K.op(dve, lambda e: e.tensor_tensor(out=ob[:].rearrange("p (h d) -> p h d", d=128), in0=od[:],
                                                        in1=self.sublnb[:, jl:jl + 1, :].to_broadcast([128, 4, 128]), op=ALU.mult),
                         reads=[bod, self.bdiffc], writes=[bob])
                    yield
                    for kc in range(4):
                        K.op(pe, lambda e, kc=kc: e.transpose(out=self.TAb[:, kc * 128:(kc + 1) * 128], in_=ob[:, kc * 128:(kc + 1) * 128],
                                                              identity=self.ident[:]),
                             reads=[bob, self.bconst], writes=[self.bTA], sig=(kc == 3))
                    K.op(act, lambda e: e.activation(out=oT[:], in_=self.TAb[:, 0:512].rearrange("p (a b) -> p a b", a=4), func=AF.Copy),
                         reads=[self.bTA], writes=[boT])
                    yb = [pbank(), pbank()]
                    for hf in range(2):
                        for kc in range(4):
                            K.op(pe, lambda e, hf=hf, kc=kc, yb=yb: e.matmul(P[yb[hf]][:], lhsT=oT[:, kc, :], rhs=w_out[:, kc, hf * 512:(hf + 1) * 512],
                                                                             start=(kc == 0), stop=(kc == 3)),
                                 reads=[boT, bw], writes=[bP[yb[hf]]], sig=(kc == 3))
                    yield
                    if g == 0:
                        for hf in range(2):
                            K.op(act, lambda e, hf=hf, yb=yb, a=a: e.activation(out=ysb[a][:, hf * 512:(hf + 1) * 512], in_=P[yb[hf]][:], func=AF.Copy),
                                 reads=[bP[yb[hf]]], writes=[bysb[a]])
                        K.dma(pool, sys_[a], self.ypart[i * 128:(i + 1) * 128, :], ysb[a][:], reads=[bysb[a]], writes=[self.ypbuf[i]])
                    else:
                        for hf in range(2):
                            K.op(dve, lambda e, hf=hf, yb=yb, a=a: e.tensor_tensor(out=ysb[a][:, hf * 512:(hf + 1) * 512], in0=P[yb[hf]][:],
                                                                                    in1=ysb[a][:, hf * 512:(hf + 1) * 512], op=ALU.add),
                                 reads=[bP[yb[hf]], bysb[a]], writes=[bysb[a]])
                        self.post_resid(vs, [(ysb[a][:], 0, D)], [bysb[a]], xa[a][:], bxa[a], xa[a], bxa[a], small[1], bsmall[1],
                                        junk, bjunk, tmp, btmp)
                        K.dma(pool, sxo[a], self.out[i * 128:(i + 1) * 128, :], xa[a][:], reads=[bxa[a]], writes=[self.xbuf[i]])
                    yield

                self.interleave(stageA(0))
                for i in range(NT):
                    nxt = stageA(i + 1) if i + 1 < NT else None
                    self.interleave(stageC(i), nxt)
                    if bgq and i % 8 == 7:
                        bgq.pop(0)()
                if g == 1:
                    while bgq:
                        bgq.pop(0)()
                K.barrier()
            K.end_scope()


def prep_weights(inp):
    wall = np.zeros((128, NTOT), np.float32)
    for i in range(DEPTH):
        for j in range(2):
            wg = np.zeros((D, FFP), np.float32); wg[:, :DFF] = inp["ffn_w_gate"][i, j]
            wu = np.zeros((D, FFP), np.float32); wu[:, :DFF] = inp["ffn_w_up"][i, j]
            gu = np.stack([wg, wu], 0).reshape(2, 8, 128, 11, 256)
            gu = gu.transpose(2, 3, 0, 1, 4).reshape(128, W_GU)
            o = WOFF[("gu", i, j)]
            wall[:, o:o + W_GU] = gu
            wdn = np.zeros((FFP, D), np.float32); wdn[:DFF] = inp["ffn_w_down"][i, j]
            o = WOFF[("d", i, j)]
            wall[:, o:o + W_D] = wdn.reshape(NF, 128, D).transpose(1, 0, 2).reshape(128, W_D)
    qperm = []
    for s in range(8):
        qperm += list(range(s * 64, s * 64 + 64)) + list(range((s + 8) * 64, (s + 8) * 64 + 64))
    qiperm = []
    for s in range(4):
        qiperm += list(range(1152 + s * 64, 1152 + s * 64 + 64)) + list(range(1152 + (s + 4) * 64, 1152 + (s + 4) * 64 + 64))
    aperm = np.array(qperm + list(range(1024, 1152)) + qiperm + list(range(1664, 1736)))
    for j in range(2):
        w = inp["dsa_w_in"][j][:, aperm]
        o = WOFF[("ain", j)]
        wall[:, o:o + W_AIN] = w.reshape(8, 128, A_IN).transpose(1, 0, 2).reshape(128, W_AIN)
        o = WOFF[("aout", j)]
        wall[:, o:o + W_AOUT] = inp["dsa_w_out"][j].reshape(8, 128, D).transpose(1, 0, 2).reshape(128, W_AOUT)
    for j in range(2):
        for g in range(2):
            cols = []
            for base in (0, 512, 1024, 1536):
                cols += list(range(base + g * 256, base + g * 256 + 256))
            cols += list(range(2048 + g * 512, 2048 + g * 512 + 512))
            w = inp["diff_w_in"][j][:, np.array(cols)]
            o = WOFF[("bin", j, g)]
            wall[:, o:o + W_BIN] = w.reshape(8, 128, 1536).transpose(1, 0, 2).reshape(128, W_BIN)
            wo = inp["diff_w_out"][j][g * 512:(g + 1) * 512]
            o = WOFF[("bout", j, g)]
            wall[:, o:o + W_BOUT] = wo.reshape(4, 128, D).transpose(1, 0, 2).reshape(128, W_BOUT)
    return wall


def prep_ada(inp):
    aw = np.asarray(inp["ada_w"], np.float32)
    a = aw.reshape(4, 4, 2, 128, 3, 6, 512)
    a = a.transpose(0, 4, 5, 1, 3, 2, 6)
    ada = np.ascontiguousarray(a).reshape(12 * 24, 128, 1024)
    adab = np.ascontiguousarray(np.asarray(inp["ada_b"], np.float32).reshape(1, 12 * 3072))
    return ada, adab


def make_in_maps(inp, T, ncores):
    wall = prep_weights(inp)
    ada, adab = prep_ada(inp)
    pre_n = np.ascontiguousarray(np.asarray(inp["pre_norm"], np.float32).reshape(12, D))
    post_n = np.ascontiguousarray(np.asarray(inp["post_norm"], np.float32).reshape(12, D))
    subln = np.ascontiguousarray(np.asarray(inp["diff_subln"], np.float32))
    lam = np.ascontiguousarray(np.asarray(inp["diff_lambda"], np.float32).reshape(2, 256))
    maps = []
    for b in range(ncores):
        maps.append({
            "x": np.ascontiguousarray(np.asarray(inp["x"][b, :T], np.float32)),
            "c": np.ascontiguousarray(np.asarray(inp["c"][b], np.float32).reshape(8, 128).T),
            "pos": np.ascontiguousarray(np.asarray(inp["positions"][b, :T], np.int32).reshape(T // 128, 128).T),
            "wall": wall, "ada": ada, "adab": adab, "pre_n": pre_n, "post_n": post_n, "subln": subln, "lam": lam,
        })
    return maps


ALL_SUBL = [(li, k) for li in range(DEPTH) for k in ("f0", "mix", "f1")]


def kernel(**inputs):
    T = 4096
    prog = Prog(T, ALL_SUBL)
    nc = prog.build()
    maps = make_in_maps(inputs, T, 8)
    res = run_bass_kernel_spmd(nc, maps, core_ids=list(range(8)))
    return np.stack([np.asarray(r["out"], np.float32) for r in res.results], 0)
```

```python
import math
from contextlib import ExitStack

import numpy as np
import concourse.bass as bass
import concourse.mybir as mybir
from concourse.bass_utils import run_bass_kernel_spmd

F32 = mybir.dt.float32
BF16 = mybir.dt.bfloat16
I32 = mybir.dt.int32
AF = mybir.ActivationFunctionType
ALU = mybir.AluOpType
AX = mybir.AxisListType

D = 1024
DFF = 2752
FFP = 2816
NF = 22
DEPTH = 4
EPS = 1e-6
A_IN = 1736
NEG = -30000.0
SEM_LIMIT = 12000
NBIS = 24
STOP = 99
PIPE = 2
YMASK = 0

W_GU = 11 * 2 * 8 * 256
W_D = NF * 1024
W_AIN = 8 * A_IN
W_AOUT = 8 * 1024
W_BIN = 8 * 1536
W_BOUT = 4 * 1024


def weight_offsets():
    off = {}
    o = 0
    for i in range(DEPTH):
        for j in range(2):
            off[("gu", i, j)] = o; o += W_GU
            off[("d", i, j)] = o; o += W_D
    for j in range(2):
        off[("ain", j)] = o; o += W_AIN
        off[("aout", j)] = o; o += W_AOUT
    for j in range(2):
        for g in range(2):
            off[("bin", j, g)] = o; o += W_BIN
            off[("bout", j, g)] = o; o += W_BOUT
    return off, o


WOFF, NTOT = weight_offsets()
CASTW = 4096
assert NTOT % CASTW == 0 or True


class Ev:
    __slots__ = ("sem", "val", "eng")

    def __init__(self, eng=None):
        self.sem = None
        self.val = 0
        self.eng = eng


class Buf:
    __slots__ = ("name", "w", "r")

    def __init__(self, name):
        self.name = name
        self.w = None
        self.r = {}


class Eng:
    def __init__(self, K, name, h):
        self.K = K
        self.name = name
        self.h = h
        self.sem = None
        self.cnt = 0
        self.seen = {}
        self.pending = []
        self.last = None
        self.n = 0


class _Slot:
    def __init__(self, K, name):
        self.sem = K.new_sem("d_" + name)
        self.cnt = 0
        self.name = name
        self.last = None
        K.slots.append(self)


def Slot(K, name):
    if K.free_slots:
        s = K.free_slots.pop()
    else:
        s = _Slot(K, name)
        s.name = f"s{len(K.slots)}"
    K.scope_slots.append(s)
    return s


class Kern:
    def __init__(self, nc):
        self.nc = nc
        self.es = ExitStack()
        self.nsem = 0
        self.slots = []
        self.pe = Eng(self, "pe", nc.tensor)
        self.act = Eng(self, "act", nc.scalar)
        self.dve = Eng(self, "dve", nc.vector)
        self.pool = Eng(self, "pool", nc.gpsimd)
        self.sp = Eng(self, "sp", nc.sync)
        self.engs = [self.pe, self.act, self.dve, self.pool, self.sp]
        self.ninstr = 0
        self.free_slots = []
        self.scope_slots = []

    def begin_scope(self):
        self._saved = self.scope_slots
        self.scope_slots = []

    def end_scope(self):
        self.free_slots.extend(self.scope_slots)
        self.scope_slots = self._saved

    def new_sem(self, name):
        self.nsem += 1
        return self.es.enter_context(self.nc.semaphore(f"{name}_{self.nsem}"))

    def _need(self, eng, ev, raw=False):
        if ev is None:
            return
        if ev.eng is eng and (not raw or eng is self.pe):
            return
        assert ev.sem is not None, "dependency on unsignaled instruction"
        k = id(ev.sem)
        if eng.seen.get(k, 0) >= ev.val:
            return
        eng.h.wait_ge(ev.sem, ev.val)
        eng.seen[k] = ev.val

    def _waits(self, eng, reads, writes):
        for b in reads:
            self._need(eng, b.w, raw=True)
        for b in writes:
            self._need(eng, b.w)
            for ev in b.r.values():
                self._need(eng, ev)

    def _record(self, ev, key, reads, writes):
        for b in writes:
            b.w = ev
            b.r = {}
        for b in reads:
            b.r[key] = ev

    def op(self, eng, fn, reads=(), writes=(), sig=True):
        self._waits(eng, reads, writes)
        ins = fn(eng.h)
        ev = Ev(eng)
        eng.n += 1
        self.ninstr += 1
        if sig:
            if eng.sem is None or eng.cnt >= SEM_LIMIT:
                eng.sem = self.new_sem(eng.name)
                eng.cnt = 0
            eng.cnt += 1
            ins.then_inc(eng.sem, 1)
            ev.sem, ev.val = eng.sem, eng.cnt
            for p in eng.pending:
                p.sem, p.val = eng.sem, eng.cnt
            eng.pending = []
            eng.last = ev
        else:
            eng.pending.append(ev)
        self._record(ev, eng.name, reads, writes)
        return ev

    def dma(self, q, slot, out, in_, reads=(), writes=()):
        self._waits(q, reads, writes)
        ins = q.h.dma_start(out=out, in_=in_)
        slot.cnt += 16
        ins.then_inc(slot.sem, 16)
        ev = Ev(None)
        ev.sem, ev.val = slot.sem, slot.cnt
        slot.last = ev
        self.ninstr += 1
        self._record(ev, "dma_" + slot.name, reads, writes)
        return ev

    def barrier(self):
        evs = []
        for e in self.engs:
            assert not e.pending, f"barrier with pending unsignaled instrs on {e.name}"
            if e.last is not None:
                evs.append(e.last)
        for s in self.slots:
            if s.last is not None:
                evs.append(s.last)
        for e in self.engs:
            for ev in evs:
                self._need(e, ev)


class Prog:
    def __init__(self, T, sublayers, do_cast=True):
        self.T = T
        self.NT = T // 128
        self.subl = sublayers
        self.do_cast = do_cast
        nc = bass.Bass("TRN2", target_bir_lowering=False)
        self.nc = nc
        self.K = Kern(nc)
        K = self.K
        NT = self.NT
        dt = nc.dram_tensor
        self.x_in = dt("x", [T, D], F32, kind="ExternalInput").ap()
        self.c_in = dt("c", [128, 8], F32, kind="ExternalInput").ap()
        self.pos_in = dt("pos", [128, NT], I32, kind="ExternalInput").ap()
        self.wall = dt("wall", [128, NTOT], F32, kind="ExternalInput").ap()
        self.ada = dt("ada", [12 * 24, 128, 1024], F32, kind="ExternalInput").ap()
        self.adab = dt("adab", [1, 12 * 3072], F32, kind="ExternalInput").ap()
        self.pre_n = dt("pre_n", [12, D], F32, kind="ExternalInput").ap()
        self.post_n = dt("post_n", [12, D], F32, kind="ExternalInput").ap()
        self.subln = dt("subln", [2, 128], F32, kind="ExternalInput").ap()
        self.lam = dt("lam", [2, 256], F32, kind="ExternalInput").ap()
        self.out = dt("out", [T, D], F32, kind="ExternalOutput").ap()
        self.debug = False
        self.dbg = None
        self.wbf = dt("wbf", [128, NTOT], BF16, kind="Internal").ap()
        self.ypart = dt("ypart", [T, D], F32, kind="Internal").ap()
        self.xbuf = [Buf(f"xd{i}") for i in range(NT)]
        self.ypbuf = [Buf(f"yp{i}") for i in range(NT)]
        self.wbf_buf = Buf("wbf")
        self.cast_pieces = []
        self.cast_slots = None
        self.gs = ExitStack()
        self._names = 0

    def sb(self, st, name, shape, dtype):
        self._names += 1
        return st.enter_context(self.nc.sbuf_tensor(f"{name}_{self._names}", shape, dtype))

    def ps(self, st, name, shape, dtype):
        self._names += 1
        return st.enter_context(self.nc.psum_tensor(f"{name}_{self._names}", shape, dtype))

    def setup_globals(self):
        K, nc, st, NT = self.K, self.nc, self.gs, self.NT
        sb = lambda n, s, d: self.sb(st, n, s, d)
        self.TA = self.ps(st, "TA", [128, 512], F32)
        self.TAb = self.TA[:].bitcast(BF16)
        self.bTA = Buf("TA")
        self.identf = sb("identf", [128, 128], F32)
        self.ident = sb("ident", [128, 128], BF16)
        self.I4 = sb("I4", [128, 4, 128], BF16)
        self.CN8 = sb("CN8", [128, 8, 128], BF16)
        self.ones_row = sb("ones_row", [1, 128], F32)
        self.neghalf = sb("neghalf", [128, 16], F32)
        self.half = sb("half", [128, 16], F32)
        self.bconst = Buf("consts")
        self.cosT = sb("cosT", [128, NT, 8], F32)
        self.sinT = sb("sinT", [128, NT, 8], F32)
        self.brope = Buf("rope")
        self.condrep = sb("condrep", [128, 8, 128], F32)
        self.bcond = Buf("cond")
        self.vecA = [sb(f"vA{i}", [128, D], F32) for i in range(2)]
        self.vecS = [sb(f"vS{i}", [128, D], F32) for i in range(2)]
        self.vecG = [sb(f"vG{i}", [128, D], F32) for i in range(2)]
        self.bvec = [Buf(f"vec{i}") for i in range(2)]
        self.adaring = [sb(f"adar{i}", [128, 2, 512], F32) for i in range(2)]
        self.badar = [Buf(f"adar{i}") for i in range(2)]
        self.sadar = [Slot(K, f"adar{i}") for i in range(2)]
        self.pgb = [sb(f"pgb{i}", [128, D], F32) for i in range(2)]
        self.bpgb = [Buf(f"pgb{i}") for i in range(2)]
        self.spgb = [Slot(K, f"pgb{i}") for i in range(2)]
        self.brow = sb("brow", [1, 512], F32)
        self.bbrow = Buf("brow")
        self.sbrow = Slot(K, "brow")
        self.neglam = sb("neglam", [128, 2], F32)
        self.sublnb = sb("sublnb", [128, 2, 128], F32)
        self.bdiffc = Buf("diffc")
        self.adacnt = 0

        pool, dve, act = K.pool, K.dve, K.act
        bc = self.bconst
        K.op(pool, lambda e: e.memset(self.identf[:], 0.0), writes=[bc])
        K.op(pool, lambda e: e.affine_select(out=self.identf[:], in_=self.identf[:], pattern=[[-1, 128]],
                                             compare_op=ALU.not_equal, fill=1.0, base=0, channel_multiplier=1),
             reads=[bc], writes=[bc])
        K.op(pool, lambda e: e.tensor_copy(out=self.ident[:], in_=self.identf[:]), reads=[bc], writes=[bc])
        for r in range(4):
            K.op(pool, lambda e, r=r: e.tensor_copy(out=self.I4[:, r, :], in_=self.identf[:]), reads=[bc], writes=[bc])
        K.op(pool, lambda e: e.memset(self.CN8[:], 0.0), writes=[bc])
        K.op(pool, lambda e: e.affine_select(out=self.CN8[:], in_=self.CN8[:], pattern=[[0, 8], [1, 128]],
                                             compare_op=ALU.is_ge, fill=NEG, base=0, channel_multiplier=-1),
             reads=[bc], writes=[bc])
        K.op(pool, lambda e: e.memset(self.ones_row[:], 1.0), writes=[bc])
        K.op(pool, lambda e: e.memset(self.neghalf[:], -0.5), writes=[bc])
        K.op(pool, lambda e: e.memset(self.half[:], 0.5), writes=[bc])

        with ExitStack() as ts:
            tsb = lambda n, s, d: self.sb(ts, n, s, d)
            c_sb = tsb("c_sb", [128, 8], F32)
            cond = tsb("cond", [128, 8], F32)
            b_c = Buf("c_sb")
            s_c = Slot(K, "c_sb")
            K.dma(K.sp, s_c, c_sb[:], self.c_in, writes=[b_c])
            K.op(act, lambda e: e.activation(out=cond[:], in_=c_sb[:], func=AF.Silu), reads=[b_c], writes=[self.bcond])
            zer = tsb("zer", [128, 128], F32)
            K.op(dve, lambda e: e.memset(zer[:], 0.0), writes=[b_c])
            for kc in range(8):
                K.op(dve, lambda e, kc=kc: e.tensor_scalar(out=self.condrep[:, kc, :], in0=zer[:],
                                                           scalar1=cond[:, kc:kc + 1], scalar2=None, op0=ALU.add),
                     reads=[b_c, self.bcond], writes=[self.bcond])
            pos_i = tsb("pos_i", [128, NT], I32)
            pos_f = tsb("pos_f", [128, NT], F32)
            invt = tsb("invt", [128, 8], F32)
            ang = tsb("ang", [128, NT, 8], F32)
            ang2 = tsb("ang2", [128, NT, 8], F32)
            b_p = Buf("pos")
            s_p = Slot(K, "pos")
            K.dma(K.sp, s_p, pos_i[:], self.pos_in, writes=[b_p])
            K.op(dve, lambda e: e.tensor_copy(out=pos_f[:], in_=pos_i[:]), reads=[b_p], writes=[b_p])
            for j in range(8):
                inv = float(np.float32(500000.0) ** np.float32(-(2.0 * j) / 16.0))
                K.op(dve, lambda e, j=j, inv=inv: e.memset(invt[:, j:j + 1], inv), writes=[b_p])
            K.op(dve, lambda e: e.tensor_tensor(out=ang[:], in0=pos_f[:].rearrange("p (n o) -> p n o", o=1).to_broadcast([128, NT, 8]),
                                                in1=invt[:].rearrange("p (o j) -> p o j", o=1).to_broadcast([128, NT, 8]),
                                                op=ALU.mult), reads=[b_p], writes=[b_p])
            TWO_PI = 2.0 * math.pi
            C1 = 6.28125
            C2 = TWO_PI - C1
            kf = tsb("kf", [128, NT, 8], F32)
            ki = tsb("ki", [128, NT, 8], I32)
            mm_ = tsb("mm_", [128, NT, 8], F32)

            def sin_of(dst, shift):
                K.op(dve, lambda e: e.tensor_scalar(out=ang2[:], in0=ang[:], scalar1=shift, scalar2=None, op0=ALU.add),
                     reads=[b_p, self.brope], writes=[b_p])
                K.op(dve, lambda e: e.tensor_scalar(out=kf[:], in0=ang2[:], scalar1=1.0 / TWO_PI, scalar2=None, op0=ALU.mult),
                     reads=[b_p], writes=[b_p])
                K.op(dve, lambda e: e.tensor_copy(out=ki[:], in_=kf[:]), reads=[b_p], writes=[b_p])
                K.op(dve, lambda e: e.tensor_copy(out=kf[:], in_=ki[:]), reads=[b_p], writes=[b_p])
                K.op(dve, lambda e: e.scalar_tensor_tensor(out=ang2[:], in0=kf[:], scalar=-C1, in1=ang2[:], op0=ALU.mult, op1=ALU.add),
                     reads=[b_p], writes=[b_p])
                K.op(dve, lambda e: e.scalar_tensor_tensor(out=ang2[:], in0=kf[:], scalar=-C2, in1=ang2[:], op0=ALU.mult, op1=ALU.add),
                     reads=[b_p], writes=[b_p])
                K.op(dve, lambda e: e.tensor_scalar(out=mm_[:], in0=ang2[:], scalar1=math.pi, scalar2=TWO_PI, op0=ALU.is_gt, op1=ALU.mult),
                     reads=[b_p], writes=[b_p])
                K.op(dve, lambda e: e.tensor_tensor(out=ang2[:], in0=ang2[:], in1=mm_[:], op=ALU.subtract), reads=[b_p], writes=[b_p])
                K.op(dve, lambda e: e.tensor_scalar(out=mm_[:], in0=ang2[:], scalar1=-math.pi, scalar2=TWO_PI, op0=ALU.is_lt, op1=ALU.mult),
                     reads=[b_p], writes=[b_p])
                K.op(dve, lambda e: e.tensor_tensor(out=ang2[:], in0=ang2[:], in1=mm_[:], op=ALU.add), reads=[b_p], writes=[b_p])
                K.op(act, lambda e: e.activation(out=dst[:], in_=ang2[:], func=AF.Sin), reads=[b_p], writes=[self.brope])

            sin_of(self.sinT, 0.0)
            sin_of(self.cosT, 0.5 * math.pi)
            lam_sb = tsb("lam_sb", [128, 2, 4, 64], F32)
            sub_sb = tsb("sub_sb", [128, 2, 128], F32)
            lp = tsb("lp", [128, 2, 2, 64], F32)
            ls = tsb("ls", [128, 4], F32)
            b_l = Buf("lam")
            s_l = Slot(K, "lam")
            s_l2 = Slot(K, "lam2")
            K.dma(K.sp, s_l, lam_sb[:].rearrange("p a b c -> p (a b c)"),
                  self.lam.rearrange("a b -> (a b)").partition_broadcast(128), writes=[b_l])
            b_l2 = Buf("sub")
            K.dma(K.sp, s_l2, sub_sb[:].rearrange("p a b -> p (a b)"),
                  self.subln.rearrange("a b -> (a b)").partition_broadcast(128), writes=[b_l2])
            K.op(dve, lambda e: e.tensor_tensor(out=lp[:], in0=lam_sb[:, :, 0:4:2, :], in1=lam_sb[:, :, 1:4:2, :],
                                                op=ALU.mult), reads=[b_l], writes=[b_l])
            K.op(dve, lambda e: e.tensor_reduce(out=ls[:], in_=lp[:].rearrange("p a b c -> p (a b) c"), axis=AX.X,
                                                op=ALU.add), reads=[b_l], writes=[b_l])
            K.op(act, lambda e: e.activation(out=ls[:], in_=ls[:], func=AF.Exp), reads=[b_l], writes=[b_l])
            for j in range(2):
                li = 0.8 - 0.6 * math.exp(-0.3 * (2 * j + 1))
                K.op(dve, lambda e, j=j, li=li: e.scalar_tensor_tensor(out=self.neglam[:, j:j + 1], in0=ls[:, 2 * j + 1:2 * j + 2],
                                                                       scalar=-li, in1=ls[:, 2 * j:2 * j + 1],
                                                                       op0=ALU.add, op1=ALU.subtract),
                     reads=[b_l], writes=[self.bdiffc])
                K.op(dve, lambda e, j=j, li=li: e.tensor_scalar(out=self.sublnb[:, j, :], in0=sub_sb[:, j, :],
                                                                scalar1=1.0 - li, scalar2=None, op0=ALU.mult),
                     reads=[b_l2], writes=[self.bdiffc])
            K.barrier()

    def cast_plan(self, ranges):
        CW = 8192
        self.wgrp = [[Buf(f"wgrp{g}_{r}") for r in range(3)] for g in range(len(ranges))]
        self.cast_ring = [Buf(f"castring{r}") for r in range(3)]
        self.cast_n = 0
        for g, (s0, ln) in enumerate(ranges):
            o = 0
            while o < ln:
                w = min(CW, ln - o)
                self.cast_pieces.append((s0 + o, w, g))
                o += w
        self.cast_slots = [Slot(self.K, f"cast{i}") for i in range(3)]

    def cast_bg(self, k=1, upto_group=None):
        K = self.K
        while self.cast_pieces and (k > 0 or (upto_group is not None and self.cast_pieces[0][2] <= upto_group)):
            c0, w, g = self.cast_pieces.pop(0)
            r = self.cast_n % 3
            self.cast_n += 1
            K.dma(K.pool, self.cast_slots[r], self.wbf[:, c0:c0 + w], self.wall[:, c0:c0 + w],
                  writes=[self.wgrp[g][r], self.cast_ring[r]])
            k -= 1

    def cast_weights(self, ranges):
        K = self.K
        K.begin_scope()
        with ExitStack() as ts:
            NB = 3
            fin = [self.sb(ts, f"cin{i}", [128, CASTW], F32) for i in range(NB)]
            fout = [self.sb(ts, f"cout{i}", [128, CASTW], BF16) for i in range(NB)]
            bin_ = [Buf(f"cin{i}") for i in range(NB)]
            bout = [Buf(f"cout{i}") for i in range(NB)]
            sin_ = [Slot(K, f"cin{i}") for i in range(NB)]
            sout = [Slot(K, f"cout{i}") for i in range(NB)]
            pieces = []
            for (s0, ln) in ranges:
                o = 0
                while o < ln:
                    w = min(CASTW, ln - o)
                    pieces.append((s0 + o, w))
                    o += w
            engs = [K.dve, K.pool, K.act]
            for n, (c0, w) in enumerate(pieces):
                i = n % NB
                K.dma(K.sp, sin_[i], fin[i][:, 0:w], self.wall[:, c0:c0 + w], writes=[bin_[i]])
                eng = engs[n % 3]
                if eng is K.act:
                    K.op(eng, lambda e, i=i, w=w: e.activation(out=fout[i][:, 0:w], in_=fin[i][:, 0:w], func=AF.Copy),
                         reads=[bin_[i]], writes=[bout[i]])
                else:
                    K.op(eng, lambda e, i=i, w=w: e.tensor_copy(out=fout[i][:, 0:w], in_=fin[i][:, 0:w]),
                         reads=[bin_[i]], writes=[bout[i]])
                K.dma(K.sp, sout[i], self.wbf[:, c0:c0 + w], fout[i][:, 0:w], reads=[bout[i]], writes=[self.wbf_buf])
            K.barrier()
        K.end_scope()

    def ada_steps(self, s_glob, vs, coef):
        K = self.K
        pe, dve, act = K.pe, K.dve, K.act

        def load_pg():
            K.dma(K.sp, self.spgb[0], self.pgb[0][:], self.pre_n[s_glob, :].partition_broadcast(128), writes=[self.bpgb[0]])
            K.dma(K.sp, self.spgb[1], self.pgb[1][:], self.post_n[s_glob, :].partition_broadcast(128), writes=[self.bpgb[1]])

        def step(c):
            if c == 0:
                load_pg()
            K.dma(K.sp, self.sbrow, self.brow[:], self.adab[0:1, s_glob * 3072 + c * 512: s_glob * 3072 + (c + 1) * 512],
                  writes=[self.bbrow])
            for kh in range(4):
                r = self.adacnt % 2
                self.adacnt += 1
                K.dma(K.sp, self.sadar[r], self.adaring[r][:].rearrange("p a b -> p (a b)"),
                      self.ada[s_glob * 24 + c * 4 + kh], writes=[self.badar[r]])
                for kl in range(2):
                    kc = kh * 2 + kl
                    K.op(pe, lambda e, r=r, kl=kl, kc=kc: e.matmul(self.TA[:], lhsT=self.condrep[:, kc, :], rhs=self.adaring[r][:, kl, :],
                                                                    start=(kc == 0), stop=False),
                         reads=[self.bcond, self.badar[r]], writes=[self.bTA], sig=(kl == 1))
            K.op(pe, lambda e: e.matmul(self.TA[:], lhsT=self.ones_row[0:1, :], rhs=self.brow[0:1, :], start=False, stop=True),
                 reads=[self.bconst, self.bbrow], writes=[self.bTA])
            cs = slice((c % 2) * 512, (c % 2) * 512 + 512)
            if c < 2:
                K.op(act, lambda e: e.activation(out=self.vecS[vs][:, cs], in_=self.TA[:], func=AF.Copy),
                     reads=[self.bTA], writes=[self.bvec[vs]])
            elif c < 4:
                K.op(dve, lambda e: e.scalar_tensor_tensor(out=self.vecA[vs][:, cs], in0=self.TA[:], scalar=1.0,
                                                           in1=self.pgb[0][:, cs], op0=ALU.add, op1=ALU.mult),
                     reads=[self.bTA, self.bpgb[0]], writes=[self.bvec[vs]])
            else:
                K.op(dve, lambda e: e.scalar_tensor_tensor(out=self.vecG[vs][:, cs], in0=self.TA[:], scalar=coef,
                                                           in1=self.pgb[1][:, cs], op0=ALU.mult, op1=ALU.mult),
                     reads=[self.bTA, self.bpgb[1]], writes=[self.bvec[vs]])

        return [lambda c=c: step(c) for c in range(6)]

    def norm_mod_T(self, vs, xt, bx, hb, bhb, tmp, btmp, small, bsmall, junk, bjunk, hT_out, bhT):
        K = self.K
        pe, dve, act, pool = K.pe, K.dve, K.act, K.pool
        ss = small[:, 0:1]
        rs = small[:, 1:2]
        K.op(act, lambda e: e.activation(out=junk[:], in_=xt, func=AF.Square, accum_out=ss), reads=[bx], writes=[bjunk, bsmall])
        K.op(pool, lambda e: e.tensor_scalar(out=rs, in0=ss, scalar1=1.0 / D, scalar2=EPS, op0=ALU.mult, op1=ALU.add),
             reads=[bsmall], writes=[bsmall])
        K.op(pool, lambda e: e.tensor_tensor(out=rs, in0=rs, in1=self.neghalf[:, 0:1], op=ALU.pow),
             reads=[bsmall, self.bconst], writes=[bsmall])
        K.op(dve, lambda e: e.scalar_tensor_tensor(out=tmp[:], in0=xt, scalar=rs, in1=self.vecA[vs][:], op0=ALU.mult, op1=ALU.mult),
             reads=[bx, bsmall, self.bvec[vs]], writes=[btmp])
        K.op(dve, lambda e: e.tensor_tensor(out=hb[:], in0=tmp[:], in1=self.vecS[vs][:], op=ALU.add),
             reads=[btmp, self.bvec[vs]], writes=[bhb])
        for kc in range(8):
            K.op(pe, lambda e, kc=kc: e.transpose(out=self.TAb[:, kc * 128:(kc + 1) * 128], in_=hb[:, kc * 128:(kc + 1) * 128],
                                                  identity=self.ident[:]),
                 reads=[bhb, self.bconst], writes=[self.bTA], sig=(kc == 7))
        K.op(act, lambda e: e.activation(out=hT_out, in_=self.TAb[:].rearrange("p (a b) -> p a b", a=8), func=AF.Copy),
             reads=[self.bTA], writes=[bhT])

    def post_resid(self, vs, y_srcs, by, xt, bx, xo, bxo, small, bsmall, junk, bjunk, tmp, btmp):
        K = self.K
        dve, act, pool = K.dve, K.act, K.pool
        n = len(y_srcs)
        for k, (yap, c0, w) in enumerate(y_srcs):
            K.op(act, lambda e, yap=yap, c0=c0, w=w, k=k: e.activation(out=junk[:, c0:c0 + w], in_=yap, func=AF.Square,
                                                                        accum_out=small[:, 4 + k:5 + k]),
                 reads=by, writes=[bjunk, bsmall])
        rs = small[:, 3:4]
        if n == 2:
            K.op(pool, lambda e: e.tensor_tensor(out=rs, in0=small[:, 4:5], in1=small[:, 5:6], op=ALU.add),
                 reads=[bsmall], writes=[bsmall])
            src = rs
        else:
            src = small[:, 4:5]
        K.op(pool, lambda e: e.tensor_scalar(out=rs, in0=src, scalar1=1.0 / D, scalar2=EPS, op0=ALU.mult, op1=ALU.add),
             reads=[bsmall], writes=[bsmall])
        K.op(pool, lambda e: e.tensor_tensor(out=rs, in0=rs, in1=self.neghalf[:, 0:1], op=ALU.pow),
             reads=[bsmall, self.bconst], writes=[bsmall])
        for (yap, c0, w) in y_srcs:
            K.op(dve, lambda e, yap=yap, c0=c0, w=w: e.scalar_tensor_tensor(out=tmp[:, c0:c0 + w], in0=yap, scalar=rs,
                                                                             in1=self.vecG[vs][:, c0:c0 + w],
                                                                             op0=ALU.mult, op1=ALU.mult),
                 reads=list(by) + [bsmall, self.bvec[vs]], writes=[btmp])
        K.op(dve, lambda e: e.tensor_tensor(out=xo[:], in0=tmp[:], in1=xt, op=ALU.add), reads=[btmp, bx], writes=[bxo])

    def ffn(self, x_src, vs, off_gu, off_d, bg):
        K, T, NT = self.K, self.T, self.NT
        pe, dve, act, pool, sp = K.pe, K.dve, K.act, K.pool, K.sp
        NTT = T // 512
        K.begin_scope()
        with ExitStack() as st:
            sb = lambda n, s, d: self.sb(st, n, s, d)
            NG = 4
            wgu = [sb(f"wgu{i}", [128, 2, 8, 256], BF16) for i in range(NG)]
            bwgu = [Buf(f"wgu{i}") for i in range(NG)]
            swgu = [Slot(K, f"wgu{i}") for i in range(NG)]
            wd = sb("wd", [128, NF, 1024], BF16)
            bwd = Buf("wd")
            swd = Slot(K, "wd")
            xa = [sb(f"xa{i}", [128, D], F32) for i in range(2)]
            bxa = [Buf(f"xa{i}") for i in range(2)]
            sxa = [Slot(K, f"xa{i}") for i in range(2)]
            xc = [sb(f"xc{i}", [128, D], F32) for i in range(2)]
            bxc = [Buf(f"xc{i}") for i in range(2)]
            sxc = [Slot(K, f"xc{i}") for i in range(2)]
            xo = [sb(f"xo{i}", [128, D], F32) for i in range(2)]
            bxo = [Buf(f"xo{i}") for i in range(2)]
            sxo = [Slot(K, f"xo{i}") for i in range(2)]
            tmp = sb("tmp", [128, D], F32)
            btmp = Buf("tmp")
            hb = [sb(f"hb{i}", [128, D], BF16) for i in range(2)]
            bhb = [Buf(f"hb{i}") for i in range(2)]
            hT = [sb(f"hT{i}", [128, 8, 512], BF16) for i in range(2)]
            bhT = [Buf(f"hT{i}") for i in range(2)]
            actT = sb("actT", [128, NF, 512], BF16)
            bactT = Buf("actT")
            gt = [sb(f"gt{i}", [128, 512], F32) for i in range(2)]
            bgt = [Buf(f"gt{i}") for i in range(2)]
            junk = sb("junk", [128, D], BF16)
            bjunk = Buf("junk")
            small = [sb(f"small{i}", [128, 8], F32) for i in range(2)]
            bsmall = [Buf(f"small{i}") for i in range(2)]
            GU = [self.ps(st, f"GU{i}", [128, 512], F32) for i in range(3)]
            bGU = [Buf(f"GU{i}") for i in range(3)]
            Y = [self.ps(st, f"Y{i}", [128, 512], F32) for i in range(3)]
            bY = [Buf(f"Y{i}") for i in range(3)]

            wcols = self.wbf
            for q4 in range(2):
                K.dma(sp, swd, wd[:, q4 * 11:(q4 + 1) * 11, :].rearrange("p a b -> p (a b)"),
                      wcols[:, off_d + q4 * 11 * 1024: off_d + (q4 + 1) * 11 * 1024], reads=self.wbf_grp, writes=[bwd])

            gu_seq = [(n, g) for n in range(NTT) for g in range(11)]
            self._gu_loaded = 0

            def load_gu(upto):
                while self._gu_loaded < min(upto, len(gu_seq)):
                    k = self._gu_loaded
                    n, g = gu_seq[k]
                    i = k % NG
                    K.dma(sp, swgu[i], wgu[i][:].rearrange("p a b c -> p (a b c)"),
                          wcols[:, off_gu + g * 4096: off_gu + (g + 1) * 4096], reads=self.wbf_grp, writes=[bwgu[i]])
                    self._gu_loaded += 1

            acnt = [0]

            def phaseA(n, j):
                a = acnt[0] % 2
                acnt[0] += 1
                tile = n * 4 + j
                K.dma(pool, sxa[a], xa[a][:], x_src[tile * 128:(tile + 1) * 128, :], reads=[self.xbuf[tile]], writes=[bxa[a]])
                self.norm_mod_T(vs, xa[a][:], bxa[a], hb[a], bhb[a], tmp, btmp, small[0], bsmall[0], junk, bjunk,
                                hT[n % 2][:, :, j * 128:(j + 1) * 128], bhT[n % 2])

            for j in range(4):
                phaseA(0, j)
            load_gu(NG)
            ccnt = [0]
            bgq = list(bg)
            for n in range(NTT):
                cur = n % 2
                for g in range(11):
                    k = n * 11 + g
                    i = k % NG
                    for fl in range(2):
                        f = 2 * g + fl
                        bg_, bu_ = (2 * f) % 3, (2 * f + 1) % 3
                        for kc in range(8):
                            K.op(pe, lambda e, i=i, fl=fl, kc=kc, bg_=bg_: e.matmul(GU[bg_][:], lhsT=wgu[i][:, 0, kc, fl * 128:(fl + 1) * 128],
                                                                                     rhs=hT[cur][:, kc, :], start=(kc == 0), stop=(kc == 7)),
                                 reads=[bwgu[i], bhT[cur]], writes=[bGU[bg_]], sig=(kc == 7))
                        for kc in range(8):
                            K.op(pe, lambda e, i=i, fl=fl, kc=kc, bu_=bu_: e.matmul(GU[bu_][:], lhsT=wgu[i][:, 1, kc, fl * 128:(fl + 1) * 128],
                                                                                     rhs=hT[cur][:, kc, :], start=(kc == 0), stop=(kc == 7)),
                                 reads=[bwgu[i], bhT[cur]], writes=[bGU[bu_]], sig=(kc == 7))
                        K.op(act, lambda e, f=f, bg_=bg_: e.activation(out=gt[f % 2][:], in_=GU[bg_][:], func=AF.Silu),
                             reads=[bGU[bg_]], writes=[bgt[f % 2]])
                        K.op(dve, lambda e, f=f, bu_=bu_: e.tensor_tensor(out=actT[:, f, :], in0=gt[f % 2][:], in1=GU[bu_][:], op=ALU.mult),
                             reads=[bgt[f % 2], bGU[bu_]], writes=[bactT])
                    load_gu(k + 1 + NG)
                    if n + 1 < NTT and g in (1, 3, 5, 7):
                        phaseA(n + 1, (g - 1) // 2)
                    if g == 9 and bgq:
                        bgq.pop(0)()
                for j in range(4):
                    tile = n * 4 + j
                    c = ccnt[0] % 2
                    ccnt[0] += 1
                    K.dma(pool, sxc[c], xc[c][:], x_src[tile * 128:(tile + 1) * 128, :], reads=[self.xbuf[tile]], writes=[bxc[c]])
                    ybs = []
                    for hf in range(2):
                        yb = (2 * j + hf) % 3
                        ybs.append(yb)
                        for f in range(NF):
                            K.op(pe, lambda e, f=f, j=j, hf=hf, yb=yb: e.matmul(Y[yb][:], lhsT=actT[:, f, j * 128:(j + 1) * 128],
                                                                                 rhs=wd[:, f, hf * 512:(hf + 1) * 512],
                                                                                 start=(f == 0), stop=(f == NF - 1)),
                                 reads=[bactT, bwd], writes=[bY[yb]], sig=(f == NF - 1))
                    self.post_resid(vs, [(Y[ybs[0]][:], 0, 512), (Y[ybs[1]][:], 512, 512)], [bY[ybs[0]], bY[ybs[1]]],
                                    xc[c][:], bxc[c], xo[c], bxo[c], small[1], bsmall[1], junk, bjunk, tmp, btmp)
                    K.dma(pool, sxo[c], self.out[tile * 128:(tile + 1) * 128, :], xo[c][:], reads=[bxo[c]], writes=[self.xbuf[tile]])
            while bgq:
                bgq.pop(0)()
            K.barrier()
        K.end_scope()

    def build(self):
        K = self.K
        self.setup_globals()
        if self.do_cast:
            need = []
            for (li, kind) in self.subl:
                if kind in ("f0", "f1"):
                    j = 0 if kind == "f0" else 1
                    need.append((WOFF[("gu", li, j)], W_GU + W_D))
                elif li % 2 == 0:
                    need.append((WOFF[("ain", li // 2)], W_AIN + W_AOUT))
                else:
                    need.append((WOFF[("bin", li // 2, 0)], 2 * (W_BIN + W_BOUT)))
            self.cast_plan(need)
            self.cast_bg(0, upto_group=0)
        x_src = self.x_in
        nsub = len(self.subl)

        def sglob(li, kind):
            return li * 3 + {"f0": 0, "mix": 1, "f1": 2}[kind]

        def coef(kind):
            return 1.0 if kind == "mix" else 0.5

        li, kind = self.subl[0]
        for stp in self.ada_steps(sglob(li, kind), 0, coef(kind)):
            stp()
        for si, (li, kind) in enumerate(self.subl):
            vs = si % 2
            bg = []
            self.wbf_grp = self.wgrp[si]
            if si + 1 < nsub:
                nl, nk = self.subl[si + 1]
                steps = self.ada_steps(sglob(nl, nk), (si + 1) % 2, coef(nk))
                bg = [(lambda s_=s_, si=si: (self.cast_bg(2), s_())) for s_ in steps]
                bg.append(lambda si=si: self.cast_bg(0, upto_group=si + 1))
            if kind in ("f0", "f1"):
                j = 0 if kind == "f0" else 1
                self.ffn(x_src, vs, WOFF[("gu", li, j)], WOFF[("d", li, j)], bg)
            elif li % 2 == 0:
                self.dsa(x_src, vs, li // 2, bg)
            else:
                self.diff(x_src, vs, li // 2, bg)
            x_src = self.out
        K.barrier()
        return self.nc

    @staticmethod
    def interleave(*gens):
        gens = [g for g in gens if g is not None]
        while gens:
            for g in list(gens):
                try:
                    next(g)
                except StopIteration:
                    gens.remove(g)

    @staticmethod
    def chain(*gens):
        for g in gens:
            if g is not None:
                yield from g

    def neg_bound(self, qsq, bq, ksq, bk, run, brun, mneg, bmneg, small, bsmall):
        K = self.K
        pe, dve, act, pool = K.pe, K.dve, K.act, K.pool
        TAr = self.TA[0:1, 0:256]
        K.op(pe, lambda e: e.transpose(out=self.TA[0:1, 0:128], in_=qsq, identity=self.identf[:]),
             reads=[bq, self.bconst], writes=[self.bTA], sig=False)
        K.op(pe, lambda e: e.transpose(out=self.TA[0:1, 128:256], in_=ksq, identity=self.identf[:]),
             reads=[bk, self.bconst], writes=[self.bTA])
        K.op(dve, lambda e: e.tensor_reduce(out=small[0:1, 0:2], in_=TAr.rearrange("p (a b) -> p a b", a=2), axis=AX.X, op=ALU.max),
             reads=[self.bTA], writes=[bsmall])
        K.op(dve, lambda e: e.tensor_tensor(out=run[0:1, 0:1], in0=run[0:1, 0:1], in1=small[0:1, 1:2], op=ALU.max),
             reads=[bsmall, brun], writes=[brun])
        K.op(dve, lambda e: e.tensor_tensor(out=small[0:1, 2:3], in0=small[0:1, 0:1], in1=run[0:1, 0:1], op=ALU.mult),
             reads=[bsmall, brun], writes=[bsmall])
        K.op(pe, lambda e: e.matmul(self.TA[:, 0:1], lhsT=self.ones_row[0:1, :], rhs=small[0:1, 2:3], start=True, stop=True),
             reads=[bsmall, self.bconst], writes=[self.bTA])
        K.op(dve, lambda e: e.tensor_copy(out=mneg[:, 2:3], in_=self.TA[:, 0:1]), reads=[self.bTA], writes=[bmneg])
        K.op(pool, lambda e: e.tensor_tensor(out=mneg[:, 2:3], in0=mneg[:, 2:3], in1=self.half[:, 0:1], op=ALU.pow),
             reads=[bmneg, self.bconst], writes=[bmneg])
        K.op(pool, lambda e: e.tensor_scalar(out=mneg[:, 0:1], in0=mneg[:, 2:3], scalar1=-0.125, scalar2=None, op0=ALU.mult),
             reads=[bmneg], writes=[bmneg])

    def rope(self, pe_t, bpe, h0, nh, ti, rt, brt):
        K = self.K
        dve = K.dve
        v = pe_t[:, h0 * 64:(h0 + nh) * 64].rearrange("p (h d) -> p h d", d=64)
        x1 = v[:, :, 0:8]
        x2 = v[:, :, 8:16]
        cb = self.cosT[:, ti:ti + 1, :].to_broadcast([128, nh, 8])
        sbb = self.sinT[:, ti:ti + 1, :].to_broadcast([128, nh, 8])
        t = [rt[:, k, 0:nh, :] for k in range(4)]
        rd = [bpe, self.brope]
        K.op(dve, lambda e: e.tensor_tensor(out=t[0], in0=x1, in1=cb, op=ALU.mult), reads=rd, writes=[brt])
        K.op(dve, lambda e: e.tensor_tensor(out=t[1], in0=x2, in1=sbb, op=ALU.mult), reads=rd, writes=[brt])
        K.op(dve, lambda e: e.tensor_tensor(out=t[2], in0=x2, in1=cb, op=ALU.mult), reads=rd, writes=[brt])
        K.op(dve, lambda e: e.tensor_tensor(out=t[3], in0=x1, in1=sbb, op=ALU.mult), reads=rd, writes=[brt])
        K.op(dve, lambda e: e.tensor_tensor(out=x1, in0=t[0], in1=t[1], op=ALU.subtract), reads=[brt], writes=[bpe])
        K.op(dve, lambda e: e.tensor_tensor(out=x2, in0=t[2], in1=t[3], op=ALU.add), reads=[brt], writes=[bpe])

    def dsa(self, x_src, vs, jl, bg):
        K, T, NT = self.K, self.T, self.NT
        pe, dve, act, pool, sp = K.pe, K.dve, K.act, K.pool, K.sp
        off_in, off_out = WOFF[("ain", jl)], WOFF[("aout", jl)]
        K.begin_scope()
        with ExitStack() as st:
            sb = lambda n, s, d: self.sb(st, n, s, d)
            w_in = sb("w_in", [128, 8, A_IN], BF16); bw = Buf("w"); sw = Slot(K, "w")
            w_out = sb("w_out", [128, 8, D], BF16)
            kT2 = sb("kT2", [128, T], BF16); bkT = Buf("kT2")
            kiT2 = sb("kiT2", [128, T], BF16); bkiT = Buf("kiT2")
            Vaug = sb("Vaug", [128, NT, 65], BF16); bV = Buf("Vaug")
            score = sb("score", [128, T], F32); bscore = Buf("score")
            NM2 = [sb(f"NM{i}", [128, T], BF16) for i in range(2)]; bNM2 = [Buf(f"NM{i}") for i in range(2)]
            rtmp = [sb(f"rtmp{i}", [128, 512], F32) for i in range(2)]; brtmp = [Buf(f"rtmp{i}") for i in range(2)]
            PT = [sb(f"PT{i}", [128, 16, 128], BF16) for i in range(2)]; bPT = [Buf(f"PT{i}") for i in range(2)]
            pev = sb("pev", [128, A_IN], F32); bpev = Buf("pev")
            rt = sb("rt", [128, 4, 17, 8], F32); brt = Buf("rt")
            qb = sb("qb", [128, 1152], BF16); bqb = Buf("qb")
            qib = sb("qib", [128, 640], BF16); bqib = Buf("qib")
            qT2 = [sb(f"qT{i}", [128, 8, 128], BF16) for i in range(2)]; bqT2 = [Buf(f"qT{i}") for i in range(2)]
            qiT = sb("qiT", [128, 4, 128], BF16); bqiT = Buf("qiT")
            wi = sb("wi", [128, 8], F32); bwi = Buf("wi")
            xa = [sb(f"xa{i}", [128, D], F32) for i in range(2)]; bxa = [Buf(f"xa{i}") for i in range(2)]
            sxa = [Slot(K, f"xa{i}") for i in range(2)]
            sxo = [Slot(K, f"xo{i}") for i in range(2)]
            tmp = sb("tmp", [128, D], F32); btmp = Buf("tmp")
            hb = sb("hb", [128, D], BF16); bhb = Buf("hb")
            hT = sb("hT", [128, 8, 128], BF16); bhT = Buf("hT")
            ob = sb("ob", [128, D], BF16); bob = Buf("ob")
            oT = sb("oT", [128, 8, 128], BF16); boT = Buf("oT")
            junk = sb("junk", [128, D], BF16); bjunk = Buf("junk")
            small = [sb(f"small{i}", [128, 8], F32) for i in range(2)]; bsmall = [Buf(f"small{i}") for i in range(2)]
            sq = sb("sq", [128, 24], F32); bsq = Buf("sq")
            ksqt = sb("ksqt", [128, 64], F32)
            run = sb("run", [128, 2], F32); brun = Buf("run")
            mneg2 = [sb(f"mneg{i}", [128, 4], F32) for i in range(2)]; bmneg2 = [Buf(f"mneg{i}") for i in range(2)]
            smallb = sb("smallb", [128, 8], F32); bsmallb = Buf("smallb")
            bis = sb("bis", [128, 8], F32); bbis = Buf("bis")
            dk = sb("dk", [128, NBIS + 2], F32)
            p2 = sb("p2", [128, NBIS + 2], F32)
            rl = sb("rl", [128, 16], F32); brl = Buf("rl")
            P = [self.ps(st, f"P{i}", [128, 512], F32) for i in range(4)]; bP = [Buf(f"P{i}") for i in range(4)]
            O = self.ps(st, "O", [128, 1536], F32); bO = Buf("O")

            K.dma(sp, sw, w_in[:].rearrange("p a b -> p (a b)"), self.wbf[:, off_in:off_in + W_AIN], reads=self.wbf_grp, writes=[bw])
            K.dma(sp, sw, w_out[:].rearrange("p a b -> p (a b)"), self.wbf[:, off_out:off_out + W_AOUT], reads=self.wbf_grp, writes=[bw])
            K.op(pool, lambda e: e.memset(Vaug[:, :, 64:65], 1.0), writes=[bV])
            K.op(pool, lambda e: e.memset(run[:], 0.0), writes=[brun])
            for k in range(NBIS + 2):
                K.op(pool, lambda e, k=k: e.memset(p2[:, k:k + 1], 2.0 ** (-(k + 1))), writes=[bbis])
            bgq = list(bg)
            pcnt = [0]
            if not hasattr(self, "fill_reg"):
                self.fill_reg = self.nc.gpsimd.to_reg(-1e30)

            def pbank():
                b = pcnt[0] % 4
                pcnt[0] += 1
                return b

            def stageA(i):
                a = i % 2
                n = (i + 1) * 128
                qT, bqT, mneg, bmneg = qT2[a], bqT2[a], mneg2[a], bmneg2[a]
                K.dma(pool, sxa[a], xa[a][:], x_src[i * 128:(i + 1) * 128, :], reads=[self.xbuf[i]], writes=[bxa[a]])
                self.norm_mod_T(vs, xa[a][:], bxa[a], hb, bhb, tmp, btmp, small[0], bsmall[0], junk, bjunk, hT[:], bhT)
                yield
                for b in range(4):
                    c0, c1 = b * 512, min((b + 1) * 512, A_IN)
                    for kc in range(8):
                        K.op(pe, lambda e, b=b, kc=kc, c0=c0, c1=c1: e.matmul(P[b][:, 0:c1 - c0], lhsT=hT[:, kc, :], rhs=w_in[:, kc, c0:c1],
                                                                             start=(kc == 0), stop=(kc == 7)),
                             reads=[bhT, bw], writes=[bP[b]], sig=(kc == 7))
                    K.op(act, lambda e, b=b, c0=c0, c1=c1: e.activation(out=pev[:, c0:c1], in_=P[b][:, 0:c1 - c0], func=AF.Copy),
                         reads=[bP[b]], writes=[bpev])
                yield
                self.rope(pev[:], bpev, 0, 17, i, rt, brt)
                self.rope(pev[:], bpev, 18, 9, i, rt, brt)
                yield
                K.op(act, lambda e: e.activation(out=qb[:, 0:1088], in_=pev[:, 0:1088], func=AF.Copy), reads=[bpev], writes=[bqb])
                K.op(act, lambda e: e.activation(out=qb[:, 1088:1152], in_=pev[:, 1024:1088], func=AF.Copy), reads=[bpev], writes=[bqb])
                K.op(act, lambda e, i=i: e.activation(out=Vaug[:, i, 0:64], in_=pev[:, 1088:1152], func=AF.Copy), reads=[bpev], writes=[bV])
                K.op(act, lambda e: e.activation(out=qib[:, 0:576], in_=pev[:, 1152:1728], func=AF.Copy), reads=[bpev], writes=[bqib])
                K.op(act, lambda e: e.activation(out=qib[:, 576:640], in_=pev[:, 1664:1728], func=AF.Copy), reads=[bpev], writes=[bqib])
                K.op(act, lambda e: e.activation(out=wi[:], in_=pev[:, 1728:1736], func=AF.Copy), reads=[bpev], writes=[bwi])
                K.op(act, lambda e: e.activation(out=tmp[:], in_=pev[:, 0:1024], func=AF.Square), reads=[bpev], writes=[btmp])
                K.op(act, lambda e: e.activation(out=ksqt[:], in_=pev[:, 1024:1088], func=AF.Square), reads=[bpev], writes=[btmp])
                K.op(dve, lambda e: e.tensor_reduce(out=sq[:, 0:16], in_=tmp[:].rearrange("p (h d) -> p h d", d=64),
                                                    axis=AX.X, op=ALU.add), reads=[btmp], writes=[bsq])
                K.op(dve, lambda e: e.tensor_reduce(out=sq[:, 16:17], in_=ksqt[:], axis=AX.X, op=ALU.add), reads=[btmp], writes=[bsq])
                K.op(dve, lambda e: e.tensor_reduce(out=sq[:, 20:21], in_=sq[:, 0:16], axis=AX.X, op=ALU.max), reads=[bsq], writes=[bsq])
                yield
                for s_ in range(8):
                    K.op(pe, lambda e, s_=s_: e.transpose(out=self.TAb[:, s_ * 128:(s_ + 1) * 128], in_=qb[:, s_ * 128:(s_ + 1) * 128],
                                                           identity=self.ident[:]),
                         reads=[bqb, self.bconst], writes=[self.bTA], sig=(s_ == 7))
                K.op(act, lambda e: e.activation(out=qT[:], in_=self.TAb[:].rearrange("p (a b) -> p a b", a=8), func=AF.Copy),
                     reads=[self.bTA], writes=[bqT])
                K.op(pe, lambda e: e.transpose(out=self.TAb[:, 0:128], in_=qb[:, 1024:1152], identity=self.ident[:]),
                     reads=[bqb, self.bconst], writes=[self.bTA], sig=False)
                for s_ in range(5):
                    K.op(pe, lambda e, s_=s_: e.transpose(out=self.TAb[:, (s_ + 1) * 128:(s_ + 2) * 128], in_=qib[:, s_ * 128:(s_ + 1) * 128],
                                                           identity=self.ident[:]),
                         reads=[bqib, self.bconst], writes=[self.bTA], sig=(s_ == 4))
                K.op(dve, lambda e, i=i: e.tensor_copy(out=kT2[:, i * 128:(i + 1) * 128], in_=self.TAb[:, 0:128]),
                     reads=[self.bTA], writes=[bkT])
                K.op(dve, lambda e: e.tensor_copy(out=qiT[:], in_=self.TAb[:, 128:640].rearrange("p (a b) -> p a b", a=4)),
                     reads=[self.bTA], writes=[bqiT])
                K.op(dve, lambda e, i=i: e.tensor_copy(out=kiT2[:, i * 128:(i + 1) * 128], in_=self.TAb[:, 640:768]),
                     reads=[self.bTA], writes=[bkiT])
                yield
                self.neg_bound(sq[:, 20:21], bsq, sq[:, 16:17], bsq, run, brun, mneg, bmneg, smallb, bsmallb)
                yield
                nblk = (n + 511) // 512
                rc = 0
                for sbk in range(nblk):
                    w = min(512, n - sbk * 512)
                    cs = slice(sbk * 512, sbk * 512 + w)
                    for h in range(8):
                        s_, hf = h % 4, h // 4
                        b = pbank()
                        K.op(pe, lambda e, b=b, s_=s_, hf=hf, cs=cs, w=w: e.matmul(P[b][:, 0:w], lhsT=qiT[hf * 64:(hf + 1) * 64, s_, :],
                                                                                    rhs=kiT2[hf * 64:(hf + 1) * 64, cs], start=True, stop=True),
                             reads=[bqiT, bkiT], writes=[bP[b]])
                        r = rc % 2
                        rc += 1
                        K.op(act, lambda e, b=b, r=r, w=w: e.activation(out=rtmp[r][:, 0:w], in_=P[b][:, 0:w], func=AF.Relu),
                             reads=[bP[b]], writes=[brtmp[r]])
                        if h == 0:
                            K.op(dve, lambda e, r=r, w=w, cs=cs, h=h: e.tensor_scalar(out=score[:, cs], in0=rtmp[r][:, 0:w], scalar1=wi[:, h:h + 1],
                                                                                       scalar2=None, op0=ALU.mult),
                                 reads=[brtmp[r], bwi], writes=[bscore])
                        else:
                            K.op(dve, lambda e, r=r, w=w, cs=cs, h=h: e.scalar_tensor_tensor(out=score[:, cs], in0=rtmp[r][:, 0:w], scalar=wi[:, h:h + 1],
                                                                                              in1=score[:, cs], op0=ALU.mult, op1=ALU.add),
                                 reads=[brtmp[r], bwi, bscore], writes=[bscore])
                    yield

            def stageB(i):
                a = i % 2
                n = (i + 1) * 128
                NM, bNM = NM2[a], bNM2[a]
                lo, mid, cnt, u, t2, d0 = (bis[:, k:k + 1] for k in range(6))
                if n > 256:
                    K.op(dve, lambda e: e.tensor_reduce(out=lo, in_=score[:, 0:n], axis=AX.X, op=ALU.min), reads=[bscore], writes=[bbis])
                    K.op(dve, lambda e: e.tensor_reduce(out=d0, in_=score[:, 0:n], axis=AX.X, op=ALU.max), reads=[bscore], writes=[bbis])
                    K.op(dve, lambda e: e.tensor_tensor(out=d0, in0=d0, in1=lo, op=ALU.subtract), reads=[bbis], writes=[bbis])
                    K.op(dve, lambda e: e.tensor_scalar(out=dk[:], in0=p2[:], scalar1=d0, scalar2=None, op0=ALU.mult), reads=[bbis], writes=[bbis])
                    K.op(dve, lambda e: e.tensor_tensor(out=mid, in0=lo, in1=dk[:, 0:1], op=ALU.add), reads=[bbis], writes=[bbis])
                K.op(pool, lambda e, i=i: e.affine_select(out=score[:, i * 128:(i + 1) * 128], in_=score[:, i * 128:(i + 1) * 128],
                                                          pattern=[[-1, 128]], compare_op=ALU.is_ge, fill=self.fill_reg, base=0, channel_multiplier=1),
                     reads=[bscore, bbis], writes=[bscore])
                if n > 256:
                    for k in range(NBIS):
                        K.op(dve, lambda e: e.tensor_scalar(out=NM[:, 0:n], in0=score[:, 0:n], scalar1=mid, scalar2=0.0, op0=ALU.is_ge,
                                                            op1=ALU.add, accum_out=cnt),
                             reads=[bscore, bbis], writes=[bNM, bbis])
                        K.op(dve, lambda e, k=k: e.tensor_scalar(out=u, in0=cnt, scalar1=255.5, scalar2=dk[:, k:k + 1], op0=ALU.is_ge, op1=ALU.mult),
                             reads=[bbis], writes=[bbis])
                        K.op(dve, lambda e, k=k: e.scalar_tensor_tensor(out=mid, in0=u, scalar=dk[:, k + 1:k + 2], in1=mid, op0=ALU.subtract, op1=ALU.add),
                             reads=[bbis], writes=[bbis])
                        yield
                    K.op(dve, lambda e: e.tensor_tensor(out=lo, in0=mid, in1=dk[:, NBIS:NBIS + 1], op=ALU.subtract), reads=[bbis], writes=[bbis])
                    K.op(dve, lambda e: e.tensor_scalar(out=NM[:, 0:n], in0=score[:, 0:n], scalar1=lo, scalar2=NEG, op0=ALU.is_lt, op1=ALU.mult),
                         reads=[bscore, bbis], writes=[bNM])
                else:
                    K.op(dve, lambda e: e.tensor_scalar(out=NM[:, 0:n], in0=score[:, 0:n], scalar1=-1e29, scalar2=NEG, op0=ALU.is_lt, op1=ALU.mult),
                         reads=[bscore], writes=[bNM])
                yield

            def stageC(i):
                a = i % 2
                n = (i + 1) * 128
                NM, bNM = NM2[a], bNM2[a]
                qT, bqT, mneg, bmneg = qT2[a], bqT2[a], mneg2[a], bmneg2[a]
                K.op(dve, lambda e: e.memset(O[:], 0.0), writes=[bO])
                for c in range(i + 1):
                    pt = c % 2
                    for hg in range(4):
                        hf, s0 = hg // 2, (hg % 2) * 4
                        b = pbank()
                        K.op(pe, lambda e, b=b, c=c: e.matmul(P[b][:], lhsT=NM[:, c * 128:(c + 1) * 128], rhs=self.I4[:].rearrange("p a b -> p (a b)"),
                                                              start=True, stop=False),
                             reads=[bNM, self.bconst], writes=[bP[b]], sig=False)
                        K.op(pe, lambda e, b=b, c=c, hf=hf, s0=s0: e.matmul(P[b][:], lhsT=kT2[hf * 64:(hf + 1) * 64, c * 128:(c + 1) * 128],
                                                                             rhs=qT[hf * 64:(hf + 1) * 64, s0:s0 + 4, :], start=False, stop=True),
                             reads=[bkT, bqT], writes=[bP[b]])
                        K.op(act, lambda e, b=b, pt=pt, hg=hg: e.activation(out=PT[pt][:, hg * 4:(hg + 1) * 4, :],
                                                                            in_=P[b][:].rearrange("p (a b) -> p a b", a=4), func=AF.Exp,
                                                                            bias=mneg[:, 0:1], scale=0.125),
                             reads=[bP[b], bmneg], writes=[bPT[pt]])
                    for hd in range(16):
                        col = (hd // 7) * 512 + (hd % 7) * 65
                        K.op(pe, lambda e, pt=pt, hd=hd, col=col, c=c: e.matmul(O[:, col:col + 65], lhsT=PT[pt][:, hd, :], rhs=Vaug[:, c, :],
                                                                                 start=False, stop=False, skip_group_check=True),
                             reads=[bPT[pt], bV, bO], writes=[bO], sig=(hd == 15))
                    yield
                if self.debug and i == 0:
                    if self.dbg is None:
                        self.dbg = self.nc.dram_tensor("dbg", [128, 4096], F32, kind="ExternalOutput").ap()
                    dsb = sb("dsb", [128, 4096], F32); bd = Buf("dsb"); sd = Slot(K, "dsb")
                    K.op(dve, lambda e: e.memset(dsb[:], 0.0), writes=[bd])
                    K.op(dve, lambda e: e.tensor_copy(out=dsb[:, 0:1536], in_=O[:]), reads=[bO], writes=[bd])
                    K.op(dve, lambda e: e.tensor_copy(out=dsb[:, 1536:1540], in_=mneg[:]), reads=[bmneg], writes=[bd])
                    K.op(dve, lambda e: e.tensor_copy(out=dsb[:, 1600:1600 + 130], in_=Vaug[:, 0:2, :].rearrange("p a b -> p (a b)")), reads=[bV], writes=[bd])
                    K.op(dve, lambda e: e.tensor_copy(out=dsb[:, 2048:4096], in_=PT[0][:].rearrange("p a b -> p (a b)")), reads=[bPT[0]], writes=[bd])
                    K.dma(sp, sd, self.dbg, dsb[:], reads=[bd])
                for bk in range(3):
                    nh = 7 if bk < 2 else 2
                    Ov = O[:, bk * 512: bk * 512 + nh * 65].rearrange("p (h e) -> p h e", e=65)
                    K.op(dve, lambda e, bk=bk, nh=nh, Ov=Ov: e.reciprocal(out=rl[:, bk * 7: bk * 7 + nh], in_=Ov[:, :, 64]),
                         reads=[bO], writes=[brl])
                    K.op(dve, lambda e, bk=bk, nh=nh, Ov=Ov: e.tensor_tensor(
                        out=ob[:, bk * 448: bk * 448 + nh * 64].rearrange("p (h d) -> p h d", d=64), in0=Ov[:, :, 0:64],
                        in1=rl[:, bk * 7: bk * 7 + nh].rearrange("p (h o) -> p h o", o=1).to_broadcast([128, nh, 64]), op=ALU.mult),
                         reads=[bO, brl], writes=[bob])
                yield
                for kc in range(8):
                    K.op(pe, lambda e, kc=kc: e.transpose(out=self.TAb[:, kc * 128:(kc + 1) * 128], in_=ob[:, kc * 128:(kc + 1) * 128],
                                                          identity=self.ident[:]),
                         reads=[bob, self.bconst], writes=[self.bTA], sig=(kc == 7))
                K.op(act, lambda e: e.activation(out=oT[:], in_=self.TAb[:].rearrange("p (a b) -> p a b", a=8), func=AF.Copy),
                     reads=[self.bTA], writes=[boT])
                yb = [pbank(), pbank()]
                for hf in range(2):
                    for kc in range(8):
                        K.op(pe, lambda e, hf=hf, kc=kc, yb=yb: e.matmul(P[yb[hf]][:], lhsT=oT[:, kc, :], rhs=w_out[:, kc, hf * 512:(hf + 1) * 512],
                                                                         start=(kc == 0), stop=(kc == 7)),
                             reads=[boT, bw], writes=[bP[yb[hf]]], sig=(kc == 7))
                self.post_resid(vs, [(P[yb[0]][:], 0, 512), (P[yb[1]][:], 512, 512)], [bP[yb[0]], bP[yb[1]]],
                                xa[a][:], bxa[a], xa[a], bxa[a], small[1], bsmall[1], junk, bjunk, tmp, btmp)
                K.dma(pool, sxo[a], self.out[i * 128:(i + 1) * 128, :], xa[a][:], reads=[bxa[a]], writes=[self.xbuf[i]])
                yield

            self.interleave(self.chain(stageA(0), stageB(0)))
            for i in range(NT):
                nxt = self.chain(stageA(i + 1), stageB(i + 1)) if i + 1 < NT else None
                if PIPE == 0:
                    self.interleave(stageC(i)); self.interleave(nxt)
                elif PIPE == 1:
                    self.interleave(nxt); self.interleave(stageC(i))
                else:
                    self.interleave(stageC(i), nxt)
                if bgq and i % 4 == 3:
                    bgq.pop(0)()
            while bgq:
                bgq.pop(0)()
            K.barrier()
        K.end_scope()

    def diff(self, x_src, vs, jl, bg):
        K, T, NT = self.K, self.T, self.NT
        pe, dve, act, pool, sp = K.pe, K.dve, K.act, K.pool, K.sp
        bgq = list(bg)
        for g in range(2):
            off_in, off_out = WOFF[("bin", jl, g)], WOFF[("bout", jl, g)]
            K.begin_scope()
            with ExitStack() as st:
                sb = lambda n, s, d: self.sb(st, n, s, d)
                w_in = sb("w_in", [128, 8, 1536], BF16); bw = Buf("w"); sw = Slot(K, "w")
                w_out = sb("w_out", [128, 4, D], BF16)
                kT = sb("kT", [128, 4, T], BF16); bkT = Buf("kT")
                Vaug = sb("Vaug", [128, NT, 4, 129], BF16); bV = Buf("Vaug")
                PT = [sb(f"PT{i}", [128, 8, 128], BF16) for i in range(2)]; bPT = [Buf(f"PT{i}") for i in range(2)]
                pev = sb("pev", [128, 1536], F32); bpev = Buf("pev")
                rt = sb("rt", [128, 4, 16, 8], F32); brt = Buf("rt")
                qb = sb("qb", [128, 1024], BF16); bqb = Buf("qb")
                qT2 = [sb(f"qT{i}", [128, 4, 128], BF16) for i in range(2)]; bqT2 = [Buf(f"qT{i}") for i in range(2)]
                smallb = sb("smallb", [128, 8], F32); bsmallb = Buf("smallb")
                sq = sb("sq", [128, 24], F32); bsq = Buf("sq")
                xa = [sb(f"xa{i}", [128, D], F32) for i in range(2)]; bxa = [Buf(f"xa{i}") for i in range(2)]
                sxa = [Slot(K, f"xa{i}") for i in range(2)]
                sxo = [Slot(K, f"xo{i}") for i in range(2)]
                ysb = [sb(f"ysb{i}", [128, D], F32) for i in range(2)]; bysb = [Buf(f"ysb{i}") for i in range(2)]
                sys_ = [Slot(K, f"ysb{i}") for i in range(2)]
                tmp = sb("tmp", [128, D], F32); btmp = Buf("tmp")
                hb = sb("hb", [128, D], BF16); bhb = Buf("hb")
                hT = sb("hT", [128, 8, 128], BF16); bhT = Buf("hT")
                on = sb("on", [128, 8, 128], F32); bon = Buf("on")
                od = sb("od", [128, 4, 128], F32); bod = Buf("od")
                ob = sb("ob", [128, 512], BF16); bob = Buf("ob")
                oT = sb("oT", [128, 4, 128], BF16); boT = Buf("oT")
                junk = sb("junk", [128, D], BF16); bjunk = Buf("junk")
                small = [sb(f"small{i}", [128, 8], F32) for i in range(2)]; bsmall = [Buf(f"small{i}") for i in range(2)]
                run = sb("run", [128, 2], F32); brun = Buf("run")
                mneg2 = [sb(f"mneg{i}", [128, 4], F32) for i in range(2)]; bmneg2 = [Buf(f"mneg{i}") for i in range(2)]
                rl = sb("rl", [128, 8], F32); brl = Buf("rl")
                ssub = sb("ssub", [128, 8], F32); bssub = Buf("ssub")
                P = [self.ps(st, f"P{i}", [128, 512], F32) for i in range(4)]; bP = [Buf(f"P{i}") for i in range(4)]
                O = self.ps(st, "O", [128, 1536], F32); bO = Buf("O")

                K.dma(sp, sw, w_in[:].rearrange("p a b -> p (a b)"), self.wbf[:, off_in:off_in + W_BIN], reads=self.wbf_grp, writes=[bw])
                K.dma(sp, sw, w_out[:].rearrange("p a b -> p (a b)"), self.wbf[:, off_out:off_out + W_BOUT], reads=self.wbf_grp, writes=[bw])
                K.op(pool, lambda e: e.memset(Vaug[:, :, :, 128:129], 1.0), writes=[bV])
                K.op(pool, lambda e: e.memset(run[:], 0.0), writes=[brun])
                pcnt = [0]

                def pbank():
                    b = pcnt[0] % 4
                    pcnt[0] += 1
                    return b

                def stageA(i):
                    a = i % 2
                    qT, bqT, mneg, bmneg = qT2[a], bqT2[a], mneg2[a], bmneg2[a]
                    K.dma(pool, sxa[a], xa[a][:], x_src[i * 128:(i + 1) * 128, :], reads=[self.xbuf[i]], writes=[bxa[a]])
                    if g == 1:
                        K.dma(pool, sys_[a], ysb[a][:], self.ypart[i * 128:(i + 1) * 128, :], reads=[self.ypbuf[i]], writes=[bysb[a]])
                    self.norm_mod_T(vs, xa[a][:], bxa[a], hb, bhb, tmp, btmp, small[0], bsmall[0], junk, bjunk, hT[:], bhT)
                    if YMASK & 1:
                        yield
                    for b in range(3):
                        for kc in range(8):
                            K.op(pe, lambda e, b=b, kc=kc: e.matmul(P[b][:], lhsT=hT[:, kc, :], rhs=w_in[:, kc, b * 512:(b + 1) * 512],
                                                                    start=(kc == 0), stop=(kc == 7)),
                                 reads=[bhT, bw], writes=[bP[b]], sig=(kc == 7))
                        K.op(act, lambda e, b=b: e.activation(out=pev[:, b * 512:(b + 1) * 512], in_=P[b][:], func=AF.Copy),
                             reads=[bP[b]], writes=[bpev])
                    if YMASK & 2:
                        yield
                    self.rope(pev[:], bpev, 0, 16, i, rt, brt)
                    if YMASK & 4:
                        yield
                    K.op(act, lambda e: e.activation(out=qb[:], in_=pev[:, 0:1024], func=AF.Copy), reads=[bpev], writes=[bqb])
                    K.op(act, lambda e, i=i: e.activation(out=Vaug[:, i, :, 0:128], in_=pev[:, 1024:1536].rearrange("p (h d) -> p h d", d=128),
                                                          func=AF.Copy), reads=[bpev], writes=[bV])
                    K.op(act, lambda e: e.activation(out=tmp[:], in_=pev[:, 0:1024], func=AF.Square), reads=[bpev], writes=[btmp])
                    K.op(dve, lambda e: e.tensor_reduce(out=sq[:, 0:16], in_=tmp[:].rearrange("p (h d) -> p h d", d=64), axis=AX.X, op=ALU.add),
                         reads=[btmp], writes=[bsq])
                    K.op(dve, lambda e: e.tensor_reduce(out=sq[:, 20:22], in_=sq[:, 0:16].rearrange("p (a b) -> p a b", a=2), axis=AX.X, op=ALU.max),
                         reads=[bsq], writes=[bsq])
                    if YMASK & 8:
                        yield
                    for s_ in range(8):
                        K.op(pe, lambda e, s_=s_: e.transpose(out=self.TAb[:, s_ * 128:(s_ + 1) * 128], in_=qb[:, s_ * 128:(s_ + 1) * 128],
                                                               identity=self.ident[:]),
                             reads=[bqb, self.bconst], writes=[self.bTA], sig=(s_ == 7))
                    K.op(act, lambda e: e.activation(out=qT[:], in_=self.TAb[:, 0:512].rearrange("p (a b) -> p a b", a=4), func=AF.Copy),
                         reads=[self.bTA], writes=[bqT])
                    K.op(dve, lambda e, i=i: e.tensor_copy(out=kT[:, :, i * 128:(i + 1) * 128],
                                                           in_=self.TAb[:, 512:1024].rearrange("p (a b) -> p a b", a=4)),
                         reads=[self.bTA], writes=[bkT])
                    if YMASK & 16:
                        yield
                    self.neg_bound(sq[:, 20:21], bsq, sq[:, 21:22], bsq, run, brun, mneg, bmneg, smallb, bsmallb)
                    if YMASK & 32:
                        yield

                def stageC(i):
                    a = i % 2
                    qT, bqT, mneg, bmneg = qT2[a], bqT2[a], mneg2[a], bmneg2[a]
                    K.op(dve, lambda e: e.memset(O[:], 0.0), writes=[bO])
                    for c in range(i + 1):
                        pt = c % 2
                        bb = [pbank(), pbank()]
                        if c == i:
                            for bnk in range(2):
                                K.op(pe, lambda e, b=bb[bnk]: e.matmul(P[b][:], lhsT=self.ident[:], rhs=self.CN8[:, 0:4, :].rearrange("p a b -> p (a b)"),
                                                                       start=True, stop=False),
                                     reads=[self.bconst], writes=[bP[bb[bnk]]], sig=False)
                        for ul in range(4):
                            for bnk in range(2):
                                b = bb[bnk]
                                K.op(pe, lambda e, b=b, ul=ul, bnk=bnk, c=c, i=i: e.matmul(
                                    P[b][:, ul * 128:(ul + 1) * 128], lhsT=kT[bnk * 64:(bnk + 1) * 64, ul, c * 128:(c + 1) * 128],
                                    rhs=qT[bnk * 64:(bnk + 1) * 64, ul, :], start=(c != i), stop=(c != i or ul == 3),
                                    skip_group_check=True),
                                     reads=[bkT, bqT], writes=[bP[b]], sig=(ul == 3))
                        for bnk in range(2):
                            b = bb[bnk]
                            K.op(act, lambda e, b=b, pt=pt, bnk=bnk: e.activation(out=PT[pt][:, bnk * 4:(bnk + 1) * 4, :],
                                                                                   in_=P[b][:].rearrange("p (a b) -> p a b", a=4), func=AF.Exp,
                                                                                   bias=mneg[:, 0:1], scale=0.125),
                                 reads=[bP[b], bmneg], writes=[bPT[pt]])
                        for u in range(8):
                            col = (u // 3) * 512 + (u % 3) * 129
                            jx = (u % 2) * 4 + u // 2
                            K.op(pe, lambda e, pt=pt, u=u, jx=jx, col=col, c=c: e.matmul(O[:, col:col + 129], lhsT=PT[pt][:, jx, :], rhs=Vaug[:, c, u % 4, :],
                                                                                          start=False, stop=False, skip_group_check=True),
                                 reads=[bPT[pt], bV, bO], writes=[bO], sig=(u == 7))
                        yield
                    for bk in range(3):
                        nh = 3 if bk < 2 else 2
                        Ov = O[:, bk * 512: bk * 512 + nh * 129].rearrange("p (h e) -> p h e", e=129)
                        K.op(dve, lambda e, bk=bk, nh=nh, Ov=Ov: e.reciprocal(out=rl[:, bk * 3: bk * 3 + nh], in_=Ov[:, :, 128]),
                             reads=[bO], writes=[brl])
                        K.op(dve, lambda e, bk=bk, nh=nh, Ov=Ov: e.tensor_tensor(
                            out=on[:, bk * 3: bk * 3 + nh, :], in0=Ov[:, :, 0:128],
                            in1=rl[:, bk * 3: bk * 3 + nh].rearrange("p (h o) -> p h o", o=1).to_broadcast([128, nh, 128]), op=ALU.mult),
                             reads=[bO, brl], writes=[bon])
                    K.op(dve, lambda e: e.scalar_tensor_tensor(out=od[:].rearrange("p a b -> p (a b)"), in0=on[:, 4:8, :].rearrange("p a b -> p (a b)"),
                                                               scalar=self.neglam[:, jl:jl + 1], in1=on[:, 0:4, :].rearrange("p a b -> p (a b)"),
                                                               op0=ALU.mult, op1=ALU.add),
                         reads=[bon, self.bdiffc], writes=[bod])
                    K.op(act, lambda e: e.activation(out=on[:, 0:4, :], in_=od[:], func=AF.Square), reads=[bod], writes=[bon])
                    K.op(dve, lambda e: e.tensor_reduce(out=ssub[:, 0:4], in_=on[:, 0:4, :], axis=AX.X, op=ALU.add), reads=[bon], writes=[bssub])
                    K.op(pool, lambda e: e.tensor_scalar(out=ssub[:, 4:8], in0=ssub[:, 0:4], scalar1=1.0 / 128.0, scalar2=EPS, op0=ALU.mult, op1=ALU.add),
                         reads=[bssub], writes=[bssub])
                    K.op(pool, lambda e: e.tensor_tensor(out=ssub[:, 4:8], in0=ssub[:, 4:8], in1=self.neghalf[:, 0:4], op=ALU.pow),
                         reads=[bssub, self.bconst], writes=[bssub])
                    K.op(dve, lambda e: e.tensor_tensor(out=od[:], in0=od[:],
                                                        in1=ssub[:, 4:8].rearrange("p (h o) -> p h o", o=1).to_broadcast([128, 4, 128]), op=ALU.mult),
                         reads=[bod, bssub], writes=[bod])
                    K.op(dve, lambda e: e.tensor_tensor(out=ob[:].rearrange("p (h d) -> p h d", d=128), in0=od[:],
                                                        in1=self.sublnb[:, jl:jl + 1, :].to_broadcast([128, 4, 128]), op=ALU.mult),
                         reads=[bod, self.bdiffc], writes=[bob])
                    yield
                    for kc in range(4):
                        K.op(pe, lambda e, kc=kc: e.transpose(out=self.TAb[:, kc * 128:(kc + 1) * 128], in_=ob[:, kc * 128:(kc + 1) * 128],
                                                              identity=self.ident[:]),
                             reads=[bob, self.bconst], writes=[self.bTA], sig=(kc == 3))
                    K.op(act, lambda e: e.activation(out=oT[:], in_=self.TAb[:, 0:512].rearrange("p (a b) -> p a b", a=4), func=AF.Copy),
                         reads=[self.bTA], writes=[boT])
                    yb = [pbank(), pbank()]
                    for hf in range(2):
                        for kc in range(4):
                            K.op(pe, lambda e, hf=hf, kc=kc, yb=yb: e.matmul(P[yb[hf]][:], lhsT=oT[:, kc, :], rhs=w_out[:, kc, hf * 512:(hf + 1) * 512],
                                                                             start=(kc == 0), stop=(kc == 3)),
                                 reads=[boT, bw], writes=[bP[yb[hf]]], sig=(kc == 3))
                    yield
                    if g == 0:
                        for hf in range(2):
                            K.op(act, lambda e, hf=hf, yb=yb, a=a: e.activation(out=ysb[a][:, hf * 512:(hf + 1) * 512], in_=P[yb[hf]][:], func=AF.Copy),
                                 reads=[bP[yb[hf]]], writes=[bysb[a]])
                        K.dma(pool, sys_[a], self.ypart[i * 128:(i + 1) * 128, :], ysb[a][:], reads=[bysb[a]], writes=[self.ypbuf[i]])
                    else:
                        for hf in range(2):
                            K.op(dve, lambda e, hf=hf, yb=yb, a=a: e.tensor_tensor(out=ysb[a][:, hf * 512:(hf + 1) * 512], in0=P[yb[hf]][:],
                                                                                    in1=ysb[a][:, hf * 512:(hf + 1) * 512], op=ALU.add),
                                 reads=[bP[yb[hf]], bysb[a]], writes=[bysb[a]])
                        self.post_resid(vs, [(ysb[a][:], 0, D)], [bysb[a]], xa[a][:], bxa[a], xa[a], bxa[a], small[1], bsmall[1],
                                        junk, bjunk, tmp, btmp)
                        K.dma(pool, sxo[a], self.out[i * 128:(i + 1) * 128, :], xa[a][:], reads=[bxa[a]], writes=[self.xbuf[i]])
                    yield

                self.interleave(stageA(0))
                for i in range(NT):
                    nxt = stageA(i + 1) if i + 1 < NT else None
                    self.interleave(stageC(i), nxt)
                    if bgq and i % 8 == 7:
                        bgq.pop(0)()
                if g == 1:
                    while bgq:
                        bgq.pop(0)()
                K.barrier()
            K.end_scope()


def prep_weights(inp):
    wall = np.zeros((128, NTOT), np.float32)
    for i in range(DEPTH):
        for j in range(2):
            wg = np.zeros((D, FFP), np.float32); wg[:, :DFF] = inp["ffn_w_gate"][i, j]
            wu = np.zeros((D, FFP), np.float32); wu[:, :DFF] = inp["ffn_w_up"][i, j]
            gu = np.stack([wg, wu], 0).reshape(2, 8, 128, 11, 256)
            gu = gu.transpose(2, 3, 0, 1, 4).reshape(128, W_GU)
            o = WOFF[("gu", i, j)]
            wall[:, o:o + W_GU] = gu
            wdn = np.zeros((FFP, D), np.float32); wdn[:DFF] = inp["ffn_w_down"][i, j]
            o = WOFF[("d", i, j)]
            wall[:, o:o + W_D] = wdn.reshape(NF, 128, D).transpose(1, 0, 2).reshape(128, W_D)
    qperm = []
    for s in range(8):
        qperm += list(range(s * 64, s * 64 + 64)) + list(range((s + 8) * 64, (s + 8) * 64 + 64))
    qiperm = []
    for s in range(4):
        qiperm += list(range(1152 + s * 64, 1152 + s * 64 + 64)) + list(range(1152 + (s + 4) * 64, 1152 + (s + 4) * 64 + 64))
    aperm = np.array(qperm + list(range(1024, 1152)) + qiperm + list(range(1664, 1736)))
    for j in range(2):
        w = inp["dsa_w_in"][j][:, aperm]
        o = WOFF[("ain", j)]
        wall[:, o:o + W_AIN] = w.reshape(8, 128, A_IN).transpose(1, 0, 2).reshape(128, W_AIN)
        o = WOFF[("aout", j)]
        wall[:, o:o + W_AOUT] = inp["dsa_w_out"][j].reshape(8, 128, D).transpose(1, 0, 2).reshape(128, W_AOUT)
    for j in range(2):
        for g in range(2):
            cols = []
            for base in (0, 512, 1024, 1536):
                cols += list(range(base + g * 256, base + g * 256 + 256))
            cols += list(range(2048 + g * 512, 2048 + g * 512 + 512))
            w = inp["diff_w_in"][j][:, np.array(cols)]
            o = WOFF[("bin", j, g)]
            wall[:, o:o + W_BIN] = w.reshape(8, 128, 1536).transpose(1, 0, 2).reshape(128, W_BIN)
            wo = inp["diff_w_out"][j][g * 512:(g + 1) * 512]
            o = WOFF[("bout", j, g)]
            wall[:, o:o + W_BOUT] = wo.reshape(4, 128, D).transpose(1, 0, 2).reshape(128, W_BOUT)
    return wall


def prep_ada(inp):
    aw = np.asarray(inp["ada_w"], np.float32)
    a = aw.reshape(4, 4, 2, 128, 3, 6, 512)
    a = a.transpose(0, 4, 5, 1, 3, 2, 6)
    ada = np.ascontiguousarray(a).reshape(12 * 24, 128, 1024)
    adab = np.ascontiguousarray(np.asarray(inp["ada_b"], np.float32).reshape(1, 12 * 3072))
    return ada, adab


def make_in_maps(inp, T, ncores):
    wall = prep_weights(inp)
    ada, adab = prep_ada(inp)
    pre_n = np.ascontiguousarray(np.asarray(inp["pre_norm"], np.float32).reshape(12, D))
    post_n = np.ascontiguousarray(np.asarray(inp["post_norm"], np.float32).reshape(12, D))
    subln = np.ascontiguousarray(np.asarray(inp["diff_subln"], np.float32))
    lam = np.ascontiguousarray(np.asarray(inp["diff_lambda"], np.float32).reshape(2, 256))
    maps = []
    for b in range(ncores):
        maps.append({
            "x": np.ascontiguousarray(np.asarray(inp["x"][b, :T], np.float32)),
            "c": np.ascontiguousarray(np.asarray(inp["c"][b], np.float32).reshape(8, 128).T),
            "pos": np.ascontiguousarray(np.asarray(inp["positions"][b, :T], np.int32).reshape(T // 128, 128).T),
            "wall": wall, "ada": ada, "adab": adab, "pre_n": pre_n, "post_n": post_n, "subln": subln, "lam": lam,
        })
    return maps


ALL_SUBL = [(li, k) for li in range(DEPTH) for k in ("f0", "mix", "f1")]


def kernel(**inputs):
    T = 4096
    prog = Prog(T, ALL_SUBL)
    nc = prog.build()
    maps = make_in_maps(inputs, T, 8)
    res = run_bass_kernel_spmd(nc, maps, core_ids=list(range(8)))
    return np.stack([np.asarray(r["out"], np.float32) for r in res.results], 0)
```
